# Optimizing a Trainium2 kernel written in Bass

```python
import math
import jax
import jax.numpy as jnp
from jax import lax
import numpy as np

D_MODEL = 1024
BATCH = 2
SEQ = 8192
DEPTH = 2

GRID_W = 64
CTX_LEN = 256
HEAD_DIM = 64
Q_BLOCK = 128
ROPE_THETA = 10000.0
EPS = 1e-6
N_EVEN = (DEPTH + 1) // 2
N_ODD = DEPTH // 2
A_WIDTH = D_MODEL // 2
A_GROUPS = A_WIDTH // HEAD_DIM
CHUNK = 128
B_HEADS = D_MODEL // (4 * HEAD_DIM)
B_WIDTH = B_HEADS * 2 * HEAD_DIM
E_SPLITS = (A_WIDTH, 2 * A_WIDTH, 2 * A_WIDTH + B_WIDTH, 2 * A_WIDTH + 2 * B_WIDTH)
E_IN = 2 * A_WIDTH + 3 * B_WIDTH
C_HEADS = (D_MODEL // 2) // HEAD_DIM
C_KV_HEADS = 2
C_GROUP = C_HEADS // C_KV_HEADS
C_WIDTH = C_HEADS * HEAD_DIM
C_KV_WIDTH = C_KV_HEADS * HEAD_DIM
D_WIDTH = D_MODEL // 2
D_BLOCKS = D_WIDTH // HEAD_DIM
CONV_W = 4
LRU_C = 8.0
O_SPLITS = (C_WIDTH, C_WIDTH + C_KV_WIDTH, C_WIDTH + 2 * C_KV_WIDTH, C_WIDTH + 2 * C_KV_WIDTH + D_WIDTH)
O_IN = C_WIDTH + 2 * C_KV_WIDTH + 2 * D_WIDTH
FFN_DIM = 2816
N_EXPERTS = 8
TOP_K = 2

kernel_name = 'hybrid_diffusion_gmlp_diffattn_gqa_rglru_moe'


def rms_norm(x, g):
    xf = x.astype(jnp.float32)
    y = xf * lax.rsqrt(jnp.mean(xf * xf, axis=-1, keepdims=True) + EPS)
    return (y * g.astype(jnp.float32)).astype(x.dtype)


def rope_1d(x, pos):
    half = x.shape[-1] // 2
    freqs = ROPE_THETA ** (-jnp.arange(half, dtype=jnp.float32) / half)
    ang = pos.astype(jnp.float32)[:, None] * freqs[None, :]
    cos = jnp.cos(ang)[None, :, None, :]
    sin = jnp.sin(ang)[None, :, None, :]
    xf = x.astype(jnp.float32)
    x1, x2 = xf[..., :half], xf[..., half:]
    return jnp.concatenate([x1 * cos - x2 * sin, x1 * sin + x2 * cos], axis=-1).astype(x.dtype)


def axial_rope(x, row, col):
    h = x.shape[-1] // 2
    return jnp.concatenate([rope_1d(x[..., :h], row), rope_1d(x[..., h:], col)], axis=-1)


def sweep_query_blocks(fn, q):
    b, l = q.shape[:2]
    nb = l // Q_BLOCK
    qb = jnp.moveaxis(q.reshape(b, nb, Q_BLOCK, *q.shape[2:]), 1, 0)
    out = lax.map(fn, qb)
    return jnp.moveaxis(out, 0, 1).reshape(b, l, *out.shape[3:])


def diff_attention(q, k, v, lam):
    s = jnp.einsum('bqhcd,bkhcd->bhcqk', q, k, preferred_element_type=jnp.float32) * (HEAD_DIM ** -0.5)
    p = jax.nn.softmax(s, axis=-1)
    w = p[:, :, 0] - lam * p[:, :, 1]
    return jnp.einsum('bhqk,bkhe->bqhe', w.astype(v.dtype), v)


def gqa_attention(q, k, v):
    s = jnp.einsum('bqngd,bknd->bngqk', q, k, preferred_element_type=jnp.float32) * (HEAD_DIM ** -0.5)
    p = jax.nn.softmax(s, axis=-1).astype(v.dtype)
    return jnp.einsum('bngqk,bknd->bqngd', p, v)


def chunk_gmlp(ua, va, norm_g, ws, bs):
    b, l, _ = ua.shape
    u = jax.nn.gelu(ua).reshape(b, l // CHUNK, CHUNK, A_GROUPS, HEAD_DIM)
    v = rms_norm(jax.nn.gelu(va), norm_g).reshape(b, l // CHUNK, CHUNK, A_GROUPS, HEAD_DIM)
    mixed = jnp.einsum('gpq,bnqgc->bnpgc', ws, v) + bs.T[:, :, None]
    return (u * mixed).reshape(b, l, A_WIDTH)


def depthwise_conv(x, w, b):
    left = CONV_W // 2
    out = lax.conv_general_dilated(x, w[:, None, :], window_strides=(1,), padding=[(left, CONV_W - 1 - left)],
                                   dimension_numbers=('NWC', 'WIO', 'NWC'), feature_group_count=x.shape[-1])
    return out + b


def block_diag(x, w, b):
    xb = x.reshape(*x.shape[:-1], D_BLOCKS, -1)
    return jnp.einsum('blnc,ncd->blnd', xb, w).reshape(x.shape) + b


def lru_coeffs(xd, wa, ba, wx, bx, lam):
    r = jax.nn.sigmoid(block_diag(xd, wa, ba).astype(jnp.float32))
    i = jax.nn.sigmoid(block_diag(xd, wx, bx).astype(jnp.float32))
    log_a = -LRU_C * r * jax.nn.softplus(-lam.astype(jnp.float32))
    a = jnp.exp(log_a)
    bterm = jnp.sqrt(-jnp.expm1(2.0 * log_a)) * (i * xd.astype(jnp.float32))
    return a, bterm


def linear_scan(a, b, h0, reverse):
    if h0 is not None:
        idx = -1 if reverse else 0
        b = b.at[:, idx].add(a[:, idx] * h0)

    def combine(left, right):
        a_l, b_l = left
        a_r, b_r = right
        return a_l * a_r, a_r * b_l + b_r

    _, h = lax.associative_scan(combine, (a, b), axis=1, reverse=reverse)
    return h


def swiglu(t, w1, w3, w2):
    return (jax.nn.silu(t @ w1) * (t @ w3)) @ w2


def moe_swiglu(t, wr, br, w1, w3, w2):
    shape = t.shape
    tf = t.reshape(-1, shape[-1])
    logits = (tf @ wr + br).astype(jnp.float32)
    top_v, top_i = lax.top_k(logits, TOP_K)
    gates = jax.nn.softmax(top_v, axis=-1)
    dense_gate = jnp.sum(jax.nn.one_hot(top_i, N_EXPERTS, dtype=jnp.float32) * gates[..., None], axis=1).astype(t.dtype)
    out = jnp.zeros_like(tf)
    for e in range(N_EXPERTS):
        out = out + dense_gate[:, e:e + 1] * swiglu(tf, w1[e], w3[e], w2[e])
    return out.reshape(shape)


def even_mixer(h, hc, row, col, w_in, a_norm_g, a_ws, a_bs, qn_g, kn_g, lq1, lk1, lq2, lk2, subln_g, lam_init, need_ctx):
    b, l, _ = h.shape
    lc = hc.shape[1]
    ua, va, q, k, v = jnp.split(h @ w_in, E_SPLITS, axis=-1)
    uac, vac, qc, kc, vc = jnp.split(hc @ w_in, E_SPLITS, axis=-1)

    def qk(t, g, n):
        return rms_norm(t.reshape(b, n, 2 * B_HEADS, HEAD_DIM), g)

    q = axial_rope(qk(q, qn_g, l), row, col).reshape(b, l, B_HEADS, 2, HEAD_DIM)
    k = axial_rope(qk(k, kn_g, l), row, col).reshape(b, l, B_HEADS, 2, HEAD_DIM)
    kc = qk(kc, kn_g, lc).reshape(b, lc, B_HEADS, 2, HEAD_DIM)
    vc = vc.reshape(b, lc, B_HEADS, 2 * HEAD_DIM)
    k_all = jnp.concatenate([kc, k], axis=1)
    v_all = jnp.concatenate([vc, v.reshape(b, l, B_HEADS, 2 * HEAD_DIM)], axis=1)
    lam = (jnp.exp(jnp.sum(lq1.astype(jnp.float32) * lk1.astype(jnp.float32)))
           - jnp.exp(jnp.sum(lq2.astype(jnp.float32) * lk2.astype(jnp.float32))) + lam_init)

    def diff_out(o):
        return (rms_norm(o, subln_g) * (1.0 - lam_init)).reshape(*o.shape[:2], B_WIDTH)

    yb = diff_out(sweep_query_blocks(lambda qb: diff_attention(qb, k_all, v_all, lam), q))
    ya = chunk_gmlp(ua, va, a_norm_g, a_ws, a_bs)
    y = jnp.concatenate([ya, yb], axis=-1)
    if not need_ctx:
        return y, None
    qc = qk(qc, qn_g, lc).reshape(b, lc, B_HEADS, 2, HEAD_DIM)
    ybc = diff_out(diff_attention(qc, kc, vc, lam))
    yac = chunk_gmlp(uac, vac, a_norm_g, a_ws, a_bs)
    return y, jnp.concatenate([yac, ybc], axis=-1)


def odd_mixer(h, hc, row, col, w_in, qn_g, kn_g, conv_w, conv_b, wa, ba, wx, bx, lam, need_ctx):
    b, l, _ = h.shape
    lc = hc.shape[1]
    q, k, v, gate, xr = jnp.split(h @ w_in, O_SPLITS, axis=-1)
    qc, kc, vc, gatec, xrc = jnp.split(hc @ w_in, O_SPLITS, axis=-1)
    q = axial_rope(rms_norm(q.reshape(b, l, C_HEADS, HEAD_DIM), qn_g), row, col).reshape(b, l, C_KV_HEADS, C_GROUP, HEAD_DIM)
    k = axial_rope(rms_norm(k.reshape(b, l, C_KV_HEADS, HEAD_DIM), kn_g), row, col)
    kc = rms_norm(kc.reshape(b, lc, C_KV_HEADS, HEAD_DIM), kn_g)
    vc = vc.reshape(b, lc, C_KV_HEADS, HEAD_DIM)
    k_all = jnp.concatenate([kc, k], axis=1)
    v_all = jnp.concatenate([vc, v.reshape(b, l, C_KV_HEADS, HEAD_DIM)], axis=1)
    y_attn = sweep_query_blocks(lambda qb: gqa_attention(qb, k_all, v_all), q).reshape(b, l, C_WIDTH)
    xd = depthwise_conv(xr, conv_w, conv_b)
    xdc = depthwise_conv(xrc, conv_w, conv_b)
    h_lat, h_ctx = [], []
    for d, rev in enumerate((False, True)):
        a_c, b_c = lru_coeffs(xdc, wa[d], ba[d], wx[d], bx[d], lam[d])
        hs_c = linear_scan(a_c, b_c, None, rev)
        a_l, b_l = lru_coeffs(xd, wa[d], ba[d], wx[d], bx[d], lam[d])
        h_lat.append(linear_scan(a_l, b_l, hs_c[:, 0] if rev else hs_c[:, -1], rev))
        h_ctx.append(hs_c)
    y_rec = (h_lat[0] + h_lat[1]).astype(h.dtype) * jax.nn.gelu(gate)
    y = jnp.concatenate([y_attn, y_rec], axis=-1)
    if not need_ctx:
        return y, None
    qc = rms_norm(qc.reshape(b, lc, C_HEADS, HEAD_DIM), qn_g).reshape(b, lc, C_KV_HEADS, C_GROUP, HEAD_DIM)
    yc_attn = gqa_attention(qc, kc, vc).reshape(b, lc, C_WIDTH)
    yc_rec = (h_ctx[0] + h_ctx[1]).astype(hc.dtype) * jax.nn.gelu(gatec)
    return y, jnp.concatenate([yc_attn, yc_rec], axis=-1)


def setup_inputs(seed: int = 0) -> dict:
    key = jax.random.key(seed)
    ks = iter(jax.random.split(key, 64))

    def nrm(shape, scale=1.0):
        return jax.random.normal(next(ks), shape, jnp.float32) * scale

    def gain(shape):
        return 1.0 + 0.05 * nrm(shape)

    D, F, E, NE, NO = D_MODEL, FFN_DIM, N_EXPERTS, N_EVEN, N_ODD
    bs = D_WIDTH // D_BLOCKS
    u = jax.random.uniform(next(ks), (NO, 2, D_WIDTH), jnp.float32, 0.9, 0.999)
    p = u ** (1.0 / LRU_C)
    d_lambda = jnp.log(p) - jnp.log1p(-p)
    return {
        'x': nrm((BATCH, SEQ, D)),
        'c': nrm((BATCH, D)),
        'ctx': nrm((BATCH, CTX_LEN, D)),
        'c_ctx': nrm((D,)),
        'w_mod': nrm((DEPTH, D, 6 * D), 0.5 * D ** -0.5),
        'b_mod': nrm((DEPTH, 6 * D), 0.01),
        'norm_mix_g': gain((DEPTH, D)),
        'norm_ffn_g': gain((DEPTH, D)),
        'w_out': nrm((DEPTH, D, D), D ** -0.5),
        'e_w_in': nrm((NE, D, E_IN), D ** -0.5),
        'a_norm_g': gain((NE, A_WIDTH)),
        'a_ws': nrm((NE, A_GROUPS, CHUNK, CHUNK), CHUNK ** -0.5),
        'a_bs': 1.0 + nrm((NE, A_GROUPS, CHUNK), 0.02),
        'b_qnorm_g': gain((NE, HEAD_DIM)),
        'b_knorm_g': gain((NE, HEAD_DIM)),
        'b_lq1': nrm((NE, HEAD_DIM), 0.1),
        'b_lk1': nrm((NE, HEAD_DIM), 0.1),
        'b_lq2': nrm((NE, HEAD_DIM), 0.1),
        'b_lk2': nrm((NE, HEAD_DIM), 0.1),
        'b_subln_g': gain((NE, 2 * HEAD_DIM)),
        'ffn_w1': nrm((NE, D, F), D ** -0.5),
        'ffn_w3': nrm((NE, D, F), D ** -0.5),
        'ffn_w2': nrm((NE, F, D), F ** -0.5),
        'o_w_in': nrm((NO, D, O_IN), D ** -0.5),
        'c_qnorm_g': gain((NO, HEAD_DIM)),
        'c_knorm_g': gain((NO, HEAD_DIM)),
        'd_conv_w': nrm((NO, CONV_W, D_WIDTH), CONV_W ** -0.5),
        'd_conv_b': nrm((NO, D_WIDTH), 0.01),
        'd_wa': nrm((NO, 2, D_BLOCKS, bs, bs), bs ** -0.5),
        'd_ba': nrm((NO, 2, D_WIDTH), 0.01),
        'd_wx': nrm((NO, 2, D_BLOCKS, bs, bs), bs ** -0.5),
        'd_bx': nrm((NO, 2, D_WIDTH), 0.01),
        'd_lambda': d_lambda,
        'router_w': nrm((NO, D, E), D ** -0.5),
        'router_b': nrm((NO, E), 0.01),
        'moe_w1': nrm((NO, E, D, F), D ** -0.5),
        'moe_w3': nrm((NO, E, D, F), D ** -0.5),
        'moe_w2': nrm((NO, E, F, D), F ** -0.5),
    }


def reference(x, c, ctx, c_ctx, w_mod, b_mod, norm_mix_g, norm_ffn_g, w_out,
              e_w_in, a_norm_g, a_ws, a_bs, b_qnorm_g, b_knorm_g, b_lq1, b_lk1, b_lq2, b_lk2, b_subln_g,
              ffn_w1, ffn_w3, ffn_w2,
              o_w_in, c_qnorm_g, c_knorm_g, d_conv_w, d_conv_b, d_wa, d_ba, d_wx, d_bx, d_lambda,
              router_w, router_b, moe_w1, moe_w3, moe_w2):
    n_lat = x.shape[1]
    rows = n_lat // GRID_W
    row = jnp.repeat(jnp.arange(rows, dtype=jnp.int32), GRID_W)
    col = jnp.tile(jnp.arange(GRID_W, dtype=jnp.int32), rows)
    xc = ctx
    for layer in range(DEPTH):
        need_ctx = layer < DEPTH - 1
        j = layer // 2
        mod = jax.nn.silu(c) @ w_mod[layer] + b_mod[layer]
        modc = jax.nn.silu(c_ctx) @ w_mod[layer] + b_mod[layer]
        sh1, sc1, g1, sh2, sc2, g2 = jnp.split(mod[:, None, :], 6, axis=-1)
        sh1c, sc1c, g1c, sh2c, sc2c, g2c = jnp.split(modc, 6, axis=-1)
        h = rms_norm(x, norm_mix_g[layer]) * (1.0 + sc1) + sh1
        hc = rms_norm(xc, norm_mix_g[layer]) * (1.0 + sc1c) + sh1c
        if layer % 2 == 0:
            lam_init = 0.8 - 0.6 * math.exp(-0.3 * layer)
            y, yc = even_mixer(h, hc, row, col, e_w_in[j], a_norm_g[j], a_ws[j], a_bs[j], b_qnorm_g[j], b_knorm_g[j],
                               b_lq1[j], b_lk1[j], b_lq2[j], b_lk2[j], b_subln_g[j], lam_init, need_ctx)

            def channel(t):
                return swiglu(t, ffn_w1[j], ffn_w3[j], ffn_w2[j])
        else:
            y, yc = odd_mixer(h, hc, row, col, o_w_in[j], c_qnorm_g[j], c_knorm_g[j], d_conv_w[j], d_conv_b[j],
                              d_wa[j], d_ba[j], d_wx[j], d_bx[j], d_lambda[j], need_ctx)

            def channel(t):
                return moe_swiglu(t, router_w[j], router_b[j], moe_w1[j], moe_w3[j], moe_w2[j])
        x = x + g1 * (y @ w_out[layer])
        x = x + g2 * channel(rms_norm(x, norm_ffn_g[layer]) * (1.0 + sc2) + sh2)
        if need_ctx:
            xc = xc + g1c * (yc @ w_out[layer])
            xc = xc + g2c * channel(rms_norm(xc, norm_ffn_g[layer]) * (1.0 + sc2c) + sh2c)
    return x
```

```python
from contextlib import ExitStack

import numpy as np
import concourse.bass as bass
import concourse.mybir as mybir
from concourse.bass_utils import run_bass_kernel_spmd

F32 = mybir.dt.float32
BF = mybir.dt.bfloat16
AF = mybir.ActivationFunctionType
ALU = mybir.AluOpType
AX = mybir.AxisListType

D = 1024
SEQ = 8192
LC = 256
NALL = SEQ + LC
NT_ALL = NALL // 128
OWN = 2048
NOWN = OWN + LC
FF = 2816
NFC = FF // 128
EPS = 1e-6


class T:
    def __init__(self, name, ap):
        self.name = name
        self.ap = ap
        self.w = None
        self.r = {}

    def __getitem__(self, k):
        return self.ap[k]


class Prog:
    ENG = ("pe", "act", "dve", "pool", "sp")
    SEM_ROLL = 30000
    N_DMA_SEMS = 32

    def __init__(self, nc):
        self.nc = nc
        self.q = {e: [] for e in self.ENG}
        self.sem = {}
        self.cnt = {}
        self.nsem = 0
        self.retired = {}
        for e in self.ENG:
            self._new_sem(e)
        self.dsem = []
        for i in range(self.N_DMA_SEMS):
            s = nc.alloc_semaphore(f"dma{i}")
            self.dsem.append([s, 0])
        self.dnext = 0
        self.seen = {e: {} for e in self.ENG}
        self.pend = {e: False for e in self.ENG}
        self.ninst = 0
        self.gstack = ExitStack()
        self.stack = None
        self.uid = 0

    def _new_sem(self, e):
        if e in self.sem and self.cnt.get(e, 0) > 0:
            self.retired.setdefault(e, []).append((self.sem[e], self.cnt[e], e))
        self.nsem += 1
        self.sem[e] = self.nc.alloc_semaphore(f"s_{e}_{self.nsem}")
        self.cnt[e] = 0

    def _deps(self, reads, writes):
        deps = []
        for t in reads:
            if t.w is not None:
                deps.append(t.w)
        for t in writes:
            if t.w is not None:
                deps.append(t.w)
            deps.extend(t.r.values())
        return deps

    def _waits(self, e, deps, same_ok):
        out = {}
        for (s, v, src) in deps:
            if src == e and same_ok:
                continue
            key = s.name
            if self.seen[e].get(key, 0) >= v:
                continue
            if key not in out or out[key][1] < v:
                out[key] = (s, v)
        for key, (s, v) in out.items():
            self.seen[e][key] = v
        return list(out.values())

    def _mark(self, tok, reads, writes):
        s, v, src = tok
        key = s.name
        for t in reads:
            old = t.r.get(key)
            if old is None or old[1] < v:
                t.r[key] = tok
        for t in writes:
            t.w = tok
            t.r = {}

    def op(self, e, fn, reads=(), writes=(), inc=True, same_ok=None):
        if same_ok is None:
            same_ok = (e == "pe")
        waits = self._waits(e, self._deps(reads, writes), same_ok)
        if self.cnt[e] >= self.SEM_ROLL and not self.pend[e]:
            self._new_sem(e)
        if inc:
            self.cnt[e] += 1
            tok = (self.sem[e], self.cnt[e], e)
            self.pend[e] = False
        else:
            tok = (self.sem[e], self.cnt[e] + 1, e)
            self.pend[e] = True
        sem = self.sem[e]

        def emit(eng, fn=fn, waits=waits, inc=inc, sem=sem):
            for (s, v) in waits:
                eng.wait_ge(s, v)
            ins = fn(eng)
            if inc:
                ins.then_inc(sem, 1)

        self.q[e].append(emit)
        self._mark(tok, reads, writes)
        self.ninst += 1
        return tok

    def dma(self, e, out_ap, in_ap, reads=(), writes=(), **kw):
        d = self.dsem[self.dnext]
        self.dnext = (self.dnext + 1) % self.N_DMA_SEMS
        s, prev = d
        deps = self._deps(reads, writes)
        if prev > 0:
            deps.append((s, prev, "dma"))
        waits = self._waits(e, deps, False)
        d[1] = prev + 16
        tok = (s, prev + 16, "dma")

        def emit(eng, waits=waits, s=s):
            for (ws, v) in waits:
                eng.wait_ge(ws, v)
            eng.dma_start(out=out_ap, in_=in_ap, **kw).then_inc(s, 16)

        self.q[e].append(emit)
        self._mark(tok, reads, writes)
        self.ninst += 1
        return tok

    def wait_all(self, e, toks):
        waits = self._waits(e, list(toks), False)

        def emit(eng, waits=waits):
            for (s, v) in waits:
                eng.wait_ge(s, v)

        self.q[e].append(emit)

    def barrier(self):
        toks = []
        for e in self.ENG:
            if self.cnt[e] > 0:
                toks.append((self.sem[e], self.cnt[e], e))
            elif self.retired.get(e):
                toks.append(self.retired[e][-1])
        toks += [(s, v, "dma") for s, v in self.dsem if v > 0]
        for e in self.ENG:
            assert not self.pend[e]
            self.wait_all(e, toks)

    def phase_begin(self):
        self.stack = ExitStack()

    def flush(self):
        if not any(self.q.values()):
            return
        q = self.q
        self.q = {e: [] for e in self.ENG}
        nc = self.nc
        with nc.Block() as block:
            @block.tensor
            def _(eng):
                for f in q["pe"]:
                    f(eng)

            @block.scalar
            def _(eng):
                for f in q["act"]:
                    f(eng)

            @block.vector
            def _(eng):
                for f in q["dve"]:
                    f(eng)

            @block.gpsimd
            def _(eng):
                for f in q["pool"]:
                    f(eng)

            @block.sync
            def _(eng):
                for f in q["sp"]:
                    f(eng)

    def phase_end(self):
        self.barrier()
        self.flush()
        self.stack.close()
        self.stack = None

    def sb(self, shape, dt=F32, name="t", glob=False):
        self.uid += 1
        nm = f"{name}_{self.uid}"
        st = self.gstack if (glob or self.stack is None) else self.stack
        h = st.enter_context(self.nc.sbuf_tensor(nm, list(shape), dt))
        return T(nm, h.ap())

    def ps(self, shape, dt=F32, name="p", glob=False):
        self.uid += 1
        nm = f"{name}_{self.uid}"
        st = self.gstack if (glob or self.stack is None) else self.stack
        h = st.enter_context(self.nc.psum_tensor(nm, list(shape), dt))
        return T(nm, h.ap())

    def finish(self, final_tiles):
        toks = [t.w for t in final_tiles if t.w is not None]
        self.wait_all("sp", toks)
        self.barrier()
        self.flush()
        self.gstack.close()


class Ring:
    def __init__(self, tiles):
        self.tiles = tiles
        self.i = 0

    def next(self):
        t = self.tiles[self.i % len(self.tiles)]
        self.i += 1
        return t


class LK:
    def __init__(self, nc):
        self.nc = nc
        self.P = Prog(nc)

    def din(self, name, shape, dt=F32):
        if not hasattr(self, "inputs"):
            self.inputs = []
        self.inputs.append(name)
        return T(name, self.nc.dram_tensor(name, list(shape), dt, kind="ExternalInput").ap())

    def dout(self, name, shape, dt=F32):
        return T(name, self.nc.dram_tensor(name, list(shape), dt, kind="ExternalOutput").ap())

    def dscratch(self, name, shape, dt=F32):
        return T(name, self.nc.dram_tensor(name, list(shape), dt, kind="Internal").ap())

    def ring(self, n, shape, dt=F32, name="r", psum=False):
        P = self.P
        return Ring([(P.ps if psum else P.sb)(shape, dt, name) for _ in range(n)])

    def load_ident(self, ident_d):
        P = self.P
        self.ident = P.sb([128, 128], F32, "ident", glob=True)
        self.identb = P.sb([128, 128], BF, "identb", glob=True)
        P.dma("sp", self.ident[:], ident_d[:, :], reads=[ident_d], writes=[self.ident])
        P.op("dve", lambda e: e.tensor_copy(self.identb[:], self.ident[:]), reads=[self.ident], writes=[self.identb])

    def setup_common(self, cvec_d, wmod_d, bmod_d, tag=""):
        P = self.P
        modrow_d = self.dscratch("modrow_d" + tag, [2, 6 * D])
        P.phase_begin()
        cv = P.sb([128, 16], F32, "cv")
        sc = P.sb([128, 16], F32, "sc")
        bm = P.sb([2, 6 * D], F32, "bm")
        mr = P.sb([2, 6 * D], F32, "mr")
        P.dma("sp", cv[:], cvec_d[:, :], reads=[cvec_d], writes=[cv])
        for j in range(2):
            P.dma("sp", bm[j:j + 1, :], bmod_d.ap.unsqueeze(0), reads=[bmod_d], writes=[bm])
        P.op("act", lambda e: e.activation(sc[:], cv[:], AF.Silu), reads=[cv], writes=[sc])
        wms = self.ring(2, [128, 8, 512], F32, "wm")
        pms = self.ring(2, [128, 512], F32, "pm", psum=True)
        for pc in range(12):
            wm = wms.next()
            pm = pms.next()
            P.dma("sp", wm[:], wmod_d[:, pc * 512:(pc + 1) * 512].rearrange("(kc p) n -> p kc n", p=128),
                  reads=[wmod_d], writes=[wm])
            for kc in range(8):
                P.op("pe", lambda e, kc=kc, wm=wm, pm=pm: e.matmul(pm[0:2, :], sc[:, kc * 2:(kc + 1) * 2], wm[:, kc, :],
                                                                 start=(kc == 0), stop=(kc == 7)),
                     reads=[sc, wm], writes=[pm], inc=(kc == 7))
            P.op("dve", lambda e, pc=pc, pm=pm: e.tensor_tensor(mr[0:2, pc * 512:(pc + 1) * 512], pm[0:2, :],
                                                              bm[0:2, pc * 512:(pc + 1) * 512], ALU.add),
                 reads=[pm, bm], writes=[mr])
        P.dma("sp", modrow_d[:, :], mr[:], reads=[mr], writes=[modrow_d])
        P.phase_end()
        return modrow_d

    def load_bc(self, dst, src_row_ap, src_t):
        self.P.dma("sp", dst[:], src_row_ap.partition_broadcast(128), reads=[src_t], writes=[dst])

    def mod_bc(self, j, idx, name):
        t = self.P.sb([128, D], F32, name)
        self.load_bc(t, self.modrow_d.ap[j, idx * D:(idx + 1) * D], self.modrow_d)
        return t

    def make_A(self, sc_t, g_t):
        self.P.op("dve", lambda e: e.scalar_tensor_tensor(sc_t[:], sc_t[:], 1.0, g_t[:], ALU.add, ALU.mult),
                  reads=[sc_t, g_t], writes=[sc_t])
        return sc_t

    def rstd(self, ss, n, dim):
        P = self.P
        P.op("dve", lambda e: e.tensor_scalar(ss[:, 0:n], ss[:, 0:n], 1.0 / dim, EPS, ALU.mult, ALU.add),
             reads=[ss], writes=[ss])
        P.op("act", lambda e: e.activation(ss[:, 0:n], ss[:, 0:n], AF.Sqrt), reads=[ss], writes=[ss])
        P.op("dve", lambda e: e.reciprocal(ss[:, 0:n], ss[:, 0:n]), reads=[ss], writes=[ss])

    def norm_tile(self, x_ap, x_t, A_bc, sh_bc, hn, junk, ss):
        P = self.P
        P.op("act", lambda e: e.activation(junk[:], x_ap, AF.Square, accum_out=ss[:, 0:1]),
             reads=[x_t], writes=[junk, ss])
        self.rstd(ss, 1, D)
        P.op("dve", lambda e: e.scalar_tensor_tensor(junk[:], x_ap, ss[:, 0:1], A_bc[:], ALU.mult, ALU.mult),
             reads=[x_t, ss, A_bc], writes=[junk])
        P.op("pool", lambda e: e.tensor_tensor(hn[:], junk[:], sh_bc[:], ALU.add), reads=[junk, sh_bc], writes=[hn])

    def transpose_cols(self, src, ncol_chunks, pT, dst_ap_fn, dst_t, eng="act"):
        P = self.P
        for c in range(ncol_chunks):
            P.op("pe", lambda e, c=c: e.transpose(pT[:, c, :], src[:, c * 128:(c + 1) * 128], self.identb[:]),
                 reads=[src, self.identb], writes=[pT], inc=(c == ncol_chunks - 1))
        if eng == "act":
            P.op("act", lambda e: e.activation(dst_ap_fn(), pT[:, 0:ncol_chunks, :], AF.Identity), reads=[pT], writes=[dst_t])
        else:
            P.op("dve", lambda e: e.tensor_copy(dst_ap_fn(), pT[:, 0:ncol_chunks, :]), reads=[pT], writes=[dst_t])

    def proj_tok(self, ps, hT, tsl, W, c0, ncols):
        for kc in range(8):
            self.P.op("pe", lambda e, kc=kc: e.matmul(ps[:, 0:ncols], hT[:, kc, tsl], W[:, kc, c0:c0 + ncols],
                                                     start=(kc == 0), stop=(kc == 7)),
                      reads=[hT, W], writes=[ps], inc=(kc == 7))

    def proj_feat(self, ps, hT, n, W, c0):
        for kc in range(8):
            self.P.op("pe", lambda e, kc=kc: e.matmul(ps[:, 0:n], W[:, kc, c0:c0 + 128], hT[:, kc, 0:n],
                                                     start=(kc == 0), stop=(kc == 7)),
                      reads=[hT, W], writes=[ps], inc=(kc == 7))

    def rope_tables(self, cs, g_bc, gs_bc, CT, ST):
        P = self.P
        P.op("pool", lambda e: e.tensor_tensor(CT[:], cs[:, 0:64], g_bc[:], ALU.mult), reads=[cs, g_bc], writes=[CT])
        P.op("pool", lambda e: e.tensor_tensor(ST[:], cs[:, 64:128], gs_bc[:], ALU.mult), reads=[cs, gs_bc], writes=[ST])

    def swap_gain(self, g_bc, gs_bc):
        P = self.P
        for a in range(2):
            for b in range(2):
                P.op("dve", lambda e, a=a, b=b: e.tensor_copy(gs_bc[:, a * 32 + b * 16:a * 32 + b * 16 + 16],
                                                              g_bc[:, a * 32 + (1 - b) * 16:a * 32 + (1 - b) * 16 + 16]),
                     reads=[g_bc], writes=[gs_bc])

    def qk_post(self, src, H, CT, ST, out, tmp):
        P = self.P
        sq, qn, t1, ssq = tmp
        n = H * 64
        P.op("act", lambda e: e.activation(sq[:, 0:n], src[:, 0:n], AF.Square), reads=[src], writes=[sq])
        P.op("dve", lambda e: e.tensor_reduce(ssq[:, 0:H], sq[:, 0:n].rearrange("p (h d) -> p h d", d=64), AX.X, ALU.add),
             reads=[sq], writes=[ssq])
        self.rstd(ssq, H, 64)
        P.op("dve", lambda e: e.tensor_tensor(qn[:, 0:n].rearrange("p (h d) -> p h d", d=64),
                                              src[:, 0:n].rearrange("p (h d) -> p h d", d=64),
                                              ssq[:, 0:H].unsqueeze(2).to_broadcast([128, H, 64]), ALU.mult),
             reads=[src, ssq], writes=[qn])
        P.op("dve", lambda e: e.tensor_tensor(t1[:, 0:n].rearrange("p (h d) -> p h d", d=64),
                                              qn[:, 0:n].rearrange("p (h d) -> p h d", d=64),
                                              CT[:].unsqueeze(1).to_broadcast([128, H, 64]), ALU.mult),
             reads=[qn, CT], writes=[t1])
        for b in range(2):
            P.op("dve", lambda e, b=b: e.tensor_tensor(
                sq[:, 0:n].rearrange("p (h a b c) -> p h a b c", a=2, b=2, c=16)[:, :, :, b, :],
                qn[:, 0:n].rearrange("p (h a b c) -> p h a b c", a=2, b=2, c=16)[:, :, :, 1 - b, :],
                ST[:].rearrange("p (a b c) -> p a b c", a=2, b=2, c=16)[:, :, b, :].unsqueeze(1).to_broadcast([128, H, 2, 16]),
                ALU.mult), reads=[qn, ST], writes=[sq])
        P.op("pool", lambda e: e.tensor_tensor(out[:, 0:n], t1[:, 0:n], sq[:, 0:n], ALU.add), reads=[t1, sq], writes=[out])


def _rope_table():
    half = 16
    freqs = 10000.0 ** (-np.arange(half, dtype=np.float32) / half)
    t = np.arange(SEQ)
    row = (t // 64).astype(np.float32)
    col = (t % 64).astype(np.float32)
    ar = row[:, None] * freqs[None, :]
    ac = col[:, None] * freqs[None, :]
    C = np.concatenate([np.cos(ar), np.cos(ar), np.cos(ac), np.cos(ac)], 1)
    S = np.concatenate([-np.sin(ar), np.sin(ar), -np.sin(ac), np.sin(ac)], 1)
    lat = np.concatenate([C, S], 1).astype(np.float32)
    ctx = np.concatenate([np.ones((LC, 64), np.float32), np.zeros((LC, 64), np.float32)], 1)
    return np.concatenate([ctx, lat], 0)


STOP = None


def emit_l0(L, io):
    nc = L.nc
    P = L.P
    _outer = P.gstack
    P.gstack = ExitStack()
    xall_tile = io["xall_tile"]; xown_blk = io["xown_blk"]; xo_blk = io["xo_blk"]
    csall = io["csall"]; csown = io["csown"]; ident_d = io["ident_d"]; cvec_d = io["cvec_d"]
    sfx = io["sfx"]
    L.modrow_d = io["modrow"]
    gmix_d = L.din("norm_mix_g" + sfx, [D])
    gffn_d = L.din("norm_ffn_g" + sfx, [D])
    wout_d = L.din("w_out" + sfx, [D, D])
    win_d = L.din("e_w_in", [D, 2560])
    anorm_d = L.din("a_norm_g", [512])
    aws_d = L.din("a_ws", [8, 128, 128])
    abs_d = L.din("a_bs", [8, 128])
    qg_d = L.din("b_qnorm_g", [64])
    kg_d = L.din("b_knorm_g", [64])
    lq1_d = L.din("b_lq1", [64]); lk1_d = L.din("b_lk1", [64]); lq2_d = L.din("b_lq2", [64]); lk2_d = L.din("b_lk2", [64])
    sub_d = L.din("b_subln_g", [128])
    w1_d = L.din("ffn_w1", [D, FF]); w3_d = L.din("ffn_w3", [D, FF]); w2_d = L.din("ffn_w2", [FF, D])
    KT_d = L.dscratch("KT_d", [4, 128, NALL], BF)
    VS_d = L.dscratch("VS_d", [NALL, 512], BF)
    LAM_INIT = 0.2


    gq = P.sb([128, 64], F32, "gq", glob=True); gqs = P.sb([128, 64], F32, "gqs", glob=True)
    gk = P.sb([128, 64], F32, "gk", glob=True); gks = P.sb([128, 64], F32, "gks", glob=True)
    L.load_bc(gq, qg_d.ap, qg_d); L.load_bc(gk, kg_d.ap, kg_d)
    L.swap_gain(gq, gqs); L.swap_gain(gk, gks)
    neglam = P.sb([128, 1], F32, "neglam", glob=True)
    P.phase_begin()
    lt = [P.sb([128, 64], F32, "l") for _ in range(4)]
    for t_, d_ in zip(lt, (lq1_d, lk1_d, lq2_d, lk2_d)):
        L.load_bc(t_, d_.ap, d_)
    e12 = P.sb([128, 2], F32, "e12")
    for i in range(2):
        P.op("dve", lambda e, i=i: e.tensor_tensor(lt[2 * i][:], lt[2 * i][:], lt[2 * i + 1][:], ALU.mult), reads=[lt[2 * i], lt[2 * i + 1]], writes=[lt[2 * i]])
        P.op("dve", lambda e, i=i: e.tensor_reduce(e12[:, i:i + 1], lt[2 * i][:], AX.X, ALU.add), reads=[lt[2 * i]], writes=[e12])
    P.op("act", lambda e: e.activation(e12[:], e12[:], AF.Exp), reads=[e12], writes=[e12])
    P.op("dve", lambda e: e.tensor_tensor(neglam[:], e12[:, 1:2], e12[:, 0:1], ALU.subtract), reads=[e12], writes=[neglam])
    P.op("dve", lambda e: e.tensor_scalar(neglam[:], neglam[:], -LAM_INIT, None, ALU.add), reads=[neglam], writes=[neglam])
    P.phase_end()

    gmix = P.sb([128, D], F32, "gmix", glob=True)
    gffn = P.sb([128, D], F32, "gffn", glob=True)
    L.load_bc(gmix, gmix_d.ap, gmix_d)
    L.load_bc(gffn, gffn_d.ap, gffn_d)

    P.phase_begin()
    A1 = [L.make_A(L.mod_bc(j, 1, "A1"), gmix) for j in range(2)]
    sh1 = [L.mod_bc(j, 0, "sh1") for j in range(2)]
    Wkv = P.sb([128, 8, 1024], BF, "Wkv")
    for h in range(2):
        P.dma("pool", Wkv[:, :, h * 512:(h + 1) * 512],
              win_d[:, 1536 + h * 512:1536 + (h + 1) * 512].rearrange("(kc p) n -> p kc n", p=128), reads=[win_d], writes=[Wkv])
    xr = L.ring(3, [128, D], F32, "xa"); csr = L.ring(3, [128, 128], F32, "csa")
    hnr = L.ring(2, [128, D], BF, "hn"); jr = L.ring(2, [128, D], F32, "junk"); ssr = L.ring(2, [128, 2], F32, "ss")
    hTr = L.ring(2, [128, 8, 128], BF, "hT")
    pTr = L.ring(2, [128, 8, 128], BF, "pT", psum=True)
    pkr = L.ring(2, [128, 512], F32, "pk", psum=True); pvr = L.ring(2, [128, 512], F32, "pv", psum=True)
    CTr = L.ring(2, [128, 64], F32, "CT"); STr = L.ring(2, [128, 64], F32, "ST")
    tmps = [tuple([P.sb([128, 512], F32, "sq"), P.sb([128, 512], F32, "qn"), P.sb([128, 512], F32, "t1"), P.sb([128, 8], F32, "ssq")]) for _ in range(2)]
    kbr = L.ring(2, [128, 512], BF, "kb"); vbr = L.ring(2, [128, 512], BF, "vb"); ktr = L.ring(2, [128, 4, 128], BF, "kt")
    for t in range(NT_ALL):
        j = 1 if t < 2 else 0
        x = xr.next(); cs = csr.next(); hn = hnr.next(); junk = jr.next(); ss = ssr.next(); hT = hTr.next(); pT = pTr.next()
        xa_ap, xa_t = xall_tile(t)
        P.dma("sp", x[:], xa_ap, reads=[xa_t], writes=[x])
        P.dma("sp", cs[:], csall[t * 128:(t + 1) * 128, :], reads=[csall], writes=[cs])
        L.norm_tile(x[:], x, A1[j], sh1[j], hn, junk, ss)
        L.transpose_cols(hn, 8, pT, lambda hT=hT: hT[:], hT)
        pk = pkr.next(); pv = pvr.next()
        L.proj_tok(pk, hT, slice(0, 128), Wkv, 0, 512)
        L.proj_tok(pv, hT, slice(0, 128), Wkv, 512, 512)
        vb = vbr.next()
        P.op("act", lambda e, vb=vb, pv=pv: e.activation(vb[:], pv[:], AF.Identity), reads=[pv], writes=[vb])
        P.dma("pool", VS_d[t * 128:(t + 1) * 128, :], vb[:], reads=[vb], writes=[VS_d])
        CT = CTr.next(); ST = STr.next()
        L.rope_tables(cs, gk, gks, CT, ST)
        kb = kbr.next()
        L.qk_post(pk, 8, CT, ST, kb, tmps[t % 2])
        pT2 = pTr.next(); kt = ktr.next()
        L.transpose_cols(kb, 4, pT2, lambda kt=kt: kt[:], kt, eng="dve")
        P.dma("pool", KT_d[:, :, t * 128:(t + 1) * 128].rearrange("h p t -> p h t"), kt[:], reads=[kt], writes=[KT_d])
    P.phase_end()

    yT = P.sb([128, 8, NOWN], BF, "yT", glob=True)
    qstack = ExitStack()
    QT = T("QT", qstack.enter_context(nc.sbuf_tensor("QT", [128, 2, 4, NOWN], BF)).ap())
    P.op("pool", lambda e: e.memset(QT[64:128, 0, :, :], 0.0), writes=[QT])
    P.op("pool", lambda e: e.memset(QT[0:64, 1, :, :], 0.0), writes=[QT])
    blocks = [(0, 2)] + [(2 + 4 * i, 4) for i in range(4)]

    P.phase_begin()
    A1 = [L.make_A(L.mod_bc(j, 1, "A1"), gmix) for j in range(2)]
    sh1 = [L.mod_bc(j, 0, "sh1") for j in range(2)]
    Win = P.sb([128, 8, 1536], BF, "Win")
    for h in range(3):
        P.dma("pool", Win[:, :, h * 512:(h + 1) * 512], win_d[:, h * 512:(h + 1) * 512].rearrange("(kc p) n -> p kc n", p=128),
              reads=[win_d], writes=[Win])
    anorm = P.sb([128, 512], F32, "anorm"); L.load_bc(anorm, anorm_d.ap, anorm_d)
    wsT = P.sb([128, 8, 128], BF, "wsT")
    wsr = P.sb([128, 8, 128], F32, "wsr")
    P.dma("sp", wsr[:], aws_d.ap.rearrange("g p q -> p g q"), reads=[aws_d], writes=[wsr])
    wsb = P.sb([128, 8, 128], BF, "wsb")
    P.op("dve", lambda e: e.tensor_copy(wsb[:], wsr[:]), reads=[wsr], writes=[wsb])
    pTr = L.ring(2, [128, 8, 128], BF, "pT", psum=True)
    pTw = pTr.next()
    for g in range(8):
        P.op("pe", lambda e, g=g: e.transpose(pTw[:, g, :], wsb[:, g, :], L.identb[:]), reads=[wsb, L.identb], writes=[pTw], inc=(g == 7))
    P.op("dve", lambda e: e.tensor_copy(wsT[:], pTw[:]), reads=[pTw], writes=[wsT])
    bsT = P.sb([128, 4, 128], F32, "bsT")
    for g in range(8):
        P.dma("sp", bsT[(g % 2) * 64:(g % 2) * 64 + 64, g // 2, :], abs_d.ap[g, :].partition_broadcast(64), reads=[abs_d], writes=[bsT])
    xb = L.ring(1, [128, 4, D], F32, "xb"); csb = L.ring(2, [128, 4, 128], F32, "csb")
    hnr = L.ring(2, [128, D], BF, "hn"); jr = L.ring(2, [128, D], F32, "junk"); ssr = L.ring(2, [128, 2], F32, "ss")
    hTb = L.ring(1, [128, 8, 512], BF, "hTb")
    uTr = L.ring(1, [128, 4, 512], F32, "uT")
    pur = L.ring(2, [128, 512], F32, "pu", psum=True)
    pvr = L.ring(2, [128, 512], F32, "pv", psum=True)
    pmr = L.ring(2, [128, 128], F32, "pm", psum=True)
    gvr = L.ring(2, [128, 512], F32, "gv"); vbr = L.ring(2, [128, 512], BF, "vb"); tmr = L.ring(2, [128, 128], F32, "tm")
    CTr = L.ring(2, [128, 64], F32, "CT"); STr = L.ring(2, [128, 64], F32, "ST")
    tmps = [tuple([P.sb([128, 512], F32, "sq"), P.sb([128, 512], F32, "qn"), P.sb([128, 512], F32, "t1"), P.sb([128, 8], F32, "ssq")]) for _ in range(2)]
    qbr = L.ring(2, [128, 512], BF, "qb")
    for bi, (t0, nt) in enumerate(blocks):
        j = 1 if bi == 0 else 0
        N = nt * 128
        x = xb.next(); cs = csb.next(); hT = hTb.next(); uT = uTr.next()
        xb_ap, xb_t = xown_blk(t0, nt)
        P.dma("sp", x[:, 0:nt, :], xb_ap.rearrange("(t p) d -> p t d", p=128), reads=[xb_t], writes=[x])
        P.dma("sp", cs[:, 0:nt, :], csown[t0 * 128:(t0 + nt) * 128, :].rearrange("(t p) d -> p t d", p=128), reads=[csown], writes=[cs])
        for t in range(nt):
            hn = hnr.next(); junk = jr.next(); ss = ssr.next(); pT = pTr.next()
            L.norm_tile(x[:, t, :], x, A1[j], sh1[j], hn, junk, ss)
            L.transpose_cols(hn, 8, pT, lambda hT=hT, t=t: hT[:, :, t * 128:(t + 1) * 128], hT)
        for c in range(4):
            pu = pur.next()
            L.proj_feat(pu, hT, N, Win, c * 128)
            P.op("act", lambda e, pu=pu, uT=uT, c=c, N=N: e.activation(uT[:, c, 0:N], pu[:, 0:N], AF.Gelu_apprx_tanh), reads=[pu], writes=[uT])
        for t in range(nt):
            tsl = slice(t * 128, (t + 1) * 128)
            gsl = slice((t0 + t) * 128, (t0 + t + 1) * 128)
            pv = pvr.next(); gv = gvr.next(); vb = vbr.next(); junk = jr.next(); ss = ssr.next()
            L.proj_tok(pv, hT, tsl, Win, 512, 512)
            P.op("act", lambda e, gv=gv, pv=pv: e.activation(gv[:], pv[:], AF.Gelu_apprx_tanh), reads=[pv], writes=[gv])
            P.op("act", lambda e, gv=gv, junk=junk, ss=ss: e.activation(junk[:, 0:512], gv[:], AF.Square, accum_out=ss[:, 0:1]), reads=[gv], writes=[junk, ss])
            L.rstd(ss, 1, 512)
            P.op("dve", lambda e, vb=vb, gv=gv, ss=ss: e.scalar_tensor_tensor(vb[:], gv[:], ss[:, 0:1], anorm[:], ALU.mult, ALU.mult),
                 reads=[gv, ss, anorm], writes=[vb])
            for jp in range(4):
                pm = pmr.next(); tm = tmr.next()
                for hh in range(2):
                    g = 2 * jp + hh
                    P.op("pe", lambda e, pm=pm, vb=vb, g=g, hh=hh: e.matmul(pm[hh * 64:(hh + 1) * 64, :], vb[:, g * 64:(g + 1) * 64], wsT[:, g, :],
                                                                         start=True, stop=True), reads=[vb, wsT], writes=[pm], inc=(hh == 1))
                P.op("dve", lambda e, pm=pm, tm=tm, jp=jp: e.tensor_tensor(tm[:], pm[:], bsT[:, jp, :], ALU.add), reads=[pm, bsT], writes=[tm])
                P.op("pool", lambda e, tm=tm, jp=jp, uT=uT, tsl=tsl, gsl=gsl: e.tensor_tensor(yT[:, jp, gsl], tm[:], uT[:, jp, tsl], ALU.mult),
                     reads=[tm, uT], writes=[yT])
            pq = pur.next(); CT = CTr.next(); ST = STr.next(); qb = qbr.next(); pT = pTr.next()
            L.proj_tok(pq, hT, tsl, Win, 1024, 512)
            L.rope_tables(T_view(cs, cs[:, t, :]), gq, gqs, CT, ST)
            L.qk_post(pq, 8, CT, ST, qb, tmps[t % 2])
            for c4 in range(4):
                P.op("pe", lambda e, c4=c4, pT=pT, qb=qb: e.transpose(pT[:, c4, :], qb[:, c4 * 128:(c4 + 1) * 128], L.identb[:]),
                     reads=[qb, L.identb], writes=[pT], inc=(c4 == 3))
            P.op("dve", lambda e, pT=pT, gsl=gsl: e.tensor_copy(QT[0:64, 0, :, gsl], pT[0:64, 0:4, :]), reads=[pT], writes=[QT])
            P.op("act", lambda e, pT=pT, gsl=gsl: e.activation(QT[64:128, 1, :, gsl], pT[64:128, 0:4, :], AF.Identity), reads=[pT], writes=[QT])
    P.phase_end()

    P.phase_begin()
    subbc = P.sb([128, 128], F32, "subbc"); L.load_bc(subbc, sub_d.ap, sub_d)
    P.op("dve", lambda e: e.tensor_scalar(subbc[:], subbc[:], 1.0 - LAM_INIT, None, ALU.mult), reads=[subbc], writes=[subbc])
    KTh = L.ring(2, [128, NALL], BF, "KTh")
    Vh = L.ring(2, [128, NT_ALL, 129], BF, "Vh")
    for v_ in Vh.tiles:
        P.op("pool", lambda e, v_=v_: e.memset(v_[:, :, 128:129], 1.0), writes=[v_])
    spr = L.ring(4, [128, 2, 256], F32, "sp", psum=True)
    LOOK = 3
    accs = [[P.ps([128, 512], F32, "acc") for c in range(2)] for qi in range(2)]
    pTr_ = L.ring(6, [128, 2, 256], BF, "pTs")
    rz = L.ring(2, [128, 4], F32, "rz"); o1r = L.ring(2, [128, 128], F32, "o1"); o2r = L.ring(2, [128, 128], F32, "o2")
    jr = L.ring(2, [128, 128], F32, "junk"); ssr = L.ring(2, [128, 2], F32, "ss"); ybr = L.ring(2, [128, 128], BF, "yb")
    for h in range(4):
        kth = KTh.next(); vh = Vh.next()
        for pc in range(6):
            P.dma("sp", kth[:, pc * 1408:(pc + 1) * 1408], KT_d[h, :, pc * 1408:(pc + 1) * 1408], reads=[KT_d], writes=[kth])
        for pc in range(11):
            P.dma("sp", vh[:, pc * 6:(pc + 1) * 6, 0:128],
                  VS_d[pc * 768:(pc + 1) * 768, h * 128:(h + 1) * 128].rearrange("(t p) e -> p t e", p=128), reads=[VS_d], writes=[vh])
        for qb_ in range(NOWN // 256):
            keys = [0, 1] if qb_ == 0 else list(range(NT_ALL))
            q0 = qb_ * 256
            def emit_scores(kt, kth=kth, q0=q0, h=h):
                sp_ = spr.next()
                for c in range(2):
                    P.op("pe", lambda e, c=c, sp_=sp_, kth=kth, kt=kt, q0=q0, h=h: e.matmul(
                        sp_[:, c, :], kth[:, kt * 128:(kt + 1) * 128], QT[:, c, h, q0:q0 + 256],
                        start=True, stop=True), reads=[kth, QT], writes=[sp_], inc=(c == 1))
                return sp_

            pend_sc = [emit_scores(keys[i]) for i in range(min(LOOK, len(keys)))]
            for ki, kt in enumerate(keys):
                sp_ = pend_sc.pop(0)
                if ki + LOOK < len(keys):
                    pend_sc.append(emit_scores(keys[ki + LOOK]))
                pt = pTr_.next()
                P.op("act", lambda e, pt=pt, sp_=sp_: e.activation(pt[:], sp_[:], AF.Exp, scale=0.125), reads=[sp_], writes=[pt])
                for qi in range(2):
                    for c in range(2):
                        P.op("pe", lambda e, qi=qi, c=c, pt=pt, vh=vh, kt=kt, ki=ki, keys=keys: e.matmul(
                            accs[qi][c][:, 0:129], pt[:, c, qi * 128:(qi + 1) * 128], vh[:, kt, :],
                            start=(ki == 0), stop=(ki == len(keys) - 1)), reads=[pt, vh], writes=[accs[qi][c]],
                            inc=(ki == len(keys) - 1))
            for qi in range(2):
                r = rz.next(); o1 = o1r.next(); o2 = o2r.next(); junk = jr.next(); ss = ssr.next(); yb = ybr.next()
                a0, a1 = accs[qi]
                P.op("dve", lambda e, r=r, a0=a0: e.reciprocal(r[:, 0:1], a0[:, 128:129]), reads=[a0], writes=[r])
                P.op("dve", lambda e, r=r, a1=a1: e.reciprocal(r[:, 1:2], a1[:, 128:129]), reads=[a1], writes=[r])
                P.op("dve", lambda e, r=r: e.tensor_tensor(r[:, 2:3], r[:, 1:2], neglam[:], ALU.mult), reads=[r, neglam], writes=[r])
                P.op("act", lambda e, o1=o1, a0=a0, r=r: e.activation(o1[:], a0[:, 0:128], AF.Identity, scale=r[:, 0:1]), reads=[a0, r], writes=[o1])
                P.op("dve", lambda e, o2=o2, a1=a1, r=r, o1=o1: e.scalar_tensor_tensor(o2[:], a1[:, 0:128], r[:, 2:3], o1[:], ALU.mult, ALU.add),
                     reads=[a1, r, o1], writes=[o2])
                P.op("act", lambda e, junk=junk, o2=o2, ss=ss: e.activation(junk[:], o2[:], AF.Square, accum_out=ss[:, 0:1]), reads=[o2], writes=[junk, ss])
                L.rstd(ss, 1, 128)
                P.op("dve", lambda e, yb=yb, o2=o2, ss=ss: e.scalar_tensor_tensor(yb[:], o2[:], ss[:, 0:1], subbc[:], ALU.mult, ALU.mult),
                     reads=[o2, ss, subbc], writes=[yb])
                gsl = slice(q0 + qi * 128, q0 + (qi + 1) * 128)
                P.op("pe", lambda e, yb=yb, a0=a0: e.transpose(a0[:].bitcast(BF)[:, 0:128], yb[:], L.identb[:]), reads=[yb, L.identb], writes=[a0])
                P.op("act", lambda e, gsl=gsl, h=h, a0=a0: e.activation(yT[:, 4 + h, gsl], a0[:].bitcast(BF)[:, 0:128], AF.Identity), reads=[a0], writes=[yT])
    P.phase_end()

    qstack.close()
    ffn_phase(L, xown_blk, xo_blk, blocks, yT, wout_d, gffn, w1_d, w3_d, w2_d, ctx_block=True)
    P.barrier()
    P.flush()
    P.gstack.close()
    P.gstack = _outer


def T_view(t, ap):
    return _View(t, ap)


class _View:
    def __init__(self, base, ap):
        object.__setattr__(self, "base", base)
        object.__setattr__(self, "ap", ap)

    def __getitem__(self, k):
        return self.ap[k]

    @property
    def w(self):
        return self.base.w

    @w.setter
    def w(self, v):
        self.base.w = v

    @property
    def r(self):
        return self.base.r

    @r.setter
    def r(self, v):
        self.base.r = v

    @property
    def name(self):
        return self.base.name


def ffn_phase(L, xown, xo, blocks, yT, wout_d, gffn, w1_d, w3_d, w2_d, ctx_block, moe=None):
    P = L.P
    P.phase_begin()
    nvar = 2 if ctx_block else 1
    g1 = [L.mod_bc(j, 2, "g1") for j in range(nvar)]
    A2 = [L.make_A(L.mod_bc(j, 4, "A2"), gffn) for j in range(nvar)]
    sh2 = [L.mod_bc(j, 3, "sh2") for j in range(nvar)]
    g2 = [L.mod_bc(j, 5, "g2") for j in range(nvar)]
    wout = P.sb([128, 8, D], BF, "wout")
    for h in range(2):
        P.dma("pool", wout[:, :, h * 512:(h + 1) * 512], wout_d[:, h * 512:(h + 1) * 512].rearrange("(kc p) n -> p kc n", p=128),
              reads=[wout_d], writes=[wout])
    NE = 1
    if moe is not None:
        rw_d, rb_d, NE = moe
        wr = P.sb([128, 8, 8], F32, "wr")
        P.dma("sp", wr[:], rw_d.ap.rearrange("(kc p) e -> p kc e", p=128), reads=[rw_d], writes=[wr])
        rb = P.sb([128, 8], F32, "rb"); L.load_bc(rb, rb_d.ap, rb_d)
        hnf_r = L.ring(2, [128, D], F32, "hnf")
        tTr = L.ring(2, [128, 8, 128], F32, "tT")
        pXr = L.ring(1, [128, 8, 128], F32, "pX", psum=True)
        dg = P.sb([128, 4, 8], F32, "dg")
        rt = [P.sb([128, 8], F32, "rt") for _ in range(4)]
        rs = P.sb([128, 4], F32, "rs")
    xb = L.ring(1, [128, 4, D], F32, "xb")
    hnr = L.ring(2, [128, D], BF, "hn"); jr = L.ring(2, [128, D], F32, "junk"); ssr = L.ring(2, [128, 2], F32, "ss")
    hTb = L.ring(1, [128, 8, 512], BF, "h2T")
    gT = P.sb([128, NFC, 512], BF, "gT")
    pTr = L.ring(1 if moe is not None else 2, [128, 8, 128], BF, "pT", psum=True)
    por = L.ring(1 if moe is not None else 2, [128, 512], F32, "po", psum=True)
    p1r = L.ring(2, [128, 512], F32, "p1", psum=True); p3r = L.ring(2, [128, 512], F32, "p3", psum=True)
    tmr = L.ring(2, [128, 512], F32, "tm")
    s1r = L.ring(2, [128, 512], F32, "s1")
    w1r = L.ring(2, [128, 8, 256], BF, "w1p"); w3r = L.ring(2, [128, 8, 256], BF, "w3p")
    w2r = L.ring(2, [128, NFC, 256], BF, "w2q")
    for bi, (t0, nt) in enumerate(blocks):
        j = 1 if (ctx_block and bi == 0) else 0
        N = nt * 128
        x = xb.next(); hT = hTb.next()
        xb_ap, xb_t = xown(t0, nt)
        P.dma("sp", x[:, 0:nt, :], xb_ap.rearrange("(t p) d -> p t d", p=128), reads=[xb_t], writes=[x])
        for t in range(nt):
            gsl = slice((t0 + t) * 128, (t0 + t + 1) * 128)
            for hf in range(2):
                po = por.next(); tm = tmr.next()
                for kc in range(8):
                    P.op("pe", lambda e, kc=kc, po=po, gsl=gsl, hf=hf: e.matmul(po[:], yT[:, kc, gsl], wout[:, kc, hf * 512:(hf + 1) * 512],
                                                                              start=(kc == 0), stop=(kc == 7)),
                         reads=[yT, wout], writes=[po], inc=(kc == 7))
                P.op("dve", lambda e, tm=tm, po=po, hf=hf, j=j: e.tensor_tensor(tm[:], po[:], g1[j][:, hf * 512:(hf + 1) * 512], ALU.mult),
                     reads=[po, g1[j]], writes=[tm])
                P.op("dve", lambda e, x=x, t=t, hf=hf, tm=tm: e.tensor_tensor(x[:, t, hf * 512:(hf + 1) * 512], x[:, t, hf * 512:(hf + 1) * 512], tm[:], ALU.add),
                     reads=[x, tm], writes=[x])
        for t in range(nt):
            hn = hnr.next(); junk = jr.next(); ss = ssr.next(); pT = pTr.next()
            if moe is None:
                L.norm_tile(x[:, t, :], x, A2[j], sh2[j], hn, junk, ss)
            else:
                hnf = hnf_r.next(); tT = tTr.next(); pX = pXr.next()
                P.op("act", lambda e, junk=junk, x=x, t=t, ss=ss: e.activation(junk[:], x[:, t, :], AF.Square, accum_out=ss[:, 0:1]),
                     reads=[x], writes=[junk, ss])
                L.rstd(ss, 1, D)
                P.op("dve", lambda e, junk=junk, x=x, t=t, ss=ss, j=j: e.scalar_tensor_tensor(junk[:], x[:, t, :], ss[:, 0:1], A2[j][:], ALU.mult, ALU.mult),
                     reads=[x, ss, A2[j]], writes=[junk])
                P.op("dve", lambda e, hnf=hnf, junk=junk, j=j: e.tensor_tensor(hnf[:], junk[:], sh2[j][:], ALU.add), reads=[junk, sh2[j]], writes=[hnf])
                P.op("act", lambda e, hn=hn, hnf=hnf: e.activation(hn[:], hnf[:], AF.Identity), reads=[hnf], writes=[hn])
                for c in range(8):
                    P.op("pe", lambda e, c=c, pX=pX, hnf=hnf: e.transpose(pX[:, c, :], hnf[:, c * 128:(c + 1) * 128], L.ident[:]),
                         reads=[hnf, L.ident], writes=[pX], inc=(c == 7))
                P.op("dve", lambda e, tT=tT, pX=pX: e.tensor_copy(tT[:], pX[:]), reads=[pX], writes=[tT])
                plg = por.next()
                for kc in range(8):
                    P.op("pe", lambda e, kc=kc, plg=plg, tT=tT: e.matmul(plg[:, 0:8], tT[:, kc, :], wr[:, kc, :], start=(kc == 0), stop=(kc == 7)),
                         reads=[tT, wr], writes=[plg], inc=(kc == 7))
                lg, mk, l2, ex = rt
                P.op("dve", lambda e, plg=plg: e.tensor_tensor(lg[:], plg[:, 0:8], rb[:], ALU.add), reads=[plg, rb], writes=[lg])
                P.op("dve", lambda e: e.tensor_reduce(rs[:, 0:1], lg[:], AX.X, ALU.max), reads=[lg], writes=[rs])
                P.op("dve", lambda e: e.tensor_scalar(mk[:], lg[:], rs[:, 0:1], None, ALU.is_equal), reads=[lg, rs], writes=[mk])
                P.op("dve", lambda e: e.scalar_tensor_tensor(l2[:], mk[:], -1e30, lg[:], ALU.mult, ALU.add), reads=[mk, lg], writes=[l2])
                P.op("dve", lambda e: e.tensor_reduce(rs[:, 1:2], l2[:], AX.X, ALU.max), reads=[l2], writes=[rs])
                P.op("dve", lambda e: e.tensor_scalar(mk[:], lg[:], rs[:, 1:2], None, ALU.is_ge), reads=[lg, rs], writes=[mk])
                P.op("dve", lambda e: e.tensor_scalar(ex[:], lg[:], rs[:, 0:1], None, ALU.subtract), reads=[lg, rs], writes=[ex])
                P.op("act", lambda e: e.activation(ex[:], ex[:], AF.Exp), reads=[ex], writes=[ex])
                P.op("dve", lambda e: e.tensor_tensor(ex[:], ex[:], mk[:], ALU.mult), reads=[ex, mk], writes=[ex])
                P.op("dve", lambda e: e.tensor_reduce(rs[:, 2:3], ex[:], AX.X, ALU.add), reads=[ex], writes=[rs])
                P.op("dve", lambda e: e.reciprocal(rs[:, 3:4], rs[:, 2:3]), reads=[rs], writes=[rs])
                P.op("dve", lambda e, t=t: e.tensor_scalar(dg[:, t, :], ex[:], rs[:, 3:4], None, ALU.mult), reads=[ex, rs], writes=[dg])
            L.transpose_cols(hn, 8, pT, lambda hT=hT, t=t: hT[:, :, t * 128:(t + 1) * 128], hT)
        for ex_i in range(NE):
            w1e = w1_d.ap[ex_i] if moe is not None else w1_d.ap
            w3e = w3_d.ap[ex_i] if moe is not None else w3_d.ap
            w2e = w2_d.ap[ex_i] if moe is not None else w2_d.ap
            for pi in range(NFC // 2):
                w1p = w1r.next(); w3p = w3r.next()
                P.dma("pool", w1p[:], w1e[:, pi * 256:(pi + 1) * 256].rearrange("(kc p) n -> p kc n", p=128), reads=[w1_d], writes=[w1p])
                P.dma("pool", w3p[:], w3e[:, pi * 256:(pi + 1) * 256].rearrange("(kc p) n -> p kc n", p=128), reads=[w3_d], writes=[w3p])
                for fc in range(2):
                    p1 = p1r.next(); p3 = p3r.next(); s1 = s1r.next()
                    L.proj_feat(p1, hT, N, w1p, fc * 128)
                    L.proj_feat(p3, hT, N, w3p, fc * 128)
                    P.op("act", lambda e, s1=s1, p1=p1, N=N: e.activation(s1[:, 0:N], p1[:, 0:N], AF.Silu), reads=[p1], writes=[s1])
                    P.op("dve", lambda e, s1=s1, p3=p3, N=N, f=pi * 2 + fc: e.tensor_tensor(gT[:, f, 0:N], s1[:, 0:N], p3[:, 0:N], ALU.mult),
                         reads=[s1, p3], writes=[gT])
            for qd in range(4):
                w2q = w2r.next()
                P.dma("pool", w2q[:], w2e[:, qd * 256:(qd + 1) * 256].rearrange("(fc p) n -> p fc n", p=128), reads=[w2_d], writes=[w2q])
                for t in range(nt):
                    po = por.next(); tm = tmr.next()
                    for fc in range(NFC):
                        P.op("pe", lambda e, fc=fc, po=po, t=t, w2q=w2q: e.matmul(po[:, 0:256], gT[:, fc, t * 128:(t + 1) * 128], w2q[:, fc, :],
                                                                                start=(fc == 0), stop=(fc == NFC - 1)),
                             reads=[gT, w2q], writes=[po], inc=(fc == NFC - 1))
                    P.op("dve", lambda e, tm=tm, po=po, qd=qd, j=j: e.tensor_tensor(tm[:, 0:256], po[:, 0:256], g2[j][:, qd * 256:(qd + 1) * 256], ALU.mult),
                         reads=[po, g2[j]], writes=[tm])
                    if moe is None:
                        P.op("dve", lambda e, x=x, t=t, qd=qd, tm=tm: e.tensor_tensor(x[:, t, qd * 256:(qd + 1) * 256], x[:, t, qd * 256:(qd + 1) * 256], tm[:, 0:256], ALU.add),
                             reads=[x, tm], writes=[x])
                    else:
                        P.op("dve", lambda e, x=x, t=t, qd=qd, tm=tm, ex_i=ex_i: e.scalar_tensor_tensor(
                            x[:, t, qd * 256:(qd + 1) * 256], tm[:, 0:256], dg[:, t, ex_i:ex_i + 1], x[:, t, qd * 256:(qd + 1) * 256], ALU.mult, ALU.add),
                            reads=[x, tm, dg], writes=[x])
        xo_ap, xo_t = xo(t0, nt)
        P.dma("sp", xo_ap.rearrange("(t p) d -> p t d", p=128), x[:, 0:nt, :], reads=[x], writes=[xo_t])
    P.phase_end()


STOP1 = None


def emit_l1(L, io):
    nc = L.nc
    P = L.P
    _outer = P.gstack
    P.gstack = ExitStack()
    xall_tile = io["xall_tile"]; xown_blk = io["xown_blk"]; xo_blk = io["xo_blk"]
    csall = io["csall"]; csown = io["csown"]; ident_d = io["ident_d"]; cvec_d = io["cvec_d"]
    sfx = io["sfx"]
    L.modrow_d = io["modrow"]
    gmix_d = L.din("norm_mix_g" + sfx, [D])
    win_d = L.din("o_w_in", [D, 1792])
    qg_d = L.din("c_qnorm_g", [64])
    kg_d = L.din("c_knorm_g", [64])
    lrup_d = L.din("lru_p", [128, 4, 12])
    wa_d = L.din("d_wa", [2, 8, 64, 64])
    wx_d = L.din("d_wx", [2, 8, 64, 64])
    segw_d = L.din("segw", [4])
    KT_d = L.dscratch("KT1_d", [128, NALL], BF)
    VS_d = L.dscratch("VS1_d", [NALL, 128], BF)
    XR_d = L.dscratch("XR_d", [4, 128, NALL], F32)

    gq = P.sb([128, 64], F32, "gq", glob=True); gqs = P.sb([128, 64], F32, "gqs", glob=True)
    gk = P.sb([128, 64], F32, "gk", glob=True); gks = P.sb([128, 64], F32, "gks", glob=True)
    L.load_bc(gq, qg_d.ap, qg_d); L.load_bc(gk, kg_d.ap, kg_d)
    L.swap_gain(gq, gqs); L.swap_gain(gk, gks)
    gmix = P.sb([128, D], F32, "gmix", glob=True)
    L.load_bc(gmix, gmix_d.ap, gmix_d)

    yT = P.sb([128, 8, NOWN], BF, "yT", glob=True)
    gstack = ExitStack()
    gateT = T("gateT", gstack.enter_context(nc.sbuf_tensor("gateT_l1", [128, 4, OWN], BF)).ap())
    qstack = ExitStack()
    QT = T("QT", qstack.enter_context(nc.sbuf_tensor("QT_l1", [128, 2, 4, NOWN], BF)).ap())
    P.op("pool", lambda e: e.memset(QT[64:128, 0, :, :], 0.0), writes=[QT])
    P.op("pool", lambda e: e.memset(QT[0:64, 1, :, :], 0.0), writes=[QT])
    blocks = [(2 + 4 * i, 4) for i in range(4)]

    P.phase_begin()
    A1 = [L.make_A(L.mod_bc(0, 1, "A1"), gmix)]
    sh1 = [L.mod_bc(0, 0, "sh1")]
    Win = P.sb([128, 8, 1024], BF, "Win")
    for g in range(4):
        for n in range(2):
            h = n * 4 + g
            P.dma("pool", Win[:, :, g * 128 + n * 64:g * 128 + (n + 1) * 64],
                  win_d[:, h * 64:(h + 1) * 64].rearrange("(kc p) n -> p kc n", p=128), reads=[win_d], writes=[Win])
    P.dma("pool", Win[:, :, 512:1024], win_d[:, 768:1280].rearrange("(kc p) n -> p kc n", p=128), reads=[win_d], writes=[Win])
    xb = L.ring(1, [128, 4, D], F32, "xb"); csb = L.ring(2, [128, 4, 128], F32, "csb")
    hnr = L.ring(2, [128, D], BF, "hn"); jr = L.ring(2, [128, D], F32, "junk"); ssr = L.ring(2, [128, 2], F32, "ss")
    hTb = L.ring(1, [128, 8, 512], BF, "hTb")
    pTr = L.ring(2, [128, 8, 128], BF, "pT", psum=True)
    pur = L.ring(2, [128, 512], F32, "pu", psum=True)
    CTr = L.ring(2, [128, 64], F32, "CT"); STr = L.ring(2, [128, 64], F32, "ST")
    tmps = [tuple([P.sb([128, 512], F32, "sq"), P.sb([128, 512], F32, "qn"), P.sb([128, 512], F32, "t1"), P.sb([128, 8], F32, "ssq")]) for _ in range(2)]
    qbr = L.ring(2, [128, 512], BF, "qb")
    for bi, (t0, nt) in enumerate(blocks):
        N = nt * 128
        x = xb.next(); cs = csb.next(); hT = hTb.next()
        xb_ap, xb_t = xown_blk(t0, nt)
        P.dma("sp", x[:, 0:nt, :], xb_ap.rearrange("(t p) d -> p t d", p=128), reads=[xb_t], writes=[x])
        P.dma("sp", cs[:, 0:nt, :], csown[t0 * 128:(t0 + nt) * 128, :].rearrange("(t p) d -> p t d", p=128), reads=[csown], writes=[cs])
        for t in range(nt):
            hn = hnr.next(); junk = jr.next(); ss = ssr.next(); pT = pTr.next()
            L.norm_tile(x[:, t, :], x, A1[0], sh1[0], hn, junk, ss)
            L.transpose_cols(hn, 8, pT, lambda hT=hT, t=t: hT[:, :, t * 128:(t + 1) * 128], hT)
        for c in range(4):
            pu = pur.next()
            L.proj_feat(pu, hT, N, Win, 512 + c * 128)
            P.op("act", lambda e, pu=pu, c=c, N=N, t0=t0: e.activation(gateT[:, c, (t0 - 2) * 128:(t0 - 2) * 128 + N], pu[:, 0:N], AF.Gelu_apprx_tanh),
                 reads=[pu], writes=[gateT])
        for t in range(nt):
            tsl = slice(t * 128, (t + 1) * 128)
            gsl = slice((t0 + t) * 128, (t0 + t + 1) * 128)
            pq = pur.next(); CT = CTr.next(); ST = STr.next(); qb = qbr.next(); pT = pTr.next()
            L.proj_tok(pq, hT, tsl, Win, 0, 512)
            L.rope_tables(T_view(cs, cs[:, t, :]), gq, gqs, CT, ST)
            L.qk_post(pq, 8, CT, ST, qb, tmps[t % 2])
            for c4 in range(4):
                P.op("pe", lambda e, c4=c4, pT=pT, qb=qb: e.transpose(pT[:, c4, :], qb[:, c4 * 128:(c4 + 1) * 128], L.identb[:]),
                     reads=[qb, L.identb], writes=[pT], inc=(c4 == 3))
            P.op("dve", lambda e, pT=pT, gsl=gsl: e.tensor_copy(QT[0:64, 0, :, gsl], pT[0:64, 0:4, :]), reads=[pT], writes=[QT])
            P.op("act", lambda e, pT=pT, gsl=gsl: e.activation(QT[64:128, 1, :, gsl], pT[64:128, 0:4, :], AF.Identity), reads=[pT], writes=[QT])
    P.phase_end()

    P.phase_begin()
    A1 = [L.make_A(L.mod_bc(j, 1, "A1"), gmix) for j in range(2)]
    sh1 = [L.mod_bc(j, 0, "sh1") for j in range(2)]
    Wk = P.sb([128, 8, 768], BF, "Wkvx")
    P.dma("pool", Wk[:, :, 0:256], win_d[:, 512:768].rearrange("(kc p) n -> p kc n", p=128), reads=[win_d], writes=[Wk])
    P.dma("pool", Wk[:, :, 256:768], win_d[:, 1280:1792].rearrange("(kc p) n -> p kc n", p=128), reads=[win_d], writes=[Wk])
    xr = L.ring(3, [128, D], F32, "xa"); csr = L.ring(3, [128, 128], F32, "csa")
    hnr = L.ring(2, [128, D], BF, "hn"); jr = L.ring(2, [128, D], F32, "junk"); ssr = L.ring(2, [128, 2], F32, "ss")
    hTr = L.ring(2, [128, 8, 128], BF, "hT")
    pTr = L.ring(2, [128, 8, 128], BF, "pT", psum=True)
    pkr = L.ring(2, [128, 512], F32, "pk", psum=True); pxr = L.ring(2, [128, 512], F32, "px", psum=True)
    pXr = L.ring(2, [128, 4, 128], F32, "pX", psum=True)
    CTr = L.ring(2, [128, 64], F32, "CT"); STr = L.ring(2, [128, 64], F32, "ST")
    tmps = [tuple([P.sb([128, 512], F32, "sq"), P.sb([128, 512], F32, "qn"), P.sb([128, 512], F32, "t1"), P.sb([128, 8], F32, "ssq")]) for _ in range(2)]
    kbr = L.ring(2, [128, 128], BF, "kb"); vbr = L.ring(2, [128, 128], BF, "vb"); ktr = L.ring(2, [128, 1, 128], BF, "kt")
    xsr = L.ring(2, [128, 512], F32, "xs"); xtr = L.ring(2, [128, 4, 128], F32, "xt")
    for t in range(NT_ALL):
        j = 1 if t < 2 else 0
        x = xr.next(); cs = csr.next(); hn = hnr.next(); junk = jr.next(); ss = ssr.next(); hT = hTr.next(); pT = pTr.next()
        xa_ap, xa_t = xall_tile(t)
        P.dma("sp", x[:], xa_ap, reads=[xa_t], writes=[x])
        P.dma("sp", cs[:], csall[t * 128:(t + 1) * 128, :], reads=[csall], writes=[cs])
        L.norm_tile(x[:], x, A1[j], sh1[j], hn, junk, ss)
        L.transpose_cols(hn, 8, pT, lambda hT=hT: hT[:], hT)
        pk = pkr.next(); px = pxr.next()
        L.proj_tok(pk, hT, slice(0, 128), Wk, 0, 256)
        L.proj_tok(px, hT, slice(0, 128), Wk, 256, 512)
        vb = vbr.next()
        P.op("act", lambda e, vb=vb, pk=pk: e.activation(vb[:], pk[:, 128:256], AF.Identity), reads=[pk], writes=[vb])
        P.dma("pool", VS_d[t * 128:(t + 1) * 128, :], vb[:], reads=[vb], writes=[VS_d])
        CT = CTr.next(); ST = STr.next()
        L.rope_tables(cs, gk, gks, CT, ST)
        kb = kbr.next()
        L.qk_post(pk, 2, CT, ST, kb, tmps[t % 2])
        pT2 = pTr.next(); kt = ktr.next()
        L.transpose_cols(kb, 1, pT2, lambda kt=kt: kt[:], kt, eng="dve")
        P.dma("pool", KT_d[:, t * 128:(t + 1) * 128], kt[:, 0, :], reads=[kt], writes=[KT_d])
        xs = xsr.next(); xt = xtr.next(); pX = pXr.next()
        P.op("act", lambda e, xs=xs, px=px: e.activation(xs[:], px[:], AF.Identity), reads=[px], writes=[xs])
        for c in range(4):
            P.op("pe", lambda e, c=c, pX=pX, xs=xs: e.transpose(pX[:, c, :], xs[:, c * 128:(c + 1) * 128], L.ident[:]),
                 reads=[xs, L.ident], writes=[pX], inc=(c == 3))
        P.op("dve", lambda e, xt=xt, pX=pX: e.tensor_copy(xt[:], pX[:]), reads=[pX], writes=[xt])
        P.dma("pool", XR_d[:, :, t * 128:(t + 1) * 128].rearrange("j p t -> p j t"), xt[:], reads=[xt], writes=[XR_d])
    P.phase_end()

    P.phase_begin()
    KT = P.sb([128, NALL], BF, "KT")
    for pc in range(6):
        P.dma("sp", KT[:, pc * 1408:(pc + 1) * 1408], KT_d[:, pc * 1408:(pc + 1) * 1408], reads=[KT_d], writes=[KT])
    Vn = [P.sb([128, NT_ALL, 128], BF, "Vn") for _ in range(2)]
    for n in range(2):
        P.op("pool", lambda e, n=n: e.memset(Vn[n][:, :, 64:128], 1.0), writes=[Vn[n]])
        for pc in range(11):
            P.dma("sp", Vn[n][:, pc * 6:(pc + 1) * 6, 0:64],
                  VS_d[pc * 768:(pc + 1) * 768, n * 64:(n + 1) * 64].rearrange("(t p) e -> p t e", p=128), reads=[VS_d], writes=[Vn[n]])
    spr = L.ring(4, [128, 512], F32, "sp", psum=True)
    accr = L.ring(2, [128, 512], F32, "acc", psum=True)
    pXo = L.ring(1, [128, 4, 128], F32, "pXo", psum=True)
    LOOK1 = 3
    pTo = P.ps([128, 4, 128], BF, "pTo")
    ptr_ = L.ring(6, [128, 512], BF, "pts")
    rzr = L.ring(2, [128, 4], F32, "rz")
    osr = L.ring(2, [128, 512], F32, "osb")
    yar = L.ring(2, [128, 512], BF, "yat")
    for qt in range(OWN // 128):
        q0 = LC + qt * 128
        yat = yar.next()
        for n in range(2):
            acc = accr.next()

            def emit_scores1(kt, n=n, q0=q0):
                sp_ = spr.next()
                P.op("pe", lambda e, n=n, sp_=sp_, kt=kt, q0=q0: e.matmul(
                    sp_[:].rearrange("p (g q) -> p g q", g=4), KT[:, kt * 128:(kt + 1) * 128],
                    QT[:, n, :, q0:q0 + 128], start=True, stop=True), reads=[KT, QT], writes=[sp_])
                return sp_

            pend_sc = [emit_scores1(i) for i in range(LOOK1)]
            for kt in range(NT_ALL):
                sp_ = pend_sc.pop(0)
                if kt + LOOK1 < NT_ALL:
                    pend_sc.append(emit_scores1(kt + LOOK1))
                pt = ptr_.next()
                P.op("act", lambda e, pt=pt, sp_=sp_: e.activation(pt[:], sp_[:], AF.Exp, scale=0.125), reads=[sp_], writes=[pt])
                P.op("pe", lambda e, pt=pt, n=n, kt=kt, acc=acc: e.matmul(acc[:], Vn[n][:, kt, :], pt[:], start=(kt == 0), stop=(kt == NT_ALL - 1)),
                     reads=[pt, Vn[n]], writes=[acc], inc=(kt == NT_ALL - 1))
            osb = osr.next(); pX = pXo.next(); rz = rzr.next()
            P.op("act", lambda e, osb=osb, acc=acc: e.activation(osb[:], acc[:], AF.Identity), reads=[acc], writes=[osb])
            for g in range(4):
                P.op("pe", lambda e, g=g, pX=pX, osb=osb: e.transpose(pX[:, g, :], osb[:, g * 128:(g + 1) * 128], L.ident[:]),
                     reads=[osb, L.ident], writes=[pX], inc=(g == 3))
            P.op("dve", lambda e, rz=rz, pX=pX: e.reciprocal(rz[:, 0:4], pX[:, :, 64]), reads=[pX], writes=[rz])
            for g in range(4):
                h = n * 4 + g
                P.op("act", lambda e, g=g, h=h, rz=rz, yat=yat, pX=pX: e.activation(yat[:, h * 64:(h + 1) * 64], pX[:, g, 0:64], AF.Identity, scale=rz[:, g:g + 1]),
                     reads=[pX, rz], writes=[yat])
        for c in range(4):
            P.op("pe", lambda e, c=c, yat=yat: e.transpose(pTo[:, c, :], yat[:, c * 128:(c + 1) * 128], L.identb[:]),
                 reads=[yat, L.identb], writes=[pTo], inc=(c == 3))
        P.op("act", lambda e, q0=q0: e.activation(yT[:, 0:4, q0:q0 + 128], pTo[:], AF.Identity), reads=[pTo], writes=[yT])
    P.phase_end()
    qstack.close()

    P.phase_begin()
    lp = P.sb([128, 4, 12], F32, "lp")
    P.dma("sp", lp[:], lrup_d[:, :, :], reads=[lrup_d], writes=[lp])
    sw = P.sb([128, 4], F32, "segw"); L.load_bc(sw, segw_d.ap, segw_d)
    nsp = P.sb([128, 4, 2], F32, "nsp")
    P.op("act", lambda e: e.activation(nsp[:], lp[:, :, 9:11], AF.Exp, scale=-1.0), reads=[lp], writes=[nsp])
    P.op("act", lambda e: e.activation(nsp[:], nsp[:], AF.Ln, bias=1.0), reads=[nsp], writes=[nsp])
    P.op("dve", lambda e: e.tensor_scalar(nsp[:], nsp[:], -8.0, None, ALU.mult), reads=[nsp], writes=[nsp])
    BD = P.sb([128, 16, 128], BF, "BD")
    P.op("dve", lambda e: e.memset(BD[:], 0.0), writes=[BD])
    for kind, wd in enumerate((wa_d, wx_d)):
        for d in range(2):
            for jj in range(4):
                for hh in range(2):
                    P.dma("pool", BD[hh * 64:(hh + 1) * 64, kind * 8 + d * 4 + jj, hh * 64:(hh + 1) * 64], wd.ap[d, 2 * jj + hh, :, :],
                          reads=[wd], writes=[BD])
    SEGL = 4096
    X = P.sb([128, NALL], F32, "Xr"); XD = P.sb([128, NALL], F32, "XD"); XDB = P.sb([128, NALL], BF, "XDB")
    Ab = T_view(X, X[:, 0:SEGL]); Bb = T_view(X, X[:, SEGL:2 * SEGL]); Hb = P.sb([128, SEGL], F32, "Hb")
    Ib = P.sb([128, SEGL], F32, "Ib")
    acc = P.sb([128, OWN], F32, "hacc")
    st = P.sb([128, 2], F32, "st")
    prr = L.ring(2, [128, 512], F32, "pr", psum=True); pir = L.ring(2, [128, 512], F32, "pi", psum=True)
    for jj in range(4):
        for pc in range(6):
            P.dma("sp", X[:, pc * 1408:(pc + 1) * 1408], XR_d[jj, :, pc * 1408:(pc + 1) * 1408], reads=[XR_d], writes=[X])
        P.op("dve", lambda e, jj=jj: e.tensor_scalar(XD[:], X[:], lp[:, jj, 2:3], lp[:, jj, 4:5], ALU.mult, ALU.add), reads=[X, lp], writes=[XD])
        for (lo, hi) in ((0, LC), (LC, NALL)):
            for tap, off in ((0, -2), (1, -1), (3, 1)):
                a = max(lo, lo - off); b = min(hi, hi - off)
                P.op("dve", lambda e, jj=jj, tap=tap, off=off, a=a, b=b: e.scalar_tensor_tensor(
                    XD[:, a:b], X[:, a + off:b + off], lp[:, jj, tap:tap + 1], XD[:, a:b], ALU.mult, ALU.add), reads=[X, lp, XD], writes=[XD])
        P.op("pool", lambda e: e.tensor_copy(XDB[:], XD[:]), reads=[XD], writes=[XDB])
        P.op("dve", lambda e: e.memset(acc[:], 0.0), writes=[acc])
        for d in range(2):
            segs = [(0, LC), (LC, LC + SEGL), (LC + SEGL, NALL)]
            order = segs if d == 0 else [segs[0], segs[2], segs[1]]
            for si, (lo, hi) in enumerate(order):
                n = hi - lo
                for b0 in range(0, n, 512):
                    bw = min(512, n - b0)
                    pr = prr.next(); pi_ = pir.next()
                    P.op("pe", lambda e, pr=pr, d=d, jj=jj, lo=lo, b0=b0, bw=bw: e.matmul(pr[:, 0:bw], BD[:, d * 4 + jj, :], XDB[:, lo + b0:lo + b0 + bw], start=True, stop=True),
                         reads=[BD, XDB], writes=[pr])
                    P.op("pe", lambda e, pi_=pi_, d=d, jj=jj, lo=lo, b0=b0, bw=bw: e.matmul(pi_[:, 0:bw], BD[:, 8 + d * 4 + jj, :], XDB[:, lo + b0:lo + b0 + bw], start=True, stop=True),
                         reads=[BD, XDB], writes=[pi_])
                    sl = slice(b0, b0 + bw)
                    P.op("act", lambda e, pr=pr, bw=bw, sl=sl, d=d, jj=jj: e.activation(Ab[:, sl], pr[:, 0:bw], AF.Sigmoid, bias=lp[:, jj, 5 + d:6 + d]), reads=[pr, lp], writes=[Ab])
                    P.op("act", lambda e, pi_=pi_, bw=bw, sl=sl, d=d, jj=jj: e.activation(Ib[:, sl], pi_[:, 0:bw], AF.Sigmoid, bias=lp[:, jj, 7 + d:8 + d]), reads=[pi_, lp], writes=[Ib])
                P.op("act", lambda e, n=n, d=d, jj=jj: e.activation(Ab[:, 0:n], Ab[:, 0:n], AF.Exp, scale=nsp[:, jj, d:d + 1]), reads=[Ab, nsp], writes=[Ab])
                P.op("dve", lambda e, n=n: e.tensor_tensor(Hb[:, 0:n], Ab[:, 0:n], Ab[:, 0:n], ALU.mult), reads=[Ab], writes=[Hb])
                P.op("act", lambda e, n=n: e.activation(Hb[:, 0:n], Hb[:, 0:n], AF.Sqrt, bias=1.0, scale=-1.0), reads=[Hb], writes=[Hb])
                P.op("dve", lambda e, n=n: e.tensor_tensor(Hb[:, 0:n], Hb[:, 0:n], Ib[:, 0:n], ALU.mult), reads=[Hb, Ib], writes=[Hb])
                P.op("pool", lambda e, n=n, lo=lo: e.tensor_tensor(Bb[:, 0:n], Hb[:, 0:n], XD[:, lo:lo + n], ALU.mult), reads=[Hb, XD], writes=[Bb])
                init = 0.0 if si == 0 else st[:, d:d + 1]
                if d == 0:
                    P.op("dve", lambda e, n=n, init=init: e.tensor_tensor_scan(Hb[:, 0:n], Ab[:, 0:n], Bb[:, 0:n], init, ALU.mult, ALU.add),
                         reads=[Ab, Bb, st], writes=[Hb])
                    P.op("dve", lambda e, n=n, d=d: e.tensor_copy(st[:, d:d + 1], Hb[:, n - 1:n]), reads=[Hb], writes=[st])
                else:
                    P.op("dve", lambda e, n=n, init=init: e.tensor_tensor_scan(Hb[:, 0:n][:, ::-1], Ab[:, 0:n][:, ::-1], Bb[:, 0:n][:, ::-1], init, ALU.mult, ALU.add),
                         reads=[Ab, Bb, st], writes=[Hb])
                    P.op("dve", lambda e, d=d: e.tensor_copy(st[:, d:d + 1], Hb[:, 0:1]), reads=[Hb], writes=[st])
                if lo >= LC:
                    s0 = (lo - LC) // OWN
                    for k in range(2):
                        P.op("dve", lambda e, k=k, s0=s0: e.scalar_tensor_tensor(acc[:], Hb[:, k * OWN:(k + 1) * OWN], sw[:, s0 + k:s0 + k + 1], acc[:], ALU.mult, ALU.add),
                             reads=[Hb, sw, acc], writes=[acc])
        P.op("dve", lambda e, jj=jj: e.tensor_tensor(yT[:, 4 + jj, LC:LC + OWN], acc[:], gateT[:, jj, :], ALU.mult), reads=[acc, gateT], writes=[yT])
    P.phase_end()
    gstack.close()

    gffn_d = L.din("norm_ffn_g" + sfx, [D])
    wout_d = L.din("w_out" + sfx, [D, D])
    rw_d = L.din("router_w", [D, 8]); rb_d = L.din("router_b", [8])
    w1_d = L.din("moe_w1", [8, D, FF]); w3_d = L.din("moe_w3", [8, D, FF]); w2_d = L.din("moe_w2", [8, FF, D])
    gffn = P.sb([128, D], F32, "gffn", glob=True)
    L.load_bc(gffn, gffn_d.ap, gffn_d)
    ffn_phase(L, xown_blk, xo_blk, blocks, yT, wout_d, gffn, w1_d, w3_d, w2_d, ctx_block=False, moe=(rw_d, rb_d, 8))
    P.barrier()
    P.flush()
    P.gstack.close()
    P.gstack = _outer


def build_fused():
    nc = bass.Bass("TRN2", target_bir_lowering=False)
    L = LK(nc)
    P = L.P
    xall = L.din("xall", [NALL, D])
    xown = L.din("xown", [NOWN, D])
    csall = L.din("csall", [NALL, 128])
    csown = L.din("csown", [NOWN, 128])
    ident_d = L.din("ident", [128, 128])
    cvec_d = L.din("cvec", [128, 16])
    xo = L.dout("xo", [OWN, D])
    xc1_d = L.dscratch("xc1_d", [LC, D])
    x1own_d = L.dscratch("x1own_d", [OWN, D])
    NCH = OWN // 256
    x1g_d = [L.dscratch(f"x1g{k}_d", [4 * 256, D]) for k in range(NCH)]
    L.load_ident(ident_d)
    modrows = []
    for lay in range(2):
        wmod_d = L.din(f"w_mod{lay}", [D, 6 * D]); bmod_d = L.din(f"b_mod{lay}", [6 * D])
        modrows.append(L.setup_common(cvec_d, wmod_d, bmod_d, f"l{lay}"))
    io0 = dict(csall=csall, csown=csown, ident_d=ident_d, cvec_d=cvec_d, sfx="0", modrow=modrows[0],
               xall_tile=lambda t: (xall[t * 128:(t + 1) * 128, :], xall),
               xown_blk=lambda t0, nt: (xown[t0 * 128:(t0 + nt) * 128, :], xown),
               xo_blk=lambda t0, nt: ((xc1_d[0:LC, :], xc1_d) if t0 == 0 else
                                      (x1own_d[(t0 - 2) * 128:(t0 - 2 + nt) * 128, :], x1own_d)))
    emit_l0(L, io0)
    for k in range(NCH):
        csem = nc.alloc_semaphore(f"ccsem{k}")
        waits = P._waits("pool", P._deps([x1own_d], [x1g_d[k]]), False)

        def emit_cc(eng, waits=waits, k=k, csem=csem):
            for (s_, v_) in waits:
                eng.wait_ge(s_, v_)
            eng.collective_compute("AllGather", ALU.bypass, replica_groups=[[0, 1, 2, 3], [4, 5, 6, 7]],
                                   ins=[x1own_d[k * 256:(k + 1) * 256, :].opt()], outs=[x1g_d[k].ap.opt()]).then_inc(csem)

        P.q["pool"].append(emit_cc)
        tok = (csem, 1, "cc")
        x1g_d[k].w = tok; x1g_d[k].r = {}
        x1own_d.r[csem.name] = tok

    def lat_tile(t):
        g = t - 2
        r, i = g // 16, g % 16
        k, h = i // 2, i % 2
        return (x1g_d[k][r * 256 + h * 128:r * 256 + (h + 1) * 128, :], x1g_d[k])

    io1 = dict(csall=csall, csown=csown, ident_d=ident_d, cvec_d=cvec_d, sfx="1", modrow=modrows[1],
               xall_tile=lambda t: ((xc1_d[t * 128:(t + 1) * 128, :], xc1_d) if t < 2 else lat_tile(t)),
               xown_blk=lambda t0, nt: (x1own_d[(t0 - 2) * 128:(t0 - 2 + nt) * 128, :], x1own_d),
               xo_blk=lambda t0, nt: (xo[(t0 - 2) * 128:(t0 - 2 + nt) * 128, :], xo))
    emit_l1(L, io1)
    P.finish([xo])
    return nc, L


_ROPE = None


def _cvec(cb, cctx):
    v = np.stack([cb, cctx], 1).astype(np.float32)
    return np.ascontiguousarray(v.reshape(8, 128, 2).transpose(1, 0, 2).reshape(128, 16))


def _lru_pack(inp):
    cw = np.asarray(inp["d_conv_w"])[0]; cb = np.asarray(inp["d_conv_b"])[0]
    ba = np.asarray(inp["d_ba"])[0]; bx = np.asarray(inp["d_bx"])[0]; lam = np.asarray(inp["d_lambda"])[0]
    cols = [cw[0], cw[1], cw[2], cw[3], cb, ba[0], ba[1], bx[0], bx[1], lam[0], lam[1], np.zeros(512, np.float32)]
    p = np.stack(cols, 1).astype(np.float32)
    return np.ascontiguousarray(p.reshape(4, 128, 12).transpose(1, 0, 2))


_NC = None


def kernel(**inputs):
    global _ROPE, _NC
    if _ROPE is None:
        _ROPE = _rope_table()
    inp = inputs
    nc, L = build_fused()
    x = np.asarray(inp["x"], np.float32); ctx = np.asarray(inp["ctx"], np.float32)
    ident = np.eye(128, dtype=np.float32)
    lru_p = _lru_pack(inp)
    g = lambda k, i=0: np.asarray(inp[k])[i]
    shared = {
        "ident": ident, "csall": _ROPE, "lru_p": lru_p,
        "w_mod0": g("w_mod", 0), "b_mod0": g("b_mod", 0), "norm_mix_g0": g("norm_mix_g", 0), "norm_ffn_g0": g("norm_ffn_g", 0),
        "w_out0": g("w_out", 0), "w_mod1": g("w_mod", 1), "b_mod1": g("b_mod", 1), "norm_mix_g1": g("norm_mix_g", 1),
        "norm_ffn_g1": g("norm_ffn_g", 1), "w_out1": g("w_out", 1),
        "e_w_in": g("e_w_in"), "a_norm_g": g("a_norm_g"), "a_ws": g("a_ws"), "a_bs": g("a_bs"),
        "b_qnorm_g": g("b_qnorm_g"), "b_knorm_g": g("b_knorm_g"), "b_lq1": g("b_lq1"), "b_lk1": g("b_lk1"),
        "b_lq2": g("b_lq2"), "b_lk2": g("b_lk2"), "b_subln_g": g("b_subln_g"),
        "ffn_w1": g("ffn_w1"), "ffn_w3": g("ffn_w3"), "ffn_w2": g("ffn_w2"),
        "o_w_in": g("o_w_in"), "c_qnorm_g": g("c_qnorm_g"), "c_knorm_g": g("c_knorm_g"),
        "d_wa": g("d_wa"), "d_wx": g("d_wx"), "router_w": g("router_w"), "router_b": g("router_b"),
        "moe_w1": g("moe_w1"), "moe_w3": g("moe_w3"), "moe_w2": g("moe_w2"),
    }
    shared = {k: np.ascontiguousarray(v, dtype=np.float32) for k, v in shared.items()}
    maps = []
    for core in range(8):
        b, s = core // 4, core % 4
        xall = np.concatenate([ctx[b], x[b]], 0)
        own = slice(LC + s * OWN, LC + (s + 1) * OWN)
        segw = np.zeros(4, np.float32); segw[s] = 1.0
        m = dict(shared)
        m.update({"xall": xall, "xown": np.concatenate([ctx[b], xall[own]], 0),
                  "csown": np.concatenate([_ROPE[:LC], _ROPE[own]], 0),
                  "cvec": _cvec(np.asarray(inp["c"])[b], np.asarray(inp["c_ctx"])), "segw": segw})
        maps.append({k: np.ascontiguousarray(m[k], dtype=np.float32) for k in L.inputs})
    res = run_bass_kernel_spmd(nc, maps, core_ids=list(range(8)))
    out = np.zeros_like(x)
    for core in range(8):
        b, s = core // 4, core % 4
        out[b, s * OWN:(s + 1) * OWN] = res.results[core]["xo"]
    return out.astype(np.float32)
```

```python
from contextlib import ExitStack

import numpy as np
import concourse.bass as bass
import concourse.mybir as mybir
from concourse.bass_utils import run_bass_kernel_spmd

F32 = mybir.dt.float32
BF = mybir.dt.bfloat16
AF = mybir.ActivationFunctionType
ALU = mybir.AluOpType
AX = mybir.AxisListType

D = 1024
SEQ = 8192
LC = 256
NALL = SEQ + LC
NT_ALL = NALL // 128
OWN = 2048
NOWN = OWN + LC
FF = 2816
NFC = FF // 128
EPS = 1e-6


class T:
    def __init__(self, name, ap):
        self.name = name
        self.ap = ap
        self.w = None
        self.r = {}

    def __getitem__(self, k):
        return self.ap[k]


class Prog:
    ENG = ("pe", "act", "dve", "pool", "sp")
    SEM_ROLL = 30000
    N_DMA_SEMS = 32

    def __init__(self, nc):
        self.nc = nc
        self.q = {e: [] for e in self.ENG}
        self.sem = {}
        self.cnt = {}
        self.nsem = 0
        self.retired = {}
        for e in self.ENG:
            self._new_sem(e)
        self.dsem = []
        for i in range(self.N_DMA_SEMS):
            s = nc.alloc_semaphore(f"dma{i}")
            self.dsem.append([s, 0])
        self.dnext = 0
        self.seen = {e: {} for e in self.ENG}
        self.pend = {e: False for e in self.ENG}
        self.ninst = 0
        self.gstack = ExitStack()
        self.stack = None
        self.uid = 0

    def _new_sem(self, e):
        if e in self.sem and self.cnt.get(e, 0) > 0:
            self.retired.setdefault(e, []).append((self.sem[e], self.cnt[e], e))
        self.nsem += 1
        self.sem[e] = self.nc.alloc_semaphore(f"s_{e}_{self.nsem}")
        self.cnt[e] = 0

    def _deps(self, reads, writes):
        deps = []
        for t in reads:
            if t.w is not None:
                deps.append(t.w)
        for t in writes:
            if t.w is not None:
                deps.append(t.w)
            deps.extend(t.r.values())
        return deps

    def _waits(self, e, deps, same_ok):
        out = {}
        for (s, v, src) in deps:
            if src == e and same_ok:
                continue
            key = s.name
            if self.seen[e].get(key, 0) >= v:
                continue
            if key not in out or out[key][1] < v:
                out[key] = (s, v)
        for key, (s, v) in out.items():
            self.seen[e][key] = v
        return list(out.values())

    def _mark(self, tok, reads, writes):
        s, v, src = tok
        key = s.name
        for t in reads:
            old = t.r.get(key)
            if old is None or old[1] < v:
                t.r[key] = tok
        for t in writes:
            t.w = tok
            t.r = {}

    def op(self, e, fn, reads=(), writes=(), inc=True, same_ok=None):
        if same_ok is None:
            same_ok = (e == "pe")
        waits = self._waits(e, self._deps(reads, writes), same_ok)
        if self.cnt[e] >= self.SEM_ROLL and not self.pend[e]:
            self._new_sem(e)
        if inc:
            self.cnt[e] += 1
            tok = (self.sem[e], self.cnt[e], e)
            self.pend[e] = False
        else:
            tok = (self.sem[e], self.cnt[e] + 1, e)
            self.pend[e] = True
        sem = self.sem[e]

        def emit(eng, fn=fn, waits=waits, inc=inc, sem=sem):
            for (s, v) in waits:
                eng.wait_ge(s, v)
            ins = fn(eng)
            if inc:
                ins.then_inc(sem, 1)

        self.q[e].append(emit)
        self._mark(tok, reads, writes)
        self.ninst += 1
        return tok

    def dma(self, e, out_ap, in_ap, reads=(), writes=(), **kw):
        d = self.dsem[self.dnext]
        self.dnext = (self.dnext + 1) % self.N_DMA_SEMS
        s, prev = d
        deps = self._deps(reads, writes)
        if prev > 0:
            deps.append((s, prev, "dma"))
        waits = self._waits(e, deps, False)
        d[1] = prev + 16
        tok = (s, prev + 16, "dma")

        def emit(eng, waits=waits, s=s):
            for (ws, v) in waits:
                eng.wait_ge(ws, v)
            eng.dma_start(out=out_ap, in_=in_ap, **kw).then_inc(s, 16)

        self.q[e].append(emit)
        self._mark(tok, reads, writes)
        self.ninst += 1
        return tok

    def wait_all(self, e, toks):
        waits = self._waits(e, list(toks), False)

        def emit(eng, waits=waits):
            for (s, v) in waits:
                eng.wait_ge(s, v)

        self.q[e].append(emit)

    def barrier(self):
        toks = []
        for e in self.ENG:
            if self.cnt[e] > 0:
                toks.append((self.sem[e], self.cnt[e], e))
            elif self.retired.get(e):
                toks.append(self.retired[e][-1])
        toks += [(s, v, "dma") for s, v in self.dsem if v > 0]
        for e in self.ENG:
            assert not self.pend[e]
            self.wait_all(e, toks)

    def phase_begin(self):
        self.stack = ExitStack()

    def flush(self):
        if not any(self.q.values()):
            return
        q = self.q
        self.q = {e: [] for e in self.ENG}
        nc = self.nc
        with nc.Block() as block:
            @block.tensor
            def _(eng):
                for f in q["pe"]:
                    f(eng)

            @block.scalar
            def _(eng):
                for f in q["act"]:
                    f(eng)

            @block.vector
            def _(eng):
                for f in q["dve"]:
                    f(eng)

            @block.gpsimd
            def _(eng):
                for f in q["pool"]:
                    f(eng)

            @block.sync
            def _(eng):
                for f in q["sp"]:
                    f(eng)

    def phase_end(self):
        self.barrier()
        self.flush()
        self.stack.close()
        self.stack = None

    def sb(self, shape, dt=F32, name="t", glob=False):
        self.uid += 1
        nm = f"{name}_{self.uid}"
        st = self.gstack if (glob or self.stack is None) else self.stack
        h = st.enter_context(self.nc.sbuf_tensor(nm, list(shape), dt))
        return T(nm, h.ap())

    def ps(self, shape, dt=F32, name="p", glob=False):
        self.uid += 1
        nm = f"{name}_{self.uid}"
        st = self.gstack if (glob or self.stack is None) else self.stack
        h = st.enter_context(self.nc.psum_tensor(nm, list(shape), dt))
        return T(nm, h.ap())

    def finish(self, final_tiles):
        toks = [t.w for t in final_tiles if t.w is not None]
        self.wait_all("sp", toks)
        self.barrier()
        self.flush()
        self.gstack.close()


class Ring:
    def __init__(self, tiles):
        self.tiles = tiles
        self.i = 0

    def next(self):
        t = self.tiles[self.i % len(self.tiles)]
        self.i += 1
        return t


class LK:
    def __init__(self, nc):
        self.nc = nc
        self.P = Prog(nc)

    def din(self, name, shape, dt=F32):
        if not hasattr(self, "inputs"):
            self.inputs = []
        self.inputs.append(name)
        return T(name, self.nc.dram_tensor(name, list(shape), dt, kind="ExternalInput").ap())

    def dout(self, name, shape, dt=F32):
        return T(name, self.nc.dram_tensor(name, list(shape), dt, kind="ExternalOutput").ap())

    def dscratch(self, name, shape, dt=F32):
        return T(name, self.nc.dram_tensor(name, list(shape), dt, kind="Internal").ap())

    def ring(self, n, shape, dt=F32, name="r", psum=False):
        P = self.P
        return Ring([(P.ps if psum else P.sb)(shape, dt, name) for _ in range(n)])

    def load_ident(self, ident_d):
        P = self.P
        self.ident = P.sb([128, 128], F32, "ident", glob=True)
        self.identb = P.sb([128, 128], BF, "identb", glob=True)
        P.dma("sp", self.ident[:], ident_d[:, :], reads=[ident_d], writes=[self.ident])
        P.op("dve", lambda e: e.tensor_copy(self.identb[:], self.ident[:]), reads=[self.ident], writes=[self.identb])
        self.epsT = P.sb([128, 1], F32, "epsT", glob=True)
        P.op("dve", lambda e: e.memset(self.epsT[:], EPS), writes=[self.epsT])

    def setup_common(self, cvec_d, wmod_d, bmod_d, tag=""):
        P = self.P
        modrow_d = self.dscratch("modrow_d" + tag, [2, 6 * D])
        P.phase_begin()
        cv = P.sb([128, 16], F32, "cv")
        sc = P.sb([128, 16], F32, "sc")
        bm = P.sb([2, 6 * D], F32, "bm")
        mr = P.sb([2, 6 * D], F32, "mr")
        P.dma("sp", cv[:], cvec_d[:, :], reads=[cvec_d], writes=[cv])
        for j in range(2):
            P.dma("sp", bm[j:j + 1, :], bmod_d.ap.unsqueeze(0), reads=[bmod_d], writes=[bm])
        P.op("act", lambda e: e.activation(sc[:], cv[:], AF.Silu), reads=[cv], writes=[sc])
        wms = self.ring(2, [128, 8, 512], F32, "wm")
        pms = self.ring(2, [128, 512], F32, "pm", psum=True)
        for pc in range(12):
            wm = wms.next()
            pm = pms.next()
            P.dma("sp", wm[:], wmod_d[:, pc * 512:(pc + 1) * 512].rearrange("(kc p) n -> p kc n", p=128),
                  reads=[wmod_d], writes=[wm])
            for kc in range(8):
                P.op("pe", lambda e, kc=kc, wm=wm, pm=pm: e.matmul(pm[0:2, :], sc[:, kc * 2:(kc + 1) * 2], wm[:, kc, :],
                                                                 start=(kc == 0), stop=(kc == 7)),
                     reads=[sc, wm], writes=[pm], inc=(kc == 7))
            P.op("dve", lambda e, pc=pc, pm=pm: e.tensor_tensor(mr[0:2, pc * 512:(pc + 1) * 512], pm[0:2, :],
                                                              bm[0:2, pc * 512:(pc + 1) * 512], ALU.add),
                 reads=[pm, bm], writes=[mr])
        P.dma("sp", modrow_d[:, :], mr[:], reads=[mr], writes=[modrow_d])
        P.phase_end()
        return modrow_d

    def load_bc(self, dst, src_row_ap, src_t):
        self.P.dma("sp", dst[:], src_row_ap.partition_broadcast(128), reads=[src_t], writes=[dst])

    def mod_bc(self, j, idx, name):
        t = self.P.sb([128, D], F32, name)
        self.load_bc(t, self.modrow_d.ap[j, idx * D:(idx + 1) * D], self.modrow_d)
        return t

    def make_A(self, sc_t, g_t):
        self.P.op("dve", lambda e: e.scalar_tensor_tensor(sc_t[:], sc_t[:], 1.0, g_t[:], ALU.add, ALU.mult),
                  reads=[sc_t, g_t], writes=[sc_t])
        return sc_t

    def rstd(self, ss, n, dim):
        P = self.P
        P.op("act", lambda e: e.activation(ss[:, 0:n], ss[:, 0:n], AF.Sqrt, bias=self.epsT[:, 0:1], scale=1.0 / dim),
             reads=[ss, self.epsT], writes=[ss])
        P.op("dve", lambda e: e.reciprocal(ss[:, 0:n], ss[:, 0:n]), reads=[ss], writes=[ss])

    def norm_tile(self, x_ap, x_t, A_bc, sh_bc, hn, junk, ss):
        P = self.P
        P.op("act", lambda e: e.activation(junk[:], x_ap, AF.Square, accum_out=ss[:, 0:1]),
             reads=[x_t], writes=[junk, ss])
        self.rstd(ss, 1, D)
        P.op("dve", lambda e: e.scalar_tensor_tensor(junk[:], x_ap, ss[:, 0:1], A_bc[:], ALU.mult, ALU.mult),
             reads=[x_t, ss, A_bc], writes=[junk])
        P.op("pool", lambda e: e.tensor_tensor(hn[:], junk[:], sh_bc[:], ALU.add), reads=[junk, sh_bc], writes=[hn])

    def transpose_cols(self, src, ncol_chunks, pT, dst_ap_fn, dst_t, eng="act"):
        P = self.P
        for c in range(ncol_chunks):
            P.op("pe", lambda e, c=c: e.transpose(pT[:, c, :], src[:, c * 128:(c + 1) * 128], self.identb[:]),
                 reads=[src, self.identb], writes=[pT], inc=(c == ncol_chunks - 1))
        if eng == "act":
            P.op("act", lambda e: e.activation(dst_ap_fn(), pT[:, 0:ncol_chunks, :], AF.Identity), reads=[pT], writes=[dst_t])
        else:
            P.op("dve", lambda e: e.tensor_copy(dst_ap_fn(), pT[:, 0:ncol_chunks, :]), reads=[pT], writes=[dst_t])

    def proj_tok(self, ps, hT, tsl, W, c0, ncols):
        for kc in range(8):
            self.P.op("pe", lambda e, kc=kc: e.matmul(ps[:, 0:ncols], hT[:, kc, tsl], W[:, kc, c0:c0 + ncols],
                                                     start=(kc == 0), stop=(kc == 7)),
                      reads=[hT, W], writes=[ps], inc=(kc == 7))

    def proj_feat(self, ps, hT, n, W, c0):
        for kc in range(8):
            self.P.op("pe", lambda e, kc=kc: e.matmul(ps[:, 0:n], W[:, kc, c0:c0 + 128], hT[:, kc, 0:n],
                                                     start=(kc == 0), stop=(kc == 7)),
                      reads=[hT, W], writes=[ps], inc=(kc == 7))

    def rope_tables(self, cs, g_bc, gs_bc, CT, ST):
        P = self.P
        P.op("pool", lambda e: e.tensor_tensor(CT[:], cs[:, 0:64], g_bc[:], ALU.mult), reads=[cs, g_bc], writes=[CT])
        P.op("pool", lambda e: e.tensor_tensor(ST[:], cs[:, 64:128], gs_bc[:], ALU.mult), reads=[cs, gs_bc], writes=[ST])

    def swap_gain(self, g_bc, gs_bc):
        P = self.P
        for a in range(2):
            for b in range(2):
                P.op("dve", lambda e, a=a, b=b: e.tensor_copy(gs_bc[:, a * 32 + b * 16:a * 32 + b * 16 + 16],
                                                              g_bc[:, a * 32 + (1 - b) * 16:a * 32 + (1 - b) * 16 + 16]),
                     reads=[g_bc], writes=[gs_bc])

    def qk_post(self, src, H, CT, ST, out, tmp):
        P = self.P
        sq, qn, t1, ssq = tmp
        n = H * 64
        P.op("act", lambda e: e.activation(sq[:, 0:n], src[:, 0:n], AF.Square), reads=[src], writes=[sq])
        P.op("dve", lambda e: e.tensor_reduce(ssq[:, 0:H], sq[:, 0:n].rearrange("p (h d) -> p h d", d=64), AX.X, ALU.add),
             reads=[sq], writes=[ssq])
        self.rstd(ssq, H, 64)
        P.op("dve", lambda e: e.tensor_tensor(qn[:, 0:n].rearrange("p (h d) -> p h d", d=64),
                                              src[:, 0:n].rearrange("p (h d) -> p h d", d=64),
                                              ssq[:, 0:H].unsqueeze(2).to_broadcast([128, H, 64]), ALU.mult),
             reads=[src, ssq], writes=[qn])
        P.op("dve", lambda e: e.tensor_tensor(t1[:, 0:n].rearrange("p (h d) -> p h d", d=64),
                                              qn[:, 0:n].rearrange("p (h d) -> p h d", d=64),
                                              CT[:].unsqueeze(1).to_broadcast([128, H, 64]), ALU.mult),
             reads=[qn, CT], writes=[t1])
        for b in range(2):
            P.op("dve", lambda e, b=b: e.tensor_tensor(
                sq[:, 0:n].rearrange("p (h a b c) -> p h a b c", a=2, b=2, c=16)[:, :, :, b, :],
                qn[:, 0:n].rearrange("p (h a b c) -> p h a b c", a=2, b=2, c=16)[:, :, :, 1 - b, :],
                ST[:].rearrange("p (a b c) -> p a b c", a=2, b=2, c=16)[:, :, b, :].unsqueeze(1).to_broadcast([128, H, 2, 16]),
                ALU.mult), reads=[qn, ST], writes=[sq])
        P.op("pool", lambda e: e.tensor_tensor(out[:, 0:n], t1[:, 0:n], sq[:, 0:n], ALU.add), reads=[t1, sq], writes=[out])


def _rope_table():
    half = 16
    freqs = 10000.0 ** (-np.arange(half, dtype=np.float32) / half)
    t = np.arange(SEQ)
    row = (t // 64).astype(np.float32)
    col = (t % 64).astype(np.float32)
    ar = row[:, None] * freqs[None, :]
    ac = col[:, None] * freqs[None, :]
    C = np.concatenate([np.cos(ar), np.cos(ar), np.cos(ac), np.cos(ac)], 1)
    S = np.concatenate([-np.sin(ar), np.sin(ar), -np.sin(ac), np.sin(ac)], 1)
    lat = np.concatenate([C, S], 1).astype(np.float32)
    ctx = np.concatenate([np.ones((LC, 64), np.float32), np.zeros((LC, 64), np.float32)], 1)
    return np.concatenate([ctx, lat], 0)


STOP = None


def emit_l0(L, io):
    nc = L.nc
    P = L.P
    _outer = P.gstack
    P.gstack = ExitStack()
    xall_tile = io["xall_tile"]; xown_blk = io["xown_blk"]; xo_blk = io["xo_blk"]
    csall = io["csall"]; csown = io["csown"]; ident_d = io["ident_d"]; cvec_d = io["cvec_d"]
    sfx = io["sfx"]
    L.modrow_d = io["modrow"]
    gmix_d = L.din("norm_mix_g" + sfx, [D])
    gffn_d = L.din("norm_ffn_g" + sfx, [D])
    wout_d = L.din("w_out" + sfx, [D, D])
    win_d = L.din("e_w_in", [D, 2560])
    anorm_d = L.din("a_norm_g", [512])
    aws_d = L.din("a_ws", [8, 128, 128])
    abs_d = L.din("a_bs", [8, 128])
    qg_d = L.din("b_qnorm_g", [64])
    kg_d = L.din("b_knorm_g", [64])
    lq1_d = L.din("b_lq1", [64]); lk1_d = L.din("b_lk1", [64]); lq2_d = L.din("b_lq2", [64]); lk2_d = L.din("b_lk2", [64])
    sub_d = L.din("b_subln_g", [128])
    w1_d = L.din("ffn_w1", [D, FF]); w3_d = L.din("ffn_w3", [D, FF]); w2_d = L.din("ffn_w2", [FF, D])
    KT_d = L.dscratch("KT_d", [4, 128, NALL], BF)
    VS_d = L.dscratch("VS_d", [NALL, 512], BF)
    LAM_INIT = 0.2


    gq = P.sb([128, 64], F32, "gq", glob=True); gqs = P.sb([128, 64], F32, "gqs", glob=True)
    gk = P.sb([128, 64], F32, "gk", glob=True); gks = P.sb([128, 64], F32, "gks", glob=True)
    L.load_bc(gq, qg_d.ap, qg_d); L.load_bc(gk, kg_d.ap, kg_d)
    L.swap_gain(gq, gqs); L.swap_gain(gk, gks)
    neglam = P.sb([128, 1], F32, "neglam", glob=True)
    P.phase_begin()
    lt = [P.sb([128, 64], F32, "l") for _ in range(4)]
    for t_, d_ in zip(lt, (lq1_d, lk1_d, lq2_d, lk2_d)):
        L.load_bc(t_, d_.ap, d_)
    e12 = P.sb([128, 2], F32, "e12")
    for i in range(2):
        P.op("dve", lambda e, i=i: e.tensor_tensor(lt[2 * i][:], lt[2 * i][:], lt[2 * i + 1][:], ALU.mult), reads=[lt[2 * i], lt[2 * i + 1]], writes=[lt[2 * i]])
        P.op("dve", lambda e, i=i: e.tensor_reduce(e12[:, i:i + 1], lt[2 * i][:], AX.X, ALU.add), reads=[lt[2 * i]], writes=[e12])
    P.op("act", lambda e: e.activation(e12[:], e12[:], AF.Exp), reads=[e12], writes=[e12])
    P.op("dve", lambda e: e.tensor_tensor(neglam[:], e12[:, 1:2], e12[:, 0:1], ALU.subtract), reads=[e12], writes=[neglam])
    P.op("dve", lambda e: e.tensor_scalar(neglam[:], neglam[:], -LAM_INIT, None, ALU.add), reads=[neglam], writes=[neglam])
    P.phase_end()

    gmix = P.sb([128, D], F32, "gmix", glob=True)
    gffn = P.sb([128, D], F32, "gffn", glob=True)
    L.load_bc(gmix, gmix_d.ap, gmix_d)
    L.load_bc(gffn, gffn_d.ap, gffn_d)

    P.phase_begin()
    A1 = [L.make_A(L.mod_bc(j, 1, "A1"), gmix) for j in range(2)]
    sh1 = [L.mod_bc(j, 0, "sh1") for j in range(2)]
    Wkv = P.sb([128, 8, 1024], BF, "Wkv")
    for h in range(2):
        P.dma("pool", Wkv[:, :, h * 512:(h + 1) * 512],
              win_d[:, 1536 + h * 512:1536 + (h + 1) * 512].rearrange("(kc p) n -> p kc n", p=128), reads=[win_d], writes=[Wkv])
    xr = L.ring(3, [128, D], F32, "xa"); csr = L.ring(3, [128, 128], F32, "csa")
    hnr = L.ring(2, [128, D], BF, "hn"); jr = L.ring(2, [128, D], F32, "junk"); ssr = L.ring(2, [128, 2], F32, "ss")
    hTr = L.ring(2, [128, 8, 128], BF, "hT")
    pTr = L.ring(2, [128, 8, 128], BF, "pT", psum=True)
    pkr = L.ring(2, [128, 512], F32, "pk", psum=True); pvr = L.ring(2, [128, 512], F32, "pv", psum=True)
    CTr = L.ring(2, [128, 64], F32, "CT"); STr = L.ring(2, [128, 64], F32, "ST")
    tmps = [tuple([P.sb([128, 512], F32, "sq"), P.sb([128, 512], F32, "qn"), P.sb([128, 512], F32, "t1"), P.sb([128, 8], F32, "ssq")]) for _ in range(2)]
    kbr = L.ring(2, [128, 512], BF, "kb"); vbr = L.ring(2, [128, 512], BF, "vb"); ktr = L.ring(2, [128, 4, 128], BF, "kt")
    for t in range(NT_ALL):
        j = 1 if t < 2 else 0
        x = xr.next(); cs = csr.next(); hn = hnr.next(); junk = jr.next(); ss = ssr.next(); hT = hTr.next(); pT = pTr.next()
        xa_ap, xa_t = xall_tile(t)
        P.dma("sp", x[:], xa_ap, reads=[xa_t], writes=[x])
        P.dma("sp", cs[:], csall[t * 128:(t + 1) * 128, :], reads=[csall], writes=[cs])
        L.norm_tile(x[:], x, A1[j], sh1[j], hn, junk, ss)
        L.transpose_cols(hn, 8, pT, lambda hT=hT: hT[:], hT)
        pk = pkr.next(); pv = pvr.next()
        L.proj_tok(pk, hT, slice(0, 128), Wkv, 0, 512)
        L.proj_tok(pv, hT, slice(0, 128), Wkv, 512, 512)
        vb = vbr.next()
        P.op("act", lambda e, vb=vb, pv=pv: e.activation(vb[:], pv[:], AF.Identity), reads=[pv], writes=[vb])
        P.dma("pool", VS_d[t * 128:(t + 1) * 128, :], vb[:], reads=[vb], writes=[VS_d])
        CT = CTr.next(); ST = STr.next()
        L.rope_tables(cs, gk, gks, CT, ST)
        kb = kbr.next()
        L.qk_post(pk, 8, CT, ST, kb, tmps[t % 2])
        pT2 = pTr.next(); kt = ktr.next()
        L.transpose_cols(kb, 4, pT2, lambda kt=kt: kt[:], kt, eng="dve")
        P.dma("pool", KT_d[:, :, t * 128:(t + 1) * 128].rearrange("h p t -> p h t"), kt[:], reads=[kt], writes=[KT_d])
    P.phase_end()

    yT = P.sb([128, 8, NOWN], BF, "yT", glob=True)
    qstack = ExitStack()
    QT = T("QT", qstack.enter_context(nc.sbuf_tensor("QT", [128, 2, 4, NOWN], BF)).ap())
    P.op("pool", lambda e: e.memset(QT[64:128, 0, :, :], 0.0), writes=[QT])
    P.op("pool", lambda e: e.memset(QT[0:64, 1, :, :], 0.0), writes=[QT])
    blocks = [(0, 2)] + [(2 + 4 * i, 4) for i in range(4)]

    P.phase_begin()
    A1 = [L.make_A(L.mod_bc(j, 1, "A1"), gmix) for j in range(2)]
    sh1 = [L.mod_bc(j, 0, "sh1") for j in range(2)]
    Win = P.sb([128, 8, 1536], BF, "Win")
    for h in range(3):
        P.dma("pool", Win[:, :, h * 512:(h + 1) * 512], win_d[:, h * 512:(h + 1) * 512].rearrange("(kc p) n -> p kc n", p=128),
              reads=[win_d], writes=[Win])
    anorm = P.sb([128, 512], F32, "anorm"); L.load_bc(anorm, anorm_d.ap, anorm_d)
    wsT = P.sb([128, 8, 128], BF, "wsT")
    wsr = P.sb([128, 8, 128], F32, "wsr")
    P.dma("sp", wsr[:], aws_d.ap.rearrange("g p q -> p g q"), reads=[aws_d], writes=[wsr])
    wsb = P.sb([128, 8, 128], BF, "wsb")
    P.op("dve", lambda e: e.tensor_copy(wsb[:], wsr[:]), reads=[wsr], writes=[wsb])
    pTr = L.ring(2, [128, 8, 128], BF, "pT", psum=True)
    pTw = pTr.next()
    for g in range(8):
        P.op("pe", lambda e, g=g: e.transpose(pTw[:, g, :], wsb[:, g, :], L.identb[:]), reads=[wsb, L.identb], writes=[pTw], inc=(g == 7))
    P.op("dve", lambda e: e.tensor_copy(wsT[:], pTw[:]), reads=[pTw], writes=[wsT])
    bsT = P.sb([128, 4, 128], F32, "bsT")
    for g in range(8):
        P.dma("sp", bsT[(g % 2) * 64:(g % 2) * 64 + 64, g // 2, :], abs_d.ap[g, :].partition_broadcast(64), reads=[abs_d], writes=[bsT])
    xb = L.ring(1, [128, 4, D], F32, "xb"); csb = L.ring(2, [128, 4, 128], F32, "csb")
    hnr = L.ring(2, [128, D], BF, "hn"); jr = L.ring(2, [128, D], F32, "junk"); ssr = L.ring(2, [128, 2], F32, "ss")
    hTb = L.ring(1, [128, 8, 512], BF, "hTb")
    uTr = L.ring(1, [128, 4, 512], F32, "uT")
    pur = L.ring(2, [128, 512], F32, "pu", psum=True)
    pvr = L.ring(2, [128, 512], F32, "pv", psum=True)
    pmr = L.ring(2, [128, 128], F32, "pm", psum=True)
    gvr = L.ring(2, [128, 512], F32, "gv"); vbr = L.ring(2, [128, 512], BF, "vb"); tmr = L.ring(2, [128, 128], F32, "tm")
    CTr = L.ring(2, [128, 64], F32, "CT"); STr = L.ring(2, [128, 64], F32, "ST")
    tmps = [tuple([P.sb([128, 512], F32, "sq"), P.sb([128, 512], F32, "qn"), P.sb([128, 512], F32, "t1"), P.sb([128, 8], F32, "ssq")]) for _ in range(2)]
    qbr = L.ring(2, [128, 512], BF, "qb")
    for bi, (t0, nt) in enumerate(blocks):
        j = 1 if bi == 0 else 0
        N = nt * 128
        x = xb.next(); cs = csb.next(); hT = hTb.next(); uT = uTr.next()
        xb_ap, xb_t = xown_blk(t0, nt)
        P.dma("sp", x[:, 0:nt, :], xb_ap.rearrange("(t p) d -> p t d", p=128), reads=[xb_t], writes=[x])
        P.dma("sp", cs[:, 0:nt, :], csown[t0 * 128:(t0 + nt) * 128, :].rearrange("(t p) d -> p t d", p=128), reads=[csown], writes=[cs])
        for t in range(nt):
            hn = hnr.next(); junk = jr.next(); ss = ssr.next(); pT = pTr.next()
            L.norm_tile(x[:, t, :], x, A1[j], sh1[j], hn, junk, ss)
            L.transpose_cols(hn, 8, pT, lambda hT=hT, t=t: hT[:, :, t * 128:(t + 1) * 128], hT)
        for c in range(4):
            pu = pur.next()
            L.proj_feat(pu, hT, N, Win, c * 128)
            P.op("act", lambda e, pu=pu, uT=uT, c=c, N=N: e.activation(uT[:, c, 0:N], pu[:, 0:N], AF.Gelu_apprx_tanh), reads=[pu], writes=[uT])
        for t in range(nt):
            tsl = slice(t * 128, (t + 1) * 128)
            gsl = slice((t0 + t) * 128, (t0 + t + 1) * 128)
            pv = pvr.next(); gv = gvr.next(); vb = vbr.next(); junk = jr.next(); ss = ssr.next()
            L.proj_tok(pv, hT, tsl, Win, 512, 512)
            P.op("act", lambda e, gv=gv, pv=pv: e.activation(gv[:], pv[:], AF.Gelu_apprx_tanh), reads=[pv], writes=[gv])
            P.op("act", lambda e, gv=gv, junk=junk, ss=ss: e.activation(junk[:, 0:512], gv[:], AF.Square, accum_out=ss[:, 0:1]), reads=[gv], writes=[junk, ss])
            L.rstd(ss, 1, 512)
            P.op("dve", lambda e, vb=vb, gv=gv, ss=ss: e.scalar_tensor_tensor(vb[:], gv[:], ss[:, 0:1], anorm[:], ALU.mult, ALU.mult),
                 reads=[gv, ss, anorm], writes=[vb])
            for jp in range(4):
                pm = pmr.next(); tm = tmr.next()
                for hh in range(2):
                    g = 2 * jp + hh
                    P.op("pe", lambda e, pm=pm, vb=vb, g=g, hh=hh: e.matmul(pm[hh * 64:(hh + 1) * 64, :], vb[:, g * 64:(g + 1) * 64], wsT[:, g, :],
                                                                         start=True, stop=True), reads=[vb, wsT], writes=[pm], inc=(hh == 1))
                P.op("dve", lambda e, pm=pm, tm=tm, jp=jp: e.tensor_tensor(tm[:], pm[:], bsT[:, jp, :], ALU.add), reads=[pm, bsT], writes=[tm])
                P.op("pool", lambda e, tm=tm, jp=jp, uT=uT, tsl=tsl, gsl=gsl: e.tensor_tensor(yT[:, jp, gsl], tm[:], uT[:, jp, tsl], ALU.mult),
                     reads=[tm, uT], writes=[yT])
            pq = pur.next(); CT = CTr.next(); ST = STr.next(); qb = qbr.next(); pT = pTr.next()
            L.proj_tok(pq, hT, tsl, Win, 1024, 512)
            L.rope_tables(T_view(cs, cs[:, t, :]), gq, gqs, CT, ST)
            L.qk_post(pq, 8, CT, ST, qb, tmps[t % 2])
            for c4 in range(4):
                P.op("pe", lambda e, c4=c4, pT=pT, qb=qb: e.transpose(pT[:, c4, :], qb[:, c4 * 128:(c4 + 1) * 128], L.identb[:]),
                     reads=[qb, L.identb], writes=[pT], inc=(c4 == 3))
            P.op("dve", lambda e, pT=pT, gsl=gsl: e.tensor_copy(QT[0:64, 0, :, gsl], pT[0:64, 0:4, :]), reads=[pT], writes=[QT])
            P.op("act", lambda e, pT=pT, gsl=gsl: e.activation(QT[64:128, 1, :, gsl], pT[64:128, 0:4, :], AF.Identity), reads=[pT], writes=[QT])
    P.phase_end()

    P.phase_begin()
    subbc = P.sb([128, 128], F32, "subbc"); L.load_bc(subbc, sub_d.ap, sub_d)
    P.op("dve", lambda e: e.tensor_scalar(subbc[:], subbc[:], 1.0 - LAM_INIT, None, ALU.mult), reads=[subbc], writes=[subbc])
    KTh = L.ring(2, [128, NALL], BF, "KTh")
    Vh = L.ring(2, [128, NT_ALL, 129], BF, "Vh")
    for v_ in Vh.tiles:
        P.op("pool", lambda e, v_=v_: e.memset(v_[:, :, 128:129], 1.0), writes=[v_])
    spr = L.ring(4, [128, 2, 256], F32, "sp", psum=True)
    LOOK = 3
    accs = [[P.ps([128, 512], F32, "acc") for c in range(2)] for qi in range(2)]
    pTr_ = L.ring(6, [128, 2, 256], BF, "pTs")
    rz = L.ring(2, [128, 4], F32, "rz"); o1r = L.ring(2, [128, 128], F32, "o1"); o2r = L.ring(2, [128, 128], F32, "o2")
    jr = L.ring(2, [128, 128], F32, "junk"); ssr = L.ring(2, [128, 2], F32, "ss"); ybr = L.ring(2, [128, 128], BF, "yb")
    for h in range(4):
        kth = KTh.next(); vh = Vh.next()
        for pc in range(6):
            P.dma("sp", kth[:, pc * 1408:(pc + 1) * 1408], KT_d[h, :, pc * 1408:(pc + 1) * 1408], reads=[KT_d], writes=[kth])
        for pc in range(11):
            P.dma("sp", vh[:, pc * 6:(pc + 1) * 6, 0:128],
                  VS_d[pc * 768:(pc + 1) * 768, h * 128:(h + 1) * 128].rearrange("(t p) e -> p t e", p=128), reads=[VS_d], writes=[vh])
        for qb_ in range(NOWN // 256):
            keys = [0, 1] if qb_ == 0 else list(range(NT_ALL))
            q0 = qb_ * 256
            def emit_scores(kt, kth=kth, q0=q0, h=h):
                sp_ = spr.next()
                for c in range(2):
                    P.op("pe", lambda e, c=c, sp_=sp_, kth=kth, kt=kt, q0=q0, h=h: e.matmul(
                        sp_[:, c, :], kth[:, kt * 128:(kt + 1) * 128], QT[:, c, h, q0:q0 + 256],
                        start=True, stop=True), reads=[kth, QT], writes=[sp_], inc=(c == 1))
                return sp_

            pend_sc = [emit_scores(keys[i]) for i in range(min(LOOK, len(keys)))]
            for ki, kt in enumerate(keys):
                sp_ = pend_sc.pop(0)
                if ki + LOOK < len(keys):
                    pend_sc.append(emit_scores(keys[ki + LOOK]))
                pt = pTr_.next()
                P.op("act", lambda e, pt=pt, sp_=sp_: e.activation(pt[:], sp_[:], AF.Exp, scale=0.125), reads=[sp_], writes=[pt])
                for qi in range(2):
                    for c in range(2):
                        P.op("pe", lambda e, qi=qi, c=c, pt=pt, vh=vh, kt=kt, ki=ki, keys=keys: e.matmul(
                            accs[qi][c][:, 0:129], pt[:, c, qi * 128:(qi + 1) * 128], vh[:, kt, :],
                            start=(ki == 0), stop=(ki == len(keys) - 1)), reads=[pt, vh], writes=[accs[qi][c]],
                            inc=(ki == len(keys) - 1))
            for qi in range(2):
                r = rz.next(); o1 = o1r.next(); o2 = o2r.next(); junk = jr.next(); ss = ssr.next(); yb = ybr.next()
                a0, a1 = accs[qi]
                P.op("dve", lambda e, r=r, a0=a0: e.reciprocal(r[:, 0:1], a0[:, 128:129]), reads=[a0], writes=[r])
                P.op("dve", lambda e, r=r, a1=a1: e.reciprocal(r[:, 1:2], a1[:, 128:129]), reads=[a1], writes=[r])
                P.op("dve", lambda e, r=r: e.tensor_tensor(r[:, 2:3], r[:, 1:2], neglam[:], ALU.mult), reads=[r, neglam], writes=[r])
                P.op("act", lambda e, o1=o1, a0=a0, r=r: e.activation(o1[:], a0[:, 0:128], AF.Identity, scale=r[:, 0:1]), reads=[a0, r], writes=[o1])
                P.op("dve", lambda e, o2=o2, a1=a1, r=r, o1=o1: e.scalar_tensor_tensor(o2[:], a1[:, 0:128], r[:, 2:3], o1[:], ALU.mult, ALU.add),
                     reads=[a1, r, o1], writes=[o2])
                P.op("act", lambda e, junk=junk, o2=o2, ss=ss: e.activation(junk[:], o2[:], AF.Square, accum_out=ss[:, 0:1]), reads=[o2], writes=[junk, ss])
                L.rstd(ss, 1, 128)
                P.op("dve", lambda e, yb=yb, o2=o2, ss=ss: e.scalar_tensor_tensor(yb[:], o2[:], ss[:, 0:1], subbc[:], ALU.mult, ALU.mult),
                     reads=[o2, ss, subbc], writes=[yb])
                gsl = slice(q0 + qi * 128, q0 + (qi + 1) * 128)
                P.op("pe", lambda e, yb=yb, a0=a0: e.transpose(a0[:].bitcast(BF)[:, 0:128], yb[:], L.identb[:]), reads=[yb, L.identb], writes=[a0])
                P.op("act", lambda e, gsl=gsl, h=h, a0=a0: e.activation(yT[:, 4 + h, gsl], a0[:].bitcast(BF)[:, 0:128], AF.Identity), reads=[a0], writes=[yT])
    P.phase_end()

    qstack.close()
    ffn_phase(L, xown_blk, xo_blk, blocks, yT, wout_d, gffn, w1_d, w3_d, w2_d, ctx_block=True)
    P.barrier()
    P.flush()
    P.gstack.close()
    P.gstack = _outer


def T_view(t, ap):
    return _View(t, ap)


class _View:
    def __init__(self, base, ap):
        object.__setattr__(self, "base", base)
        object.__setattr__(self, "ap", ap)

    def __getitem__(self, k):
        return self.ap[k]

    @property
    def w(self):
        return self.base.w

    @w.setter
    def w(self, v):
        self.base.w = v

    @property
    def r(self):
        return self.base.r

    @r.setter
    def r(self, v):
        self.base.r = v

    @property
    def name(self):
        return self.base.name


def ffn_phase(L, xown, xo, blocks, yT, wout_d, gffn, w1_d, w3_d, w2_d, ctx_block, moe=None):
    P = L.P
    P.phase_begin()
    nvar = 2 if ctx_block else 1
    g1 = [L.mod_bc(j, 2, "g1") for j in range(nvar)]
    A2 = [L.make_A(L.mod_bc(j, 4, "A2"), gffn) for j in range(nvar)]
    sh2 = [L.mod_bc(j, 3, "sh2") for j in range(nvar)]
    g2 = [L.mod_bc(j, 5, "g2") for j in range(nvar)]
    wout = P.sb([128, 8, D], BF, "wout")
    for h in range(2):
        P.dma("pool", wout[:, :, h * 512:(h + 1) * 512], wout_d[:, h * 512:(h + 1) * 512].rearrange("(kc p) n -> p kc n", p=128),
              reads=[wout_d], writes=[wout])
    NE = 1
    if moe is not None:
        rw_d, rb_d, NE = moe
        wr = P.sb([128, 8, 8], F32, "wr")
        P.dma("sp", wr[:], rw_d.ap.rearrange("(kc p) e -> p kc e", p=128), reads=[rw_d], writes=[wr])
        rb = P.sb([128, 8], F32, "rb"); L.load_bc(rb, rb_d.ap, rb_d)
        hnf_r = L.ring(2, [128, D], F32, "hnf")
        tTr = L.ring(2, [128, 8, 128], F32, "tT")
        pXr = L.ring(1, [128, 8, 128], F32, "pX", psum=True)
        dg = P.sb([128, 4, 8], F32, "dg")
        rt = [P.sb([128, 8], F32, "rt") for _ in range(4)]
        rs = P.sb([128, 4], F32, "rs")
    xb = L.ring(1, [128, 4, D], F32, "xb")
    hnr = L.ring(2, [128, D], BF, "hn"); jr = L.ring(2, [128, D], F32, "junk"); ssr = L.ring(2, [128, 2], F32, "ss")
    hTb = L.ring(1, [128, 8, 512], BF, "h2T")
    gT = P.sb([128, NFC, 512], BF, "gT")
    pTr = L.ring(1 if moe is not None else 2, [128, 8, 128], BF, "pT", psum=True)
    por = L.ring(1 if moe is not None else 2, [128, 512], F32, "po", psum=True)
    p1r = L.ring(2, [128, 512], F32, "p1", psum=True); p3r = L.ring(2, [128, 512], F32, "p3", psum=True)
    tmr = L.ring(2, [128, 512], F32, "tm")
    s1r = L.ring(2, [128, 512], F32, "s1")
    w1r = L.ring(2, [128, 8, 256], BF, "w1p"); w3r = L.ring(2, [128, 8, 256], BF, "w3p")
    w2r = L.ring(2, [128, NFC, 256], BF, "w2q")
    for bi, (t0, nt) in enumerate(blocks):
        j = 1 if (ctx_block and bi == 0) else 0
        N = nt * 128
        x = xb.next(); hT = hTb.next()
        xb_ap, xb_t = xown(t0, nt)
        P.dma("sp", x[:, 0:nt, :], xb_ap.rearrange("(t p) d -> p t d", p=128), reads=[xb_t], writes=[x])
        for t in range(nt):
            gsl = slice((t0 + t) * 128, (t0 + t + 1) * 128)
            for hf in range(2):
                po = por.next(); tm = tmr.next()
                for kc in range(8):
                    P.op("pe", lambda e, kc=kc, po=po, gsl=gsl, hf=hf: e.matmul(po[:], yT[:, kc, gsl], wout[:, kc, hf * 512:(hf + 1) * 512],
                                                                              start=(kc == 0), stop=(kc == 7)),
                         reads=[yT, wout], writes=[po], inc=(kc == 7))
                P.op("dve", lambda e, tm=tm, po=po, hf=hf, j=j: e.tensor_tensor(tm[:], po[:], g1[j][:, hf * 512:(hf + 1) * 512], ALU.mult),
                     reads=[po, g1[j]], writes=[tm])
                P.op("dve", lambda e, x=x, t=t, hf=hf, tm=tm: e.tensor_tensor(x[:, t, hf * 512:(hf + 1) * 512], x[:, t, hf * 512:(hf + 1) * 512], tm[:], ALU.add),
                     reads=[x, tm], writes=[x])
        for t in range(nt):
            hn = hnr.next(); junk = jr.next(); ss = ssr.next(); pT = pTr.next()
            if moe is None:
                L.norm_tile(x[:, t, :], x, A2[j], sh2[j], hn, junk, ss)
            else:
                hnf = hnf_r.next(); tT = tTr.next(); pX = pXr.next()
                P.op("act", lambda e, junk=junk, x=x, t=t, ss=ss: e.activation(junk[:], x[:, t, :], AF.Square, accum_out=ss[:, 0:1]),
                     reads=[x], writes=[junk, ss])
                L.rstd(ss, 1, D)
                P.op("dve", lambda e, junk=junk, x=x, t=t, ss=ss, j=j: e.scalar_tensor_tensor(junk[:], x[:, t, :], ss[:, 0:1], A2[j][:], ALU.mult, ALU.mult),
                     reads=[x, ss, A2[j]], writes=[junk])
                P.op("dve", lambda e, hnf=hnf, junk=junk, j=j: e.tensor_tensor(hnf[:], junk[:], sh2[j][:], ALU.add), reads=[junk, sh2[j]], writes=[hnf])
                P.op("act", lambda e, hn=hn, hnf=hnf: e.activation(hn[:], hnf[:], AF.Identity), reads=[hnf], writes=[hn])
                for c in range(8):
                    P.op("pe", lambda e, c=c, pX=pX, hnf=hnf: e.transpose(pX[:, c, :], hnf[:, c * 128:(c + 1) * 128], L.ident[:]),
                         reads=[hnf, L.ident], writes=[pX], inc=(c == 7))
                P.op("dve", lambda e, tT=tT, pX=pX: e.tensor_copy(tT[:], pX[:]), reads=[pX], writes=[tT])
                plg = por.next()
                for kc in range(8):
                    P.op("pe", lambda e, kc=kc, plg=plg, tT=tT: e.matmul(plg[:, 0:8], tT[:, kc, :], wr[:, kc, :], start=(kc == 0), stop=(kc == 7)),
                         reads=[tT, wr], writes=[plg], inc=(kc == 7))
                lg, mk, l2, ex = rt
                P.op("dve", lambda e, plg=plg: e.tensor_tensor(lg[:], plg[:, 0:8], rb[:], ALU.add), reads=[plg, rb], writes=[lg])
                P.op("dve", lambda e: e.tensor_reduce(rs[:, 0:1], lg[:], AX.X, ALU.max), reads=[lg], writes=[rs])
                P.op("dve", lambda e: e.tensor_scalar(mk[:], lg[:], rs[:, 0:1], None, ALU.is_equal), reads=[lg, rs], writes=[mk])
                P.op("dve", lambda e: e.scalar_tensor_tensor(l2[:], mk[:], -1e30, lg[:], ALU.mult, ALU.add), reads=[mk, lg], writes=[l2])
                P.op("dve", lambda e: e.tensor_reduce(rs[:, 1:2], l2[:], AX.X, ALU.max), reads=[l2], writes=[rs])
                P.op("dve", lambda e: e.tensor_scalar(mk[:], lg[:], rs[:, 1:2], None, ALU.is_ge), reads=[lg, rs], writes=[mk])
                P.op("dve", lambda e: e.tensor_scalar(ex[:], lg[:], rs[:, 0:1], None, ALU.subtract), reads=[lg, rs], writes=[ex])
                P.op("act", lambda e: e.activation(ex[:], ex[:], AF.Exp), reads=[ex], writes=[ex])
                P.op("dve", lambda e: e.tensor_tensor(ex[:], ex[:], mk[:], ALU.mult), reads=[ex, mk], writes=[ex])
                P.op("dve", lambda e: e.tensor_reduce(rs[:, 2:3], ex[:], AX.X, ALU.add), reads=[ex], writes=[rs])
                P.op("dve", lambda e: e.reciprocal(rs[:, 3:4], rs[:, 2:3]), reads=[rs], writes=[rs])
                P.op("dve", lambda e, t=t: e.tensor_scalar(dg[:, t, :], ex[:], rs[:, 3:4], None, ALU.mult), reads=[ex, rs], writes=[dg])
            L.transpose_cols(hn, 8, pT, lambda hT=hT, t=t: hT[:, :, t * 128:(t + 1) * 128], hT)
        for ex_i in range(NE):
            w1e = w1_d.ap[ex_i] if moe is not None else w1_d.ap
            w3e = w3_d.ap[ex_i] if moe is not None else w3_d.ap
            w2e = w2_d.ap[ex_i] if moe is not None else w2_d.ap
            for pi in range(NFC // 2):
                w1p = w1r.next(); w3p = w3r.next()
                P.dma("pool", w1p[:], w1e[:, pi * 256:(pi + 1) * 256].rearrange("(kc p) n -> p kc n", p=128), reads=[w1_d], writes=[w1p])
                P.dma("pool", w3p[:], w3e[:, pi * 256:(pi + 1) * 256].rearrange("(kc p) n -> p kc n", p=128), reads=[w3_d], writes=[w3p])
                for fc in range(2):
                    p1 = p1r.next(); p3 = p3r.next(); s1 = s1r.next()
                    L.proj_feat(p1, hT, N, w1p, fc * 128)
                    L.proj_feat(p3, hT, N, w3p, fc * 128)
                    P.op("act", lambda e, s1=s1, p1=p1, N=N: e.activation(s1[:, 0:N], p1[:, 0:N], AF.Silu), reads=[p1], writes=[s1])
                    P.op("dve", lambda e, s1=s1, p3=p3, N=N, f=pi * 2 + fc: e.tensor_tensor(gT[:, f, 0:N], s1[:, 0:N], p3[:, 0:N], ALU.mult),
                         reads=[s1, p3], writes=[gT])
            for qd in range(4):
                w2q = w2r.next()
                P.dma("pool", w2q[:], w2e[:, qd * 256:(qd + 1) * 256].rearrange("(fc p) n -> p fc n", p=128), reads=[w2_d], writes=[w2q])
                for t in range(nt):
                    po = por.next(); tm = tmr.next()
                    for fc in range(NFC):
                        P.op("pe", lambda e, fc=fc, po=po, t=t, w2q=w2q: e.matmul(po[:, 0:256], gT[:, fc, t * 128:(t + 1) * 128], w2q[:, fc, :],
                                                                                start=(fc == 0), stop=(fc == NFC - 1)),
                             reads=[gT, w2q], writes=[po], inc=(fc == NFC - 1))
                    P.op("dve", lambda e, tm=tm, po=po, qd=qd, j=j: e.tensor_tensor(tm[:, 0:256], po[:, 0:256], g2[j][:, qd * 256:(qd + 1) * 256], ALU.mult),
                         reads=[po, g2[j]], writes=[tm])
                    if moe is None:
                        P.op("dve", lambda e, x=x, t=t, qd=qd, tm=tm: e.tensor_tensor(x[:, t, qd * 256:(qd + 1) * 256], x[:, t, qd * 256:(qd + 1) * 256], tm[:, 0:256], ALU.add),
                             reads=[x, tm], writes=[x])
                    else:
                        P.op("dve", lambda e, x=x, t=t, qd=qd, tm=tm, ex_i=ex_i: e.scalar_tensor_tensor(
                            x[:, t, qd * 256:(qd + 1) * 256], tm[:, 0:256], dg[:, t, ex_i:ex_i + 1], x[:, t, qd * 256:(qd + 1) * 256], ALU.mult, ALU.add),
                            reads=[x, tm, dg], writes=[x])
        xo_ap, xo_t = xo(t0, nt)
        P.dma("sp", xo_ap.rearrange("(t p) d -> p t d", p=128), x[:, 0:nt, :], reads=[x], writes=[xo_t])
    P.phase_end()


STOP1 = None


def emit_l1(L, io):
    nc = L.nc
    P = L.P
    _outer = P.gstack
    P.gstack = ExitStack()
    xall_tile = io["xall_tile"]; xown_blk = io["xown_blk"]; xo_blk = io["xo_blk"]
    csall = io["csall"]; csown = io["csown"]; ident_d = io["ident_d"]; cvec_d = io["cvec_d"]
    sfx = io["sfx"]
    L.modrow_d = io["modrow"]
    gmix_d = L.din("norm_mix_g" + sfx, [D])
    win_d = L.din("o_w_in", [D, 1792])
    qg_d = L.din("c_qnorm_g", [64])
    kg_d = L.din("c_knorm_g", [64])
    lrup_d = L.din("lru_p", [128, 4, 12])
    wa_d = L.din("d_wa", [2, 8, 64, 64])
    wx_d = L.din("d_wx", [2, 8, 64, 64])
    segw_d = L.din("segw", [4])
    KT_d = L.dscratch("KT1_d", [128, NALL], BF)
    VS_d = L.dscratch("VS1_d", [NALL, 128], BF)
    XR_d = L.dscratch("XR_d", [4, 128, NALL], F32)

    gq = P.sb([128, 64], F32, "gq", glob=True); gqs = P.sb([128, 64], F32, "gqs", glob=True)
    gk = P.sb([128, 64], F32, "gk", glob=True); gks = P.sb([128, 64], F32, "gks", glob=True)
    L.load_bc(gq, qg_d.ap, qg_d); L.load_bc(gk, kg_d.ap, kg_d)
    L.swap_gain(gq, gqs); L.swap_gain(gk, gks)
    gmix = P.sb([128, D], F32, "gmix", glob=True)
    L.load_bc(gmix, gmix_d.ap, gmix_d)

    yT = P.sb([128, 8, NOWN], BF, "yT", glob=True)
    gstack = ExitStack()
    gateT = T("gateT", gstack.enter_context(nc.sbuf_tensor("gateT_l1", [128, 4, OWN], BF)).ap())
    qstack = ExitStack()
    QT = T("QT", qstack.enter_context(nc.sbuf_tensor("QT_l1", [128, 2, 4, NOWN], BF)).ap())
    P.op("pool", lambda e: e.memset(QT[64:128, 0, :, :], 0.0), writes=[QT])
    P.op("pool", lambda e: e.memset(QT[0:64, 1, :, :], 0.0), writes=[QT])
    blocks = [(2 + 4 * i, 4) for i in range(4)]

    P.phase_begin()
    A1 = [L.make_A(L.mod_bc(0, 1, "A1"), gmix)]
    sh1 = [L.mod_bc(0, 0, "sh1")]
    Win = P.sb([128, 8, 1024], BF, "Win")
    for g in range(4):
        for n in range(2):
            h = n * 4 + g
            P.dma("pool", Win[:, :, g * 128 + n * 64:g * 128 + (n + 1) * 64],
                  win_d[:, h * 64:(h + 1) * 64].rearrange("(kc p) n -> p kc n", p=128), reads=[win_d], writes=[Win])
    P.dma("pool", Win[:, :, 512:1024], win_d[:, 768:1280].rearrange("(kc p) n -> p kc n", p=128), reads=[win_d], writes=[Win])
    xb = L.ring(1, [128, 4, D], F32, "xb"); csb = L.ring(2, [128, 4, 128], F32, "csb")
    hnr = L.ring(2, [128, D], BF, "hn"); jr = L.ring(2, [128, D], F32, "junk"); ssr = L.ring(2, [128, 2], F32, "ss")
    hTb = L.ring(1, [128, 8, 512], BF, "hTb")
    pTr = L.ring(2, [128, 8, 128], BF, "pT", psum=True)
    pur = L.ring(2, [128, 512], F32, "pu", psum=True)
    CTr = L.ring(2, [128, 64], F32, "CT"); STr = L.ring(2, [128, 64], F32, "ST")
    tmps = [tuple([P.sb([128, 512], F32, "sq"), P.sb([128, 512], F32, "qn"), P.sb([128, 512], F32, "t1"), P.sb([128, 8], F32, "ssq")]) for _ in range(2)]
    qbr = L.ring(2, [128, 512], BF, "qb")
    for bi, (t0, nt) in enumerate(blocks):
        N = nt * 128
        x = xb.next(); cs = csb.next(); hT = hTb.next()
        xb_ap, xb_t = xown_blk(t0, nt)
        P.dma("sp", x[:, 0:nt, :], xb_ap.rearrange("(t p) d -> p t d", p=128), reads=[xb_t], writes=[x])
        P.dma("sp", cs[:, 0:nt, :], csown[t0 * 128:(t0 + nt) * 128, :].rearrange("(t p) d -> p t d", p=128), reads=[csown], writes=[cs])
        for t in range(nt):
            hn = hnr.next(); junk = jr.next(); ss = ssr.next(); pT = pTr.next()
            L.norm_tile(x[:, t, :], x, A1[0], sh1[0], hn, junk, ss)
            L.transpose_cols(hn, 8, pT, lambda hT=hT, t=t: hT[:, :, t * 128:(t + 1) * 128], hT)
        for c in range(4):
            pu = pur.next()
            L.proj_feat(pu, hT, N, Win, 512 + c * 128)
            P.op("act", lambda e, pu=pu, c=c, N=N, t0=t0: e.activation(gateT[:, c, (t0 - 2) * 128:(t0 - 2) * 128 + N], pu[:, 0:N], AF.Gelu_apprx_tanh),
                 reads=[pu], writes=[gateT])
        for t in range(nt):
            tsl = slice(t * 128, (t + 1) * 128)
            gsl = slice((t0 + t) * 128, (t0 + t + 1) * 128)
            pq = pur.next(); CT = CTr.next(); ST = STr.next(); qb = qbr.next(); pT = pTr.next()
            L.proj_tok(pq, hT, tsl, Win, 0, 512)
            L.rope_tables(T_view(cs, cs[:, t, :]), gq, gqs, CT, ST)
            L.qk_post(pq, 8, CT, ST, qb, tmps[t % 2])
            for c4 in range(4):
                P.op("pe", lambda e, c4=c4, pT=pT, qb=qb: e.transpose(pT[:, c4, :], qb[:, c4 * 128:(c4 + 1) * 128], L.identb[:]),
                     reads=[qb, L.identb], writes=[pT], inc=(c4 == 3))
            P.op("dve", lambda e, pT=pT, gsl=gsl: e.tensor_copy(QT[0:64, 0, :, gsl], pT[0:64, 0:4, :]), reads=[pT], writes=[QT])
            P.op("act", lambda e, pT=pT, gsl=gsl: e.activation(QT[64:128, 1, :, gsl], pT[64:128, 0:4, :], AF.Identity), reads=[pT], writes=[QT])
    P.phase_end()

    P.phase_begin()
    A1 = [L.make_A(L.mod_bc(j, 1, "A1"), gmix) for j in range(2)]
    sh1 = [L.mod_bc(j, 0, "sh1") for j in range(2)]
    Wk = P.sb([128, 8, 768], BF, "Wkvx")
    P.dma("pool", Wk[:, :, 0:256], win_d[:, 512:768].rearrange("(kc p) n -> p kc n", p=128), reads=[win_d], writes=[Wk])
    P.dma("pool", Wk[:, :, 256:768], win_d[:, 1280:1792].rearrange("(kc p) n -> p kc n", p=128), reads=[win_d], writes=[Wk])
    xr = L.ring(3, [128, D], F32, "xa"); csr = L.ring(3, [128, 128], F32, "csa")
    hnr = L.ring(2, [128, D], BF, "hn"); jr = L.ring(2, [128, D], F32, "junk"); ssr = L.ring(2, [128, 2], F32, "ss")
    hTr = L.ring(2, [128, 8, 128], BF, "hT")
    pTr = L.ring(2, [128, 8, 128], BF, "pT", psum=True)
    pkr = L.ring(2, [128, 512], F32, "pk", psum=True); pxr = L.ring(2, [128, 512], F32, "px", psum=True)
    pXr = L.ring(2, [128, 4, 128], F32, "pX", psum=True)
    CTr = L.ring(2, [128, 64], F32, "CT"); STr = L.ring(2, [128, 64], F32, "ST")
    tmps = [tuple([P.sb([128, 512], F32, "sq"), P.sb([128, 512], F32, "qn"), P.sb([128, 512], F32, "t1"), P.sb([128, 8], F32, "ssq")]) for _ in range(2)]
    kbr = L.ring(2, [128, 128], BF, "kb"); vbr = L.ring(2, [128, 128], BF, "vb"); ktr = L.ring(2, [128, 1, 128], BF, "kt")
    xsr = L.ring(2, [128, 512], F32, "xs"); xtr = L.ring(2, [128, 4, 128], F32, "xt")
    for t in range(NT_ALL):
        j = 1 if t < 2 else 0
        x = xr.next(); cs = csr.next(); hn = hnr.next(); junk = jr.next(); ss = ssr.next(); hT = hTr.next(); pT = pTr.next()
        xa_ap, xa_t = xall_tile(t)
        P.dma("sp", x[:], xa_ap, reads=[xa_t], writes=[x])
        P.dma("sp", cs[:], csall[t * 128:(t + 1) * 128, :], reads=[csall], writes=[cs])
        L.norm_tile(x[:], x, A1[j], sh1[j], hn, junk, ss)
        L.transpose_cols(hn, 8, pT, lambda hT=hT: hT[:], hT)
        pk = pkr.next(); px = pxr.next()
        L.proj_tok(pk, hT, slice(0, 128), Wk, 0, 256)
        L.proj_tok(px, hT, slice(0, 128), Wk, 256, 512)
        vb = vbr.next()
        P.op("act", lambda e, vb=vb, pk=pk: e.activation(vb[:], pk[:, 128:256], AF.Identity), reads=[pk], writes=[vb])
        P.dma("pool", VS_d[t * 128:(t + 1) * 128, :], vb[:], reads=[vb], writes=[VS_d])
        CT = CTr.next(); ST = STr.next()
        L.rope_tables(cs, gk, gks, CT, ST)
        kb = kbr.next()
        L.qk_post(pk, 2, CT, ST, kb, tmps[t % 2])
        pT2 = pTr.next(); kt = ktr.next()
        L.transpose_cols(kb, 1, pT2, lambda kt=kt: kt[:], kt, eng="dve")
        P.dma("pool", KT_d[:, t * 128:(t + 1) * 128], kt[:, 0, :], reads=[kt], writes=[KT_d])
        xs = xsr.next(); xt = xtr.next(); pX = pXr.next()
        P.op("act", lambda e, xs=xs, px=px: e.activation(xs[:], px[:], AF.Identity), reads=[px], writes=[xs])
        for c in range(4):
            P.op("pe", lambda e, c=c, pX=pX, xs=xs: e.transpose(pX[:, c, :], xs[:, c * 128:(c + 1) * 128], L.ident[:]),
                 reads=[xs, L.ident], writes=[pX], inc=(c == 3))
        P.op("dve", lambda e, xt=xt, pX=pX: e.tensor_copy(xt[:], pX[:]), reads=[pX], writes=[xt])
        P.dma("pool", XR_d[:, :, t * 128:(t + 1) * 128].rearrange("j p t -> p j t"), xt[:], reads=[xt], writes=[XR_d])
    P.phase_end()

    P.phase_begin()
    KT = P.sb([128, NALL], BF, "KT")
    for pc in range(6):
        P.dma("sp", KT[:, pc * 1408:(pc + 1) * 1408], KT_d[:, pc * 1408:(pc + 1) * 1408], reads=[KT_d], writes=[KT])
    Vn = [P.sb([128, NT_ALL, 128], BF, "Vn") for _ in range(2)]
    for n in range(2):
        P.op("pool", lambda e, n=n: e.memset(Vn[n][:, :, 64:128], 1.0), writes=[Vn[n]])
        for pc in range(11):
            P.dma("sp", Vn[n][:, pc * 6:(pc + 1) * 6, 0:64],
                  VS_d[pc * 768:(pc + 1) * 768, n * 64:(n + 1) * 64].rearrange("(t p) e -> p t e", p=128), reads=[VS_d], writes=[Vn[n]])
    spr = L.ring(4, [128, 512], F32, "sp", psum=True)
    accr = L.ring(2, [128, 512], F32, "acc", psum=True)
    pXo = L.ring(1, [128, 4, 128], F32, "pXo", psum=True)
    LOOK1 = 3
    pTo = P.ps([128, 4, 128], BF, "pTo")
    ptr_ = L.ring(6, [128, 512], BF, "pts")
    rzr = L.ring(2, [128, 4], F32, "rz")
    osr = L.ring(2, [128, 512], F32, "osb")
    yar = L.ring(2, [128, 512], BF, "yat")
    for qt in range(OWN // 128):
        q0 = LC + qt * 128
        yat = yar.next()
        for n in range(2):
            acc = accr.next()

            def emit_scores1(kt, n=n, q0=q0):
                sp_ = spr.next()
                P.op("pe", lambda e, n=n, sp_=sp_, kt=kt, q0=q0: e.matmul(
                    sp_[:].rearrange("p (g q) -> p g q", g=4), KT[:, kt * 128:(kt + 1) * 128],
                    QT[:, n, :, q0:q0 + 128], start=True, stop=True), reads=[KT, QT], writes=[sp_])
                return sp_

            pend_sc = [emit_scores1(i) for i in range(LOOK1)]
            for kt in range(NT_ALL):
                sp_ = pend_sc.pop(0)
                if kt + LOOK1 < NT_ALL:
                    pend_sc.append(emit_scores1(kt + LOOK1))
                pt = ptr_.next()
                P.op("act", lambda e, pt=pt, sp_=sp_: e.activation(pt[:], sp_[:], AF.Exp, scale=0.125), reads=[sp_], writes=[pt])
                P.op("pe", lambda e, pt=pt, n=n, kt=kt, acc=acc: e.matmul(acc[:], Vn[n][:, kt, :], pt[:], start=(kt == 0), stop=(kt == NT_ALL - 1)),
                     reads=[pt, Vn[n]], writes=[acc], inc=(kt == NT_ALL - 1))
            osb = osr.next(); pX = pXo.next(); rz = rzr.next()
            P.op("act", lambda e, osb=osb, acc=acc: e.activation(osb[:], acc[:], AF.Identity), reads=[acc], writes=[osb])
            for g in range(4):
                P.op("pe", lambda e, g=g, pX=pX, osb=osb: e.transpose(pX[:, g, :], osb[:, g * 128:(g + 1) * 128], L.ident[:]),
                     reads=[osb, L.ident], writes=[pX], inc=(g == 3))
            P.op("dve", lambda e, rz=rz, pX=pX: e.reciprocal(rz[:, 0:4], pX[:, :, 64]), reads=[pX], writes=[rz])
            for g in range(4):
                h = n * 4 + g
                P.op("act", lambda e, g=g, h=h, rz=rz, yat=yat, pX=pX: e.activation(yat[:, h * 64:(h + 1) * 64], pX[:, g, 0:64], AF.Identity, scale=rz[:, g:g + 1]),
                     reads=[pX, rz], writes=[yat])
        for c in range(4):
            P.op("pe", lambda e, c=c, yat=yat: e.transpose(pTo[:, c, :], yat[:, c * 128:(c + 1) * 128], L.identb[:]),
                 reads=[yat, L.identb], writes=[pTo], inc=(c == 3))
        P.op("act", lambda e, q0=q0: e.activation(yT[:, 0:4, q0:q0 + 128], pTo[:], AF.Identity), reads=[pTo], writes=[yT])
    P.phase_end()
    qstack.close()

    P.phase_begin()
    lp = P.sb([128, 4, 12], F32, "lp")
    P.dma("sp", lp[:], lrup_d[:, :, :], reads=[lrup_d], writes=[lp])
    sw = P.sb([128, 4], F32, "segw"); L.load_bc(sw, segw_d.ap, segw_d)
    nsp = P.sb([128, 4, 2], F32, "nsp")
    P.op("act", lambda e: e.activation(nsp[:], lp[:, :, 9:11], AF.Exp, scale=-1.0), reads=[lp], writes=[nsp])
    P.op("act", lambda e: e.activation(nsp[:], nsp[:], AF.Ln, bias=1.0), reads=[nsp], writes=[nsp])
    P.op("dve", lambda e: e.tensor_scalar(nsp[:], nsp[:], -8.0, None, ALU.mult), reads=[nsp], writes=[nsp])
    BD = P.sb([128, 16, 128], BF, "BD")
    P.op("dve", lambda e: e.memset(BD[:], 0.0), writes=[BD])
    for kind, wd in enumerate((wa_d, wx_d)):
        for d in range(2):
            for jj in range(4):
                for hh in range(2):
                    P.dma("pool", BD[hh * 64:(hh + 1) * 64, kind * 8 + d * 4 + jj, hh * 64:(hh + 1) * 64], wd.ap[d, 2 * jj + hh, :, :],
                          reads=[wd], writes=[BD])
    SEGL = 4096
    X = P.sb([128, NALL], F32, "Xr"); XD = P.sb([128, NALL], F32, "XD"); XDB = P.sb([128, NALL], BF, "XDB")
    Ab = T_view(X, X[:, 0:SEGL]); Bb = T_view(X, X[:, SEGL:2 * SEGL]); Hb = P.sb([128, SEGL], F32, "Hb")
    Ib = P.sb([128, SEGL], F32, "Ib")
    acc = P.sb([128, OWN], F32, "hacc")
    st = P.sb([128, 2], F32, "st")
    prr = L.ring(2, [128, 512], F32, "pr", psum=True); pir = L.ring(2, [128, 512], F32, "pi", psum=True)
    for jj in range(4):
        for pc in range(6):
            P.dma("sp", X[:, pc * 1408:(pc + 1) * 1408], XR_d[jj, :, pc * 1408:(pc + 1) * 1408], reads=[XR_d], writes=[X])
        P.op("dve", lambda e, jj=jj: e.tensor_scalar(XD[:], X[:], lp[:, jj, 2:3], lp[:, jj, 4:5], ALU.mult, ALU.add), reads=[X, lp], writes=[XD])
        for (lo, hi) in ((0, LC), (LC, NALL)):
            for tap, off in ((0, -2), (1, -1), (3, 1)):
                a = max(lo, lo - off); b = min(hi, hi - off)
                P.op("dve", lambda e, jj=jj, tap=tap, off=off, a=a, b=b: e.scalar_tensor_tensor(
                    XD[:, a:b], X[:, a + off:b + off], lp[:, jj, tap:tap + 1], XD[:, a:b], ALU.mult, ALU.add), reads=[X, lp, XD], writes=[XD])
        P.op("pool", lambda e: e.tensor_copy(XDB[:], XD[:]), reads=[XD], writes=[XDB])
        P.op("dve", lambda e: e.memset(acc[:], 0.0), writes=[acc])
        for d in range(2):
            segs = [(0, LC), (LC, LC + SEGL), (LC + SEGL, NALL)]
            order = segs if d == 0 else [segs[0], segs[2], segs[1]]
            for si, (lo, hi) in enumerate(order):
                n = hi - lo
                for b0 in range(0, n, 512):
                    bw = min(512, n - b0)
                    pr = prr.next(); pi_ = pir.next()
                    P.op("pe", lambda e, pr=pr, d=d, jj=jj, lo=lo, b0=b0, bw=bw: e.matmul(pr[:, 0:bw], BD[:, d * 4 + jj, :], XDB[:, lo + b0:lo + b0 + bw], start=True, stop=True),
                         reads=[BD, XDB], writes=[pr])
                    P.op("pe", lambda e, pi_=pi_, d=d, jj=jj, lo=lo, b0=b0, bw=bw: e.matmul(pi_[:, 0:bw], BD[:, 8 + d * 4 + jj, :], XDB[:, lo + b0:lo + b0 + bw], start=True, stop=True),
                         reads=[BD, XDB], writes=[pi_])
                    sl = slice(b0, b0 + bw)
                    P.op("act", lambda e, pr=pr, bw=bw, sl=sl, d=d, jj=jj: e.activation(Ab[:, sl], pr[:, 0:bw], AF.Sigmoid, bias=lp[:, jj, 5 + d:6 + d]), reads=[pr, lp], writes=[Ab])
                    P.op("act", lambda e, pi_=pi_, bw=bw, sl=sl, d=d, jj=jj: e.activation(Ib[:, sl], pi_[:, 0:bw], AF.Sigmoid, bias=lp[:, jj, 7 + d:8 + d]), reads=[pi_, lp], writes=[Ib])
                P.op("act", lambda e, n=n, d=d, jj=jj: e.activation(Ab[:, 0:n], Ab[:, 0:n], AF.Exp, scale=nsp[:, jj, d:d + 1]), reads=[Ab, nsp], writes=[Ab])
                P.op("dve", lambda e, n=n: e.tensor_tensor(Hb[:, 0:n], Ab[:, 0:n], Ab[:, 0:n], ALU.mult), reads=[Ab], writes=[Hb])
                P.op("act", lambda e, n=n: e.activation(Hb[:, 0:n], Hb[:, 0:n], AF.Sqrt, bias=1.0, scale=-1.0), reads=[Hb], writes=[Hb])
                P.op("dve", lambda e, n=n: e.tensor_tensor(Hb[:, 0:n], Hb[:, 0:n], Ib[:, 0:n], ALU.mult), reads=[Hb, Ib], writes=[Hb])
                P.op("pool", lambda e, n=n, lo=lo: e.tensor_tensor(Bb[:, 0:n], Hb[:, 0:n], XD[:, lo:lo + n], ALU.mult), reads=[Hb, XD], writes=[Bb])
                init = 0.0 if si == 0 else st[:, d:d + 1]
                if d == 0:
                    P.op("dve", lambda e, n=n, init=init: e.tensor_tensor_scan(Hb[:, 0:n], Ab[:, 0:n], Bb[:, 0:n], init, ALU.mult, ALU.add),
                         reads=[Ab, Bb, st], writes=[Hb])
                    P.op("dve", lambda e, n=n, d=d: e.tensor_copy(st[:, d:d + 1], Hb[:, n - 1:n]), reads=[Hb], writes=[st])
                else:
                    P.op("dve", lambda e, n=n, init=init: e.tensor_tensor_scan(Hb[:, 0:n][:, ::-1], Ab[:, 0:n][:, ::-1], Bb[:, 0:n][:, ::-1], init, ALU.mult, ALU.add),
                         reads=[Ab, Bb, st], writes=[Hb])
                    P.op("dve", lambda e, d=d: e.tensor_copy(st[:, d:d + 1], Hb[:, 0:1]), reads=[Hb], writes=[st])
                if lo >= LC:
                    s0 = (lo - LC) // OWN
                    for k in range(2):
                        P.op("dve", lambda e, k=k, s0=s0: e.scalar_tensor_tensor(acc[:], Hb[:, k * OWN:(k + 1) * OWN], sw[:, s0 + k:s0 + k + 1], acc[:], ALU.mult, ALU.add),
                             reads=[Hb, sw, acc], writes=[acc])
        P.op("dve", lambda e, jj=jj: e.tensor_tensor(yT[:, 4 + jj, LC:LC + OWN], acc[:], gateT[:, jj, :], ALU.mult), reads=[acc, gateT], writes=[yT])
    P.phase_end()
    gstack.close()

    gffn_d = L.din("norm_ffn_g" + sfx, [D])
    wout_d = L.din("w_out" + sfx, [D, D])
    rw_d = L.din("router_w", [D, 8]); rb_d = L.din("router_b", [8])
    w1_d = L.din("moe_w1", [8, D, FF]); w3_d = L.din("moe_w3", [8, D, FF]); w2_d = L.din("moe_w2", [8, FF, D])
    gffn = P.sb([128, D], F32, "gffn", glob=True)
    L.load_bc(gffn, gffn_d.ap, gffn_d)
    ffn_phase(L, xown_blk, xo_blk, blocks, yT, wout_d, gffn, w1_d, w3_d, w2_d, ctx_block=False, moe=(rw_d, rb_d, 8))
    P.barrier()
    P.flush()
    P.gstack.close()
    P.gstack = _outer


def build_fused():
    nc = bass.Bass("TRN2", target_bir_lowering=False)
    L = LK(nc)
    P = L.P
    xall = L.din("xall", [NALL, D])
    xown = L.din("xown", [NOWN, D])
    csall = L.din("csall", [NALL, 128])
    csown = L.din("csown", [NOWN, 128])
    ident_d = L.din("ident", [128, 128])
    cvec_d = L.din("cvec", [128, 16])
    xo = L.dout("xo", [OWN, D])
    xc1_d = L.dscratch("xc1_d", [LC, D])
    x1own_d = L.dscratch("x1own_d", [OWN, D])
    NCH = OWN // 256
    x1g_d = [L.dscratch(f"x1g{k}_d", [4 * 256, D]) for k in range(NCH)]
    L.load_ident(ident_d)
    modrows = []
    for lay in range(2):
        wmod_d = L.din(f"w_mod{lay}", [D, 6 * D]); bmod_d = L.din(f"b_mod{lay}", [6 * D])
        modrows.append(L.setup_common(cvec_d, wmod_d, bmod_d, f"l{lay}"))
    io0 = dict(csall=csall, csown=csown, ident_d=ident_d, cvec_d=cvec_d, sfx="0", modrow=modrows[0],
               xall_tile=lambda t: (xall[t * 128:(t + 1) * 128, :], xall),
               xown_blk=lambda t0, nt: (xown[t0 * 128:(t0 + nt) * 128, :], xown),
               xo_blk=lambda t0, nt: ((xc1_d[0:LC, :], xc1_d) if t0 == 0 else
                                      (x1own_d[(t0 - 2) * 128:(t0 - 2 + nt) * 128, :], x1own_d)))
    emit_l0(L, io0)
    for k in range(NCH):
        csem = nc.alloc_semaphore(f"ccsem{k}")
        waits = P._waits("pool", P._deps([x1own_d], [x1g_d[k]]), False)

        def emit_cc(eng, waits=waits, k=k, csem=csem):
            for (s_, v_) in waits:
                eng.wait_ge(s_, v_)
            eng.collective_compute("AllGather", ALU.bypass, replica_groups=[[0, 1, 2, 3], [4, 5, 6, 7]],
                                   ins=[x1own_d[k * 256:(k + 1) * 256, :].opt()], outs=[x1g_d[k].ap.opt()]).then_inc(csem)

        P.q["pool"].append(emit_cc)
        tok = (csem, 1, "cc")
        x1g_d[k].w = tok; x1g_d[k].r = {}
        x1own_d.r[csem.name] = tok

    def lat_tile(t):
        g = t - 2
        r, i = g // 16, g % 16
        k, h = i // 2, i % 2
        return (x1g_d[k][r * 256 + h * 128:r * 256 + (h + 1) * 128, :], x1g_d[k])

    io1 = dict(csall=csall, csown=csown, ident_d=ident_d, cvec_d=cvec_d, sfx="1", modrow=modrows[1],
               xall_tile=lambda t: ((xc1_d[t * 128:(t + 1) * 128, :], xc1_d) if t < 2 else lat_tile(t)),
               xown_blk=lambda t0, nt: (x1own_d[(t0 - 2) * 128:(t0 - 2 + nt) * 128, :], x1own_d),
               xo_blk=lambda t0, nt: (xo[(t0 - 2) * 128:(t0 - 2 + nt) * 128, :], xo))
    emit_l1(L, io1)
    P.finish([xo])
    return nc, L


_ROPE = None


def _cvec(cb, cctx):
    v = np.stack([cb, cctx], 1).astype(np.float32)
    return np.ascontiguousarray(v.reshape(8, 128, 2).transpose(1, 0, 2).reshape(128, 16))


def _lru_pack(inp):
    cw = np.asarray(inp["d_conv_w"])[0]; cb = np.asarray(inp["d_conv_b"])[0]
    ba = np.asarray(inp["d_ba"])[0]; bx = np.asarray(inp["d_bx"])[0]; lam = np.asarray(inp["d_lambda"])[0]
    cols = [cw[0], cw[1], cw[2], cw[3], cb, ba[0], ba[1], bx[0], bx[1], lam[0], lam[1], np.zeros(512, np.float32)]
    p = np.stack(cols, 1).astype(np.float32)
    return np.ascontiguousarray(p.reshape(4, 128, 12).transpose(1, 0, 2))


_NC = None


def kernel(**inputs):
    global _ROPE, _NC
    if _ROPE is None:
        _ROPE = _rope_table()
    inp = inputs
    nc, L = build_fused()
    x = np.asarray(inp["x"], np.float32); ctx = np.asarray(inp["ctx"], np.float32)
    ident = np.eye(128, dtype=np.float32)
    lru_p = _lru_pack(inp)
    g = lambda k, i=0: np.asarray(inp[k])[i]
    shared = {
        "ident": ident, "csall": _ROPE, "lru_p": lru_p,
        "w_mod0": g("w_mod", 0), "b_mod0": g("b_mod", 0), "norm_mix_g0": g("norm_mix_g", 0), "norm_ffn_g0": g("norm_ffn_g", 0),
        "w_out0": g("w_out", 0), "w_mod1": g("w_mod", 1), "b_mod1": g("b_mod", 1), "norm_mix_g1": g("norm_mix_g", 1),
        "norm_ffn_g1": g("norm_ffn_g", 1), "w_out1": g("w_out", 1),
        "e_w_in": g("e_w_in"), "a_norm_g": g("a_norm_g"), "a_ws": g("a_ws"), "a_bs": g("a_bs"),
        "b_qnorm_g": g("b_qnorm_g"), "b_knorm_g": g("b_knorm_g"), "b_lq1": g("b_lq1"), "b_lk1": g("b_lk1"),
        "b_lq2": g("b_lq2"), "b_lk2": g("b_lk2"), "b_subln_g": g("b_subln_g"),
        "ffn_w1": g("ffn_w1"), "ffn_w3": g("ffn_w3"), "ffn_w2": g("ffn_w2"),
        "o_w_in": g("o_w_in"), "c_qnorm_g": g("c_qnorm_g"), "c_knorm_g": g("c_knorm_g"),
        "d_wa": g("d_wa"), "d_wx": g("d_wx"), "router_w": g("router_w"), "router_b": g("router_b"),
        "moe_w1": g("moe_w1"), "moe_w3": g("moe_w3"), "moe_w2": g("moe_w2"),
    }
    shared = {k: np.ascontiguousarray(v, dtype=np.float32) for k, v in shared.items()}
    maps = []
    for core in range(8):
        b, s = core // 4, core % 4
        xall = np.concatenate([ctx[b], x[b]], 0)
        own = slice(LC + s * OWN, LC + (s + 1) * OWN)
        segw = np.zeros(4, np.float32); segw[s] = 1.0
        m = dict(shared)
        m.update({"xall": xall, "xown": np.concatenate([ctx[b], xall[own]], 0),
                  "csown": np.concatenate([_ROPE[:LC], _ROPE[own]], 0),
                  "cvec": _cvec(np.asarray(inp["c"])[b], np.asarray(inp["c_ctx"])), "segw": segw})
        maps.append({k: np.ascontiguousarray(m[k], dtype=np.float32) for k in L.inputs})
    res = run_bass_kernel_spmd(nc, maps, core_ids=list(range(8)))
    out = np.zeros_like(x)
    for core in range(8):
        b, s = core // 4, core % 4
        out[b, s * OWN:(s + 1) * OWN] = res.results[core]["xo"]
    return out.astype(np.float32)
```

```python
from contextlib import ExitStack

import numpy as np
import concourse.bass as bass
import concourse.mybir as mybir
from concourse.bass_utils import run_bass_kernel_spmd

F32 = mybir.dt.float32
BF = mybir.dt.bfloat16
AF = mybir.ActivationFunctionType
ALU = mybir.AluOpType
AX = mybir.AxisListType

D = 1024
SEQ = 8192
LC = 256
NALL = SEQ + LC
NT_ALL = NALL // 128
OWN = 2048
NOWN = OWN + LC
FF = 2816
NFC = FF // 128
EPS = 1e-6


class T:
    def __init__(self, name, ap):
        self.name = name
        self.ap = ap
        self.w = None
        self.r = {}

    def __getitem__(self, k):
        return self.ap[k]


class Prog:
    ENG = ("pe", "act", "dve", "pool", "sp")
    SEM_ROLL = 30000
    N_DMA_SEMS = 32

    def __init__(self, nc):
        self.nc = nc
        self.q = {e: [] for e in self.ENG}
        self.sem = {}
        self.cnt = {}
        self.nsem = 0
        self.retired = {}
        self._rec = None
        for e in self.ENG:
            self._new_sem(e)
        self.dsem = []
        for i in range(self.N_DMA_SEMS):
            s = nc.alloc_semaphore(f"dma{i}")
            self.dsem.append([s, 0])
        self.dnext = 0
        self.seen = {e: {} for e in self.ENG}
        self.pend = {e: False for e in self.ENG}
        self.ninst = 0
        self.gstack = ExitStack()
        self.stack = None
        self.uid = 0

    def _new_sem(self, e):
        if e in self.sem and self.cnt.get(e, 0) > 0:
            self.retired.setdefault(e, []).append((self.sem[e], self.cnt[e], e))
        self.nsem += 1
        self.sem[e] = self.nc.alloc_semaphore(f"s_{e}_{self.nsem}")
        self.cnt[e] = 0

    def _deps(self, reads, writes):
        deps = []
        for t in reads:
            if t.w is not None:
                deps.append(t.w)
        for t in writes:
            if t.w is not None:
                deps.append(t.w)
            deps.extend(t.r.values())
        return deps

    def _waits(self, e, deps, same_ok):
        out = {}
        for (s, v, src) in deps:
            if src == e and same_ok:
                continue
            key = s.name
            if self.seen[e].get(key, 0) >= v:
                continue
            if key not in out or out[key][1] < v:
                out[key] = (s, v)
        for key, (s, v) in out.items():
            self.seen[e][key] = v
        return list(out.values())

    def _mark(self, tok, reads, writes):
        s, v, src = tok
        key = s.name
        for t in reads:
            old = t.r.get(key)
            if old is None or old[1] < v:
                t.r[key] = tok
        for t in writes:
            t.w = tok
            t.r = {}

    def record_begin(self):
        self._rec = []

    def record_end(self):
        r, self._rec = self._rec, None
        return r

    def mark(self):
        if self._rec is not None:
            self._rec.append(("mark", (), {}))

    def _replay(self, ops):
        for kind, a, kw in ops:
            if kind == "op":
                self.op(*a, **kw)
            elif kind == "dma":
                self.dma(*a, **kw)

    def replay_skewed(self, recs):
        split = []
        for r in recs:
            k = next(i for i, o in enumerate(r) if o[0] == "mark")
            split.append((r[:k], r[k + 1:]))
        n = len(split)
        self._replay(split[0][0])
        for t in range(n):
            if t + 1 < n:
                self._replay(split[t + 1][0])
            self._replay(split[t][1])

    def op(self, e, fn, reads=(), writes=(), inc=True, same_ok=None):
        if getattr(self, "_rec", None) is not None:
            self._rec.append(("op", (e, fn), dict(reads=reads, writes=writes, inc=inc, same_ok=same_ok)))
            return None
        if same_ok is None:
            same_ok = (e == "pe")
        waits = self._waits(e, self._deps(reads, writes), same_ok)
        if self.cnt[e] >= self.SEM_ROLL and not self.pend[e]:
            self._new_sem(e)
        if inc:
            self.cnt[e] += 1
            tok = (self.sem[e], self.cnt[e], e)
            self.pend[e] = False
        else:
            tok = (self.sem[e], self.cnt[e] + 1, e)
            self.pend[e] = True
        sem = self.sem[e]

        def emit(eng, fn=fn, waits=waits, inc=inc, sem=sem):
            for (s, v) in waits:
                eng.wait_ge(s, v)
            ins = fn(eng)
            if inc:
                ins.then_inc(sem, 1)

        self.q[e].append(emit)
        self._mark(tok, reads, writes)
        self.ninst += 1
        return tok

    def dma(self, e, out_ap, in_ap, reads=(), writes=(), **kw):
        if getattr(self, "_rec", None) is not None:
            self._rec.append(("dma", (e, out_ap, in_ap), dict(reads=reads, writes=writes, **kw)))
            return None
        d = self.dsem[self.dnext]
        self.dnext = (self.dnext + 1) % self.N_DMA_SEMS
        s, prev = d
        deps = self._deps(reads, writes)
        if prev > 0:
            deps.append((s, prev, "dma"))
        waits = self._waits(e, deps, False)
        d[1] = prev + 16
        tok = (s, prev + 16, "dma")

        def emit(eng, waits=waits, s=s):
            for (ws, v) in waits:
                eng.wait_ge(ws, v)
            eng.dma_start(out=out_ap, in_=in_ap, **kw).then_inc(s, 16)

        self.q[e].append(emit)
        self._mark(tok, reads, writes)
        self.ninst += 1
        return tok

    def wait_all(self, e, toks):
        waits = self._waits(e, list(toks), False)

        def emit(eng, waits=waits):
            for (s, v) in waits:
                eng.wait_ge(s, v)

        self.q[e].append(emit)

    def barrier(self):
        toks = []
        for e in self.ENG:
            if self.cnt[e] > 0:
                toks.append((self.sem[e], self.cnt[e], e))
            elif self.retired.get(e):
                toks.append(self.retired[e][-1])
        toks += [(s, v, "dma") for s, v in self.dsem if v > 0]
        for e in self.ENG:
            assert not self.pend[e]
            self.wait_all(e, toks)

    def phase_begin(self):
        self.stack = ExitStack()

    def flush(self):
        if not any(self.q.values()):
            return
        q = self.q
        self.q = {e: [] for e in self.ENG}
        nc = self.nc
        with nc.Block() as block:
            @block.tensor
            def _(eng):
                for f in q["pe"]:
                    f(eng)

            @block.scalar
            def _(eng):
                for f in q["act"]:
                    f(eng)

            @block.vector
            def _(eng):
                for f in q["dve"]:
                    f(eng)

            @block.gpsimd
            def _(eng):
                for f in q["pool"]:
                    f(eng)

            @block.sync
            def _(eng):
                for f in q["sp"]:
                    f(eng)

    def phase_end(self):
        self.barrier()
        self.flush()
        self.stack.close()
        self.stack = None

    def sb(self, shape, dt=F32, name="t", glob=False):
        self.uid += 1
        nm = f"{name}_{self.uid}"
        st = self.gstack if (glob or self.stack is None) else self.stack
        h = st.enter_context(self.nc.sbuf_tensor(nm, list(shape), dt))
        return T(nm, h.ap())

    def ps(self, shape, dt=F32, name="p", glob=False):
        self.uid += 1
        nm = f"{name}_{self.uid}"
        st = self.gstack if (glob or self.stack is None) else self.stack
        h = st.enter_context(self.nc.psum_tensor(nm, list(shape), dt))
        return T(nm, h.ap())

    def finish(self, final_tiles):
        toks = [t.w for t in final_tiles if t.w is not None]
        self.wait_all("sp", toks)
        self.barrier()
        self.flush()
        self.gstack.close()


class Ring:
    def __init__(self, tiles):
        self.tiles = tiles
        self.i = 0

    def next(self):
        t = self.tiles[self.i % len(self.tiles)]
        self.i += 1
        return t


class LK:
    def __init__(self, nc):
        self.nc = nc
        self.P = Prog(nc)

    def din(self, name, shape, dt=F32):
        if not hasattr(self, "inputs"):
            self.inputs = []
        self.inputs.append(name)
        return T(name, self.nc.dram_tensor(name, list(shape), dt, kind="ExternalInput").ap())

    def dout(self, name, shape, dt=F32):
        return T(name, self.nc.dram_tensor(name, list(shape), dt, kind="ExternalOutput").ap())

    def dscratch(self, name, shape, dt=F32):
        return T(name, self.nc.dram_tensor(name, list(shape), dt, kind="Internal").ap())

    def ring(self, n, shape, dt=F32, name="r", psum=False):
        P = self.P
        return Ring([(P.ps if psum else P.sb)(shape, dt, name) for _ in range(n)])

    def load_ident(self, ident_d):
        P = self.P
        self.ident = P.sb([128, 128], F32, "ident", glob=True)
        self.identb = P.sb([128, 128], BF, "identb", glob=True)
        P.dma("sp", self.ident[:], ident_d[:, :], reads=[ident_d], writes=[self.ident])
        P.op("dve", lambda e: e.tensor_copy(self.identb[:], self.ident[:]), reads=[self.ident], writes=[self.identb])
        self.epsT = P.sb([128, 1], F32, "epsT", glob=True)
        P.op("dve", lambda e: e.memset(self.epsT[:], EPS), writes=[self.epsT])

    def setup_common(self, cvec_d, wmod_d, bmod_d, tag=""):
        P = self.P
        modrow_d = self.dscratch("modrow_d" + tag, [2, 6 * D])
        P.phase_begin()
        cv = P.sb([128, 16], F32, "cv")
        sc = P.sb([128, 16], F32, "sc")
        bm = P.sb([2, 6 * D], F32, "bm")
        mr = P.sb([2, 6 * D], F32, "mr")
        P.dma("sp", cv[:], cvec_d[:, :], reads=[cvec_d], writes=[cv])
        for j in range(2):
            P.dma("sp", bm[j:j + 1, :], bmod_d.ap.unsqueeze(0), reads=[bmod_d], writes=[bm])
        P.op("act", lambda e: e.activation(sc[:], cv[:], AF.Silu), reads=[cv], writes=[sc])
        wms = self.ring(2, [128, 8, 512], F32, "wm")
        pms = self.ring(2, [128, 512], F32, "pm", psum=True)
        for pc in range(12):
            wm = wms.next()
            pm = pms.next()
            P.dma("sp", wm[:], wmod_d[:, pc * 512:(pc + 1) * 512].rearrange("(kc p) n -> p kc n", p=128),
                  reads=[wmod_d], writes=[wm])
            for kc in range(8):
                P.op("pe", lambda e, kc=kc, wm=wm, pm=pm: e.matmul(pm[0:2, :], sc[:, kc * 2:(kc + 1) * 2], wm[:, kc, :],
                                                                 start=(kc == 0), stop=(kc == 7)),
                     reads=[sc, wm], writes=[pm], inc=(kc == 7))
            P.op("dve", lambda e, pc=pc, pm=pm: e.tensor_tensor(mr[0:2, pc * 512:(pc + 1) * 512], pm[0:2, :],
                                                              bm[0:2, pc * 512:(pc + 1) * 512], ALU.add),
                 reads=[pm, bm], writes=[mr])
        P.dma("sp", modrow_d[:, :], mr[:], reads=[mr], writes=[modrow_d])
        P.phase_end()
        return modrow_d

    def load_bc(self, dst, src_row_ap, src_t):
        self.P.dma("sp", dst[:], src_row_ap.partition_broadcast(128), reads=[src_t], writes=[dst])

    def mod_bc(self, j, idx, name):
        t = self.P.sb([128, D], F32, name)
        self.load_bc(t, self.modrow_d.ap[j, idx * D:(idx + 1) * D], self.modrow_d)
        return t

    def make_A(self, sc_t, g_t):
        self.P.op("dve", lambda e: e.scalar_tensor_tensor(sc_t[:], sc_t[:], 1.0, g_t[:], ALU.add, ALU.mult),
                  reads=[sc_t, g_t], writes=[sc_t])
        return sc_t

    def rstd(self, ss, n, dim):
        P = self.P
        P.op("act", lambda e: e.activation(ss[:, 0:n], ss[:, 0:n], AF.Sqrt, bias=self.epsT[:, 0:1], scale=1.0 / dim),
             reads=[ss, self.epsT], writes=[ss])
        P.op("dve", lambda e: e.reciprocal(ss[:, 0:n], ss[:, 0:n]), reads=[ss], writes=[ss])

    def norm_tile(self, x_ap, x_t, A_bc, sh_bc, hn, junk, ss):
        P = self.P
        P.op("act", lambda e: e.activation(junk[:], x_ap, AF.Square, accum_out=ss[:, 0:1]),
             reads=[x_t], writes=[junk, ss])
        self.rstd(ss, 1, D)
        P.op("dve", lambda e: e.scalar_tensor_tensor(junk[:], x_ap, ss[:, 0:1], A_bc[:], ALU.mult, ALU.mult),
             reads=[x_t, ss, A_bc], writes=[junk])
        P.op("pool", lambda e: e.tensor_tensor(hn[:], junk[:], sh_bc[:], ALU.add), reads=[junk, sh_bc], writes=[hn])

    def transpose_cols(self, src, ncol_chunks, pT, dst_ap_fn, dst_t, eng="act"):
        P = self.P
        for c in range(ncol_chunks):
            P.op("pe", lambda e, c=c: e.transpose(pT[:, c, :], src[:, c * 128:(c + 1) * 128], self.identb[:]),
                 reads=[src, self.identb], writes=[pT], inc=(c == ncol_chunks - 1))
        if eng == "act":
            P.op("act", lambda e: e.activation(dst_ap_fn(), pT[:, 0:ncol_chunks, :], AF.Identity), reads=[pT], writes=[dst_t])
        else:
            P.op("dve", lambda e: e.tensor_copy(dst_ap_fn(), pT[:, 0:ncol_chunks, :]), reads=[pT], writes=[dst_t])

    def proj_tok(self, ps, hT, tsl, W, c0, ncols):
        for kc in range(8):
            self.P.op("pe", lambda e, kc=kc: e.matmul(ps[:, 0:ncols], hT[:, kc, tsl], W[:, kc, c0:c0 + ncols],
                                                     start=(kc == 0), stop=(kc == 7)),
                      reads=[hT, W], writes=[ps], inc=(kc == 7))

    def proj_feat(self, ps, hT, n, W, c0):
        for kc in range(8):
            self.P.op("pe", lambda e, kc=kc: e.matmul(ps[:, 0:n], W[:, kc, c0:c0 + 128], hT[:, kc, 0:n],
                                                     start=(kc == 0), stop=(kc == 7)),
                      reads=[hT, W], writes=[ps], inc=(kc == 7))

    def rope_tables(self, cs, g_bc, gs_bc, CT, ST):
        P = self.P
        P.op("pool", lambda e: e.tensor_tensor(CT[:], cs[:, 0:64], g_bc[:], ALU.mult), reads=[cs, g_bc], writes=[CT])
        P.op("pool", lambda e: e.tensor_tensor(ST[:], cs[:, 64:128], gs_bc[:], ALU.mult), reads=[cs, gs_bc], writes=[ST])

    def swap_gain(self, g_bc, gs_bc):
        P = self.P
        for a in range(2):
            for b in range(2):
                P.op("dve", lambda e, a=a, b=b: e.tensor_copy(gs_bc[:, a * 32 + b * 16:a * 32 + b * 16 + 16],
                                                              g_bc[:, a * 32 + (1 - b) * 16:a * 32 + (1 - b) * 16 + 16]),
                     reads=[g_bc], writes=[gs_bc])

    def qk_post(self, src, H, CT, ST, out, tmp):
        P = self.P
        sq, qn, t1, ssq = tmp
        n = H * 64
        P.op("act", lambda e: e.activation(sq[:, 0:n], src[:, 0:n], AF.Square), reads=[src], writes=[sq])
        P.op("dve", lambda e: e.tensor_reduce(ssq[:, 0:H], sq[:, 0:n].rearrange("p (h d) -> p h d", d=64), AX.X, ALU.add),
             reads=[sq], writes=[ssq])
        self.rstd(ssq, H, 64)
        P.op("dve", lambda e: e.tensor_tensor(qn[:, 0:n].rearrange("p (h d) -> p h d", d=64),
                                              src[:, 0:n].rearrange("p (h d) -> p h d", d=64),
                                              ssq[:, 0:H].unsqueeze(2).to_broadcast([128, H, 64]), ALU.mult),
             reads=[src, ssq], writes=[qn])
        P.op("dve", lambda e: e.tensor_tensor(t1[:, 0:n].rearrange("p (h d) -> p h d", d=64),
                                              qn[:, 0:n].rearrange("p (h d) -> p h d", d=64),
                                              CT[:].unsqueeze(1).to_broadcast([128, H, 64]), ALU.mult),
             reads=[qn, CT], writes=[t1])
        for b in range(2):
            P.op("dve", lambda e, b=b: e.tensor_tensor(
                sq[:, 0:n].rearrange("p (h a b c) -> p h a b c", a=2, b=2, c=16)[:, :, :, b, :],
                qn[:, 0:n].rearrange("p (h a b c) -> p h a b c", a=2, b=2, c=16)[:, :, :, 1 - b, :],
                ST[:].rearrange("p (a b c) -> p a b c", a=2, b=2, c=16)[:, :, b, :].unsqueeze(1).to_broadcast([128, H, 2, 16]),
                ALU.mult), reads=[qn, ST], writes=[sq])
        P.op("pool", lambda e: e.tensor_tensor(out[:, 0:n], t1[:, 0:n], sq[:, 0:n], ALU.add), reads=[t1, sq], writes=[out])


def _rope_table():
    half = 16
    freqs = 10000.0 ** (-np.arange(half, dtype=np.float32) / half)
    t = np.arange(SEQ)
    row = (t // 64).astype(np.float32)
    col = (t % 64).astype(np.float32)
    ar = row[:, None] * freqs[None, :]
    ac = col[:, None] * freqs[None, :]
    C = np.concatenate([np.cos(ar), np.cos(ar), np.cos(ac), np.cos(ac)], 1)
    S = np.concatenate([-np.sin(ar), np.sin(ar), -np.sin(ac), np.sin(ac)], 1)
    lat = np.concatenate([C, S], 1).astype(np.float32)
    ctx = np.concatenate([np.ones((LC, 64), np.float32), np.zeros((LC, 64), np.float32)], 1)
    return np.concatenate([ctx, lat], 0)


STOP = None


def emit_l0(L, io):
    nc = L.nc
    P = L.P
    _outer = P.gstack
    P.gstack = ExitStack()
    xall_tile = io["xall_tile"]; xown_blk = io["xown_blk"]; xo_blk = io["xo_blk"]
    csall = io["csall"]; csown = io["csown"]; ident_d = io["ident_d"]; cvec_d = io["cvec_d"]
    sfx = io["sfx"]
    L.modrow_d = io["modrow"]
    gmix_d = L.din("norm_mix_g" + sfx, [D])
    gffn_d = L.din("norm_ffn_g" + sfx, [D])
    wout_d = L.din("w_out" + sfx, [D, D])
    win_d = L.din("e_w_in", [D, 2560])
    anorm_d = L.din("a_norm_g", [512])
    aws_d = L.din("a_ws", [8, 128, 128])
    abs_d = L.din("a_bs", [8, 128])
    qg_d = L.din("b_qnorm_g", [64])
    kg_d = L.din("b_knorm_g", [64])
    lq1_d = L.din("b_lq1", [64]); lk1_d = L.din("b_lk1", [64]); lq2_d = L.din("b_lq2", [64]); lk2_d = L.din("b_lk2", [64])
    sub_d = L.din("b_subln_g", [128])
    w1_d = L.din("ffn_w1", [D, FF]); w3_d = L.din("ffn_w3", [D, FF]); w2_d = L.din("ffn_w2", [FF, D])
    KT_d = L.dscratch("KT_d", [4, 128, NALL], BF)
    VS_d = L.dscratch("VS_d", [NALL, 512], BF)
    LAM_INIT = 0.2


    gq = P.sb([128, 64], F32, "gq", glob=True); gqs = P.sb([128, 64], F32, "gqs", glob=True)
    gk = P.sb([128, 64], F32, "gk", glob=True); gks = P.sb([128, 64], F32, "gks", glob=True)
    L.load_bc(gq, qg_d.ap, qg_d); L.load_bc(gk, kg_d.ap, kg_d)
    L.swap_gain(gq, gqs); L.swap_gain(gk, gks)
    neglam = P.sb([128, 1], F32, "neglam", glob=True)
    P.phase_begin()
    lt = [P.sb([128, 64], F32, "l") for _ in range(4)]
    for t_, d_ in zip(lt, (lq1_d, lk1_d, lq2_d, lk2_d)):
        L.load_bc(t_, d_.ap, d_)
    e12 = P.sb([128, 2], F32, "e12")
    for i in range(2):
        P.op("dve", lambda e, i=i: e.tensor_tensor(lt[2 * i][:], lt[2 * i][:], lt[2 * i + 1][:], ALU.mult), reads=[lt[2 * i], lt[2 * i + 1]], writes=[lt[2 * i]])
        P.op("dve", lambda e, i=i: e.tensor_reduce(e12[:, i:i + 1], lt[2 * i][:], AX.X, ALU.add), reads=[lt[2 * i]], writes=[e12])
    P.op("act", lambda e: e.activation(e12[:], e12[:], AF.Exp), reads=[e12], writes=[e12])
    P.op("dve", lambda e: e.tensor_tensor(neglam[:], e12[:, 1:2], e12[:, 0:1], ALU.subtract), reads=[e12], writes=[neglam])
    P.op("dve", lambda e: e.tensor_scalar(neglam[:], neglam[:], -LAM_INIT, None, ALU.add), reads=[neglam], writes=[neglam])
    P.phase_end()

    gmix = P.sb([128, D], F32, "gmix", glob=True)
    gffn = P.sb([128, D], F32, "gffn", glob=True)
    L.load_bc(gmix, gmix_d.ap, gmix_d)
    L.load_bc(gffn, gffn_d.ap, gffn_d)

    P.phase_begin()
    A1 = [L.make_A(L.mod_bc(j, 1, "A1"), gmix) for j in range(2)]
    sh1 = [L.mod_bc(j, 0, "sh1") for j in range(2)]
    Wkv = P.sb([128, 8, 1024], BF, "Wkv")
    for h in range(2):
        P.dma("pool", Wkv[:, :, h * 512:(h + 1) * 512],
              win_d[:, 1536 + h * 512:1536 + (h + 1) * 512].rearrange("(kc p) n -> p kc n", p=128), reads=[win_d], writes=[Wkv])
    xr = L.ring(3, [128, D], F32, "xa"); csr = L.ring(3, [128, 128], F32, "csa")
    hnr = L.ring(2, [128, D], BF, "hn"); jr = L.ring(2, [128, D], F32, "junk"); ssr = L.ring(2, [128, 2], F32, "ss")
    hTr = L.ring(2, [128, 8, 128], BF, "hT")
    pTr = L.ring(2, [128, 8, 128], BF, "pT", psum=True)
    pkr = L.ring(2, [128, 512], F32, "pk", psum=True); pvr = L.ring(2, [128, 512], F32, "pv", psum=True)
    CTr = L.ring(2, [128, 64], F32, "CT"); STr = L.ring(2, [128, 64], F32, "ST")
    tmps = [tuple([P.sb([128, 512], F32, "sq"), P.sb([128, 512], F32, "qn"), P.sb([128, 512], F32, "t1"), P.sb([128, 8], F32, "ssq")]) for _ in range(2)]
    kbr = L.ring(2, [128, 512], BF, "kb"); vbr = L.ring(2, [128, 512], BF, "vb"); ktr = L.ring(2, [128, 4, 128], BF, "kt")
    def tile_A(t):
        j = 1 if t < 2 else 0
        x = xr.next(); cs = csr.next(); hn = hnr.next(); junk = jr.next(); ss = ssr.next(); hT = hTr.next(); pT = pTr.next()
        xa_ap, xa_t = xall_tile(t)
        P.dma("sp", x[:], xa_ap, reads=[xa_t], writes=[x])
        P.dma("sp", cs[:], csall[t * 128:(t + 1) * 128, :], reads=[csall], writes=[cs])
        L.norm_tile(x[:], x, A1[j], sh1[j], hn, junk, ss)
        L.transpose_cols(hn, 8, pT, lambda hT=hT: hT[:], hT)
        pk = pkr.next(); pv = pvr.next()
        L.proj_tok(pk, hT, slice(0, 128), Wkv, 0, 512)
        L.proj_tok(pv, hT, slice(0, 128), Wkv, 512, 512)
        P.mark()
        vb = vbr.next()
        P.op("act", lambda e, vb=vb, pv=pv: e.activation(vb[:], pv[:], AF.Identity), reads=[pv], writes=[vb])
        P.dma("pool", VS_d[t * 128:(t + 1) * 128, :], vb[:], reads=[vb], writes=[VS_d])
        CT = CTr.next(); ST = STr.next()
        L.rope_tables(cs, gk, gks, CT, ST)
        kb = kbr.next()
        L.qk_post(pk, 8, CT, ST, kb, tmps[t % 2])
        kt = ktr.next()
        L.transpose_cols(kb, 4, pT, lambda kt=kt: kt[:], kt, eng="dve")
        P.dma("pool", KT_d[:, :, t * 128:(t + 1) * 128].rearrange("h p t -> p h t"), kt[:], reads=[kt], writes=[KT_d])

    recs = []
    for t in range(NT_ALL):
        P.record_begin(); tile_A(t); recs.append(P.record_end())
    P.replay_skewed(recs)
    P.phase_end()

    yT = P.sb([128, 8, NOWN], BF, "yT", glob=True)
    qstack = ExitStack()
    QT = T("QT", qstack.enter_context(nc.sbuf_tensor("QT", [128, 2, 4, NOWN], BF)).ap())
    P.op("pool", lambda e: e.memset(QT[64:128, 0, :, :], 0.0), writes=[QT])
    P.op("pool", lambda e: e.memset(QT[0:64, 1, :, :], 0.0), writes=[QT])
    blocks = [(0, 2)] + [(2 + 4 * i, 4) for i in range(4)]

    P.phase_begin()
    A1 = [L.make_A(L.mod_bc(j, 1, "A1"), gmix) for j in range(2)]
    sh1 = [L.mod_bc(j, 0, "sh1") for j in range(2)]
    Win = P.sb([128, 8, 1536], BF, "Win")
    for h in range(3):
        P.dma("pool", Win[:, :, h * 512:(h + 1) * 512], win_d[:, h * 512:(h + 1) * 512].rearrange("(kc p) n -> p kc n", p=128),
              reads=[win_d], writes=[Win])
    anorm = P.sb([128, 512], F32, "anorm"); L.load_bc(anorm, anorm_d.ap, anorm_d)
    wsT = P.sb([128, 8, 128], BF, "wsT")
    wsr = P.sb([128, 8, 128], F32, "wsr")
    P.dma("sp", wsr[:], aws_d.ap.rearrange("g p q -> p g q"), reads=[aws_d], writes=[wsr])
    wsb = P.sb([128, 8, 128], BF, "wsb")
    P.op("dve", lambda e: e.tensor_copy(wsb[:], wsr[:]), reads=[wsr], writes=[wsb])
    pTr = L.ring(2, [128, 8, 128], BF, "pT", psum=True)
    pTw = pTr.next()
    for g in range(8):
        P.op("pe", lambda e, g=g: e.transpose(pTw[:, g, :], wsb[:, g, :], L.identb[:]), reads=[wsb, L.identb], writes=[pTw], inc=(g == 7))
    P.op("dve", lambda e: e.tensor_copy(wsT[:], pTw[:]), reads=[pTw], writes=[wsT])
    bsT = P.sb([128, 4, 128], F32, "bsT")
    for g in range(8):
        P.dma("sp", bsT[(g % 2) * 64:(g % 2) * 64 + 64, g // 2, :], abs_d.ap[g, :].partition_broadcast(64), reads=[abs_d], writes=[bsT])
    xb = L.ring(1, [128, 4, D], F32, "xb"); csb = L.ring(2, [128, 4, 128], F32, "csb")
    hnr = L.ring(2, [128, D], BF, "hn"); jr = L.ring(2, [128, D], F32, "junk"); ssr = L.ring(2, [128, 2], F32, "ss")
    hTb = L.ring(1, [128, 8, 512], BF, "hTb")
    uTr = L.ring(1, [128, 4, 512], F32, "uT")
    pur = L.ring(2, [128, 512], F32, "pu", psum=True)
    pvr = L.ring(2, [128, 512], F32, "pv", psum=True)
    pmr = L.ring(2, [128, 128], F32, "pm", psum=True)
    gvr = L.ring(2, [128, 512], F32, "gv"); vbr = L.ring(2, [128, 512], BF, "vb"); tmr = L.ring(2, [128, 128], F32, "tm")
    CTr = L.ring(2, [128, 64], F32, "CT"); STr = L.ring(2, [128, 64], F32, "ST")
    tmps = [tuple([P.sb([128, 512], F32, "sq"), P.sb([128, 512], F32, "qn"), P.sb([128, 512], F32, "t1"), P.sb([128, 8], F32, "ssq")]) for _ in range(2)]
    qbr = L.ring(2, [128, 512], BF, "qb")
    for bi, (t0, nt) in enumerate(blocks):
        j = 1 if bi == 0 else 0
        N = nt * 128
        x = xb.next(); cs = csb.next(); hT = hTb.next(); uT = uTr.next()
        xb_ap, xb_t = xown_blk(t0, nt)
        P.dma("sp", x[:, 0:nt, :], xb_ap.rearrange("(t p) d -> p t d", p=128), reads=[xb_t], writes=[x])
        P.dma("sp", cs[:, 0:nt, :], csown[t0 * 128:(t0 + nt) * 128, :].rearrange("(t p) d -> p t d", p=128), reads=[csown], writes=[cs])
        for t in range(nt):
            hn = hnr.next(); junk = jr.next(); ss = ssr.next(); pT = pTr.next()
            L.norm_tile(x[:, t, :], x, A1[j], sh1[j], hn, junk, ss)
            L.transpose_cols(hn, 8, pT, lambda hT=hT, t=t: hT[:, :, t * 128:(t + 1) * 128], hT)
        for c in range(4):
            pu = pur.next()
            L.proj_feat(pu, hT, N, Win, c * 128)
            P.op("act", lambda e, pu=pu, uT=uT, c=c, N=N: e.activation(uT[:, c, 0:N], pu[:, 0:N], AF.Gelu_apprx_tanh), reads=[pu], writes=[uT])
        for t in range(nt):
            tsl = slice(t * 128, (t + 1) * 128)
            gsl = slice((t0 + t) * 128, (t0 + t + 1) * 128)
            pv = pvr.next(); gv = gvr.next(); vb = vbr.next(); junk = jr.next(); ss = ssr.next()
            L.proj_tok(pv, hT, tsl, Win, 512, 512)
            P.op("act", lambda e, gv=gv, pv=pv: e.activation(gv[:], pv[:], AF.Gelu_apprx_tanh), reads=[pv], writes=[gv])
            P.op("act", lambda e, gv=gv, junk=junk, ss=ss: e.activation(junk[:, 0:512], gv[:], AF.Square, accum_out=ss[:, 0:1]), reads=[gv], writes=[junk, ss])
            L.rstd(ss, 1, 512)
            P.op("dve", lambda e, vb=vb, gv=gv, ss=ss: e.scalar_tensor_tensor(vb[:], gv[:], ss[:, 0:1], anorm[:], ALU.mult, ALU.mult),
                 reads=[gv, ss, anorm], writes=[vb])
            for jp in range(4):
                pm = pmr.next(); tm = tmr.next()
                for hh in range(2):
                    g = 2 * jp + hh
                    P.op("pe", lambda e, pm=pm, vb=vb, g=g, hh=hh: e.matmul(pm[hh * 64:(hh + 1) * 64, :], vb[:, g * 64:(g + 1) * 64], wsT[:, g, :],
                                                                         start=True, stop=True), reads=[vb, wsT], writes=[pm], inc=(hh == 1))
                P.op("dve", lambda e, pm=pm, tm=tm, jp=jp: e.tensor_tensor(tm[:], pm[:], bsT[:, jp, :], ALU.add), reads=[pm, bsT], writes=[tm])
                P.op("pool", lambda e, tm=tm, jp=jp, uT=uT, tsl=tsl, gsl=gsl: e.tensor_tensor(yT[:, jp, gsl], tm[:], uT[:, jp, tsl], ALU.mult),
                     reads=[tm, uT], writes=[yT])
            pq = pur.next(); CT = CTr.next(); ST = STr.next(); qb = qbr.next(); pT = pTr.next()
            L.proj_tok(pq, hT, tsl, Win, 1024, 512)
            L.rope_tables(T_view(cs, cs[:, t, :]), gq, gqs, CT, ST)
            L.qk_post(pq, 8, CT, ST, qb, tmps[t % 2])
            for c4 in range(4):
                P.op("pe", lambda e, c4=c4, pT=pT, qb=qb: e.transpose(pT[:, c4, :], qb[:, c4 * 128:(c4 + 1) * 128], L.identb[:]),
                     reads=[qb, L.identb], writes=[pT], inc=(c4 == 3))
            P.op("dve", lambda e, pT=pT, gsl=gsl: e.tensor_copy(QT[0:64, 0, :, gsl], pT[0:64, 0:4, :]), reads=[pT], writes=[QT])
            P.op("act", lambda e, pT=pT, gsl=gsl: e.activation(QT[64:128, 1, :, gsl], pT[64:128, 0:4, :], AF.Identity), reads=[pT], writes=[QT])
    P.phase_end()

    P.phase_begin()
    subbc = P.sb([128, 128], F32, "subbc"); L.load_bc(subbc, sub_d.ap, sub_d)
    P.op("dve", lambda e: e.tensor_scalar(subbc[:], subbc[:], 1.0 - LAM_INIT, None, ALU.mult), reads=[subbc], writes=[subbc])
    KTh = L.ring(2, [128, NALL], BF, "KTh")
    Vh = L.ring(2, [128, NT_ALL, 129], BF, "Vh")
    for v_ in Vh.tiles:
        P.op("pool", lambda e, v_=v_: e.memset(v_[:, :, 128:129], 1.0), writes=[v_])
    spr = L.ring(4, [128, 2, 256], F32, "sp", psum=True)
    LOOK = 3
    accs = [[P.ps([128, 512], F32, "acc") for c in range(2)] for qi in range(2)]
    pTr_ = L.ring(6, [128, 2, 256], BF, "pTs")
    rz = L.ring(2, [128, 4], F32, "rz"); o1r = L.ring(2, [128, 128], F32, "o1"); o2r = L.ring(2, [128, 128], F32, "o2")
    jr = L.ring(2, [128, 128], F32, "junk"); ssr = L.ring(2, [128, 2], F32, "ss"); ybr = L.ring(2, [128, 128], BF, "yb")
    for h in range(4):
        kth = KTh.next(); vh = Vh.next()
        for pc in range(6):
            P.dma("sp", kth[:, pc * 1408:(pc + 1) * 1408], KT_d[h, :, pc * 1408:(pc + 1) * 1408], reads=[KT_d], writes=[kth])
        for pc in range(11):
            P.dma("sp", vh[:, pc * 6:(pc + 1) * 6, 0:128],
                  VS_d[pc * 768:(pc + 1) * 768, h * 128:(h + 1) * 128].rearrange("(t p) e -> p t e", p=128), reads=[VS_d], writes=[vh])
        for qb_ in range(NOWN // 256):
            keys = [0, 1] if qb_ == 0 else list(range(NT_ALL))
            q0 = qb_ * 256
            def emit_scores(kt, kth=kth, q0=q0, h=h):
                sp_ = spr.next()
                for c in range(2):
                    P.op("pe", lambda e, c=c, sp_=sp_, kth=kth, kt=kt, q0=q0, h=h: e.matmul(
                        sp_[:, c, :], kth[:, kt * 128:(kt + 1) * 128], QT[:, c, h, q0:q0 + 256],
                        start=True, stop=True), reads=[kth, QT], writes=[sp_], inc=(c == 1))
                return sp_

            pend_sc = [emit_scores(keys[i]) for i in range(min(LOOK, len(keys)))]
            for ki, kt in enumerate(keys):
                sp_ = pend_sc.pop(0)
                if ki + LOOK < len(keys):
                    pend_sc.append(emit_scores(keys[ki + LOOK]))
                pt = pTr_.next()
                P.op("act", lambda e, pt=pt, sp_=sp_: e.activation(pt[:], sp_[:], AF.Exp, scale=0.125), reads=[sp_], writes=[pt])
                for qi in range(2):
                    for c in range(2):
                        P.op("pe", lambda e, qi=qi, c=c, pt=pt, vh=vh, kt=kt, ki=ki, keys=keys: e.matmul(
                            accs[qi][c][:, 0:129], pt[:, c, qi * 128:(qi + 1) * 128], vh[:, kt, :],
                            start=(ki == 0), stop=(ki == len(keys) - 1)), reads=[pt, vh], writes=[accs[qi][c]],
                            inc=(ki == len(keys) - 1))
            for qi in range(2):
                r = rz.next(); o1 = o1r.next(); o2 = o2r.next(); junk = jr.next(); ss = ssr.next(); yb = ybr.next()
                a0, a1 = accs[qi]
                P.op("dve", lambda e, r=r, a0=a0: e.reciprocal(r[:, 0:1], a0[:, 128:129]), reads=[a0], writes=[r])
                P.op("dve", lambda e, r=r, a1=a1: e.reciprocal(r[:, 1:2], a1[:, 128:129]), reads=[a1], writes=[r])
                P.op("dve", lambda e, r=r: e.tensor_tensor(r[:, 2:3], r[:, 1:2], neglam[:], ALU.mult), reads=[r, neglam], writes=[r])
                P.op("act", lambda e, o1=o1, a0=a0, r=r: e.activation(o1[:], a0[:, 0:128], AF.Identity, scale=r[:, 0:1]), reads=[a0, r], writes=[o1])
                P.op("dve", lambda e, o2=o2, a1=a1, r=r, o1=o1: e.scalar_tensor_tensor(o2[:], a1[:, 0:128], r[:, 2:3], o1[:], ALU.mult, ALU.add),
                     reads=[a1, r, o1], writes=[o2])
                P.op("act", lambda e, junk=junk, o2=o2, ss=ss: e.activation(junk[:], o2[:], AF.Square, accum_out=ss[:, 0:1]), reads=[o2], writes=[junk, ss])
                L.rstd(ss, 1, 128)
                P.op("dve", lambda e, yb=yb, o2=o2, ss=ss: e.scalar_tensor_tensor(yb[:], o2[:], ss[:, 0:1], subbc[:], ALU.mult, ALU.mult),
                     reads=[o2, ss, subbc], writes=[yb])
                gsl = slice(q0 + qi * 128, q0 + (qi + 1) * 128)
                P.op("pe", lambda e, yb=yb, a0=a0: e.transpose(a0[:].bitcast(BF)[:, 0:128], yb[:], L.identb[:]), reads=[yb, L.identb], writes=[a0])
                P.op("act", lambda e, gsl=gsl, h=h, a0=a0: e.activation(yT[:, 4 + h, gsl], a0[:].bitcast(BF)[:, 0:128], AF.Identity), reads=[a0], writes=[yT])
    P.phase_end()

    qstack.close()
    ffn_phase(L, xown_blk, xo_blk, blocks, yT, wout_d, gffn, w1_d, w3_d, w2_d, ctx_block=True)
    P.barrier()
    P.flush()
    P.gstack.close()
    P.gstack = _outer


def T_view(t, ap):
    return _View(t, ap)


class _View:
    def __init__(self, base, ap):
        object.__setattr__(self, "base", base)
        object.__setattr__(self, "ap", ap)

    def __getitem__(self, k):
        return self.ap[k]

    @property
    def w(self):
        return self.base.w

    @w.setter
    def w(self, v):
        self.base.w = v

    @property
    def r(self):
        return self.base.r

    @r.setter
    def r(self, v):
        self.base.r = v

    @property
    def name(self):
        return self.base.name


def ffn_phase(L, xown, xo, blocks, yT, wout_d, gffn, w1_d, w3_d, w2_d, ctx_block, moe=None):
    P = L.P
    P.phase_begin()
    nvar = 2 if ctx_block else 1
    g1 = [L.mod_bc(j, 2, "g1") for j in range(nvar)]
    A2 = [L.make_A(L.mod_bc(j, 4, "A2"), gffn) for j in range(nvar)]
    sh2 = [L.mod_bc(j, 3, "sh2") for j in range(nvar)]
    g2 = [L.mod_bc(j, 5, "g2") for j in range(nvar)]
    wout = P.sb([128, 8, D], BF, "wout")
    for h in range(2):
        P.dma("pool", wout[:, :, h * 512:(h + 1) * 512], wout_d[:, h * 512:(h + 1) * 512].rearrange("(kc p) n -> p kc n", p=128),
              reads=[wout_d], writes=[wout])
    NE = 1
    if moe is not None:
        rw_d, rb_d, NE = moe
        wr = P.sb([128, 8, 8], F32, "wr")
        P.dma("sp", wr[:], rw_d.ap.rearrange("(kc p) e -> p kc e", p=128), reads=[rw_d], writes=[wr])
        rb = P.sb([128, 8], F32, "rb"); L.load_bc(rb, rb_d.ap, rb_d)
        hnf_r = L.ring(2, [128, D], F32, "hnf")
        tTr = L.ring(2, [128, 8, 128], F32, "tT")
        pXr = L.ring(1, [128, 8, 128], F32, "pX", psum=True)
        dg = P.sb([128, 4, 8], F32, "dg")
        rt = [P.sb([128, 8], F32, "rt") for _ in range(4)]
        rs = P.sb([128, 4], F32, "rs")
    xb = L.ring(1, [128, 4, D], F32, "xb")
    hnr = L.ring(2, [128, D], BF, "hn"); jr = L.ring(2, [128, D], F32, "junk"); ssr = L.ring(2, [128, 2], F32, "ss")
    hTb = L.ring(1, [128, 8, 512], BF, "h2T")
    gT = P.sb([128, NFC, 512], BF, "gT")
    pTr = L.ring(1 if moe is not None else 2, [128, 8, 128], BF, "pT", psum=True)
    por = L.ring(1 if moe is not None else 2, [128, 512], F32, "po", psum=True)
    p1r = L.ring(2, [128, 512], F32, "p1", psum=True); p3r = L.ring(2, [128, 512], F32, "p3", psum=True)
    tmr = L.ring(2, [128, 512], F32, "tm")
    s1r = L.ring(2, [128, 512], F32, "s1")
    w1r = L.ring(2, [128, 8, 256], BF, "w1p"); w3r = L.ring(2, [128, 8, 256], BF, "w3p")
    w2r = L.ring(2, [128, NFC, 256], BF, "w2q")
    for bi, (t0, nt) in enumerate(blocks):
        j = 1 if (ctx_block and bi == 0) else 0
        N = nt * 128
        x = xb.next(); hT = hTb.next()
        xb_ap, xb_t = xown(t0, nt)
        P.dma("sp", x[:, 0:nt, :], xb_ap.rearrange("(t p) d -> p t d", p=128), reads=[xb_t], writes=[x])
        for t in range(nt):
            gsl = slice((t0 + t) * 128, (t0 + t + 1) * 128)
            for hf in range(2):
                po = por.next(); tm = tmr.next()
                for kc in range(8):
                    P.op("pe", lambda e, kc=kc, po=po, gsl=gsl, hf=hf: e.matmul(po[:], yT[:, kc, gsl], wout[:, kc, hf * 512:(hf + 1) * 512],
                                                                              start=(kc == 0), stop=(kc == 7)),
                         reads=[yT, wout], writes=[po], inc=(kc == 7))
                P.op("dve", lambda e, tm=tm, po=po, hf=hf, j=j: e.tensor_tensor(tm[:], po[:], g1[j][:, hf * 512:(hf + 1) * 512], ALU.mult),
                     reads=[po, g1[j]], writes=[tm])
                P.op("dve", lambda e, x=x, t=t, hf=hf, tm=tm: e.tensor_tensor(x[:, t, hf * 512:(hf + 1) * 512], x[:, t, hf * 512:(hf + 1) * 512], tm[:], ALU.add),
                     reads=[x, tm], writes=[x])
        for t in range(nt):
            hn = hnr.next(); junk = jr.next(); ss = ssr.next(); pT = pTr.next()
            if moe is None:
                L.norm_tile(x[:, t, :], x, A2[j], sh2[j], hn, junk, ss)
            else:
                hnf = hnf_r.next(); tT = tTr.next(); pX = pXr.next()
                P.op("act", lambda e, junk=junk, x=x, t=t, ss=ss: e.activation(junk[:], x[:, t, :], AF.Square, accum_out=ss[:, 0:1]),
                     reads=[x], writes=[junk, ss])
                L.rstd(ss, 1, D)
                P.op("dve", lambda e, junk=junk, x=x, t=t, ss=ss, j=j: e.scalar_tensor_tensor(junk[:], x[:, t, :], ss[:, 0:1], A2[j][:], ALU.mult, ALU.mult),
                     reads=[x, ss, A2[j]], writes=[junk])
                P.op("dve", lambda e, hnf=hnf, junk=junk, j=j: e.tensor_tensor(hnf[:], junk[:], sh2[j][:], ALU.add), reads=[junk, sh2[j]], writes=[hnf])
                P.op("act", lambda e, hn=hn, hnf=hnf: e.activation(hn[:], hnf[:], AF.Identity), reads=[hnf], writes=[hn])
                for c in range(8):
                    P.op("pe", lambda e, c=c, pX=pX, hnf=hnf: e.transpose(pX[:, c, :], hnf[:, c * 128:(c + 1) * 128], L.ident[:]),
                         reads=[hnf, L.ident], writes=[pX], inc=(c == 7))
                P.op("dve", lambda e, tT=tT, pX=pX: e.tensor_copy(tT[:], pX[:]), reads=[pX], writes=[tT])
                plg = por.next()
                for kc in range(8):
                    P.op("pe", lambda e, kc=kc, plg=plg, tT=tT: e.matmul(plg[:, 0:8], tT[:, kc, :], wr[:, kc, :], start=(kc == 0), stop=(kc == 7)),
                         reads=[tT, wr], writes=[plg], inc=(kc == 7))
                lg, mk, l2, ex = rt
                P.op("dve", lambda e, plg=plg: e.tensor_tensor(lg[:], plg[:, 0:8], rb[:], ALU.add), reads=[plg, rb], writes=[lg])
                P.op("dve", lambda e: e.tensor_reduce(rs[:, 0:1], lg[:], AX.X, ALU.max), reads=[lg], writes=[rs])
                P.op("dve", lambda e: e.tensor_scalar(mk[:], lg[:], rs[:, 0:1], None, ALU.is_equal), reads=[lg, rs], writes=[mk])
                P.op("dve", lambda e: e.scalar_tensor_tensor(l2[:], mk[:], -1e30, lg[:], ALU.mult, ALU.add), reads=[mk, lg], writes=[l2])
                P.op("dve", lambda e: e.tensor_reduce(rs[:, 1:2], l2[:], AX.X, ALU.max), reads=[l2], writes=[rs])
                P.op("dve", lambda e: e.tensor_scalar(mk[:], lg[:], rs[:, 1:2], None, ALU.is_ge), reads=[lg, rs], writes=[mk])
                P.op("dve", lambda e: e.tensor_scalar(ex[:], lg[:], rs[:, 0:1], None, ALU.subtract), reads=[lg, rs], writes=[ex])
                P.op("act", lambda e: e.activation(ex[:], ex[:], AF.Exp), reads=[ex], writes=[ex])
                P.op("dve", lambda e: e.tensor_tensor(ex[:], ex[:], mk[:], ALU.mult), reads=[ex, mk], writes=[ex])
                P.op("dve", lambda e: e.tensor_reduce(rs[:, 2:3], ex[:], AX.X, ALU.add), reads=[ex], writes=[rs])
                P.op("dve", lambda e: e.reciprocal(rs[:, 3:4], rs[:, 2:3]), reads=[rs], writes=[rs])
                P.op("dve", lambda e, t=t: e.tensor_scalar(dg[:, t, :], ex[:], rs[:, 3:4], None, ALU.mult), reads=[ex, rs], writes=[dg])
            L.transpose_cols(hn, 8, pT, lambda hT=hT, t=t: hT[:, :, t * 128:(t + 1) * 128], hT)
        for ex_i in range(NE):
            w1e = w1_d.ap[ex_i] if moe is not None else w1_d.ap
            w3e = w3_d.ap[ex_i] if moe is not None else w3_d.ap
            w2e = w2_d.ap[ex_i] if moe is not None else w2_d.ap
            for pi in range(NFC // 2):
                w1p = w1r.next(); w3p = w3r.next()
                P.dma("pool", w1p[:], w1e[:, pi * 256:(pi + 1) * 256].rearrange("(kc p) n -> p kc n", p=128), reads=[w1_d], writes=[w1p])
                P.dma("pool", w3p[:], w3e[:, pi * 256:(pi + 1) * 256].rearrange("(kc p) n -> p kc n", p=128), reads=[w3_d], writes=[w3p])
                for fc in range(2):
                    p1 = p1r.next(); p3 = p3r.next(); s1 = s1r.next()
                    L.proj_feat(p1, hT, N, w1p, fc * 128)
                    L.proj_feat(p3, hT, N, w3p, fc * 128)
                    P.op("act", lambda e, s1=s1, p1=p1, N=N: e.activation(s1[:, 0:N], p1[:, 0:N], AF.Silu), reads=[p1], writes=[s1])
                    P.op("dve", lambda e, s1=s1, p3=p3, N=N, f=pi * 2 + fc: e.tensor_tensor(gT[:, f, 0:N], s1[:, 0:N], p3[:, 0:N], ALU.mult),
                         reads=[s1, p3], writes=[gT])
            for qd in range(4):
                w2q = w2r.next()
                P.dma("pool", w2q[:], w2e[:, qd * 256:(qd + 1) * 256].rearrange("(fc p) n -> p fc n", p=128), reads=[w2_d], writes=[w2q])
                for t in range(nt):
                    po = por.next(); tm = tmr.next()
                    for fc in range(NFC):
                        P.op("pe", lambda e, fc=fc, po=po, t=t, w2q=w2q: e.matmul(po[:, 0:256], gT[:, fc, t * 128:(t + 1) * 128], w2q[:, fc, :],
                                                                                start=(fc == 0), stop=(fc == NFC - 1)),
                             reads=[gT, w2q], writes=[po], inc=(fc == NFC - 1))
                    P.op("dve", lambda e, tm=tm, po=po, qd=qd, j=j: e.tensor_tensor(tm[:, 0:256], po[:, 0:256], g2[j][:, qd * 256:(qd + 1) * 256], ALU.mult),
                         reads=[po, g2[j]], writes=[tm])
                    if moe is None:
                        P.op("dve", lambda e, x=x, t=t, qd=qd, tm=tm: e.tensor_tensor(x[:, t, qd * 256:(qd + 1) * 256], x[:, t, qd * 256:(qd + 1) * 256], tm[:, 0:256], ALU.add),
                             reads=[x, tm], writes=[x])
                    else:
                        P.op("dve", lambda e, x=x, t=t, qd=qd, tm=tm, ex_i=ex_i: e.scalar_tensor_tensor(
                            x[:, t, qd * 256:(qd + 1) * 256], tm[:, 0:256], dg[:, t, ex_i:ex_i + 1], x[:, t, qd * 256:(qd + 1) * 256], ALU.mult, ALU.add),
                            reads=[x, tm, dg], writes=[x])
        xo_ap, xo_t = xo(t0, nt)
        P.dma("sp", xo_ap.rearrange("(t p) d -> p t d", p=128), x[:, 0:nt, :], reads=[x], writes=[xo_t])
    P.phase_end()


STOP1 = None


def emit_l1(L, io):
    nc = L.nc
    P = L.P
    _outer = P.gstack
    P.gstack = ExitStack()
    xall_tile = io["xall_tile"]; xown_blk = io["xown_blk"]; xo_blk = io["xo_blk"]
    csall = io["csall"]; csown = io["csown"]; ident_d = io["ident_d"]; cvec_d = io["cvec_d"]
    sfx = io["sfx"]
    L.modrow_d = io["modrow"]
    gmix_d = L.din("norm_mix_g" + sfx, [D])
    win_d = L.din("o_w_in", [D, 1792])
    qg_d = L.din("c_qnorm_g", [64])
    kg_d = L.din("c_knorm_g", [64])
    lrup_d = L.din("lru_p", [128, 4, 12])
    wa_d = L.din("d_wa", [2, 8, 64, 64])
    wx_d = L.din("d_wx", [2, 8, 64, 64])
    segw_d = L.din("segw", [4])
    KT_d = L.dscratch("KT1_d", [128, NALL], BF)
    VS_d = L.dscratch("VS1_d", [NALL, 128], BF)
    XR_d = L.dscratch("XR_d", [4, 128, NALL], F32)

    gq = P.sb([128, 64], F32, "gq", glob=True); gqs = P.sb([128, 64], F32, "gqs", glob=True)
    gk = P.sb([128, 64], F32, "gk", glob=True); gks = P.sb([128, 64], F32, "gks", glob=True)
    L.load_bc(gq, qg_d.ap, qg_d); L.load_bc(gk, kg_d.ap, kg_d)
    L.swap_gain(gq, gqs); L.swap_gain(gk, gks)
    gmix = P.sb([128, D], F32, "gmix", glob=True)
    L.load_bc(gmix, gmix_d.ap, gmix_d)

    yT = P.sb([128, 8, NOWN], BF, "yT", glob=True)
    gstack = ExitStack()
    gateT = T("gateT", gstack.enter_context(nc.sbuf_tensor("gateT_l1", [128, 4, OWN], BF)).ap())
    qstack = ExitStack()
    QT = T("QT", qstack.enter_context(nc.sbuf_tensor("QT_l1", [128, 2, 4, NOWN], BF)).ap())
    P.op("pool", lambda e: e.memset(QT[64:128, 0, :, :], 0.0), writes=[QT])
    P.op("pool", lambda e: e.memset(QT[0:64, 1, :, :], 0.0), writes=[QT])
    blocks = [(2 + 4 * i, 4) for i in range(4)]

    P.phase_begin()
    A1 = [L.make_A(L.mod_bc(0, 1, "A1"), gmix)]
    sh1 = [L.mod_bc(0, 0, "sh1")]
    Win = P.sb([128, 8, 1024], BF, "Win")
    for g in range(4):
        for n in range(2):
            h = n * 4 + g
            P.dma("pool", Win[:, :, g * 128 + n * 64:g * 128 + (n + 1) * 64],
                  win_d[:, h * 64:(h + 1) * 64].rearrange("(kc p) n -> p kc n", p=128), reads=[win_d], writes=[Win])
    P.dma("pool", Win[:, :, 512:1024], win_d[:, 768:1280].rearrange("(kc p) n -> p kc n", p=128), reads=[win_d], writes=[Win])
    xb = L.ring(1, [128, 4, D], F32, "xb"); csb = L.ring(2, [128, 4, 128], F32, "csb")
    hnr = L.ring(2, [128, D], BF, "hn"); jr = L.ring(2, [128, D], F32, "junk"); ssr = L.ring(2, [128, 2], F32, "ss")
    hTb = L.ring(1, [128, 8, 512], BF, "hTb")
    pTr = L.ring(2, [128, 8, 128], BF, "pT", psum=True)
    pur = L.ring(2, [128, 512], F32, "pu", psum=True)
    CTr = L.ring(2, [128, 64], F32, "CT"); STr = L.ring(2, [128, 64], F32, "ST")
    tmps = [tuple([P.sb([128, 512], F32, "sq"), P.sb([128, 512], F32, "qn"), P.sb([128, 512], F32, "t1"), P.sb([128, 8], F32, "ssq")]) for _ in range(2)]
    qbr = L.ring(2, [128, 512], BF, "qb")
    for bi, (t0, nt) in enumerate(blocks):
        N = nt * 128
        x = xb.next(); cs = csb.next(); hT = hTb.next()
        xb_ap, xb_t = xown_blk(t0, nt)
        P.dma("sp", x[:, 0:nt, :], xb_ap.rearrange("(t p) d -> p t d", p=128), reads=[xb_t], writes=[x])
        P.dma("sp", cs[:, 0:nt, :], csown[t0 * 128:(t0 + nt) * 128, :].rearrange("(t p) d -> p t d", p=128), reads=[csown], writes=[cs])
        for t in range(nt):
            hn = hnr.next(); junk = jr.next(); ss = ssr.next(); pT = pTr.next()
            L.norm_tile(x[:, t, :], x, A1[0], sh1[0], hn, junk, ss)
            L.transpose_cols(hn, 8, pT, lambda hT=hT, t=t: hT[:, :, t * 128:(t + 1) * 128], hT)
        for c in range(4):
            pu = pur.next()
            L.proj_feat(pu, hT, N, Win, 512 + c * 128)
            P.op("act", lambda e, pu=pu, c=c, N=N, t0=t0: e.activation(gateT[:, c, (t0 - 2) * 128:(t0 - 2) * 128 + N], pu[:, 0:N], AF.Gelu_apprx_tanh),
                 reads=[pu], writes=[gateT])
        for t in range(nt):
            tsl = slice(t * 128, (t + 1) * 128)
            gsl = slice((t0 + t) * 128, (t0 + t + 1) * 128)
            pq = pur.next(); CT = CTr.next(); ST = STr.next(); qb = qbr.next(); pT = pTr.next()
            L.proj_tok(pq, hT, tsl, Win, 0, 512)
            L.rope_tables(T_view(cs, cs[:, t, :]), gq, gqs, CT, ST)
            L.qk_post(pq, 8, CT, ST, qb, tmps[t % 2])
            for c4 in range(4):
                P.op("pe", lambda e, c4=c4, pT=pT, qb=qb: e.transpose(pT[:, c4, :], qb[:, c4 * 128:(c4 + 1) * 128], L.identb[:]),
                     reads=[qb, L.identb], writes=[pT], inc=(c4 == 3))
            P.op("dve", lambda e, pT=pT, gsl=gsl: e.tensor_copy(QT[0:64, 0, :, gsl], pT[0:64, 0:4, :]), reads=[pT], writes=[QT])
            P.op("act", lambda e, pT=pT, gsl=gsl: e.activation(QT[64:128, 1, :, gsl], pT[64:128, 0:4, :], AF.Identity), reads=[pT], writes=[QT])
    P.phase_end()

    P.phase_begin()
    A1 = [L.make_A(L.mod_bc(j, 1, "A1"), gmix) for j in range(2)]
    sh1 = [L.mod_bc(j, 0, "sh1") for j in range(2)]
    Wk = P.sb([128, 8, 768], BF, "Wkvx")
    P.dma("pool", Wk[:, :, 0:256], win_d[:, 512:768].rearrange("(kc p) n -> p kc n", p=128), reads=[win_d], writes=[Wk])
    P.dma("pool", Wk[:, :, 256:768], win_d[:, 1280:1792].rearrange("(kc p) n -> p kc n", p=128), reads=[win_d], writes=[Wk])
    xr = L.ring(3, [128, D], F32, "xa"); csr = L.ring(3, [128, 128], F32, "csa")
    hnr = L.ring(2, [128, D], BF, "hn"); jr = L.ring(2, [128, D], F32, "junk"); ssr = L.ring(2, [128, 2], F32, "ss")
    hTr = L.ring(2, [128, 8, 128], BF, "hT")
    pTr = L.ring(2, [128, 8, 128], BF, "pT", psum=True)
    pkr = L.ring(2, [128, 512], F32, "pk", psum=True); pxr = L.ring(2, [128, 512], F32, "px", psum=True)
    pXr = L.ring(2, [128, 4, 128], F32, "pX", psum=True)
    CTr = L.ring(2, [128, 64], F32, "CT"); STr = L.ring(2, [128, 64], F32, "ST")
    tmps = [tuple([P.sb([128, 512], F32, "sq"), P.sb([128, 512], F32, "qn"), P.sb([128, 512], F32, "t1"), P.sb([128, 8], F32, "ssq")]) for _ in range(2)]
    kbr = L.ring(2, [128, 128], BF, "kb"); vbr = L.ring(2, [128, 128], BF, "vb"); ktr = L.ring(2, [128, 1, 128], BF, "kt")
    xsr = L.ring(2, [128, 512], F32, "xs"); xtr = L.ring(2, [128, 4, 128], F32, "xt")
    def tile_A(t):
        j = 1 if t < 2 else 0
        x = xr.next(); cs = csr.next(); hn = hnr.next(); junk = jr.next(); ss = ssr.next(); hT = hTr.next(); pT = pTr.next()
        xa_ap, xa_t = xall_tile(t)
        P.dma("sp", x[:], xa_ap, reads=[xa_t], writes=[x])
        P.dma("sp", cs[:], csall[t * 128:(t + 1) * 128, :], reads=[csall], writes=[cs])
        L.norm_tile(x[:], x, A1[j], sh1[j], hn, junk, ss)
        L.transpose_cols(hn, 8, pT, lambda hT=hT: hT[:], hT)
        pk = pkr.next(); px = pxr.next()
        L.proj_tok(pk, hT, slice(0, 128), Wk, 0, 256)
        L.proj_tok(px, hT, slice(0, 128), Wk, 256, 512)
        P.mark()
        vb = vbr.next()
        P.op("act", lambda e, vb=vb, pk=pk: e.activation(vb[:], pk[:, 128:256], AF.Identity), reads=[pk], writes=[vb])
        P.dma("pool", VS_d[t * 128:(t + 1) * 128, :], vb[:], reads=[vb], writes=[VS_d])
        CT = CTr.next(); ST = STr.next()
        L.rope_tables(cs, gk, gks, CT, ST)
        kb = kbr.next()
        L.qk_post(pk, 2, CT, ST, kb, tmps[t % 2])
        kt = ktr.next()
        L.transpose_cols(kb, 1, pT, lambda kt=kt: kt[:], kt, eng="dve")
        P.dma("pool", KT_d[:, t * 128:(t + 1) * 128], kt[:, 0, :], reads=[kt], writes=[KT_d])
        xs = xsr.next(); xt = xtr.next(); pX = pXr.next()
        P.op("act", lambda e, xs=xs, px=px: e.activation(xs[:], px[:], AF.Identity), reads=[px], writes=[xs])
        for c in range(4):
            P.op("pe", lambda e, c=c, pX=pX, xs=xs: e.transpose(pX[:, c, :], xs[:, c * 128:(c + 1) * 128], L.ident[:]),
                 reads=[xs, L.ident], writes=[pX], inc=(c == 3))
        P.op("dve", lambda e, xt=xt, pX=pX: e.tensor_copy(xt[:], pX[:]), reads=[pX], writes=[xt])
        P.dma("pool", XR_d[:, :, t * 128:(t + 1) * 128].rearrange("j p t -> p j t"), xt[:], reads=[xt], writes=[XR_d])

    recs = []
    for t in range(NT_ALL):
        P.record_begin(); tile_A(t); recs.append(P.record_end())
    P.replay_skewed(recs)
    P.phase_end()

    P.phase_begin()
    KT = P.sb([128, NALL], BF, "KT")
    for pc in range(6):
        P.dma("sp", KT[:, pc * 1408:(pc + 1) * 1408], KT_d[:, pc * 1408:(pc + 1) * 1408], reads=[KT_d], writes=[KT])
    Vn = [P.sb([128, NT_ALL, 128], BF, "Vn") for _ in range(2)]
    for n in range(2):
        P.op("pool", lambda e, n=n: e.memset(Vn[n][:, :, 64:128], 1.0), writes=[Vn[n]])
        for pc in range(11):
            P.dma("sp", Vn[n][:, pc * 6:(pc + 1) * 6, 0:64],
                  VS_d[pc * 768:(pc + 1) * 768, n * 64:(n + 1) * 64].rearrange("(t p) e -> p t e", p=128), reads=[VS_d], writes=[Vn[n]])
    spr = L.ring(4, [128, 512], F32, "sp", psum=True)
    accr = L.ring(2, [128, 512], F32, "acc", psum=True)
    pXo = L.ring(1, [128, 4, 128], F32, "pXo", psum=True)
    LOOK1 = 3
    pTo = P.ps([128, 4, 128], BF, "pTo")
    ptr_ = L.ring(6, [128, 512], BF, "pts")
    rzr = L.ring(2, [128, 4], F32, "rz")
    osr = L.ring(2, [128, 512], F32, "osb")
    yar = L.ring(2, [128, 512], BF, "yat")
    for qt in range(OWN // 128):
        q0 = LC + qt * 128
        yat = yar.next()
        for n in range(2):
            acc = accr.next()

            def emit_scores1(kt, n=n, q0=q0):
                sp_ = spr.next()
                P.op("pe", lambda e, n=n, sp_=sp_, kt=kt, q0=q0: e.matmul(
                    sp_[:].rearrange("p (g q) -> p g q", g=4), KT[:, kt * 128:(kt + 1) * 128],
                    QT[:, n, :, q0:q0 + 128], start=True, stop=True), reads=[KT, QT], writes=[sp_])
                return sp_

            pend_sc = [emit_scores1(i) for i in range(LOOK1)]
            for kt in range(NT_ALL):
                sp_ = pend_sc.pop(0)
                if kt + LOOK1 < NT_ALL:
                    pend_sc.append(emit_scores1(kt + LOOK1))
                pt = ptr_.next()
                P.op("act", lambda e, pt=pt, sp_=sp_: e.activation(pt[:], sp_[:], AF.Exp, scale=0.125), reads=[sp_], writes=[pt])
                P.op("pe", lambda e, pt=pt, n=n, kt=kt, acc=acc: e.matmul(acc[:], Vn[n][:, kt, :], pt[:], start=(kt == 0), stop=(kt == NT_ALL - 1)),
                     reads=[pt, Vn[n]], writes=[acc], inc=(kt == NT_ALL - 1))
            osb = osr.next(); pX = pXo.next(); rz = rzr.next()
            P.op("act", lambda e, osb=osb, acc=acc: e.activation(osb[:], acc[:], AF.Identity), reads=[acc], writes=[osb])
            for g in range(4):
                P.op("pe", lambda e, g=g, pX=pX, osb=osb: e.transpose(pX[:, g, :], osb[:, g * 128:(g + 1) * 128], L.ident[:]),
                     reads=[osb, L.ident], writes=[pX], inc=(g == 3))
            P.op("dve", lambda e, rz=rz, pX=pX: e.reciprocal(rz[:, 0:4], pX[:, :, 64]), reads=[pX], writes=[rz])
            for g in range(4):
                h = n * 4 + g
                P.op("act", lambda e, g=g, h=h, rz=rz, yat=yat, pX=pX: e.activation(yat[:, h * 64:(h + 1) * 64], pX[:, g, 0:64], AF.Identity, scale=rz[:, g:g + 1]),
                     reads=[pX, rz], writes=[yat])
        for c in range(4):
            P.op("pe", lambda e, c=c, yat=yat: e.transpose(pTo[:, c, :], yat[:, c * 128:(c + 1) * 128], L.identb[:]),
                 reads=[yat, L.identb], writes=[pTo], inc=(c == 3))
        P.op("act", lambda e, q0=q0: e.activation(yT[:, 0:4, q0:q0 + 128], pTo[:], AF.Identity), reads=[pTo], writes=[yT])
    P.phase_end()
    qstack.close()

    P.phase_begin()
    lp = P.sb([128, 4, 12], F32, "lp")
    P.dma("sp", lp[:], lrup_d[:, :, :], reads=[lrup_d], writes=[lp])
    sw = P.sb([128, 4], F32, "segw"); L.load_bc(sw, segw_d.ap, segw_d)
    nsp = P.sb([128, 4, 2], F32, "nsp")
    P.op("act", lambda e: e.activation(nsp[:], lp[:, :, 9:11], AF.Exp, scale=-1.0), reads=[lp], writes=[nsp])
    P.op("act", lambda e: e.activation(nsp[:], nsp[:], AF.Ln, bias=1.0), reads=[nsp], writes=[nsp])
    P.op("dve", lambda e: e.tensor_scalar(nsp[:], nsp[:], -8.0, None, ALU.mult), reads=[nsp], writes=[nsp])
    BD = P.sb([128, 16, 128], BF, "BD")
    P.op("dve", lambda e: e.memset(BD[:], 0.0), writes=[BD])
    for kind, wd in enumerate((wa_d, wx_d)):
        for d in range(2):
            for jj in range(4):
                for hh in range(2):
                    P.dma("pool", BD[hh * 64:(hh + 1) * 64, kind * 8 + d * 4 + jj, hh * 64:(hh + 1) * 64], wd.ap[d, 2 * jj + hh, :, :],
                          reads=[wd], writes=[BD])
    SEGL = 4096
    X = P.sb([128, NALL], F32, "Xr"); XD = P.sb([128, NALL], F32, "XD"); XDB = P.sb([128, NALL], BF, "XDB")
    Ab = T_view(X, X[:, 0:SEGL]); Bb = T_view(X, X[:, SEGL:2 * SEGL]); Hb = P.sb([128, SEGL], F32, "Hb")
    Ib = P.sb([128, SEGL], F32, "Ib")
    acc = P.sb([128, OWN], F32, "hacc")
    st = P.sb([128, 2], F32, "st")
    prr = L.ring(2, [128, 512], F32, "pr", psum=True); pir = L.ring(2, [128, 512], F32, "pi", psum=True)
    for jj in range(4):
        for pc in range(6):
            P.dma("sp", X[:, pc * 1408:(pc + 1) * 1408], XR_d[jj, :, pc * 1408:(pc + 1) * 1408], reads=[XR_d], writes=[X])
        P.op("dve", lambda e, jj=jj: e.tensor_scalar(XD[:], X[:], lp[:, jj, 2:3], lp[:, jj, 4:5], ALU.mult, ALU.add), reads=[X, lp], writes=[XD])
        for (lo, hi) in ((0, LC), (LC, NALL)):
            for tap, off in ((0, -2), (1, -1), (3, 1)):
                a = max(lo, lo - off); b = min(hi, hi - off)
                P.op("dve", lambda e, jj=jj, tap=tap, off=off, a=a, b=b: e.scalar_tensor_tensor(
                    XD[:, a:b], X[:, a + off:b + off], lp[:, jj, tap:tap + 1], XD[:, a:b], ALU.mult, ALU.add), reads=[X, lp, XD], writes=[XD])
        P.op("pool", lambda e: e.tensor_copy(XDB[:], XD[:]), reads=[XD], writes=[XDB])
        P.op("dve", lambda e: e.memset(acc[:], 0.0), writes=[acc])
        for d in range(2):
            segs = [(0, LC), (LC, LC + SEGL), (LC + SEGL, NALL)]
            order = segs if d == 0 else [segs[0], segs[2], segs[1]]
            for si, (lo, hi) in enumerate(order):
                n = hi - lo
                for b0 in range(0, n, 512):
                    bw = min(512, n - b0)
                    pr = prr.next(); pi_ = pir.next()
                    P.op("pe", lambda e, pr=pr, d=d, jj=jj, lo=lo, b0=b0, bw=bw: e.matmul(pr[:, 0:bw], BD[:, d * 4 + jj, :], XDB[:, lo + b0:lo + b0 + bw], start=True, stop=True),
                         reads=[BD, XDB], writes=[pr])
                    P.op("pe", lambda e, pi_=pi_, d=d, jj=jj, lo=lo, b0=b0, bw=bw: e.matmul(pi_[:, 0:bw], BD[:, 8 + d * 4 + jj, :], XDB[:, lo + b0:lo + b0 + bw], start=True, stop=True),
                         reads=[BD, XDB], writes=[pi_])
                    sl = slice(b0, b0 + bw)
                    P.op("act", lambda e, pr=pr, bw=bw, sl=sl, d=d, jj=jj: e.activation(Ab[:, sl], pr[:, 0:bw], AF.Sigmoid, bias=lp[:, jj, 5 + d:6 + d]), reads=[pr, lp], writes=[Ab])
                    P.op("act", lambda e, pi_=pi_, bw=bw, sl=sl, d=d, jj=jj: e.activation(Ib[:, sl], pi_[:, 0:bw], AF.Sigmoid, bias=lp[:, jj, 7 + d:8 + d]), reads=[pi_, lp], writes=[Ib])
                P.op("act", lambda e, n=n, d=d, jj=jj: e.activation(Ab[:, 0:n], Ab[:, 0:n], AF.Exp, scale=nsp[:, jj, d:d + 1]), reads=[Ab, nsp], writes=[Ab])
                P.op("dve", lambda e, n=n: e.tensor_tensor(Hb[:, 0:n], Ab[:, 0:n], Ab[:, 0:n], ALU.mult), reads=[Ab], writes=[Hb])
                P.op("act", lambda e, n=n: e.activation(Hb[:, 0:n], Hb[:, 0:n], AF.Sqrt, bias=1.0, scale=-1.0), reads=[Hb], writes=[Hb])
                P.op("dve", lambda e, n=n: e.tensor_tensor(Hb[:, 0:n], Hb[:, 0:n], Ib[:, 0:n], ALU.mult), reads=[Hb, Ib], writes=[Hb])
                P.op("pool", lambda e, n=n, lo=lo: e.tensor_tensor(Bb[:, 0:n], Hb[:, 0:n], XD[:, lo:lo + n], ALU.mult), reads=[Hb, XD], writes=[Bb])
                init = 0.0 if si == 0 else st[:, d:d + 1]
                if d == 0:
                    P.op("dve", lambda e, n=n, init=init: e.tensor_tensor_scan(Hb[:, 0:n], Ab[:, 0:n], Bb[:, 0:n], init, ALU.mult, ALU.add),
                         reads=[Ab, Bb, st], writes=[Hb])
                    P.op("dve", lambda e, n=n, d=d: e.tensor_copy(st[:, d:d + 1], Hb[:, n - 1:n]), reads=[Hb], writes=[st])
                else:
                    P.op("dve", lambda e, n=n, init=init: e.tensor_tensor_scan(Hb[:, 0:n][:, ::-1], Ab[:, 0:n][:, ::-1], Bb[:, 0:n][:, ::-1], init, ALU.mult, ALU.add),
                         reads=[Ab, Bb, st], writes=[Hb])
                    P.op("dve", lambda e, d=d: e.tensor_copy(st[:, d:d + 1], Hb[:, 0:1]), reads=[Hb], writes=[st])
                if lo >= LC:
                    s0 = (lo - LC) // OWN
                    for k in range(2):
                        P.op("dve", lambda e, k=k, s0=s0: e.scalar_tensor_tensor(acc[:], Hb[:, k * OWN:(k + 1) * OWN], sw[:, s0 + k:s0 + k + 1], acc[:], ALU.mult, ALU.add),
                             reads=[Hb, sw, acc], writes=[acc])
        P.op("dve", lambda e, jj=jj: e.tensor_tensor(yT[:, 4 + jj, LC:LC + OWN], acc[:], gateT[:, jj, :], ALU.mult), reads=[acc, gateT], writes=[yT])
    P.phase_end()
    gstack.close()

    gffn_d = L.din("norm_ffn_g" + sfx, [D])
    wout_d = L.din("w_out" + sfx, [D, D])
    rw_d = L.din("router_w", [D, 8]); rb_d = L.din("router_b", [8])
    w1_d = L.din("moe_w1", [8, D, FF]); w3_d = L.din("moe_w3", [8, D, FF]); w2_d = L.din("moe_w2", [8, FF, D])
    gffn = P.sb([128, D], F32, "gffn", glob=True)
    L.load_bc(gffn, gffn_d.ap, gffn_d)
    ffn_phase(L, xown_blk, xo_blk, blocks, yT, wout_d, gffn, w1_d, w3_d, w2_d, ctx_block=False, moe=(rw_d, rb_d, 8))
    P.barrier()
    P.flush()
    P.gstack.close()
    P.gstack = _outer


def build_fused():
    nc = bass.Bass("TRN2", target_bir_lowering=False)
    L = LK(nc)
    P = L.P
    xall = L.din("xall", [NALL, D])
    xown = L.din("xown", [NOWN, D])
    csall = L.din("csall", [NALL, 128])
    csown = L.din("csown", [NOWN, 128])
    ident_d = L.din("ident", [128, 128])
    cvec_d = L.din("cvec", [128, 16])
    xo = L.dout("xo", [OWN, D])
    xc1_d = L.dscratch("xc1_d", [LC, D])
    x1own_d = L.dscratch("x1own_d", [OWN, D])
    NCH = OWN // 256
    x1g_d = [L.dscratch(f"x1g{k}_d", [4 * 256, D]) for k in range(NCH)]
    L.load_ident(ident_d)
    modrows = []
    for lay in range(2):
        wmod_d = L.din(f"w_mod{lay}", [D, 6 * D]); bmod_d = L.din(f"b_mod{lay}", [6 * D])
        modrows.append(L.setup_common(cvec_d, wmod_d, bmod_d, f"l{lay}"))
    io0 = dict(csall=csall, csown=csown, ident_d=ident_d, cvec_d=cvec_d, sfx="0", modrow=modrows[0],
               xall_tile=lambda t: (xall[t * 128:(t + 1) * 128, :], xall),
               xown_blk=lambda t0, nt: (xown[t0 * 128:(t0 + nt) * 128, :], xown),
               xo_blk=lambda t0, nt: ((xc1_d[0:LC, :], xc1_d) if t0 == 0 else
                                      (x1own_d[(t0 - 2) * 128:(t0 - 2 + nt) * 128, :], x1own_d)))
    emit_l0(L, io0)
    for k in range(NCH):
        csem = nc.alloc_semaphore(f"ccsem{k}")
        waits = P._waits("pool", P._deps([x1own_d], [x1g_d[k]]), False)

        def emit_cc(eng, waits=waits, k=k, csem=csem):
            for (s_, v_) in waits:
                eng.wait_ge(s_, v_)
            eng.collective_compute("AllGather", ALU.bypass, replica_groups=[[0, 1, 2, 3], [4, 5, 6, 7]],
                                   ins=[x1own_d[k * 256:(k + 1) * 256, :].opt()], outs=[x1g_d[k].ap.opt()]).then_inc(csem)

        P.q["pool"].append(emit_cc)
        tok = (csem, 1, "cc")
        x1g_d[k].w = tok; x1g_d[k].r = {}
        x1own_d.r[csem.name] = tok

    def lat_tile(t):
        g = t - 2
        r, i = g // 16, g % 16
        k, h = i // 2, i % 2
        return (x1g_d[k][r * 256 + h * 128:r * 256 + (h + 1) * 128, :], x1g_d[k])

    io1 = dict(csall=csall, csown=csown, ident_d=ident_d, cvec_d=cvec_d, sfx="1", modrow=modrows[1],
               xall_tile=lambda t: ((xc1_d[t * 128:(t + 1) * 128, :], xc1_d) if t < 2 else lat_tile(t)),
               xown_blk=lambda t0, nt: (x1own_d[(t0 - 2) * 128:(t0 - 2 + nt) * 128, :], x1own_d),
               xo_blk=lambda t0, nt: (xo[(t0 - 2) * 128:(t0 - 2 + nt) * 128, :], xo))
    emit_l1(L, io1)
    P.finish([xo])
    return nc, L


_ROPE = None


def _cvec(cb, cctx):
    v = np.stack([cb, cctx], 1).astype(np.float32)
    return np.ascontiguousarray(v.reshape(8, 128, 2).transpose(1, 0, 2).reshape(128, 16))


def _lru_pack(inp):
    cw = np.asarray(inp["d_conv_w"])[0]; cb = np.asarray(inp["d_conv_b"])[0]
    ba = np.asarray(inp["d_ba"])[0]; bx = np.asarray(inp["d_bx"])[0]; lam = np.asarray(inp["d_lambda"])[0]
    cols = [cw[0], cw[1], cw[2], cw[3], cb, ba[0], ba[1], bx[0], bx[1], lam[0], lam[1], np.zeros(512, np.float32)]
    p = np.stack(cols, 1).astype(np.float32)
    return np.ascontiguousarray(p.reshape(4, 128, 12).transpose(1, 0, 2))


_NC = None


def kernel(**inputs):
    global _ROPE, _NC
    if _ROPE is None:
        _ROPE = _rope_table()
    inp = inputs
    nc, L = build_fused()
    x = np.asarray(inp["x"], np.float32); ctx = np.asarray(inp["ctx"], np.float32)
    ident = np.eye(128, dtype=np.float32)
    lru_p = _lru_pack(inp)
    g = lambda k, i=0: np.asarray(inp[k])[i]
    shared = {
        "ident": ident, "csall": _ROPE, "lru_p": lru_p,
        "w_mod0": g("w_mod", 0), "b_mod0": g("b_mod", 0), "norm_mix_g0": g("norm_mix_g", 0), "norm_ffn_g0": g("norm_ffn_g", 0),
        "w_out0": g("w_out", 0), "w_mod1": g("w_mod", 1), "b_mod1": g("b_mod", 1), "norm_mix_g1": g("norm_mix_g", 1),
        "norm_ffn_g1": g("norm_ffn_g", 1), "w_out1": g("w_out", 1),
        "e_w_in": g("e_w_in"), "a_norm_g": g("a_norm_g"), "a_ws": g("a_ws"), "a_bs": g("a_bs"),
        "b_qnorm_g": g("b_qnorm_g"), "b_knorm_g": g("b_knorm_g"), "b_lq1": g("b_lq1"), "b_lk1": g("b_lk1"),
        "b_lq2": g("b_lq2"), "b_lk2": g("b_lk2"), "b_subln_g": g("b_subln_g"),
        "ffn_w1": g("ffn_w1"), "ffn_w3": g("ffn_w3"), "ffn_w2": g("ffn_w2"),
        "o_w_in": g("o_w_in"), "c_qnorm_g": g("c_qnorm_g"), "c_knorm_g": g("c_knorm_g"),
        "d_wa": g("d_wa"), "d_wx": g("d_wx"), "router_w": g("router_w"), "router_b": g("router_b"),
        "moe_w1": g("moe_w1"), "moe_w3": g("moe_w3"), "moe_w2": g("moe_w2"),
    }
    shared = {k: np.ascontiguousarray(v, dtype=np.float32) for k, v in shared.items()}
    maps = []
    for core in range(8):
        b, s = core // 4, core % 4
        xall = np.concatenate([ctx[b], x[b]], 0)
        own = slice(LC + s * OWN, LC + (s + 1) * OWN)
        segw = np.zeros(4, np.float32); segw[s] = 1.0
        m = dict(shared)
        m.update({"xall": xall, "xown": np.concatenate([ctx[b], xall[own]], 0),
                  "csown": np.concatenate([_ROPE[:LC], _ROPE[own]], 0),
                  "cvec": _cvec(np.asarray(inp["c"])[b], np.asarray(inp["c_ctx"])), "segw": segw})
        maps.append({k: np.ascontiguousarray(m[k], dtype=np.float32) for k in L.inputs})
    res = run_bass_kernel_spmd(nc, maps, core_ids=list(range(8)))
    out = np.zeros_like(x)
    for core in range(8):
        b, s = core // 4, core % 4
        out[b, s * OWN:(s + 1) * OWN] = res.results[core]["xo"]
    return out.astype(np.float32)
```

```python
from contextlib import ExitStack

import numpy as np
import concourse.bass as bass
import concourse.mybir as mybir
from concourse.bass_utils import run_bass_kernel_spmd

F32 = mybir.dt.float32
BF = mybir.dt.bfloat16
AF = mybir.ActivationFunctionType
ALU = mybir.AluOpType
AX = mybir.AxisListType

D = 1024
SEQ = 8192
LC = 256
NALL = SEQ + LC
NT_ALL = NALL // 128
OWN = 2048
NOWN = OWN + LC
FF = 2816
NFC = FF // 128
EPS = 1e-6


class T:
    def __init__(self, name, ap):
        self.name = name
        self.ap = ap
        self.w = None
        self.r = {}

    def __getitem__(self, k):
        return self.ap[k]


class Prog:
    ENG = ("pe", "act", "dve", "pool", "sp")
    SEM_ROLL = 30000
    N_DMA_SEMS = 32

    def __init__(self, nc):
        self.nc = nc
        self.q = {e: [] for e in self.ENG}
        self.sem = {}
        self.cnt = {}
        self.nsem = 0
        self.retired = {}
        self._rec = None
        for e in self.ENG:
            self._new_sem(e)
        self.dsem = []
        for i in range(self.N_DMA_SEMS):
            s = nc.alloc_semaphore(f"dma{i}")
            self.dsem.append([s, 0])
        self.dnext = 0
        self.seen = {e: {} for e in self.ENG}
        self.pend = {e: False for e in self.ENG}
        self.ninst = 0
        self.gstack = ExitStack()
        self.stack = None
        self.uid = 0

    def _new_sem(self, e):
        if e in self.sem and self.cnt.get(e, 0) > 0:
            self.retired.setdefault(e, []).append((self.sem[e], self.cnt[e], e))
        self.nsem += 1
        self.sem[e] = self.nc.alloc_semaphore(f"s_{e}_{self.nsem}")
        self.cnt[e] = 0

    def _deps(self, reads, writes):
        deps = []
        for t in reads:
            if t.w is not None:
                deps.append(t.w)
        for t in writes:
            if t.w is not None:
                deps.append(t.w)
            deps.extend(t.r.values())
        return deps

    def _waits(self, e, deps, same_ok):
        out = {}
        for (s, v, src) in deps:
            if src == e and same_ok:
                continue
            key = s.name
            if self.seen[e].get(key, 0) >= v:
                continue
            if key not in out or out[key][1] < v:
                out[key] = (s, v)
        for key, (s, v) in out.items():
            self.seen[e][key] = v
        return list(out.values())

    def _mark(self, tok, reads, writes):
        s, v, src = tok
        key = s.name
        for t in reads:
            old = t.r.get(key)
            if old is None or old[1] < v:
                t.r[key] = tok
        for t in writes:
            t.w = tok
            t.r = {}

    def record_begin(self):
        self._rec = []

    def record_end(self):
        r, self._rec = self._rec, None
        return r

    def mark(self):
        if self._rec is not None:
            self._rec.append(("mark", (), {}))

    def _replay(self, ops):
        for kind, a, kw in ops:
            if kind == "op":
                self.op(*a, **kw)
            elif kind == "dma":
                self.dma(*a, **kw)

    def replay_skewed(self, recs):
        split = []
        for r in recs:
            k = next(i for i, o in enumerate(r) if o[0] == "mark")
            split.append((r[:k], r[k + 1:]))
        n = len(split)
        self._replay(split[0][0])
        for t in range(n):
            if t + 1 < n:
                self._replay(split[t + 1][0])
            self._replay(split[t][1])

    def op(self, e, fn, reads=(), writes=(), inc=True, same_ok=None):
        if getattr(self, "_rec", None) is not None:
            self._rec.append(("op", (e, fn), dict(reads=reads, writes=writes, inc=inc, same_ok=same_ok)))
            return None
        if same_ok is None:
            same_ok = (e == "pe")
        waits = self._waits(e, self._deps(reads, writes), same_ok)
        if self.cnt[e] >= self.SEM_ROLL and not self.pend[e]:
            self._new_sem(e)
        if inc:
            self.cnt[e] += 1
            tok = (self.sem[e], self.cnt[e], e)
            self.pend[e] = False
        else:
            tok = (self.sem[e], self.cnt[e] + 1, e)
            self.pend[e] = True
        sem = self.sem[e]

        def emit(eng, fn=fn, waits=waits, inc=inc, sem=sem):
            for (s, v) in waits:
                eng.wait_ge(s, v)
            ins = fn(eng)
            if inc:
                ins.then_inc(sem, 1)

        self.q[e].append(emit)
        self._mark(tok, reads, writes)
        self.ninst += 1
        return tok

    def dma(self, e, out_ap, in_ap, reads=(), writes=(), **kw):
        if getattr(self, "_rec", None) is not None:
            self._rec.append(("dma", (e, out_ap, in_ap), dict(reads=reads, writes=writes, **kw)))
            return None
        d = self.dsem[self.dnext]
        self.dnext = (self.dnext + 1) % self.N_DMA_SEMS
        s, prev = d
        deps = self._deps(reads, writes)
        if prev > 0:
            deps.append((s, prev, "dma"))
        waits = self._waits(e, deps, False)
        d[1] = prev + 16
        tok = (s, prev + 16, "dma")

        def emit(eng, waits=waits, s=s):
            for (ws, v) in waits:
                eng.wait_ge(ws, v)
            eng.dma_start(out=out_ap, in_=in_ap, **kw).then_inc(s, 16)

        self.q[e].append(emit)
        self._mark(tok, reads, writes)
        self.ninst += 1
        return tok

    def wait_all(self, e, toks):
        waits = self._waits(e, list(toks), False)

        def emit(eng, waits=waits):
            for (s, v) in waits:
                eng.wait_ge(s, v)

        self.q[e].append(emit)

    def barrier(self):
        toks = []
        for e in self.ENG:
            if self.cnt[e] > 0:
                toks.append((self.sem[e], self.cnt[e], e))
            elif self.retired.get(e):
                toks.append(self.retired[e][-1])
        toks += [(s, v, "dma") for s, v in self.dsem if v > 0]
        for e in self.ENG:
            assert not self.pend[e]
            self.wait_all(e, toks)

    def phase_begin(self):
        self.stack = ExitStack()

    def flush(self):
        if not any(self.q.values()):
            return
        q = self.q
        self.q = {e: [] for e in self.ENG}
        nc = self.nc
        with nc.Block() as block:
            @block.tensor
            def _(eng):
                for f in q["pe"]:
                    f(eng)

            @block.scalar
            def _(eng):
                for f in q["act"]:
                    f(eng)

            @block.vector
            def _(eng):
                for f in q["dve"]:
                    f(eng)

            @block.gpsimd
            def _(eng):
                for f in q["pool"]:
                    f(eng)

            @block.sync
            def _(eng):
                for f in q["sp"]:
                    f(eng)

    def phase_end(self):
        self.barrier()
        self.flush()
        self.stack.close()
        self.stack = None

    def sb(self, shape, dt=F32, name="t", glob=False):
        self.uid += 1
        nm = f"{name}_{self.uid}"
        st = self.gstack if (glob or self.stack is None) else self.stack
        h = st.enter_context(self.nc.sbuf_tensor(nm, list(shape), dt))
        return T(nm, h.ap())

    def ps(self, shape, dt=F32, name="p", glob=False):
        self.uid += 1
        nm = f"{name}_{self.uid}"
        st = self.gstack if (glob or self.stack is None) else self.stack
        h = st.enter_context(self.nc.psum_tensor(nm, list(shape), dt))
        return T(nm, h.ap())

    def finish(self, final_tiles):
        toks = [t.w for t in final_tiles if t.w is not None]
        self.wait_all("sp", toks)
        self.barrier()
        self.flush()
        self.gstack.close()


class Ring:
    def __init__(self, tiles):
        self.tiles = tiles
        self.i = 0

    def next(self):
        t = self.tiles[self.i % len(self.tiles)]
        self.i += 1
        return t


class LK:
    def __init__(self, nc):
        self.nc = nc
        self.P = Prog(nc)

    def din(self, name, shape, dt=F32):
        if not hasattr(self, "inputs"):
            self.inputs = []
        self.inputs.append(name)
        return T(name, self.nc.dram_tensor(name, list(shape), dt, kind="ExternalInput").ap())

    def dout(self, name, shape, dt=F32):
        return T(name, self.nc.dram_tensor(name, list(shape), dt, kind="ExternalOutput").ap())

    def dscratch(self, name, shape, dt=F32):
        return T(name, self.nc.dram_tensor(name, list(shape), dt, kind="Internal").ap())

    def ring(self, n, shape, dt=F32, name="r", psum=False):
        P = self.P
        return Ring([(P.ps if psum else P.sb)(shape, dt, name) for _ in range(n)])

    def load_ident(self, ident_d):
        P = self.P
        self.ident = P.sb([128, 128], F32, "ident", glob=True)
        self.identb = P.sb([128, 128], BF, "identb", glob=True)
        P.dma("sp", self.ident[:], ident_d[:, :], reads=[ident_d], writes=[self.ident])
        P.op("dve", lambda e: e.tensor_copy(self.identb[:], self.ident[:]), reads=[self.ident], writes=[self.identb])
        self.epsT = P.sb([128, 1], F32, "epsT", glob=True)
        P.op("dve", lambda e: e.memset(self.epsT[:], EPS), writes=[self.epsT])

    def setup_common(self, cvec_d, wmod_d, bmod_d, tag=""):
        P = self.P
        modrow_d = self.dscratch("modrow_d" + tag, [2, 6 * D])
        P.phase_begin()
        cv = P.sb([128, 16], F32, "cv")
        sc = P.sb([128, 16], F32, "sc")
        bm = P.sb([2, 6 * D], F32, "bm")
        mr = P.sb([2, 6 * D], F32, "mr")
        P.dma("sp", cv[:], cvec_d[:, :], reads=[cvec_d], writes=[cv])
        for j in range(2):
            P.dma("sp", bm[j:j + 1, :], bmod_d.ap.unsqueeze(0), reads=[bmod_d], writes=[bm])
        P.op("act", lambda e: e.activation(sc[:], cv[:], AF.Silu), reads=[cv], writes=[sc])
        wms = self.ring(2, [128, 8, 512], F32, "wm")
        pms = self.ring(2, [128, 512], F32, "pm", psum=True)
        for pc in range(12):
            wm = wms.next()
            pm = pms.next()
            P.dma("sp", wm[:], wmod_d[:, pc * 512:(pc + 1) * 512].rearrange("(kc p) n -> p kc n", p=128),
                  reads=[wmod_d], writes=[wm])
            for kc in range(8):
                P.op("pe", lambda e, kc=kc, wm=wm, pm=pm: e.matmul(pm[0:2, :], sc[:, kc * 2:(kc + 1) * 2], wm[:, kc, :],
                                                                 start=(kc == 0), stop=(kc == 7)),
                     reads=[sc, wm], writes=[pm], inc=(kc == 7))
            P.op("dve", lambda e, pc=pc, pm=pm: e.tensor_tensor(mr[0:2, pc * 512:(pc + 1) * 512], pm[0:2, :],
                                                              bm[0:2, pc * 512:(pc + 1) * 512], ALU.add),
                 reads=[pm, bm], writes=[mr])
        P.dma("sp", modrow_d[:, :], mr[:], reads=[mr], writes=[modrow_d])
        P.phase_end()
        return modrow_d

    def load_bc(self, dst, src_row_ap, src_t):
        self.P.dma("sp", dst[:], src_row_ap.partition_broadcast(128), reads=[src_t], writes=[dst])

    def mod_bc(self, j, idx, name):
        t = self.P.sb([128, D], F32, name)
        self.load_bc(t, self.modrow_d.ap[j, idx * D:(idx + 1) * D], self.modrow_d)
        return t

    def make_A(self, sc_t, g_t):
        self.P.op("dve", lambda e: e.scalar_tensor_tensor(sc_t[:], sc_t[:], 1.0, g_t[:], ALU.add, ALU.mult),
                  reads=[sc_t, g_t], writes=[sc_t])
        return sc_t

    def rstd(self, ss, n, dim):
        P = self.P
        P.op("act", lambda e: e.activation(ss[:, 0:n], ss[:, 0:n], AF.Sqrt, bias=self.epsT[:, 0:1], scale=1.0 / dim),
             reads=[ss, self.epsT], writes=[ss])
        P.op("dve", lambda e: e.reciprocal(ss[:, 0:n], ss[:, 0:n]), reads=[ss], writes=[ss])

    def norm_tile(self, x_ap, x_t, A_bc, sh_bc, hn, junk, ss):
        P = self.P
        P.op("act", lambda e: e.activation(junk[:], x_ap, AF.Square, accum_out=ss[:, 0:1]),
             reads=[x_t], writes=[junk, ss])
        self.rstd(ss, 1, D)
        P.op("dve", lambda e: e.scalar_tensor_tensor(junk[:], x_ap, ss[:, 0:1], A_bc[:], ALU.mult, ALU.mult),
             reads=[x_t, ss, A_bc], writes=[junk])
        P.op("pool", lambda e: e.tensor_tensor(hn[:], junk[:], sh_bc[:], ALU.add), reads=[junk, sh_bc], writes=[hn])

    def transpose_cols(self, src, ncol_chunks, pT, dst_ap_fn, dst_t, eng="act"):
        P = self.P
        for c in range(ncol_chunks):
            P.op("pe", lambda e, c=c: e.transpose(pT[:, c, :], src[:, c * 128:(c + 1) * 128], self.identb[:]),
                 reads=[src, self.identb], writes=[pT], inc=(c == ncol_chunks - 1))
        if eng == "act":
            P.op("act", lambda e: e.activation(dst_ap_fn(), pT[:, 0:ncol_chunks, :], AF.Identity), reads=[pT], writes=[dst_t])
        else:
            P.op("dve", lambda e: e.tensor_copy(dst_ap_fn(), pT[:, 0:ncol_chunks, :]), reads=[pT], writes=[dst_t])

    def proj_tok(self, ps, hT, tsl, W, c0, ncols):
        for kc in range(8):
            self.P.op("pe", lambda e, kc=kc: e.matmul(ps[:, 0:ncols], hT[:, kc, tsl], W[:, kc, c0:c0 + ncols],
                                                     start=(kc == 0), stop=(kc == 7)),
                      reads=[hT, W], writes=[ps], inc=(kc == 7))

    def proj_feat(self, ps, hT, n, W, c0):
        for kc in range(8):
            self.P.op("pe", lambda e, kc=kc: e.matmul(ps[:, 0:n], W[:, kc, c0:c0 + 128], hT[:, kc, 0:n],
                                                     start=(kc == 0), stop=(kc == 7)),
                      reads=[hT, W], writes=[ps], inc=(kc == 7))

    def rope_tables(self, cs, g_bc, gs_bc, CT, ST):
        P = self.P
        P.op("pool", lambda e: e.tensor_tensor(CT[:], cs[:, 0:64], g_bc[:], ALU.mult), reads=[cs, g_bc], writes=[CT])
        P.op("pool", lambda e: e.tensor_tensor(ST[:], cs[:, 64:128], gs_bc[:], ALU.mult), reads=[cs, gs_bc], writes=[ST])

    def swap_gain(self, g_bc, gs_bc):
        P = self.P
        for a in range(2):
            for b in range(2):
                P.op("dve", lambda e, a=a, b=b: e.tensor_copy(gs_bc[:, a * 32 + b * 16:a * 32 + b * 16 + 16],
                                                              g_bc[:, a * 32 + (1 - b) * 16:a * 32 + (1 - b) * 16 + 16]),
                     reads=[g_bc], writes=[gs_bc])

    def qk_post(self, src, H, CT, ST, out, tmp):
        P = self.P
        sq, qn, t1, ssq = tmp
        n = H * 64
        P.op("act", lambda e: e.activation(sq[:, 0:n], src[:, 0:n], AF.Square), reads=[src], writes=[sq])
        P.op("dve", lambda e: e.tensor_reduce(ssq[:, 0:H], sq[:, 0:n].rearrange("p (h d) -> p h d", d=64), AX.X, ALU.add),
             reads=[sq], writes=[ssq])
        self.rstd(ssq, H, 64)
        P.op("dve", lambda e: e.tensor_tensor(qn[:, 0:n].rearrange("p (h d) -> p h d", d=64),
                                              src[:, 0:n].rearrange("p (h d) -> p h d", d=64),
                                              ssq[:, 0:H].unsqueeze(2).to_broadcast([128, H, 64]), ALU.mult),
             reads=[src, ssq], writes=[qn])
        P.op("dve", lambda e: e.tensor_tensor(t1[:, 0:n].rearrange("p (h d) -> p h d", d=64),
                                              qn[:, 0:n].rearrange("p (h d) -> p h d", d=64),
                                              CT[:].unsqueeze(1).to_broadcast([128, H, 64]), ALU.mult),
             reads=[qn, CT], writes=[t1])
        for b in range(2):
            P.op("dve", lambda e, b=b: e.tensor_tensor(
                sq[:, 0:n].rearrange("p (h a b c) -> p h a b c", a=2, b=2, c=16)[:, :, :, b, :],
                qn[:, 0:n].rearrange("p (h a b c) -> p h a b c", a=2, b=2, c=16)[:, :, :, 1 - b, :],
                ST[:].rearrange("p (a b c) -> p a b c", a=2, b=2, c=16)[:, :, b, :].unsqueeze(1).to_broadcast([128, H, 2, 16]),
                ALU.mult), reads=[qn, ST], writes=[sq])
        P.op("pool", lambda e: e.tensor_tensor(out[:, 0:n], t1[:, 0:n], sq[:, 0:n], ALU.add), reads=[t1, sq], writes=[out])


def _rope_table():
    half = 16
    freqs = 10000.0 ** (-np.arange(half, dtype=np.float32) / half)
    t = np.arange(SEQ)
    row = (t // 64).astype(np.float32)
    col = (t % 64).astype(np.float32)
    ar = row[:, None] * freqs[None, :]
    ac = col[:, None] * freqs[None, :]
    C = np.concatenate([np.cos(ar), np.cos(ar), np.cos(ac), np.cos(ac)], 1)
    S = np.concatenate([-np.sin(ar), np.sin(ar), -np.sin(ac), np.sin(ac)], 1)
    lat = np.concatenate([C, S], 1).astype(np.float32)
    ctx = np.concatenate([np.ones((LC, 64), np.float32), np.zeros((LC, 64), np.float32)], 1)
    return np.concatenate([ctx, lat], 0)


STOP = None


def emit_l0(L, io):
    nc = L.nc
    P = L.P
    _outer = P.gstack
    P.gstack = ExitStack()
    xall_tile = io["xall_tile"]; xown_blk = io["xown_blk"]; xo_blk = io["xo_blk"]
    csall = io["csall"]; csown = io["csown"]; ident_d = io["ident_d"]; cvec_d = io["cvec_d"]
    sfx = io["sfx"]
    L.modrow_d = io["modrow"]
    gmix_d = L.din("norm_mix_g" + sfx, [D])
    gffn_d = L.din("norm_ffn_g" + sfx, [D])
    wout_d = L.din("w_out" + sfx, [D, D])
    win_d = L.din("e_w_in", [D, 2560])
    anorm_d = L.din("a_norm_g", [512])
    aws_d = L.din("a_ws", [8, 128, 128])
    abs_d = L.din("a_bs", [8, 128])
    qg_d = L.din("b_qnorm_g", [64])
    kg_d = L.din("b_knorm_g", [64])
    lq1_d = L.din("b_lq1", [64]); lk1_d = L.din("b_lk1", [64]); lq2_d = L.din("b_lq2", [64]); lk2_d = L.din("b_lk2", [64])
    sub_d = L.din("b_subln_g", [128])
    w1_d = L.din("ffn_w1", [D, FF]); w3_d = L.din("ffn_w3", [D, FF]); w2_d = L.din("ffn_w2", [FF, D])
    KT_d = L.dscratch("KT_d", [4, 128, NALL], BF)
    VS_d = L.dscratch("VS_d", [NALL, 512], BF)
    LAM_INIT = 0.2


    gq = P.sb([128, 64], F32, "gq", glob=True); gqs = P.sb([128, 64], F32, "gqs", glob=True)
    gk = P.sb([128, 64], F32, "gk", glob=True); gks = P.sb([128, 64], F32, "gks", glob=True)
    L.load_bc(gq, qg_d.ap, qg_d); L.load_bc(gk, kg_d.ap, kg_d)
    L.swap_gain(gq, gqs); L.swap_gain(gk, gks)
    neglam = P.sb([128, 1], F32, "neglam", glob=True)
    P.phase_begin()
    lt = [P.sb([128, 64], F32, "l") for _ in range(4)]
    for t_, d_ in zip(lt, (lq1_d, lk1_d, lq2_d, lk2_d)):
        L.load_bc(t_, d_.ap, d_)
    e12 = P.sb([128, 2], F32, "e12")
    for i in range(2):
        P.op("dve", lambda e, i=i: e.tensor_tensor(lt[2 * i][:], lt[2 * i][:], lt[2 * i + 1][:], ALU.mult), reads=[lt[2 * i], lt[2 * i + 1]], writes=[lt[2 * i]])
        P.op("dve", lambda e, i=i: e.tensor_reduce(e12[:, i:i + 1], lt[2 * i][:], AX.X, ALU.add), reads=[lt[2 * i]], writes=[e12])
    P.op("act", lambda e: e.activation(e12[:], e12[:], AF.Exp), reads=[e12], writes=[e12])
    P.op("dve", lambda e: e.tensor_tensor(neglam[:], e12[:, 1:2], e12[:, 0:1], ALU.subtract), reads=[e12], writes=[neglam])
    P.op("dve", lambda e: e.tensor_scalar(neglam[:], neglam[:], -LAM_INIT, None, ALU.add), reads=[neglam], writes=[neglam])
    P.phase_end()

    gmix = P.sb([128, D], F32, "gmix", glob=True)
    gffn = P.sb([128, D], F32, "gffn", glob=True)
    L.load_bc(gmix, gmix_d.ap, gmix_d)
    L.load_bc(gffn, gffn_d.ap, gffn_d)

    P.phase_begin()
    A1 = [L.make_A(L.mod_bc(j, 1, "A1"), gmix) for j in range(2)]
    sh1 = [L.mod_bc(j, 0, "sh1") for j in range(2)]
    Wkv = P.sb([128, 8, 1024], BF, "Wkv")
    for h in range(2):
        P.dma("pool", Wkv[:, :, h * 512:(h + 1) * 512],
              win_d[:, 1536 + h * 512:1536 + (h + 1) * 512].rearrange("(kc p) n -> p kc n", p=128), reads=[win_d], writes=[Wkv])
    xr = L.ring(3, [128, D], F32, "xa"); csr = L.ring(3, [128, 128], F32, "csa")
    hnr = L.ring(2, [128, D], BF, "hn"); jr = L.ring(2, [128, D], F32, "junk"); ssr = L.ring(2, [128, 2], F32, "ss")
    hTr = L.ring(2, [128, 8, 128], BF, "hT")
    pTr = L.ring(2, [128, 8, 128], BF, "pT", psum=True)
    pkr = L.ring(2, [128, 512], F32, "pk", psum=True); pvr = L.ring(2, [128, 512], F32, "pv", psum=True)
    CTr = L.ring(2, [128, 64], F32, "CT"); STr = L.ring(2, [128, 64], F32, "ST")
    tmps = [tuple([P.sb([128, 512], F32, "sq"), P.sb([128, 512], F32, "qn"), P.sb([128, 512], F32, "t1"), P.sb([128, 8], F32, "ssq")]) for _ in range(2)]
    kbr = L.ring(2, [128, 512], BF, "kb"); vbr = L.ring(2, [128, 512], BF, "vb"); ktr = L.ring(2, [128, 4, 128], BF, "kt")
    def tile_A(t):
        j = 1 if t < 2 else 0
        x = xr.next(); cs = csr.next(); hn = hnr.next(); junk = jr.next(); ss = ssr.next(); hT = hTr.next(); pT = pTr.next()
        xa_ap, xa_t = xall_tile(t)
        P.dma("sp", x[:], xa_ap, reads=[xa_t], writes=[x])
        P.dma("sp", cs[:], csall[t * 128:(t + 1) * 128, :], reads=[csall], writes=[cs])
        L.norm_tile(x[:], x, A1[j], sh1[j], hn, junk, ss)
        L.transpose_cols(hn, 8, pT, lambda hT=hT: hT[:], hT)
        pk = pkr.next(); pv = pvr.next()
        L.proj_tok(pk, hT, slice(0, 128), Wkv, 0, 512)
        L.proj_tok(pv, hT, slice(0, 128), Wkv, 512, 512)
        P.mark()
        vb = vbr.next()
        P.op("act", lambda e, vb=vb, pv=pv: e.activation(vb[:], pv[:], AF.Identity), reads=[pv], writes=[vb])
        P.dma("pool", VS_d[t * 128:(t + 1) * 128, :], vb[:], reads=[vb], writes=[VS_d])
        CT = CTr.next(); ST = STr.next()
        L.rope_tables(cs, gk, gks, CT, ST)
        kb = kbr.next()
        L.qk_post(pk, 8, CT, ST, kb, tmps[t % 2])
        kt = ktr.next()
        L.transpose_cols(kb, 4, pT, lambda kt=kt: kt[:], kt, eng="dve")
        P.dma("pool", KT_d[:, :, t * 128:(t + 1) * 128].rearrange("h p t -> p h t"), kt[:], reads=[kt], writes=[KT_d])

    recs = []
    for t in range(NT_ALL):
        P.record_begin(); tile_A(t); recs.append(P.record_end())
    P.replay_skewed(recs)
    P.phase_end()

    yT = P.sb([128, 8, NOWN], BF, "yT", glob=True)
    qstack = ExitStack()
    QT = T("QT", qstack.enter_context(nc.sbuf_tensor("QT", [128, 2, 4, NOWN], BF)).ap())
    P.op("pool", lambda e: e.memset(QT[64:128, 0, :, :], 0.0), writes=[QT])
    P.op("pool", lambda e: e.memset(QT[0:64, 1, :, :], 0.0), writes=[QT])
    blocks = [(0, 2)] + [(2 + 4 * i, 4) for i in range(4)]

    P.phase_begin()
    A1 = [L.make_A(L.mod_bc(j, 1, "A1"), gmix) for j in range(2)]
    sh1 = [L.mod_bc(j, 0, "sh1") for j in range(2)]
    Win = P.sb([128, 8, 1536], BF, "Win")
    for h in range(3):
        P.dma("pool", Win[:, :, h * 512:(h + 1) * 512], win_d[:, h * 512:(h + 1) * 512].rearrange("(kc p) n -> p kc n", p=128),
              reads=[win_d], writes=[Win])
    anorm = P.sb([128, 512], F32, "anorm"); L.load_bc(anorm, anorm_d.ap, anorm_d)
    wsT = P.sb([128, 8, 128], BF, "wsT")
    wsr = P.sb([128, 8, 128], F32, "wsr")
    P.dma("sp", wsr[:], aws_d.ap.rearrange("g p q -> p g q"), reads=[aws_d], writes=[wsr])
    wsb = P.sb([128, 8, 128], BF, "wsb")
    P.op("dve", lambda e: e.tensor_copy(wsb[:], wsr[:]), reads=[wsr], writes=[wsb])
    pTr = L.ring(2, [128, 8, 128], BF, "pT", psum=True)
    pTw = pTr.next()
    for g in range(8):
        P.op("pe", lambda e, g=g: e.transpose(pTw[:, g, :], wsb[:, g, :], L.identb[:]), reads=[wsb, L.identb], writes=[pTw], inc=(g == 7))
    P.op("dve", lambda e: e.tensor_copy(wsT[:], pTw[:]), reads=[pTw], writes=[wsT])
    bsT = P.sb([128, 4, 128], F32, "bsT")
    for g in range(8):
        P.dma("sp", bsT[(g % 2) * 64:(g % 2) * 64 + 64, g // 2, :], abs_d.ap[g, :].partition_broadcast(64), reads=[abs_d], writes=[bsT])
    xb = L.ring(1, [128, 4, D], F32, "xb"); csb = L.ring(2, [128, 4, 128], F32, "csb")
    hnr = L.ring(2, [128, D], BF, "hn"); jr = L.ring(2, [128, D], F32, "junk"); ssr = L.ring(2, [128, 2], F32, "ss")
    hTb = L.ring(1, [128, 8, 512], BF, "hTb")
    uTr = L.ring(1, [128, 4, 512], F32, "uT")
    pur = L.ring(2, [128, 512], F32, "pu", psum=True)
    pvr = L.ring(2, [128, 512], F32, "pv", psum=True)
    pmr = L.ring(2, [128, 128], F32, "pm", psum=True)
    gvr = L.ring(2, [128, 512], F32, "gv"); vbr = L.ring(2, [128, 512], BF, "vb"); tmr = L.ring(2, [128, 128], F32, "tm")
    CTr = L.ring(2, [128, 64], F32, "CT"); STr = L.ring(2, [128, 64], F32, "ST")
    tmps = [tuple([P.sb([128, 512], F32, "sq"), P.sb([128, 512], F32, "qn"), P.sb([128, 512], F32, "t1"), P.sb([128, 8], F32, "ssq")]) for _ in range(2)]
    qbr = L.ring(2, [128, 512], BF, "qb")
    for bi, (t0, nt) in enumerate(blocks):
        j = 1 if bi == 0 else 0
        N = nt * 128
        x = xb.next(); cs = csb.next(); hT = hTb.next(); uT = uTr.next()
        xb_ap, xb_t = xown_blk(t0, nt)
        P.dma("sp", x[:, 0:nt, :], xb_ap.rearrange("(t p) d -> p t d", p=128), reads=[xb_t], writes=[x])
        P.dma("sp", cs[:, 0:nt, :], csown[t0 * 128:(t0 + nt) * 128, :].rearrange("(t p) d -> p t d", p=128), reads=[csown], writes=[cs])
        for t in range(nt):
            hn = hnr.next(); junk = jr.next(); ss = ssr.next(); pT = pTr.next()
            L.norm_tile(x[:, t, :], x, A1[j], sh1[j], hn, junk, ss)
            L.transpose_cols(hn, 8, pT, lambda hT=hT, t=t: hT[:, :, t * 128:(t + 1) * 128], hT)
        for c in range(4):
            pu = pur.next()
            L.proj_feat(pu, hT, N, Win, c * 128)
            P.op("act", lambda e, pu=pu, uT=uT, c=c, N=N: e.activation(uT[:, c, 0:N], pu[:, 0:N], AF.Gelu_apprx_tanh), reads=[pu], writes=[uT])
        recs = []
        for t in range(nt):
            tsl = slice(t * 128, (t + 1) * 128)
            gsl = slice((t0 + t) * 128, (t0 + t + 1) * 128)
            P.record_begin()
            pv = pvr.next(); gv = gvr.next(); vb = vbr.next(); junk = jr.next(); ss = ssr.next()
            L.proj_tok(pv, hT, tsl, Win, 512, 512)
            P.op("act", lambda e, gv=gv, pv=pv: e.activation(gv[:], pv[:], AF.Gelu_apprx_tanh), reads=[pv], writes=[gv])
            P.op("act", lambda e, gv=gv, junk=junk, ss=ss: e.activation(junk[:, 0:512], gv[:], AF.Square, accum_out=ss[:, 0:1]), reads=[gv], writes=[junk, ss])
            L.rstd(ss, 1, 512)
            P.op("dve", lambda e, vb=vb, gv=gv, ss=ss: e.scalar_tensor_tensor(vb[:], gv[:], ss[:, 0:1], anorm[:], ALU.mult, ALU.mult),
                 reads=[gv, ss, anorm], writes=[vb])
            for jp in range(4):
                pm = pmr.next(); tm = tmr.next()
                for hh in range(2):
                    g = 2 * jp + hh
                    P.op("pe", lambda e, pm=pm, vb=vb, g=g, hh=hh: e.matmul(pm[hh * 64:(hh + 1) * 64, :], vb[:, g * 64:(g + 1) * 64], wsT[:, g, :],
                                                                         start=True, stop=True), reads=[vb, wsT], writes=[pm], inc=(hh == 1))
                P.op("dve", lambda e, pm=pm, tm=tm, jp=jp: e.tensor_tensor(tm[:], pm[:], bsT[:, jp, :], ALU.add), reads=[pm, bsT], writes=[tm])
                P.op("pool", lambda e, tm=tm, jp=jp, uT=uT, tsl=tsl, gsl=gsl: e.tensor_tensor(yT[:, jp, gsl], tm[:], uT[:, jp, tsl], ALU.mult),
                     reads=[tm, uT], writes=[yT])
            P.mark()
            pq = pur.next(); CT = CTr.next(); ST = STr.next(); qb = qbr.next(); pT = pTr.next()
            L.proj_tok(pq, hT, tsl, Win, 1024, 512)
            L.rope_tables(T_view(cs, cs[:, t, :]), gq, gqs, CT, ST)
            L.qk_post(pq, 8, CT, ST, qb, tmps[t % 2])
            for c4 in range(4):
                P.op("pe", lambda e, c4=c4, pT=pT, qb=qb: e.transpose(pT[:, c4, :], qb[:, c4 * 128:(c4 + 1) * 128], L.identb[:]),
                     reads=[qb, L.identb], writes=[pT], inc=(c4 == 3))
            P.op("dve", lambda e, pT=pT, gsl=gsl: e.tensor_copy(QT[0:64, 0, :, gsl], pT[0:64, 0:4, :]), reads=[pT], writes=[QT])
            P.op("act", lambda e, pT=pT, gsl=gsl: e.activation(QT[64:128, 1, :, gsl], pT[64:128, 0:4, :], AF.Identity), reads=[pT], writes=[QT])
            recs.append(P.record_end())
        P.replay_skewed(recs)
    P.phase_end()

    P.phase_begin()
    subbc = P.sb([128, 128], F32, "subbc"); L.load_bc(subbc, sub_d.ap, sub_d)
    P.op("dve", lambda e: e.tensor_scalar(subbc[:], subbc[:], 1.0 - LAM_INIT, None, ALU.mult), reads=[subbc], writes=[subbc])
    KTh = L.ring(2, [128, NALL], BF, "KTh")
    Vh = L.ring(2, [128, NT_ALL, 129], BF, "Vh")
    for v_ in Vh.tiles:
        P.op("pool", lambda e, v_=v_: e.memset(v_[:, :, 128:129], 1.0), writes=[v_])
    spr = L.ring(4, [128, 2, 256], F32, "sp", psum=True)
    LOOK = 3
    accs = [[P.ps([128, 512], F32, "acc") for c in range(2)] for qi in range(2)]
    pTr_ = L.ring(6, [128, 2, 256], BF, "pTs")
    rz = L.ring(2, [128, 4], F32, "rz"); o1r = L.ring(2, [128, 128], F32, "o1"); o2r = L.ring(2, [128, 128], F32, "o2")
    jr = L.ring(2, [128, 128], F32, "junk"); ssr = L.ring(2, [128, 2], F32, "ss"); ybr = L.ring(2, [128, 128], BF, "yb")
    for h in range(4):
        kth = KTh.next(); vh = Vh.next()
        for pc in range(6):
            P.dma("sp", kth[:, pc * 1408:(pc + 1) * 1408], KT_d[h, :, pc * 1408:(pc + 1) * 1408], reads=[KT_d], writes=[kth])
        for pc in range(11):
            P.dma("sp", vh[:, pc * 6:(pc + 1) * 6, 0:128],
                  VS_d[pc * 768:(pc + 1) * 768, h * 128:(h + 1) * 128].rearrange("(t p) e -> p t e", p=128), reads=[VS_d], writes=[vh])
        for qb_ in range(NOWN // 256):
            keys = [0, 1] if qb_ == 0 else list(range(NT_ALL))
            q0 = qb_ * 256
            def emit_scores(kt, kth=kth, q0=q0, h=h):
                sp_ = spr.next()
                for c in range(2):
                    P.op("pe", lambda e, c=c, sp_=sp_, kth=kth, kt=kt, q0=q0, h=h: e.matmul(
                        sp_[:, c, :], kth[:, kt * 128:(kt + 1) * 128], QT[:, c, h, q0:q0 + 256],
                        start=True, stop=True), reads=[kth, QT], writes=[sp_], inc=(c == 1))
                return sp_

            pend_sc = [emit_scores(keys[i]) for i in range(min(LOOK, len(keys)))]
            for ki, kt in enumerate(keys):
                sp_ = pend_sc.pop(0)
                if ki + LOOK < len(keys):
                    pend_sc.append(emit_scores(keys[ki + LOOK]))
                pt = pTr_.next()
                P.op("act", lambda e, pt=pt, sp_=sp_: e.activation(pt[:], sp_[:], AF.Exp, scale=0.125), reads=[sp_], writes=[pt])
                for qi in range(2):
                    for c in range(2):
                        P.op("pe", lambda e, qi=qi, c=c, pt=pt, vh=vh, kt=kt, ki=ki, keys=keys: e.matmul(
                            accs[qi][c][:, 0:129], pt[:, c, qi * 128:(qi + 1) * 128], vh[:, kt, :],
                            start=(ki == 0), stop=(ki == len(keys) - 1)), reads=[pt, vh], writes=[accs[qi][c]],
                            inc=(ki == len(keys) - 1))
            for qi in range(2):
                r = rz.next(); o1 = o1r.next(); o2 = o2r.next(); junk = jr.next(); ss = ssr.next(); yb = ybr.next()
                a0, a1 = accs[qi]
                P.op("dve", lambda e, r=r, a0=a0: e.reciprocal(r[:, 0:1], a0[:, 128:129]), reads=[a0], writes=[r])
                P.op("dve", lambda e, r=r, a1=a1: e.reciprocal(r[:, 1:2], a1[:, 128:129]), reads=[a1], writes=[r])
                P.op("dve", lambda e, r=r: e.tensor_tensor(r[:, 2:3], r[:, 1:2], neglam[:], ALU.mult), reads=[r, neglam], writes=[r])
                P.op("act", lambda e, o1=o1, a0=a0, r=r: e.activation(o1[:], a0[:, 0:128], AF.Identity, scale=r[:, 0:1]), reads=[a0, r], writes=[o1])
                P.op("dve", lambda e, o2=o2, a1=a1, r=r, o1=o1: e.scalar_tensor_tensor(o2[:], a1[:, 0:128], r[:, 2:3], o1[:], ALU.mult, ALU.add),
                     reads=[a1, r, o1], writes=[o2])
                P.op("act", lambda e, junk=junk, o2=o2, ss=ss: e.activation(junk[:], o2[:], AF.Square, accum_out=ss[:, 0:1]), reads=[o2], writes=[junk, ss])
                L.rstd(ss, 1, 128)
                P.op("dve", lambda e, yb=yb, o2=o2, ss=ss: e.scalar_tensor_tensor(yb[:], o2[:], ss[:, 0:1], subbc[:], ALU.mult, ALU.mult),
                     reads=[o2, ss, subbc], writes=[yb])
                gsl = slice(q0 + qi * 128, q0 + (qi + 1) * 128)
                P.op("pe", lambda e, yb=yb, a0=a0: e.transpose(a0[:].bitcast(BF)[:, 0:128], yb[:], L.identb[:]), reads=[yb, L.identb], writes=[a0])
                P.op("act", lambda e, gsl=gsl, h=h, a0=a0: e.activation(yT[:, 4 + h, gsl], a0[:].bitcast(BF)[:, 0:128], AF.Identity), reads=[a0], writes=[yT])
    P.phase_end()

    qstack.close()
    ffn_phase(L, xown_blk, xo_blk, blocks, yT, wout_d, gffn, w1_d, w3_d, w2_d, ctx_block=True)
    P.barrier()
    P.flush()
    P.gstack.close()
    P.gstack = _outer


def T_view(t, ap):
    return _View(t, ap)


class _View:
    def __init__(self, base, ap):
        object.__setattr__(self, "base", base)
        object.__setattr__(self, "ap", ap)

    def __getitem__(self, k):
        return self.ap[k]

    @property
    def w(self):
        return self.base.w

    @w.setter
    def w(self, v):
        self.base.w = v

    @property
    def r(self):
        return self.base.r

    @r.setter
    def r(self, v):
        self.base.r = v

    @property
    def name(self):
        return self.base.name


def ffn_phase(L, xown, xo, blocks, yT, wout_d, gffn, w1_d, w3_d, w2_d, ctx_block, moe=None):
    P = L.P
    P.phase_begin()
    nvar = 2 if ctx_block else 1
    g1 = [L.mod_bc(j, 2, "g1") for j in range(nvar)]
    A2 = [L.make_A(L.mod_bc(j, 4, "A2"), gffn) for j in range(nvar)]
    sh2 = [L.mod_bc(j, 3, "sh2") for j in range(nvar)]
    g2 = [L.mod_bc(j, 5, "g2") for j in range(nvar)]
    wout = P.sb([128, 8, D], BF, "wout")
    for h in range(2):
        P.dma("pool", wout[:, :, h * 512:(h + 1) * 512], wout_d[:, h * 512:(h + 1) * 512].rearrange("(kc p) n -> p kc n", p=128),
              reads=[wout_d], writes=[wout])
    NE = 1
    if moe is not None:
        rw_d, rb_d, NE = moe
        wr = P.sb([128, 8, 8], F32, "wr")
        P.dma("sp", wr[:], rw_d.ap.rearrange("(kc p) e -> p kc e", p=128), reads=[rw_d], writes=[wr])
        rb = P.sb([128, 8], F32, "rb"); L.load_bc(rb, rb_d.ap, rb_d)
        hnf_r = L.ring(2, [128, D], F32, "hnf")
        tTr = L.ring(2, [128, 8, 128], F32, "tT")
        pXr = L.ring(1, [128, 8, 128], F32, "pX", psum=True)
        dg = P.sb([128, 4, 8], F32, "dg")
        rt = [P.sb([128, 8], F32, "rt") for _ in range(4)]
        rs = P.sb([128, 4], F32, "rs")
    xb = L.ring(1, [128, 4, D], F32, "xb")
    hnr = L.ring(2, [128, D], BF, "hn"); jr = L.ring(2, [128, D], F32, "junk"); ssr = L.ring(2, [128, 2], F32, "ss")
    hTb = L.ring(1, [128, 8, 512], BF, "h2T")
    gT = P.sb([128, NFC, 512], BF, "gT")
    pTr = L.ring(1 if moe is not None else 2, [128, 8, 128], BF, "pT", psum=True)
    por = L.ring(1 if moe is not None else 2, [128, 512], F32, "po", psum=True)
    p1r = L.ring(2, [128, 512], F32, "p1", psum=True); p3r = L.ring(2, [128, 512], F32, "p3", psum=True)
    tmr = L.ring(2, [128, 512], F32, "tm")
    s1r = L.ring(2, [128, 512], F32, "s1")
    w1r = L.ring(2, [128, 8, 256], BF, "w1p"); w3r = L.ring(2, [128, 8, 256], BF, "w3p")
    w2r = L.ring(2, [128, NFC, 256], BF, "w2q")
    for bi, (t0, nt) in enumerate(blocks):
        j = 1 if (ctx_block and bi == 0) else 0
        N = nt * 128
        x = xb.next(); hT = hTb.next()
        xb_ap, xb_t = xown(t0, nt)
        P.dma("sp", x[:, 0:nt, :], xb_ap.rearrange("(t p) d -> p t d", p=128), reads=[xb_t], writes=[x])
        for t in range(nt):
            gsl = slice((t0 + t) * 128, (t0 + t + 1) * 128)
            for hf in range(2):
                po = por.next(); tm = tmr.next()
                for kc in range(8):
                    P.op("pe", lambda e, kc=kc, po=po, gsl=gsl, hf=hf: e.matmul(po[:], yT[:, kc, gsl], wout[:, kc, hf * 512:(hf + 1) * 512],
                                                                              start=(kc == 0), stop=(kc == 7)),
                         reads=[yT, wout], writes=[po], inc=(kc == 7))
                P.op("dve", lambda e, tm=tm, po=po, hf=hf, j=j: e.tensor_tensor(tm[:], po[:], g1[j][:, hf * 512:(hf + 1) * 512], ALU.mult),
                     reads=[po, g1[j]], writes=[tm])
                P.op("dve", lambda e, x=x, t=t, hf=hf, tm=tm: e.tensor_tensor(x[:, t, hf * 512:(hf + 1) * 512], x[:, t, hf * 512:(hf + 1) * 512], tm[:], ALU.add),
                     reads=[x, tm], writes=[x])
        for t in range(nt):
            hn = hnr.next(); junk = jr.next(); ss = ssr.next(); pT = pTr.next()
            if moe is None:
                L.norm_tile(x[:, t, :], x, A2[j], sh2[j], hn, junk, ss)
            else:
                hnf = hnf_r.next(); tT = tTr.next(); pX = pXr.next()
                P.op("act", lambda e, junk=junk, x=x, t=t, ss=ss: e.activation(junk[:], x[:, t, :], AF.Square, accum_out=ss[:, 0:1]),
                     reads=[x], writes=[junk, ss])
                L.rstd(ss, 1, D)
                P.op("dve", lambda e, junk=junk, x=x, t=t, ss=ss, j=j: e.scalar_tensor_tensor(junk[:], x[:, t, :], ss[:, 0:1], A2[j][:], ALU.mult, ALU.mult),
                     reads=[x, ss, A2[j]], writes=[junk])
                P.op("dve", lambda e, hnf=hnf, junk=junk, j=j: e.tensor_tensor(hnf[:], junk[:], sh2[j][:], ALU.add), reads=[junk, sh2[j]], writes=[hnf])
                P.op("act", lambda e, hn=hn, hnf=hnf: e.activation(hn[:], hnf[:], AF.Identity), reads=[hnf], writes=[hn])
                for c in range(8):
                    P.op("pe", lambda e, c=c, pX=pX, hnf=hnf: e.transpose(pX[:, c, :], hnf[:, c * 128:(c + 1) * 128], L.ident[:]),
                         reads=[hnf, L.ident], writes=[pX], inc=(c == 7))
                P.op("dve", lambda e, tT=tT, pX=pX: e.tensor_copy(tT[:], pX[:]), reads=[pX], writes=[tT])
                plg = por.next()
                for kc in range(8):
                    P.op("pe", lambda e, kc=kc, plg=plg, tT=tT: e.matmul(plg[:, 0:8], tT[:, kc, :], wr[:, kc, :], start=(kc == 0), stop=(kc == 7)),
                         reads=[tT, wr], writes=[plg], inc=(kc == 7))
                lg, mk, l2, ex = rt
                P.op("dve", lambda e, plg=plg: e.tensor_tensor(lg[:], plg[:, 0:8], rb[:], ALU.add), reads=[plg, rb], writes=[lg])
                P.op("dve", lambda e: e.tensor_reduce(rs[:, 0:1], lg[:], AX.X, ALU.max), reads=[lg], writes=[rs])
                P.op("dve", lambda e: e.tensor_scalar(mk[:], lg[:], rs[:, 0:1], None, ALU.is_equal), reads=[lg, rs], writes=[mk])
                P.op("dve", lambda e: e.scalar_tensor_tensor(l2[:], mk[:], -1e30, lg[:], ALU.mult, ALU.add), reads=[mk, lg], writes=[l2])
                P.op("dve", lambda e: e.tensor_reduce(rs[:, 1:2], l2[:], AX.X, ALU.max), reads=[l2], writes=[rs])
                P.op("dve", lambda e: e.tensor_scalar(mk[:], lg[:], rs[:, 1:2], None, ALU.is_ge), reads=[lg, rs], writes=[mk])
                P.op("dve", lambda e: e.tensor_scalar(ex[:], lg[:], rs[:, 0:1], None, ALU.subtract), reads=[lg, rs], writes=[ex])
                P.op("act", lambda e: e.activation(ex[:], ex[:], AF.Exp), reads=[ex], writes=[ex])
                P.op("dve", lambda e: e.tensor_tensor(ex[:], ex[:], mk[:], ALU.mult), reads=[ex, mk], writes=[ex])
                P.op("dve", lambda e: e.tensor_reduce(rs[:, 2:3], ex[:], AX.X, ALU.add), reads=[ex], writes=[rs])
                P.op("dve", lambda e: e.reciprocal(rs[:, 3:4], rs[:, 2:3]), reads=[rs], writes=[rs])
                P.op("dve", lambda e, t=t: e.tensor_scalar(dg[:, t, :], ex[:], rs[:, 3:4], None, ALU.mult), reads=[ex, rs], writes=[dg])
            L.transpose_cols(hn, 8, pT, lambda hT=hT, t=t: hT[:, :, t * 128:(t + 1) * 128], hT)
        for ex_i in range(NE):
            w1e = w1_d.ap[ex_i] if moe is not None else w1_d.ap
            w3e = w3_d.ap[ex_i] if moe is not None else w3_d.ap
            w2e = w2_d.ap[ex_i] if moe is not None else w2_d.ap
            for pi in range(NFC // 2):
                w1p = w1r.next(); w3p = w3r.next()
                P.dma("pool", w1p[:], w1e[:, pi * 256:(pi + 1) * 256].rearrange("(kc p) n -> p kc n", p=128), reads=[w1_d], writes=[w1p])
                P.dma("pool", w3p[:], w3e[:, pi * 256:(pi + 1) * 256].rearrange("(kc p) n -> p kc n", p=128), reads=[w3_d], writes=[w3p])
                for fc in range(2):
                    p1 = p1r.next(); p3 = p3r.next(); s1 = s1r.next()
                    L.proj_feat(p1, hT, N, w1p, fc * 128)
                    L.proj_feat(p3, hT, N, w3p, fc * 128)
                    P.op("act", lambda e, s1=s1, p1=p1, N=N: e.activation(s1[:, 0:N], p1[:, 0:N], AF.Silu), reads=[p1], writes=[s1])
                    P.op("dve", lambda e, s1=s1, p3=p3, N=N, f=pi * 2 + fc: e.tensor_tensor(gT[:, f, 0:N], s1[:, 0:N], p3[:, 0:N], ALU.mult),
                         reads=[s1, p3], writes=[gT])
            for qd in range(4):
                w2q = w2r.next()
                P.dma("pool", w2q[:], w2e[:, qd * 256:(qd + 1) * 256].rearrange("(fc p) n -> p fc n", p=128), reads=[w2_d], writes=[w2q])
                for t in range(nt):
                    po = por.next(); tm = tmr.next()
                    for fc in range(NFC):
                        P.op("pe", lambda e, fc=fc, po=po, t=t, w2q=w2q: e.matmul(po[:, 0:256], gT[:, fc, t * 128:(t + 1) * 128], w2q[:, fc, :],
                                                                                start=(fc == 0), stop=(fc == NFC - 1)),
                             reads=[gT, w2q], writes=[po], inc=(fc == NFC - 1))
                    P.op("dve", lambda e, tm=tm, po=po, qd=qd, j=j: e.tensor_tensor(tm[:, 0:256], po[:, 0:256], g2[j][:, qd * 256:(qd + 1) * 256], ALU.mult),
                         reads=[po, g2[j]], writes=[tm])
                    if moe is None:
                        P.op("dve", lambda e, x=x, t=t, qd=qd, tm=tm: e.tensor_tensor(x[:, t, qd * 256:(qd + 1) * 256], x[:, t, qd * 256:(qd + 1) * 256], tm[:, 0:256], ALU.add),
                             reads=[x, tm], writes=[x])
                    else:
                        P.op("dve", lambda e, x=x, t=t, qd=qd, tm=tm, ex_i=ex_i: e.scalar_tensor_tensor(
                            x[:, t, qd * 256:(qd + 1) * 256], tm[:, 0:256], dg[:, t, ex_i:ex_i + 1], x[:, t, qd * 256:(qd + 1) * 256], ALU.mult, ALU.add),
                            reads=[x, tm, dg], writes=[x])
        xo_ap, xo_t = xo(t0, nt)
        P.dma("sp", xo_ap.rearrange("(t p) d -> p t d", p=128), x[:, 0:nt, :], reads=[x], writes=[xo_t])
    P.phase_end()


STOP1 = None


def emit_l1(L, io):
    nc = L.nc
    P = L.P
    _outer = P.gstack
    P.gstack = ExitStack()
    xall_tile = io["xall_tile"]; xown_blk = io["xown_blk"]; xo_blk = io["xo_blk"]
    csall = io["csall"]; csown = io["csown"]; ident_d = io["ident_d"]; cvec_d = io["cvec_d"]
    sfx = io["sfx"]
    L.modrow_d = io["modrow"]
    gmix_d = L.din("norm_mix_g" + sfx, [D])
    win_d = L.din("o_w_in", [D, 1792])
    qg_d = L.din("c_qnorm_g", [64])
    kg_d = L.din("c_knorm_g", [64])
    lrup_d = L.din("lru_p", [128, 4, 12])
    wa_d = L.din("d_wa", [2, 8, 64, 64])
    wx_d = L.din("d_wx", [2, 8, 64, 64])
    segw_d = L.din("segw", [4])
    KT_d = L.dscratch("KT1_d", [128, NALL], BF)
    VS_d = L.dscratch("VS1_d", [NALL, 128], BF)
    XR_d = L.dscratch("XR_d", [4, 128, NALL], F32)

    gq = P.sb([128, 64], F32, "gq", glob=True); gqs = P.sb([128, 64], F32, "gqs", glob=True)
    gk = P.sb([128, 64], F32, "gk", glob=True); gks = P.sb([128, 64], F32, "gks", glob=True)
    L.load_bc(gq, qg_d.ap, qg_d); L.load_bc(gk, kg_d.ap, kg_d)
    L.swap_gain(gq, gqs); L.swap_gain(gk, gks)
    gmix = P.sb([128, D], F32, "gmix", glob=True)
    L.load_bc(gmix, gmix_d.ap, gmix_d)

    yT = P.sb([128, 8, NOWN], BF, "yT", glob=True)
    gstack = ExitStack()
    gateT = T("gateT", gstack.enter_context(nc.sbuf_tensor("gateT_l1", [128, 4, OWN], BF)).ap())
    qstack = ExitStack()
    QT = T("QT", qstack.enter_context(nc.sbuf_tensor("QT_l1", [128, 2, 4, NOWN], BF)).ap())
    P.op("pool", lambda e: e.memset(QT[64:128, 0, :, :], 0.0), writes=[QT])
    P.op("pool", lambda e: e.memset(QT[0:64, 1, :, :], 0.0), writes=[QT])
    blocks = [(2 + 4 * i, 4) for i in range(4)]

    P.phase_begin()
    A1 = [L.make_A(L.mod_bc(0, 1, "A1"), gmix)]
    sh1 = [L.mod_bc(0, 0, "sh1")]
    Win = P.sb([128, 8, 1024], BF, "Win")
    for g in range(4):
        for n in range(2):
            h = n * 4 + g
            P.dma("pool", Win[:, :, g * 128 + n * 64:g * 128 + (n + 1) * 64],
                  win_d[:, h * 64:(h + 1) * 64].rearrange("(kc p) n -> p kc n", p=128), reads=[win_d], writes=[Win])
    P.dma("pool", Win[:, :, 512:1024], win_d[:, 768:1280].rearrange("(kc p) n -> p kc n", p=128), reads=[win_d], writes=[Win])
    xb = L.ring(1, [128, 4, D], F32, "xb"); csb = L.ring(2, [128, 4, 128], F32, "csb")
    hnr = L.ring(2, [128, D], BF, "hn"); jr = L.ring(2, [128, D], F32, "junk"); ssr = L.ring(2, [128, 2], F32, "ss")
    hTb = L.ring(1, [128, 8, 512], BF, "hTb")
    pTr = L.ring(2, [128, 8, 128], BF, "pT", psum=True)
    pur = L.ring(2, [128, 512], F32, "pu", psum=True)
    CTr = L.ring(2, [128, 64], F32, "CT"); STr = L.ring(2, [128, 64], F32, "ST")
    tmps = [tuple([P.sb([128, 512], F32, "sq"), P.sb([128, 512], F32, "qn"), P.sb([128, 512], F32, "t1"), P.sb([128, 8], F32, "ssq")]) for _ in range(2)]
    qbr = L.ring(2, [128, 512], BF, "qb")
    for bi, (t0, nt) in enumerate(blocks):
        N = nt * 128
        x = xb.next(); cs = csb.next(); hT = hTb.next()
        xb_ap, xb_t = xown_blk(t0, nt)
        P.dma("sp", x[:, 0:nt, :], xb_ap.rearrange("(t p) d -> p t d", p=128), reads=[xb_t], writes=[x])
        P.dma("sp", cs[:, 0:nt, :], csown[t0 * 128:(t0 + nt) * 128, :].rearrange("(t p) d -> p t d", p=128), reads=[csown], writes=[cs])
        for t in range(nt):
            hn = hnr.next(); junk = jr.next(); ss = ssr.next(); pT = pTr.next()
            L.norm_tile(x[:, t, :], x, A1[0], sh1[0], hn, junk, ss)
            L.transpose_cols(hn, 8, pT, lambda hT=hT, t=t: hT[:, :, t * 128:(t + 1) * 128], hT)
        for c in range(4):
            pu = pur.next()
            L.proj_feat(pu, hT, N, Win, 512 + c * 128)
            P.op("act", lambda e, pu=pu, c=c, N=N, t0=t0: e.activation(gateT[:, c, (t0 - 2) * 128:(t0 - 2) * 128 + N], pu[:, 0:N], AF.Gelu_apprx_tanh),
                 reads=[pu], writes=[gateT])
        recs = []
        for t in range(nt):
            tsl = slice(t * 128, (t + 1) * 128)
            gsl = slice((t0 + t) * 128, (t0 + t + 1) * 128)
            P.record_begin()
            pq = pur.next(); CT = CTr.next(); ST = STr.next(); qb = qbr.next(); pT = pTr.next()
            L.proj_tok(pq, hT, tsl, Win, 0, 512)
            P.mark()
            L.rope_tables(T_view(cs, cs[:, t, :]), gq, gqs, CT, ST)
            L.qk_post(pq, 8, CT, ST, qb, tmps[t % 2])
            for c4 in range(4):
                P.op("pe", lambda e, c4=c4, pT=pT, qb=qb: e.transpose(pT[:, c4, :], qb[:, c4 * 128:(c4 + 1) * 128], L.identb[:]),
                     reads=[qb, L.identb], writes=[pT], inc=(c4 == 3))
            P.op("dve", lambda e, pT=pT, gsl=gsl: e.tensor_copy(QT[0:64, 0, :, gsl], pT[0:64, 0:4, :]), reads=[pT], writes=[QT])
            P.op("act", lambda e, pT=pT, gsl=gsl: e.activation(QT[64:128, 1, :, gsl], pT[64:128, 0:4, :], AF.Identity), reads=[pT], writes=[QT])
            recs.append(P.record_end())
        P.replay_skewed(recs)
    P.phase_end()

    P.phase_begin()
    A1 = [L.make_A(L.mod_bc(j, 1, "A1"), gmix) for j in range(2)]
    sh1 = [L.mod_bc(j, 0, "sh1") for j in range(2)]
    Wk = P.sb([128, 8, 768], BF, "Wkvx")
    P.dma("pool", Wk[:, :, 0:256], win_d[:, 512:768].rearrange("(kc p) n -> p kc n", p=128), reads=[win_d], writes=[Wk])
    P.dma("pool", Wk[:, :, 256:768], win_d[:, 1280:1792].rearrange("(kc p) n -> p kc n", p=128), reads=[win_d], writes=[Wk])
    xr = L.ring(3, [128, D], F32, "xa"); csr = L.ring(3, [128, 128], F32, "csa")
    hnr = L.ring(2, [128, D], BF, "hn"); jr = L.ring(2, [128, D], F32, "junk"); ssr = L.ring(2, [128, 2], F32, "ss")
    hTr = L.ring(2, [128, 8, 128], BF, "hT")
    pTr = L.ring(2, [128, 8, 128], BF, "pT", psum=True)
    pkr = L.ring(2, [128, 512], F32, "pk", psum=True); pxr = L.ring(2, [128, 512], F32, "px", psum=True)
    pXr = L.ring(2, [128, 4, 128], F32, "pX", psum=True)
    CTr = L.ring(2, [128, 64], F32, "CT"); STr = L.ring(2, [128, 64], F32, "ST")
    tmps = [tuple([P.sb([128, 512], F32, "sq"), P.sb([128, 512], F32, "qn"), P.sb([128, 512], F32, "t1"), P.sb([128, 8], F32, "ssq")]) for _ in range(2)]
    kbr = L.ring(2, [128, 128], BF, "kb"); vbr = L.ring(2, [128, 128], BF, "vb"); ktr = L.ring(2, [128, 1, 128], BF, "kt")
    xsr = L.ring(2, [128, 512], F32, "xs"); xtr = L.ring(2, [128, 4, 128], F32, "xt")
    def tile_A(t):
        j = 1 if t < 2 else 0
        x = xr.next(); cs = csr.next(); hn = hnr.next(); junk = jr.next(); ss = ssr.next(); hT = hTr.next(); pT = pTr.next()
        xa_ap, xa_t = xall_tile(t)
        P.dma("sp", x[:], xa_ap, reads=[xa_t], writes=[x])
        P.dma("sp", cs[:], csall[t * 128:(t + 1) * 128, :], reads=[csall], writes=[cs])
        L.norm_tile(x[:], x, A1[j], sh1[j], hn, junk, ss)
        L.transpose_cols(hn, 8, pT, lambda hT=hT: hT[:], hT)
        pk = pkr.next(); px = pxr.next()
        L.proj_tok(pk, hT, slice(0, 128), Wk, 0, 256)
        L.proj_tok(px, hT, slice(0, 128), Wk, 256, 512)
        P.mark()
        vb = vbr.next()
        P.op("act", lambda e, vb=vb, pk=pk: e.activation(vb[:], pk[:, 128:256], AF.Identity), reads=[pk], writes=[vb])
        P.dma("pool", VS_d[t * 128:(t + 1) * 128, :], vb[:], reads=[vb], writes=[VS_d])
        CT = CTr.next(); ST = STr.next()
        L.rope_tables(cs, gk, gks, CT, ST)
        kb = kbr.next()
        L.qk_post(pk, 2, CT, ST, kb, tmps[t % 2])
        kt = ktr.next()
        L.transpose_cols(kb, 1, pT, lambda kt=kt: kt[:], kt, eng="dve")
        P.dma("pool", KT_d[:, t * 128:(t + 1) * 128], kt[:, 0, :], reads=[kt], writes=[KT_d])
        xs = xsr.next(); xt = xtr.next(); pX = pXr.next()
        P.op("act", lambda e, xs=xs, px=px: e.activation(xs[:], px[:], AF.Identity), reads=[px], writes=[xs])
        for c in range(4):
            P.op("pe", lambda e, c=c, pX=pX, xs=xs: e.transpose(pX[:, c, :], xs[:, c * 128:(c + 1) * 128], L.ident[:]),
                 reads=[xs, L.ident], writes=[pX], inc=(c == 3))
        P.op("dve", lambda e, xt=xt, pX=pX: e.tensor_copy(xt[:], pX[:]), reads=[pX], writes=[xt])
        P.dma("pool", XR_d[:, :, t * 128:(t + 1) * 128].rearrange("j p t -> p j t"), xt[:], reads=[xt], writes=[XR_d])

    recs = []
    for t in range(NT_ALL):
        P.record_begin(); tile_A(t); recs.append(P.record_end())
    P.replay_skewed(recs)
    P.phase_end()

    P.phase_begin()
    KT = P.sb([128, NALL], BF, "KT")
    for pc in range(6):
        P.dma("sp", KT[:, pc * 1408:(pc + 1) * 1408], KT_d[:, pc * 1408:(pc + 1) * 1408], reads=[KT_d], writes=[KT])
    Vn = [P.sb([128, NT_ALL, 128], BF, "Vn") for _ in range(2)]
    for n in range(2):
        P.op("pool", lambda e, n=n: e.memset(Vn[n][:, :, 64:128], 1.0), writes=[Vn[n]])
        for pc in range(11):
            P.dma("sp", Vn[n][:, pc * 6:(pc + 1) * 6, 0:64],
                  VS_d[pc * 768:(pc + 1) * 768, n * 64:(n + 1) * 64].rearrange("(t p) e -> p t e", p=128), reads=[VS_d], writes=[Vn[n]])
    spr = L.ring(4, [128, 512], F32, "sp", psum=True)
    accr = L.ring(2, [128, 512], F32, "acc", psum=True)
    pXo = L.ring(1, [128, 4, 128], F32, "pXo", psum=True)
    LOOK1 = 3
    pTo = P.ps([128, 4, 128], BF, "pTo")
    ptr_ = L.ring(6, [128, 512], BF, "pts")
    rzr = L.ring(2, [128, 4], F32, "rz")
    osr = L.ring(2, [128, 512], F32, "osb")
    yar = L.ring(2, [128, 512], BF, "yat")
    for qt in range(OWN // 128):
        q0 = LC + qt * 128
        yat = yar.next()
        for n in range(2):
            acc = accr.next()

            def emit_scores1(kt, n=n, q0=q0):
                sp_ = spr.next()
                P.op("pe", lambda e, n=n, sp_=sp_, kt=kt, q0=q0: e.matmul(
                    sp_[:].rearrange("p (g q) -> p g q", g=4), KT[:, kt * 128:(kt + 1) * 128],
                    QT[:, n, :, q0:q0 + 128], start=True, stop=True), reads=[KT, QT], writes=[sp_])
                return sp_

            pend_sc = [emit_scores1(i) for i in range(LOOK1)]
            for kt in range(NT_ALL):
                sp_ = pend_sc.pop(0)
                if kt + LOOK1 < NT_ALL:
                    pend_sc.append(emit_scores1(kt + LOOK1))
                pt = ptr_.next()
                P.op("act", lambda e, pt=pt, sp_=sp_: e.activation(pt[:], sp_[:], AF.Exp, scale=0.125), reads=[sp_], writes=[pt])
                P.op("pe", lambda e, pt=pt, n=n, kt=kt, acc=acc: e.matmul(acc[:], Vn[n][:, kt, :], pt[:], start=(kt == 0), stop=(kt == NT_ALL - 1)),
                     reads=[pt, Vn[n]], writes=[acc], inc=(kt == NT_ALL - 1))
            osb = osr.next(); pX = pXo.next(); rz = rzr.next()
            P.op("act", lambda e, osb=osb, acc=acc: e.activation(osb[:], acc[:], AF.Identity), reads=[acc], writes=[osb])
            for g in range(4):
                P.op("pe", lambda e, g=g, pX=pX, osb=osb: e.transpose(pX[:, g, :], osb[:, g * 128:(g + 1) * 128], L.ident[:]),
                     reads=[osb, L.ident], writes=[pX], inc=(g == 3))
            P.op("dve", lambda e, rz=rz, pX=pX: e.reciprocal(rz[:, 0:4], pX[:, :, 64]), reads=[pX], writes=[rz])
            for g in range(4):
                h = n * 4 + g
                P.op("act", lambda e, g=g, h=h, rz=rz, yat=yat, pX=pX: e.activation(yat[:, h * 64:(h + 1) * 64], pX[:, g, 0:64], AF.Identity, scale=rz[:, g:g + 1]),
                     reads=[pX, rz], writes=[yat])
        for c in range(4):
            P.op("pe", lambda e, c=c, yat=yat: e.transpose(pTo[:, c, :], yat[:, c * 128:(c + 1) * 128], L.identb[:]),
                 reads=[yat, L.identb], writes=[pTo], inc=(c == 3))
        P.op("act", lambda e, q0=q0: e.activation(yT[:, 0:4, q0:q0 + 128], pTo[:], AF.Identity), reads=[pTo], writes=[yT])
    P.phase_end()
    qstack.close()

    P.phase_begin()
    lp = P.sb([128, 4, 12], F32, "lp")
    P.dma("sp", lp[:], lrup_d[:, :, :], reads=[lrup_d], writes=[lp])
    sw = P.sb([128, 4], F32, "segw"); L.load_bc(sw, segw_d.ap, segw_d)
    nsp = P.sb([128, 4, 2], F32, "nsp")
    P.op("act", lambda e: e.activation(nsp[:], lp[:, :, 9:11], AF.Exp, scale=-1.0), reads=[lp], writes=[nsp])
    P.op("act", lambda e: e.activation(nsp[:], nsp[:], AF.Ln, bias=1.0), reads=[nsp], writes=[nsp])
    P.op("dve", lambda e: e.tensor_scalar(nsp[:], nsp[:], -8.0, None, ALU.mult), reads=[nsp], writes=[nsp])
    BD = P.sb([128, 16, 128], BF, "BD")
    P.op("dve", lambda e: e.memset(BD[:], 0.0), writes=[BD])
    for kind, wd in enumerate((wa_d, wx_d)):
        for d in range(2):
            for jj in range(4):
                for hh in range(2):
                    P.dma("pool", BD[hh * 64:(hh + 1) * 64, kind * 8 + d * 4 + jj, hh * 64:(hh + 1) * 64], wd.ap[d, 2 * jj + hh, :, :],
                          reads=[wd], writes=[BD])
    SEGL = 4096
    X = P.sb([128, NALL], F32, "Xr"); XD = P.sb([128, NALL], F32, "XD"); XDB = P.sb([128, NALL], BF, "XDB")
    Ab = T_view(X, X[:, 0:SEGL]); Bb = T_view(X, X[:, SEGL:2 * SEGL]); Hb = P.sb([128, SEGL], F32, "Hb")
    Ib = P.sb([128, SEGL], F32, "Ib")
    acc = P.sb([128, OWN], F32, "hacc")
    st = P.sb([128, 2], F32, "st")
    prr = L.ring(2, [128, 512], F32, "pr", psum=True); pir = L.ring(2, [128, 512], F32, "pi", psum=True)
    for jj in range(4):
        for pc in range(6):
            P.dma("sp", X[:, pc * 1408:(pc + 1) * 1408], XR_d[jj, :, pc * 1408:(pc + 1) * 1408], reads=[XR_d], writes=[X])
        P.op("dve", lambda e, jj=jj: e.tensor_scalar(XD[:], X[:], lp[:, jj, 2:3], lp[:, jj, 4:5], ALU.mult, ALU.add), reads=[X, lp], writes=[XD])
        for (lo, hi) in ((0, LC), (LC, NALL)):
            for tap, off in ((0, -2), (1, -1), (3, 1)):
                a = max(lo, lo - off); b = min(hi, hi - off)
                P.op("dve", lambda e, jj=jj, tap=tap, off=off, a=a, b=b: e.scalar_tensor_tensor(
                    XD[:, a:b], X[:, a + off:b + off], lp[:, jj, tap:tap + 1], XD[:, a:b], ALU.mult, ALU.add), reads=[X, lp, XD], writes=[XD])
        P.op("pool", lambda e: e.tensor_copy(XDB[:], XD[:]), reads=[XD], writes=[XDB])
        P.op("dve", lambda e: e.memset(acc[:], 0.0), writes=[acc])
        for d in range(2):
            segs = [(0, LC), (LC, LC + SEGL), (LC + SEGL, NALL)]
            order = segs if d == 0 else [segs[0], segs[2], segs[1]]
            for si, (lo, hi) in enumerate(order):
                n = hi - lo
                for b0 in range(0, n, 512):
                    bw = min(512, n - b0)
                    pr = prr.next(); pi_ = pir.next()
                    P.op("pe", lambda e, pr=pr, d=d, jj=jj, lo=lo, b0=b0, bw=bw: e.matmul(pr[:, 0:bw], BD[:, d * 4 + jj, :], XDB[:, lo + b0:lo + b0 + bw], start=True, stop=True),
                         reads=[BD, XDB], writes=[pr])
                    P.op("pe", lambda e, pi_=pi_, d=d, jj=jj, lo=lo, b0=b0, bw=bw: e.matmul(pi_[:, 0:bw], BD[:, 8 + d * 4 + jj, :], XDB[:, lo + b0:lo + b0 + bw], start=True, stop=True),
                         reads=[BD, XDB], writes=[pi_])
                    sl = slice(b0, b0 + bw)
                    P.op("act", lambda e, pr=pr, bw=bw, sl=sl, d=d, jj=jj: e.activation(Ab[:, sl], pr[:, 0:bw], AF.Sigmoid, bias=lp[:, jj, 5 + d:6 + d]), reads=[pr, lp], writes=[Ab])
                    P.op("act", lambda e, pi_=pi_, bw=bw, sl=sl, d=d, jj=jj: e.activation(Ib[:, sl], pi_[:, 0:bw], AF.Sigmoid, bias=lp[:, jj, 7 + d:8 + d]), reads=[pi_, lp], writes=[Ib])
                P.op("act", lambda e, n=n, d=d, jj=jj: e.activation(Ab[:, 0:n], Ab[:, 0:n], AF.Exp, scale=nsp[:, jj, d:d + 1]), reads=[Ab, nsp], writes=[Ab])
                P.op("dve", lambda e, n=n: e.tensor_tensor(Hb[:, 0:n], Ab[:, 0:n], Ab[:, 0:n], ALU.mult), reads=[Ab], writes=[Hb])
                P.op("act", lambda e, n=n: e.activation(Hb[:, 0:n], Hb[:, 0:n], AF.Sqrt, bias=1.0, scale=-1.0), reads=[Hb], writes=[Hb])
                P.op("dve", lambda e, n=n: e.tensor_tensor(Hb[:, 0:n], Hb[:, 0:n], Ib[:, 0:n], ALU.mult), reads=[Hb, Ib], writes=[Hb])
                P.op("pool", lambda e, n=n, lo=lo: e.tensor_tensor(Bb[:, 0:n], Hb[:, 0:n], XD[:, lo:lo + n], ALU.mult), reads=[Hb, XD], writes=[Bb])
                init = 0.0 if si == 0 else st[:, d:d + 1]
                if d == 0:
                    P.op("dve", lambda e, n=n, init=init: e.tensor_tensor_scan(Hb[:, 0:n], Ab[:, 0:n], Bb[:, 0:n], init, ALU.mult, ALU.add),
                         reads=[Ab, Bb, st], writes=[Hb])
                    P.op("dve", lambda e, n=n, d=d: e.tensor_copy(st[:, d:d + 1], Hb[:, n - 1:n]), reads=[Hb], writes=[st])
                else:
                    P.op("dve", lambda e, n=n, init=init: e.tensor_tensor_scan(Hb[:, 0:n][:, ::-1], Ab[:, 0:n][:, ::-1], Bb[:, 0:n][:, ::-1], init, ALU.mult, ALU.add),
                         reads=[Ab, Bb, st], writes=[Hb])
                    P.op("dve", lambda e, d=d: e.tensor_copy(st[:, d:d + 1], Hb[:, 0:1]), reads=[Hb], writes=[st])
                if lo >= LC:
                    s0 = (lo - LC) // OWN
                    for k in range(2):
                        P.op("dve", lambda e, k=k, s0=s0: e.scalar_tensor_tensor(acc[:], Hb[:, k * OWN:(k + 1) * OWN], sw[:, s0 + k:s0 + k + 1], acc[:], ALU.mult, ALU.add),
                             reads=[Hb, sw, acc], writes=[acc])
        P.op("dve", lambda e, jj=jj: e.tensor_tensor(yT[:, 4 + jj, LC:LC + OWN], acc[:], gateT[:, jj, :], ALU.mult), reads=[acc, gateT], writes=[yT])
    P.phase_end()
    gstack.close()

    gffn_d = L.din("norm_ffn_g" + sfx, [D])
    wout_d = L.din("w_out" + sfx, [D, D])
    rw_d = L.din("router_w", [D, 8]); rb_d = L.din("router_b", [8])
    w1_d = L.din("moe_w1", [8, D, FF]); w3_d = L.din("moe_w3", [8, D, FF]); w2_d = L.din("moe_w2", [8, FF, D])
    gffn = P.sb([128, D], F32, "gffn", glob=True)
    L.load_bc(gffn, gffn_d.ap, gffn_d)
    ffn_phase(L, xown_blk, xo_blk, blocks, yT, wout_d, gffn, w1_d, w3_d, w2_d, ctx_block=False, moe=(rw_d, rb_d, 8))
    P.barrier()
    P.flush()
    P.gstack.close()
    P.gstack = _outer


def build_fused():
    nc = bass.Bass("TRN2", target_bir_lowering=False)
    L = LK(nc)
    P = L.P
    xall = L.din("xall", [NALL, D])
    xown = L.din("xown", [NOWN, D])
    csall = L.din("csall", [NALL, 128])
    csown = L.din("csown", [NOWN, 128])
    ident_d = L.din("ident", [128, 128])
    cvec_d = L.din("cvec", [128, 16])
    xo = L.dout("xo", [OWN, D])
    xc1_d = L.dscratch("xc1_d", [LC, D])
    x1own_d = L.dscratch("x1own_d", [OWN, D])
    NCH = OWN // 256
    x1g_d = [L.dscratch(f"x1g{k}_d", [4 * 256, D]) for k in range(NCH)]
    L.load_ident(ident_d)
    modrows = []
    for lay in range(2):
        wmod_d = L.din(f"w_mod{lay}", [D, 6 * D]); bmod_d = L.din(f"b_mod{lay}", [6 * D])
        modrows.append(L.setup_common(cvec_d, wmod_d, bmod_d, f"l{lay}"))
    io0 = dict(csall=csall, csown=csown, ident_d=ident_d, cvec_d=cvec_d, sfx="0", modrow=modrows[0],
               xall_tile=lambda t: (xall[t * 128:(t + 1) * 128, :], xall),
               xown_blk=lambda t0, nt: (xown[t0 * 128:(t0 + nt) * 128, :], xown),
               xo_blk=lambda t0, nt: ((xc1_d[0:LC, :], xc1_d) if t0 == 0 else
                                      (x1own_d[(t0 - 2) * 128:(t0 - 2 + nt) * 128, :], x1own_d)))
    emit_l0(L, io0)
    for k in range(NCH):
        csem = nc.alloc_semaphore(f"ccsem{k}")
        waits = P._waits("pool", P._deps([x1own_d], [x1g_d[k]]), False)

        def emit_cc(eng, waits=waits, k=k, csem=csem):
            for (s_, v_) in waits:
                eng.wait_ge(s_, v_)
            eng.collective_compute("AllGather", ALU.bypass, replica_groups=[[0, 1, 2, 3], [4, 5, 6, 7]],
                                   ins=[x1own_d[k * 256:(k + 1) * 256, :].opt()], outs=[x1g_d[k].ap.opt()]).then_inc(csem)

        P.q["pool"].append(emit_cc)
        tok = (csem, 1, "cc")
        x1g_d[k].w = tok; x1g_d[k].r = {}
        x1own_d.r[csem.name] = tok

    def lat_tile(t):
        g = t - 2
        r, i = g // 16, g % 16
        k, h = i // 2, i % 2
        return (x1g_d[k][r * 256 + h * 128:r * 256 + (h + 1) * 128, :], x1g_d[k])

    io1 = dict(csall=csall, csown=csown, ident_d=ident_d, cvec_d=cvec_d, sfx="1", modrow=modrows[1],
               xall_tile=lambda t: ((xc1_d[t * 128:(t + 1) * 128, :], xc1_d) if t < 2 else lat_tile(t)),
               xown_blk=lambda t0, nt: (x1own_d[(t0 - 2) * 128:(t0 - 2 + nt) * 128, :], x1own_d),
               xo_blk=lambda t0, nt: (xo[(t0 - 2) * 128:(t0 - 2 + nt) * 128, :], xo))
    emit_l1(L, io1)
    P.finish([xo])
    return nc, L


_ROPE = None


def _cvec(cb, cctx):
    v = np.stack([cb, cctx], 1).astype(np.float32)
    return np.ascontiguousarray(v.reshape(8, 128, 2).transpose(1, 0, 2).reshape(128, 16))


def _lru_pack(inp):
    cw = np.asarray(inp["d_conv_w"])[0]; cb = np.asarray(inp["d_conv_b"])[0]
    ba = np.asarray(inp["d_ba"])[0]; bx = np.asarray(inp["d_bx"])[0]; lam = np.asarray(inp["d_lambda"])[0]
    cols = [cw[0], cw[1], cw[2], cw[3], cb, ba[0], ba[1], bx[0], bx[1], lam[0], lam[1], np.zeros(512, np.float32)]
    p = np.stack(cols, 1).astype(np.float32)
    return np.ascontiguousarray(p.reshape(4, 128, 12).transpose(1, 0, 2))


_NC = None


def kernel(**inputs):
    global _ROPE, _NC
    if _ROPE is None:
        _ROPE = _rope_table()
    inp = inputs
    nc, L = build_fused()
    x = np.asarray(inp["x"], np.float32); ctx = np.asarray(inp["ctx"], np.float32)
    ident = np.eye(128, dtype=np.float32)
    lru_p = _lru_pack(inp)
    g = lambda k, i=0: np.asarray(inp[k])[i]
    shared = {
        "ident": ident, "csall": _ROPE, "lru_p": lru_p,
        "w_mod0": g("w_mod", 0), "b_mod0": g("b_mod", 0), "norm_mix_g0": g("norm_mix_g", 0), "norm_ffn_g0": g("norm_ffn_g", 0),
        "w_out0": g("w_out", 0), "w_mod1": g("w_mod", 1), "b_mod1": g("b_mod", 1), "norm_mix_g1": g("norm_mix_g", 1),
        "norm_ffn_g1": g("norm_ffn_g", 1), "w_out1": g("w_out", 1),
        "e_w_in": g("e_w_in"), "a_norm_g": g("a_norm_g"), "a_ws": g("a_ws"), "a_bs": g("a_bs"),
        "b_qnorm_g": g("b_qnorm_g"), "b_knorm_g": g("b_knorm_g"), "b_lq1": g("b_lq1"), "b_lk1": g("b_lk1"),
        "b_lq2": g("b_lq2"), "b_lk2": g("b_lk2"), "b_subln_g": g("b_subln_g"),
        "ffn_w1": g("ffn_w1"), "ffn_w3": g("ffn_w3"), "ffn_w2": g("ffn_w2"),
        "o_w_in": g("o_w_in"), "c_qnorm_g": g("c_qnorm_g"), "c_knorm_g": g("c_knorm_g"),
        "d_wa": g("d_wa"), "d_wx": g("d_wx"), "router_w": g("router_w"), "router_b": g("router_b"),
        "moe_w1": g("moe_w1"), "moe_w3": g("moe_w3"), "moe_w2": g("moe_w2"),
    }
    shared = {k: np.ascontiguousarray(v, dtype=np.float32) for k, v in shared.items()}
    maps = []
    for core in range(8):
        b, s = core // 4, core % 4
        xall = np.concatenate([ctx[b], x[b]], 0)
        own = slice(LC + s * OWN, LC + (s + 1) * OWN)
        segw = np.zeros(4, np.float32); segw[s] = 1.0
        m = dict(shared)
        m.update({"xall": xall, "xown": np.concatenate([ctx[b], xall[own]], 0),
                  "csown": np.concatenate([_ROPE[:LC], _ROPE[own]], 0),
                  "cvec": _cvec(np.asarray(inp["c"])[b], np.asarray(inp["c_ctx"])), "segw": segw})
        maps.append({k: np.ascontiguousarray(m[k], dtype=np.float32) for k in L.inputs})
    res = run_bass_kernel_spmd(nc, maps, core_ids=list(range(8)))
    out = np.zeros_like(x)
    for core in range(8):
        b, s = core // 4, core % 4
        out[b, s * OWN:(s + 1) * OWN] = res.results[core]["xo"]
    return out.astype(np.float32)
```

```python
from contextlib import ExitStack

import numpy as np
import concourse.bass as bass
import concourse.mybir as mybir
from concourse.bass_utils import run_bass_kernel_spmd

F32 = mybir.dt.float32
BF = mybir.dt.bfloat16
AF = mybir.ActivationFunctionType
ALU = mybir.AluOpType
AX = mybir.AxisListType

D = 1024
SEQ = 8192
LC = 256
NALL = SEQ + LC
NT_ALL = NALL // 128
OWN = 2048
NOWN = OWN + LC
FF = 2816
NFC = FF // 128
EPS = 1e-6


class T:
    def __init__(self, name, ap):
        self.name = name
        self.ap = ap
        self.w = None
        self.r = {}

    def __getitem__(self, k):
        return self.ap[k]


class Prog:
    ENG = ("pe", "act", "dve", "pool", "sp")
    SEM_ROLL = 30000
    N_DMA_SEMS = 32

    def __init__(self, nc):
        self.nc = nc
        self.q = {e: [] for e in self.ENG}
        self.sem = {}
        self.cnt = {}
        self.nsem = 0
        self.retired = {}
        self._rec = None
        for e in self.ENG:
            self._new_sem(e)
        self.dsem = []
        for i in range(self.N_DMA_SEMS):
            s = nc.alloc_semaphore(f"dma{i}")
            self.dsem.append([s, 0])
        self.dnext = 0
        self.seen = {e: {} for e in self.ENG}
        self.pend = {e: False for e in self.ENG}
        self.ninst = 0
        self.gstack = ExitStack()
        self.stack = None
        self.uid = 0

    def _new_sem(self, e):
        if e in self.sem and self.cnt.get(e, 0) > 0:
            self.retired.setdefault(e, []).append((self.sem[e], self.cnt[e], e))
        self.nsem += 1
        self.sem[e] = self.nc.alloc_semaphore(f"s_{e}_{self.nsem}")
        self.cnt[e] = 0

    def _deps(self, reads, writes):
        deps = []
        for t in reads:
            if t.w is not None:
                deps.append(t.w)
        for t in writes:
            if t.w is not None:
                deps.append(t.w)
            deps.extend(t.r.values())
        return deps

    def _waits(self, e, deps, same_ok):
        out = {}
        for (s, v, src) in deps:
            if src == e and same_ok:
                continue
            key = s.name
            if self.seen[e].get(key, 0) >= v:
                continue
            if key not in out or out[key][1] < v:
                out[key] = (s, v)
        for key, (s, v) in out.items():
            self.seen[e][key] = v
        return list(out.values())

    def _mark(self, tok, reads, writes):
        s, v, src = tok
        key = s.name
        for t in reads:
            old = t.r.get(key)
            if old is None or old[1] < v:
                t.r[key] = tok
        for t in writes:
            t.w = tok
            t.r = {}

    def record_begin(self):
        self._rec = []

    def record_end(self):
        r, self._rec = self._rec, None
        return r

    def mark(self):
        if self._rec is not None:
            self._rec.append(("mark", (), {}))

    def _replay(self, ops):
        for kind, a, kw in ops:
            if kind == "op":
                self.op(*a, **kw)
            elif kind == "dma":
                self.dma(*a, **kw)

    def replay_skewed(self, recs):
        split = []
        for r in recs:
            k = next(i for i, o in enumerate(r) if o[0] == "mark")
            split.append((r[:k], r[k + 1:]))
        n = len(split)
        self._replay(split[0][0])
        for t in range(n):
            if t + 1 < n:
                self._replay(split[t + 1][0])
            self._replay(split[t][1])

    def op(self, e, fn, reads=(), writes=(), inc=True, same_ok=None):
        if getattr(self, "_rec", None) is not None:
            self._rec.append(("op", (e, fn), dict(reads=reads, writes=writes, inc=inc, same_ok=same_ok)))
            return None
        if same_ok is None:
            same_ok = (e == "pe")
        waits = self._waits(e, self._deps(reads, writes), same_ok)
        if self.cnt[e] >= self.SEM_ROLL and not self.pend[e]:
            self._new_sem(e)
        if inc:
            self.cnt[e] += 1
            tok = (self.sem[e], self.cnt[e], e)
            self.pend[e] = False
        else:
            tok = (self.sem[e], self.cnt[e] + 1, e)
            self.pend[e] = True
        sem = self.sem[e]

        def emit(eng, fn=fn, waits=waits, inc=inc, sem=sem):
            for (s, v) in waits:
                eng.wait_ge(s, v)
            ins = fn(eng)
            if inc:
                ins.then_inc(sem, 1)

        self.q[e].append(emit)
        self._mark(tok, reads, writes)
        self.ninst += 1
        return tok

    def dma(self, e, out_ap, in_ap, reads=(), writes=(), **kw):
        if getattr(self, "_rec", None) is not None:
            self._rec.append(("dma", (e, out_ap, in_ap), dict(reads=reads, writes=writes, **kw)))
            return None
        d = self.dsem[self.dnext]
        self.dnext = (self.dnext + 1) % self.N_DMA_SEMS
        s, prev = d
        deps = self._deps(reads, writes)
        if prev > 0:
            deps.append((s, prev, "dma"))
        waits = self._waits(e, deps, False)
        d[1] = prev + 16
        tok = (s, prev + 16, "dma")

        def emit(eng, waits=waits, s=s):
            for (ws, v) in waits:
                eng.wait_ge(ws, v)
            eng.dma_start(out=out_ap, in_=in_ap, **kw).then_inc(s, 16)

        self.q[e].append(emit)
        self._mark(tok, reads, writes)
        self.ninst += 1
        return tok

    def wait_all(self, e, toks):
        waits = self._waits(e, list(toks), False)

        def emit(eng, waits=waits):
            for (s, v) in waits:
                eng.wait_ge(s, v)

        self.q[e].append(emit)

    def barrier(self):
        toks = []
        for e in self.ENG:
            if self.cnt[e] > 0:
                toks.append((self.sem[e], self.cnt[e], e))
            elif self.retired.get(e):
                toks.append(self.retired[e][-1])
        toks += [(s, v, "dma") for s, v in self.dsem if v > 0]
        for e in self.ENG:
            assert not self.pend[e]
            self.wait_all(e, toks)

    def phase_begin(self):
        self.stack = ExitStack()

    def flush(self):
        if not any(self.q.values()):
            return
        q = self.q
        self.q = {e: [] for e in self.ENG}
        nc = self.nc
        with nc.Block() as block:
            @block.tensor
            def _(eng):
                for f in q["pe"]:
                    f(eng)

            @block.scalar
            def _(eng):
                for f in q["act"]:
                    f(eng)

            @block.vector
            def _(eng):
                for f in q["dve"]:
                    f(eng)

            @block.gpsimd
            def _(eng):
                for f in q["pool"]:
                    f(eng)

            @block.sync
            def _(eng):
                for f in q["sp"]:
                    f(eng)

    def phase_end(self):
        self.barrier()
        self.flush()
        self.stack.close()
        self.stack = None

    def sb(self, shape, dt=F32, name="t", glob=False):
        self.uid += 1
        nm = f"{name}_{self.uid}"
        st = self.gstack if (glob or self.stack is None) else self.stack
        h = st.enter_context(self.nc.sbuf_tensor(nm, list(shape), dt))
        return T(nm, h.ap())

    def ps(self, shape, dt=F32, name="p", glob=False):
        self.uid += 1
        nm = f"{name}_{self.uid}"
        st = self.gstack if (glob or self.stack is None) else self.stack
        h = st.enter_context(self.nc.psum_tensor(nm, list(shape), dt))
        return T(nm, h.ap())

    def finish(self, final_tiles):
        toks = [t.w for t in final_tiles if t.w is not None]
        self.wait_all("sp", toks)
        self.barrier()
        self.flush()
        self.gstack.close()


class Ring:
    def __init__(self, tiles):
        self.tiles = tiles
        self.i = 0

    def next(self):
        t = self.tiles[self.i % len(self.tiles)]
        self.i += 1
        return t


class LK:
    def __init__(self, nc):
        self.nc = nc
        self.P = Prog(nc)

    def din(self, name, shape, dt=F32):
        if not hasattr(self, "inputs"):
            self.inputs = []
        self.inputs.append(name)
        return T(name, self.nc.dram_tensor(name, list(shape), dt, kind="ExternalInput").ap())

    def dout(self, name, shape, dt=F32):
        return T(name, self.nc.dram_tensor(name, list(shape), dt, kind="ExternalOutput").ap())

    def dscratch(self, name, shape, dt=F32):
        return T(name, self.nc.dram_tensor(name, list(shape), dt, kind="Internal").ap())

    def ring(self, n, shape, dt=F32, name="r", psum=False):
        P = self.P
        return Ring([(P.ps if psum else P.sb)(shape, dt, name) for _ in range(n)])

    def load_ident(self, ident_d):
        P = self.P
        self.ident = P.sb([128, 128], F32, "ident", glob=True)
        self.identb = P.sb([128, 128], BF, "identb", glob=True)
        P.dma("sp", self.ident[:], ident_d[:, :], reads=[ident_d], writes=[self.ident])
        P.op("dve", lambda e: e.tensor_copy(self.identb[:], self.ident[:]), reads=[self.ident], writes=[self.identb])
        self.epsT = P.sb([128, 1], F32, "epsT", glob=True)
        P.op("dve", lambda e: e.memset(self.epsT[:], EPS), writes=[self.epsT])

    def setup_common(self, cvec_d, wmod_d, bmod_d, tag=""):
        P = self.P
        modrow_d = self.dscratch("modrow_d" + tag, [2, 6 * D])
        P.phase_begin()
        cv = P.sb([128, 16], F32, "cv")
        sc = P.sb([128, 16], F32, "sc")
        bm = P.sb([2, 6 * D], F32, "bm")
        mr = P.sb([2, 6 * D], F32, "mr")
        P.dma("sp", cv[:], cvec_d[:, :], reads=[cvec_d], writes=[cv])
        for j in range(2):
            P.dma("sp", bm[j:j + 1, :], bmod_d.ap.unsqueeze(0), reads=[bmod_d], writes=[bm])
        P.op("act", lambda e: e.activation(sc[:], cv[:], AF.Silu), reads=[cv], writes=[sc])
        wms = self.ring(2, [128, 8, 512], F32, "wm")
        pms = self.ring(2, [128, 512], F32, "pm", psum=True)
        for pc in range(12):
            wm = wms.next()
            pm = pms.next()
            P.dma("sp", wm[:], wmod_d[:, pc * 512:(pc + 1) * 512].rearrange("(kc p) n -> p kc n", p=128),
                  reads=[wmod_d], writes=[wm])
            for kc in range(8):
                P.op("pe", lambda e, kc=kc, wm=wm, pm=pm: e.matmul(pm[0:2, :], sc[:, kc * 2:(kc + 1) * 2], wm[:, kc, :],
                                                                 start=(kc == 0), stop=(kc == 7)),
                     reads=[sc, wm], writes=[pm], inc=(kc == 7))
            P.op("dve", lambda e, pc=pc, pm=pm: e.tensor_tensor(mr[0:2, pc * 512:(pc + 1) * 512], pm[0:2, :],
                                                              bm[0:2, pc * 512:(pc + 1) * 512], ALU.add),
                 reads=[pm, bm], writes=[mr])
        P.dma("sp", modrow_d[:, :], mr[:], reads=[mr], writes=[modrow_d])
        P.phase_end()
        return modrow_d

    def load_bc(self, dst, src_row_ap, src_t):
        self.P.dma("sp", dst[:], src_row_ap.partition_broadcast(128), reads=[src_t], writes=[dst])

    def mod_bc(self, j, idx, name):
        t = self.P.sb([128, D], F32, name)
        self.load_bc(t, self.modrow_d.ap[j, idx * D:(idx + 1) * D], self.modrow_d)
        return t

    def make_A(self, sc_t, g_t):
        self.P.op("dve", lambda e: e.scalar_tensor_tensor(sc_t[:], sc_t[:], 1.0, g_t[:], ALU.add, ALU.mult),
                  reads=[sc_t, g_t], writes=[sc_t])
        return sc_t

    def rstd(self, ss, n, dim):
        P = self.P
        P.op("act", lambda e: e.activation(ss[:, 0:n], ss[:, 0:n], AF.Sqrt, bias=self.epsT[:, 0:1], scale=1.0 / dim),
             reads=[ss, self.epsT], writes=[ss])
        P.op("dve", lambda e: e.reciprocal(ss[:, 0:n], ss[:, 0:n]), reads=[ss], writes=[ss])

    def norm_tile(self, x_ap, x_t, A_bc, sh_bc, hn, junk, ss):
        P = self.P
        P.op("act", lambda e: e.activation(junk[:], x_ap, AF.Square, accum_out=ss[:, 0:1]),
             reads=[x_t], writes=[junk, ss])
        self.rstd(ss, 1, D)
        P.op("dve", lambda e: e.scalar_tensor_tensor(junk[:], x_ap, ss[:, 0:1], A_bc[:], ALU.mult, ALU.mult),
             reads=[x_t, ss, A_bc], writes=[junk])
        P.op("pool", lambda e: e.tensor_tensor(hn[:], junk[:], sh_bc[:], ALU.add), reads=[junk, sh_bc], writes=[hn])

    def transpose_cols(self, src, ncol_chunks, pT, dst_ap_fn, dst_t, eng="act"):
        P = self.P
        for c in range(ncol_chunks):
            P.op("pe", lambda e, c=c: e.transpose(pT[:, c, :], src[:, c * 128:(c + 1) * 128], self.identb[:]),
                 reads=[src, self.identb], writes=[pT], inc=(c == ncol_chunks - 1))
        if eng == "act":
            P.op("act", lambda e: e.activation(dst_ap_fn(), pT[:, 0:ncol_chunks, :], AF.Identity), reads=[pT], writes=[dst_t])
        else:
            P.op("dve", lambda e: e.tensor_copy(dst_ap_fn(), pT[:, 0:ncol_chunks, :]), reads=[pT], writes=[dst_t])

    def proj_tok(self, ps, hT, tsl, W, c0, ncols):
        for kc in range(8):
            self.P.op("pe", lambda e, kc=kc: e.matmul(ps[:, 0:ncols], hT[:, kc, tsl], W[:, kc, c0:c0 + ncols],
                                                     start=(kc == 0), stop=(kc == 7)),
                      reads=[hT, W], writes=[ps], inc=(kc == 7))

    def proj_feat(self, ps, hT, n, W, c0):
        for kc in range(8):
            self.P.op("pe", lambda e, kc=kc: e.matmul(ps[:, 0:n], W[:, kc, c0:c0 + 128], hT[:, kc, 0:n],
                                                     start=(kc == 0), stop=(kc == 7)),
                      reads=[hT, W], writes=[ps], inc=(kc == 7))

    def rope_tables(self, cs, g_bc, gs_bc, CT, ST):
        P = self.P
        P.op("pool", lambda e: e.tensor_tensor(CT[:], cs[:, 0:64], g_bc[:], ALU.mult), reads=[cs, g_bc], writes=[CT])
        P.op("pool", lambda e: e.tensor_tensor(ST[:], cs[:, 64:128], gs_bc[:], ALU.mult), reads=[cs, gs_bc], writes=[ST])

    def swap_gain(self, g_bc, gs_bc):
        P = self.P
        for a in range(2):
            for b in range(2):
                P.op("dve", lambda e, a=a, b=b: e.tensor_copy(gs_bc[:, a * 32 + b * 16:a * 32 + b * 16 + 16],
                                                              g_bc[:, a * 32 + (1 - b) * 16:a * 32 + (1 - b) * 16 + 16]),
                     reads=[g_bc], writes=[gs_bc])

    def qk_post(self, src, H, CT, ST, out, tmp):
        P = self.P
        sq, qn, t1, ssq = tmp
        n = H * 64
        P.op("act", lambda e: e.activation(sq[:, 0:n], src[:, 0:n], AF.Square), reads=[src], writes=[sq])
        P.op("dve", lambda e: e.tensor_reduce(ssq[:, 0:H], sq[:, 0:n].rearrange("p (h d) -> p h d", d=64), AX.X, ALU.add),
             reads=[sq], writes=[ssq])
        self.rstd(ssq, H, 64)
        P.op("dve", lambda e: e.tensor_tensor(qn[:, 0:n].rearrange("p (h d) -> p h d", d=64),
                                              src[:, 0:n].rearrange("p (h d) -> p h d", d=64),
                                              ssq[:, 0:H].unsqueeze(2).to_broadcast([128, H, 64]), ALU.mult),
             reads=[src, ssq], writes=[qn])
        P.op("dve", lambda e: e.tensor_tensor(t1[:, 0:n].rearrange("p (h d) -> p h d", d=64),
                                              qn[:, 0:n].rearrange("p (h d) -> p h d", d=64),
                                              CT[:].unsqueeze(1).to_broadcast([128, H, 64]), ALU.mult),
             reads=[qn, CT], writes=[t1])
        for b in range(2):
            P.op("dve", lambda e, b=b: e.tensor_tensor(
                sq[:, 0:n].rearrange("p (h a b c) -> p h a b c", a=2, b=2, c=16)[:, :, :, b, :],
                qn[:, 0:n].rearrange("p (h a b c) -> p h a b c", a=2, b=2, c=16)[:, :, :, 1 - b, :],
                ST[:].rearrange("p (a b c) -> p a b c", a=2, b=2, c=16)[:, :, b, :].unsqueeze(1).to_broadcast([128, H, 2, 16]),
                ALU.mult), reads=[qn, ST], writes=[sq])
        P.op("pool", lambda e: e.tensor_tensor(out[:, 0:n], t1[:, 0:n], sq[:, 0:n], ALU.add), reads=[t1, sq], writes=[out])


def _rope_table():
    half = 16
    freqs = 10000.0 ** (-np.arange(half, dtype=np.float32) / half)
    t = np.arange(SEQ)
    row = (t // 64).astype(np.float32)
    col = (t % 64).astype(np.float32)
    ar = row[:, None] * freqs[None, :]
    ac = col[:, None] * freqs[None, :]
    C = np.concatenate([np.cos(ar), np.cos(ar), np.cos(ac), np.cos(ac)], 1)
    S = np.concatenate([-np.sin(ar), np.sin(ar), -np.sin(ac), np.sin(ac)], 1)
    lat = np.concatenate([C, S], 1).astype(np.float32)
    ctx = np.concatenate([np.ones((LC, 64), np.float32), np.zeros((LC, 64), np.float32)], 1)
    return np.concatenate([ctx, lat], 0)


STOP = None


def emit_l0(L, io):
    nc = L.nc
    P = L.P
    _outer = P.gstack
    P.gstack = ExitStack()
    xall_tile = io["xall_tile"]; xown_blk = io["xown_blk"]; xo_blk = io["xo_blk"]
    csall = io["csall"]; csown = io["csown"]; ident_d = io["ident_d"]; cvec_d = io["cvec_d"]
    sfx = io["sfx"]
    L.modrow_d = io["modrow"]
    gmix_d = L.din("norm_mix_g" + sfx, [D])
    gffn_d = L.din("norm_ffn_g" + sfx, [D])
    wout_d = L.din("w_out" + sfx, [D, D])
    win_d = L.din("e_w_in", [D, 2560])
    anorm_d = L.din("a_norm_g", [512])
    aws_d = L.din("a_ws", [8, 128, 128])
    abs_d = L.din("a_bs", [8, 128])
    qg_d = L.din("b_qnorm_g", [64])
    kg_d = L.din("b_knorm_g", [64])
    lq1_d = L.din("b_lq1", [64]); lk1_d = L.din("b_lk1", [64]); lq2_d = L.din("b_lq2", [64]); lk2_d = L.din("b_lk2", [64])
    sub_d = L.din("b_subln_g", [128])
    w1_d = L.din("ffn_w1", [D, FF]); w3_d = L.din("ffn_w3", [D, FF]); w2_d = L.din("ffn_w2", [FF, D])
    KT_d = L.dscratch("KT_d", [4, 128, NALL], BF)
    VS_d = L.dscratch("VS_d", [NALL, 512], BF)
    LAM_INIT = 0.2


    gq = P.sb([128, 64], F32, "gq", glob=True); gqs = P.sb([128, 64], F32, "gqs", glob=True)
    gk = P.sb([128, 64], F32, "gk", glob=True); gks = P.sb([128, 64], F32, "gks", glob=True)
    L.load_bc(gq, qg_d.ap, qg_d); L.load_bc(gk, kg_d.ap, kg_d)
    L.swap_gain(gq, gqs); L.swap_gain(gk, gks)
    neglam = P.sb([128, 1], F32, "neglam", glob=True)
    P.phase_begin()
    lt = [P.sb([128, 64], F32, "l") for _ in range(4)]
    for t_, d_ in zip(lt, (lq1_d, lk1_d, lq2_d, lk2_d)):
        L.load_bc(t_, d_.ap, d_)
    e12 = P.sb([128, 2], F32, "e12")
    for i in range(2):
        P.op("dve", lambda e, i=i: e.tensor_tensor(lt[2 * i][:], lt[2 * i][:], lt[2 * i + 1][:], ALU.mult), reads=[lt[2 * i], lt[2 * i + 1]], writes=[lt[2 * i]])
        P.op("dve", lambda e, i=i: e.tensor_reduce(e12[:, i:i + 1], lt[2 * i][:], AX.X, ALU.add), reads=[lt[2 * i]], writes=[e12])
    P.op("act", lambda e: e.activation(e12[:], e12[:], AF.Exp), reads=[e12], writes=[e12])
    P.op("dve", lambda e: e.tensor_tensor(neglam[:], e12[:, 1:2], e12[:, 0:1], ALU.subtract), reads=[e12], writes=[neglam])
    P.op("dve", lambda e: e.tensor_scalar(neglam[:], neglam[:], -LAM_INIT, None, ALU.add), reads=[neglam], writes=[neglam])
    P.phase_end()

    gmix = P.sb([128, D], F32, "gmix", glob=True)
    gffn = P.sb([128, D], F32, "gffn", glob=True)
    L.load_bc(gmix, gmix_d.ap, gmix_d)
    L.load_bc(gffn, gffn_d.ap, gffn_d)

    P.phase_begin()
    A1 = [L.make_A(L.mod_bc(j, 1, "A1"), gmix) for j in range(2)]
    sh1 = [L.mod_bc(j, 0, "sh1") for j in range(2)]
    Wkv = P.sb([128, 8, 1024], BF, "Wkv")
    for h in range(2):
        P.dma("pool", Wkv[:, :, h * 512:(h + 1) * 512],
              win_d[:, 1536 + h * 512:1536 + (h + 1) * 512].rearrange("(kc p) n -> p kc n", p=128), reads=[win_d], writes=[Wkv])
    xr = L.ring(3, [128, D], F32, "xa"); csr = L.ring(3, [128, 128], F32, "csa")
    hnr = L.ring(2, [128, D], BF, "hn"); jr = L.ring(2, [128, D], F32, "junk"); ssr = L.ring(2, [128, 2], F32, "ss")
    hTr = L.ring(2, [128, 8, 128], BF, "hT")
    pTr = L.ring(2, [128, 8, 128], BF, "pT", psum=True)
    pkr = L.ring(2, [128, 512], F32, "pk", psum=True); pvr = L.ring(2, [128, 512], F32, "pv", psum=True)
    CTr = L.ring(2, [128, 64], F32, "CT"); STr = L.ring(2, [128, 64], F32, "ST")
    tmps = [tuple([P.sb([128, 512], F32, "sq"), P.sb([128, 512], F32, "qn"), P.sb([128, 512], F32, "t1"), P.sb([128, 8], F32, "ssq")]) for _ in range(2)]
    kbr = L.ring(2, [128, 512], BF, "kb"); vbr = L.ring(2, [128, 512], BF, "vb"); ktr = L.ring(2, [128, 4, 128], BF, "kt")
    def tile_A(t):
        j = 1 if t < 2 else 0
        x = xr.next(); cs = csr.next(); hn = hnr.next(); junk = jr.next(); ss = ssr.next(); hT = hTr.next(); pT = pTr.next()
        xa_ap, xa_t = xall_tile(t)
        P.dma("sp", x[:], xa_ap, reads=[xa_t], writes=[x])
        P.dma("sp", cs[:], csall[t * 128:(t + 1) * 128, :], reads=[csall], writes=[cs])
        L.norm_tile(x[:], x, A1[j], sh1[j], hn, junk, ss)
        L.transpose_cols(hn, 8, pT, lambda hT=hT: hT[:], hT)
        pk = pkr.next(); pv = pvr.next()
        L.proj_tok(pk, hT, slice(0, 128), Wkv, 0, 512)
        L.proj_tok(pv, hT, slice(0, 128), Wkv, 512, 512)
        P.mark()
        vb = vbr.next()
        P.op("act", lambda e, vb=vb, pv=pv: e.activation(vb[:], pv[:], AF.Identity), reads=[pv], writes=[vb])
        P.dma("pool", VS_d[t * 128:(t + 1) * 128, :], vb[:], reads=[vb], writes=[VS_d])
        CT = CTr.next(); ST = STr.next()
        L.rope_tables(cs, gk, gks, CT, ST)
        kb = kbr.next()
        L.qk_post(pk, 8, CT, ST, kb, tmps[t % 2])
        kt = ktr.next()
        L.transpose_cols(kb, 4, pT, lambda kt=kt: kt[:], kt, eng="dve")
        P.dma("pool", KT_d[:, :, t * 128:(t + 1) * 128].rearrange("h p t -> p h t"), kt[:], reads=[kt], writes=[KT_d])

    recs = []
    for t in range(NT_ALL):
        P.record_begin(); tile_A(t); recs.append(P.record_end())
    P.replay_skewed(recs)
    P.phase_end()

    yT = P.sb([128, 8, NOWN], BF, "yT", glob=True)
    qstack = ExitStack()
    QT = T("QT", qstack.enter_context(nc.sbuf_tensor("QT", [128, 2, 4, NOWN], BF)).ap())
    P.op("pool", lambda e: e.memset(QT[64:128, 0, :, :], 0.0), writes=[QT])
    P.op("pool", lambda e: e.memset(QT[0:64, 1, :, :], 0.0), writes=[QT])
    blocks = [(0, 2)] + [(2 + 4 * i, 4) for i in range(4)]

    P.phase_begin()
    A1 = [L.make_A(L.mod_bc(j, 1, "A1"), gmix) for j in range(2)]
    sh1 = [L.mod_bc(j, 0, "sh1") for j in range(2)]
    Win = P.sb([128, 8, 1536], BF, "Win")
    for h in range(3):
        P.dma("pool", Win[:, :, h * 512:(h + 1) * 512], win_d[:, h * 512:(h + 1) * 512].rearrange("(kc p) n -> p kc n", p=128),
              reads=[win_d], writes=[Win])
    anorm = P.sb([128, 512], F32, "anorm"); L.load_bc(anorm, anorm_d.ap, anorm_d)
    wsT = P.sb([128, 8, 128], BF, "wsT")
    wsr = P.sb([128, 8, 128], F32, "wsr")
    P.dma("sp", wsr[:], aws_d.ap.rearrange("g p q -> p g q"), reads=[aws_d], writes=[wsr])
    wsb = P.sb([128, 8, 128], BF, "wsb")
    P.op("dve", lambda e: e.tensor_copy(wsb[:], wsr[:]), reads=[wsr], writes=[wsb])
    pTr = L.ring(2, [128, 8, 128], BF, "pT", psum=True)
    pTw = pTr.next()
    for g in range(8):
        P.op("pe", lambda e, g=g: e.transpose(pTw[:, g, :], wsb[:, g, :], L.identb[:]), reads=[wsb, L.identb], writes=[pTw], inc=(g == 7))
    P.op("dve", lambda e: e.tensor_copy(wsT[:], pTw[:]), reads=[pTw], writes=[wsT])
    bsT = P.sb([128, 4, 128], F32, "bsT")
    for g in range(8):
        P.dma("sp", bsT[(g % 2) * 64:(g % 2) * 64 + 64, g // 2, :], abs_d.ap[g, :].partition_broadcast(64), reads=[abs_d], writes=[bsT])
    xb = L.ring(1, [128, 4, D], F32, "xb"); csb = L.ring(2, [128, 4, 128], F32, "csb")
    hnr = L.ring(2, [128, D], BF, "hn"); jr = L.ring(2, [128, D], F32, "junk"); ssr = L.ring(2, [128, 2], F32, "ss")
    hTb = L.ring(1, [128, 8, 512], BF, "hTb")
    uTr = L.ring(1, [128, 4, 512], F32, "uT")
    pur = L.ring(2, [128, 512], F32, "pu", psum=True)
    pvr = L.ring(2, [128, 512], F32, "pv", psum=True)
    pmr = L.ring(2, [128, 128], F32, "pm", psum=True)
    gvr = L.ring(2, [128, 512], F32, "gv"); vbr = L.ring(2, [128, 512], BF, "vb"); tmr = L.ring(2, [128, 128], F32, "tm")
    CTr = L.ring(2, [128, 64], F32, "CT"); STr = L.ring(2, [128, 64], F32, "ST")
    tmps = [tuple([P.sb([128, 512], F32, "sq"), P.sb([128, 512], F32, "qn"), P.sb([128, 512], F32, "t1"), P.sb([128, 8], F32, "ssq")]) for _ in range(2)]
    qbr = L.ring(2, [128, 512], BF, "qb")
    for bi, (t0, nt) in enumerate(blocks):
        j = 1 if bi == 0 else 0
        N = nt * 128
        x = xb.next(); cs = csb.next(); hT = hTb.next(); uT = uTr.next()
        xb_ap, xb_t = xown_blk(t0, nt)
        P.dma("sp", x[:, 0:nt, :], xb_ap.rearrange("(t p) d -> p t d", p=128), reads=[xb_t], writes=[x])
        P.dma("sp", cs[:, 0:nt, :], csown[t0 * 128:(t0 + nt) * 128, :].rearrange("(t p) d -> p t d", p=128), reads=[csown], writes=[cs])
        for t in range(nt):
            hn = hnr.next(); junk = jr.next(); ss = ssr.next(); pT = pTr.next()
            L.norm_tile(x[:, t, :], x, A1[j], sh1[j], hn, junk, ss)
            L.transpose_cols(hn, 8, pT, lambda hT=hT, t=t: hT[:, :, t * 128:(t + 1) * 128], hT)
        for c in range(4):
            pu = pur.next()
            L.proj_feat(pu, hT, N, Win, c * 128)
            P.op("act", lambda e, pu=pu, uT=uT, c=c, N=N: e.activation(uT[:, c, 0:N], pu[:, 0:N], AF.Gelu_apprx_tanh), reads=[pu], writes=[uT])
        recs = []
        for t in range(nt):
            tsl = slice(t * 128, (t + 1) * 128)
            gsl = slice((t0 + t) * 128, (t0 + t + 1) * 128)
            P.record_begin()
            pv = pvr.next(); gv = gvr.next(); vb = vbr.next(); junk = jr.next(); ss = ssr.next()
            L.proj_tok(pv, hT, tsl, Win, 512, 512)
            P.op("act", lambda e, gv=gv, pv=pv: e.activation(gv[:], pv[:], AF.Gelu_apprx_tanh), reads=[pv], writes=[gv])
            P.op("act", lambda e, gv=gv, junk=junk, ss=ss: e.activation(junk[:, 0:512], gv[:], AF.Square, accum_out=ss[:, 0:1]), reads=[gv], writes=[junk, ss])
            L.rstd(ss, 1, 512)
            P.op("dve", lambda e, vb=vb, gv=gv, ss=ss: e.scalar_tensor_tensor(vb[:], gv[:], ss[:, 0:1], anorm[:], ALU.mult, ALU.mult),
                 reads=[gv, ss, anorm], writes=[vb])
            for jp in range(4):
                pm = pmr.next(); tm = tmr.next()
                for hh in range(2):
                    g = 2 * jp + hh
                    P.op("pe", lambda e, pm=pm, vb=vb, g=g, hh=hh: e.matmul(pm[hh * 64:(hh + 1) * 64, :], vb[:, g * 64:(g + 1) * 64], wsT[:, g, :],
                                                                         start=True, stop=True), reads=[vb, wsT], writes=[pm], inc=(hh == 1))
                P.op("dve", lambda e, pm=pm, tm=tm, jp=jp: e.tensor_tensor(tm[:], pm[:], bsT[:, jp, :], ALU.add), reads=[pm, bsT], writes=[tm])
                P.op("pool", lambda e, tm=tm, jp=jp, uT=uT, tsl=tsl, gsl=gsl: e.tensor_tensor(yT[:, jp, gsl], tm[:], uT[:, jp, tsl], ALU.mult),
                     reads=[tm, uT], writes=[yT])
            P.mark()
            pq = pur.next(); CT = CTr.next(); ST = STr.next(); qb = qbr.next(); pT = pTr.next()
            L.proj_tok(pq, hT, tsl, Win, 1024, 512)
            L.rope_tables(T_view(cs, cs[:, t, :]), gq, gqs, CT, ST)
            L.qk_post(pq, 8, CT, ST, qb, tmps[t % 2])
            for c4 in range(4):
                P.op("pe", lambda e, c4=c4, pT=pT, qb=qb: e.transpose(pT[:, c4, :], qb[:, c4 * 128:(c4 + 1) * 128], L.identb[:]),
                     reads=[qb, L.identb], writes=[pT], inc=(c4 == 3))
            P.op("dve", lambda e, pT=pT, gsl=gsl: e.tensor_copy(QT[0:64, 0, :, gsl], pT[0:64, 0:4, :]), reads=[pT], writes=[QT])
            P.op("act", lambda e, pT=pT, gsl=gsl: e.activation(QT[64:128, 1, :, gsl], pT[64:128, 0:4, :], AF.Identity), reads=[pT], writes=[QT])
            recs.append(P.record_end())
        P.replay_skewed(recs)
    P.phase_end()

    P.phase_begin()
    subbc = P.sb([128, 128], F32, "subbc"); L.load_bc(subbc, sub_d.ap, sub_d)
    P.op("dve", lambda e: e.tensor_scalar(subbc[:], subbc[:], 1.0 - LAM_INIT, None, ALU.mult), reads=[subbc], writes=[subbc])
    KTh = L.ring(2, [128, NALL], BF, "KTh")
    Vh = L.ring(2, [128, NT_ALL, 129], BF, "Vh")
    for v_ in Vh.tiles:
        P.op("pool", lambda e, v_=v_: e.memset(v_[:, :, 128:129], 1.0), writes=[v_])
    spr = L.ring(4, [128, 2, 256], F32, "sp", psum=True)
    LOOK = 3
    accs = [[P.ps([128, 512], F32, "acc") for c in range(2)] for qi in range(2)]
    pTr_ = L.ring(6, [128, 2, 256], BF, "pTs")
    rz = L.ring(2, [128, 4], F32, "rz"); o1r = L.ring(2, [128, 128], F32, "o1"); o2r = L.ring(2, [128, 128], F32, "o2")
    jr = L.ring(2, [128, 128], F32, "junk"); ssr = L.ring(2, [128, 2], F32, "ss"); ybr = L.ring(2, [128, 128], BF, "yb")
    for h in range(4):
        kth = KTh.next(); vh = Vh.next()
        for pc in range(6):
            P.dma("sp", kth[:, pc * 1408:(pc + 1) * 1408], KT_d[h, :, pc * 1408:(pc + 1) * 1408], reads=[KT_d], writes=[kth])
        for pc in range(11):
            P.dma("sp", vh[:, pc * 6:(pc + 1) * 6, 0:128],
                  VS_d[pc * 768:(pc + 1) * 768, h * 128:(h + 1) * 128].rearrange("(t p) e -> p t e", p=128), reads=[VS_d], writes=[vh])
        for qb_ in range(NOWN // 256):
            keys = [0, 1] if qb_ == 0 else list(range(NT_ALL))
            q0 = qb_ * 256
            def emit_scores(kt, kth=kth, q0=q0, h=h):
                sp_ = spr.next()
                for c in range(2):
                    P.op("pe", lambda e, c=c, sp_=sp_, kth=kth, kt=kt, q0=q0, h=h: e.matmul(
                        sp_[:, c, :], kth[:, kt * 128:(kt + 1) * 128], QT[:, c, h, q0:q0 + 256],
                        start=True, stop=True), reads=[kth, QT], writes=[sp_], inc=(c == 1))
                return sp_

            pend_sc = [emit_scores(keys[i]) for i in range(min(LOOK, len(keys)))]
            for ki, kt in enumerate(keys):
                sp_ = pend_sc.pop(0)
                if ki + LOOK < len(keys):
                    pend_sc.append(emit_scores(keys[ki + LOOK]))
                pt = pTr_.next()
                P.op("act", lambda e, pt=pt, sp_=sp_: e.activation(pt[:], sp_[:], AF.Exp, scale=0.125), reads=[sp_], writes=[pt])
                for qi in range(2):
                    for c in range(2):
                        P.op("pe", lambda e, qi=qi, c=c, pt=pt, vh=vh, kt=kt, ki=ki, keys=keys: e.matmul(
                            accs[qi][c][:, 0:129], pt[:, c, qi * 128:(qi + 1) * 128], vh[:, kt, :],
                            start=(ki == 0), stop=(ki == len(keys) - 1)), reads=[pt, vh], writes=[accs[qi][c]],
                            inc=(ki == len(keys) - 1))
            for qi in range(2):
                r = rz.next(); o1 = o1r.next(); o2 = o2r.next(); junk = jr.next(); ss = ssr.next(); yb = ybr.next()
                a0, a1 = accs[qi]
                P.op("dve", lambda e, r=r, a0=a0: e.reciprocal(r[:, 0:1], a0[:, 128:129]), reads=[a0], writes=[r])
                P.op("dve", lambda e, r=r, a1=a1: e.reciprocal(r[:, 1:2], a1[:, 128:129]), reads=[a1], writes=[r])
                P.op("dve", lambda e, r=r: e.tensor_tensor(r[:, 2:3], r[:, 1:2], neglam[:], ALU.mult), reads=[r, neglam], writes=[r])
                P.op("act", lambda e, o1=o1, a0=a0, r=r: e.activation(o1[:], a0[:, 0:128], AF.Identity, scale=r[:, 0:1]), reads=[a0, r], writes=[o1])
                P.op("dve", lambda e, o2=o2, a1=a1, r=r, o1=o1: e.scalar_tensor_tensor(o2[:], a1[:, 0:128], r[:, 2:3], o1[:], ALU.mult, ALU.add),
                     reads=[a1, r, o1], writes=[o2])
                P.op("act", lambda e, junk=junk, o2=o2, ss=ss: e.activation(junk[:], o2[:], AF.Square, accum_out=ss[:, 0:1]), reads=[o2], writes=[junk, ss])
                L.rstd(ss, 1, 128)
                P.op("dve", lambda e, yb=yb, o2=o2, ss=ss: e.scalar_tensor_tensor(yb[:], o2[:], ss[:, 0:1], subbc[:], ALU.mult, ALU.mult),
                     reads=[o2, ss, subbc], writes=[yb])
                gsl = slice(q0 + qi * 128, q0 + (qi + 1) * 128)
                P.op("pe", lambda e, yb=yb, a0=a0: e.transpose(a0[:].bitcast(BF)[:, 0:128], yb[:], L.identb[:]), reads=[yb, L.identb], writes=[a0])
                P.op("act", lambda e, gsl=gsl, h=h, a0=a0: e.activation(yT[:, 4 + h, gsl], a0[:].bitcast(BF)[:, 0:128], AF.Identity), reads=[a0], writes=[yT])
    P.phase_end()

    qstack.close()
    ffn_phase(L, xown_blk, xo_blk, blocks, yT, wout_d, gffn, w1_d, w3_d, w2_d, ctx_block=True)
    P.barrier()
    P.flush()
    P.gstack.close()
    P.gstack = _outer


def T_view(t, ap):
    return _View(t, ap)


class _View:
    def __init__(self, base, ap):
        object.__setattr__(self, "base", base)
        object.__setattr__(self, "ap", ap)

    def __getitem__(self, k):
        return self.ap[k]

    @property
    def w(self):
        return self.base.w

    @w.setter
    def w(self, v):
        self.base.w = v

    @property
    def r(self):
        return self.base.r

    @r.setter
    def r(self, v):
        self.base.r = v

    @property
    def name(self):
        return self.base.name


def ffn_phase(L, xown, xo, blocks, yT, wout_d, gffn, w1_d, w3_d, w2_d, ctx_block, moe=None):
    P = L.P
    P.phase_begin()
    nvar = 2 if ctx_block else 1
    g1 = [L.mod_bc(j, 2, "g1") for j in range(nvar)]
    A2 = [L.make_A(L.mod_bc(j, 4, "A2"), gffn) for j in range(nvar)]
    sh2 = [L.mod_bc(j, 3, "sh2") for j in range(nvar)]
    g2 = [L.mod_bc(j, 5, "g2") for j in range(nvar)]
    wout = P.sb([128, 8, D], BF, "wout")
    for h in range(2):
        P.dma("pool", wout[:, :, h * 512:(h + 1) * 512], wout_d[:, h * 512:(h + 1) * 512].rearrange("(kc p) n -> p kc n", p=128),
              reads=[wout_d], writes=[wout])
    NE = 1
    if moe is not None:
        rw_d, rb_d, NE = moe
        wr = P.sb([128, 8, 8], F32, "wr")
        P.dma("sp", wr[:], rw_d.ap.rearrange("(kc p) e -> p kc e", p=128), reads=[rw_d], writes=[wr])
        rb = P.sb([128, 8], F32, "rb"); L.load_bc(rb, rb_d.ap, rb_d)
        hnf_r = L.ring(2, [128, D], F32, "hnf")
        tTr = L.ring(2, [128, 8, 128], F32, "tT")
        pXr = L.ring(1, [128, 8, 128], F32, "pX", psum=True)
        dg = P.sb([128, 4, 8], F32, "dg")
        rt = [P.sb([128, 8], F32, "rt") for _ in range(4)]
        rs = P.sb([128, 4], F32, "rs")
    xb = L.ring(1, [128, 4, D], F32, "xb")
    hnr = L.ring(2, [128, D], BF, "hn"); jr = L.ring(2, [128, D], F32, "junk"); ssr = L.ring(2, [128, 2], F32, "ss")
    hTb = L.ring(1, [128, 8, 512], BF, "h2T")
    gT = P.sb([128, NFC, 512], BF, "gT")
    pTr = L.ring(1 if moe is not None else 2, [128, 8, 128], BF, "pT", psum=True)
    por = L.ring(1 if moe is not None else 2, [128, 512], F32, "po", psum=True)
    p1r = L.ring(2, [128, 512], F32, "p1", psum=True); p3r = L.ring(2, [128, 512], F32, "p3", psum=True)
    tmr = L.ring(2, [128, 512], F32, "tm")
    s1r = L.ring(2, [128, 512], F32, "s1")
    w1r = L.ring(2, [128, 8, 256], BF, "w1p"); w3r = L.ring(2, [128, 8, 256], BF, "w3p")
    w2r = L.ring(2, [128, NFC, 256], BF, "w2q")
    for bi, (t0, nt) in enumerate(blocks):
        j = 1 if (ctx_block and bi == 0) else 0
        N = nt * 128
        x = xb.next(); hT = hTb.next()
        xb_ap, xb_t = xown(t0, nt)
        P.dma("sp", x[:, 0:nt, :], xb_ap.rearrange("(t p) d -> p t d", p=128), reads=[xb_t], writes=[x])
        for t in range(nt):
            gsl = slice((t0 + t) * 128, (t0 + t + 1) * 128)
            for hf in range(2):
                po = por.next(); tm = tmr.next()
                for kc in range(8):
                    P.op("pe", lambda e, kc=kc, po=po, gsl=gsl, hf=hf: e.matmul(po[:], yT[:, kc, gsl], wout[:, kc, hf * 512:(hf + 1) * 512],
                                                                              start=(kc == 0), stop=(kc == 7)),
                         reads=[yT, wout], writes=[po], inc=(kc == 7))
                P.op("dve", lambda e, tm=tm, po=po, hf=hf, j=j: e.tensor_tensor(tm[:], po[:], g1[j][:, hf * 512:(hf + 1) * 512], ALU.mult),
                     reads=[po, g1[j]], writes=[tm])
                P.op("dve", lambda e, x=x, t=t, hf=hf, tm=tm: e.tensor_tensor(x[:, t, hf * 512:(hf + 1) * 512], x[:, t, hf * 512:(hf + 1) * 512], tm[:], ALU.add),
                     reads=[x, tm], writes=[x])
        recs = []
        for t in range(nt):
            hn = hnr.next(); junk = jr.next(); ss = ssr.next(); pT = pTr.next()
            P.record_begin()
            if moe is None:
                L.norm_tile(x[:, t, :], x, A2[j], sh2[j], hn, junk, ss)
                P.mark()
            else:
                hnf = hnf_r.next(); tT = tTr.next(); pX = pXr.next()
                P.op("act", lambda e, junk=junk, x=x, t=t, ss=ss: e.activation(junk[:], x[:, t, :], AF.Square, accum_out=ss[:, 0:1]),
                     reads=[x], writes=[junk, ss])
                L.rstd(ss, 1, D)
                P.op("dve", lambda e, junk=junk, x=x, t=t, ss=ss, j=j: e.scalar_tensor_tensor(junk[:], x[:, t, :], ss[:, 0:1], A2[j][:], ALU.mult, ALU.mult),
                     reads=[x, ss, A2[j]], writes=[junk])
                P.op("dve", lambda e, hnf=hnf, junk=junk, j=j: e.tensor_tensor(hnf[:], junk[:], sh2[j][:], ALU.add), reads=[junk, sh2[j]], writes=[hnf])
                P.op("act", lambda e, hn=hn, hnf=hnf: e.activation(hn[:], hnf[:], AF.Identity), reads=[hnf], writes=[hn])
                P.mark()
                for c in range(8):
                    P.op("pe", lambda e, c=c, pX=pX, hnf=hnf: e.transpose(pX[:, c, :], hnf[:, c * 128:(c + 1) * 128], L.ident[:]),
                         reads=[hnf, L.ident], writes=[pX], inc=(c == 7))
                P.op("dve", lambda e, tT=tT, pX=pX: e.tensor_copy(tT[:], pX[:]), reads=[pX], writes=[tT])
                plg = por.next()
                for kc in range(8):
                    P.op("pe", lambda e, kc=kc, plg=plg, tT=tT: e.matmul(plg[:, 0:8], tT[:, kc, :], wr[:, kc, :], start=(kc == 0), stop=(kc == 7)),
                         reads=[tT, wr], writes=[plg], inc=(kc == 7))
                lg, mk, l2, ex = rt
                P.op("dve", lambda e, plg=plg: e.tensor_tensor(lg[:], plg[:, 0:8], rb[:], ALU.add), reads=[plg, rb], writes=[lg])
                P.op("dve", lambda e: e.tensor_reduce(rs[:, 0:1], lg[:], AX.X, ALU.max), reads=[lg], writes=[rs])
                P.op("dve", lambda e: e.tensor_scalar(mk[:], lg[:], rs[:, 0:1], None, ALU.is_equal), reads=[lg, rs], writes=[mk])
                P.op("dve", lambda e: e.scalar_tensor_tensor(l2[:], mk[:], -1e30, lg[:], ALU.mult, ALU.add), reads=[mk, lg], writes=[l2])
                P.op("dve", lambda e: e.tensor_reduce(rs[:, 1:2], l2[:], AX.X, ALU.max), reads=[l2], writes=[rs])
                P.op("dve", lambda e: e.tensor_scalar(mk[:], lg[:], rs[:, 1:2], None, ALU.is_ge), reads=[lg, rs], writes=[mk])
                P.op("dve", lambda e: e.tensor_scalar(ex[:], lg[:], rs[:, 0:1], None, ALU.subtract), reads=[lg, rs], writes=[ex])
                P.op("act", lambda e: e.activation(ex[:], ex[:], AF.Exp), reads=[ex], writes=[ex])
                P.op("dve", lambda e: e.tensor_tensor(ex[:], ex[:], mk[:], ALU.mult), reads=[ex, mk], writes=[ex])
                P.op("dve", lambda e: e.tensor_reduce(rs[:, 2:3], ex[:], AX.X, ALU.add), reads=[ex], writes=[rs])
                P.op("dve", lambda e: e.reciprocal(rs[:, 3:4], rs[:, 2:3]), reads=[rs], writes=[rs])
                P.op("dve", lambda e, t=t: e.tensor_scalar(dg[:, t, :], ex[:], rs[:, 3:4], None, ALU.mult), reads=[ex, rs], writes=[dg])
            L.transpose_cols(hn, 8, pT, lambda hT=hT, t=t: hT[:, :, t * 128:(t + 1) * 128], hT)
            recs.append(P.record_end())
        P.replay_skewed(recs)
        for ex_i in range(NE):
            w1e = w1_d.ap[ex_i] if moe is not None else w1_d.ap
            w3e = w3_d.ap[ex_i] if moe is not None else w3_d.ap
            w2e = w2_d.ap[ex_i] if moe is not None else w2_d.ap
            for pi in range(NFC // 2):
                w1p = w1r.next(); w3p = w3r.next()
                P.dma("pool", w1p[:], w1e[:, pi * 256:(pi + 1) * 256].rearrange("(kc p) n -> p kc n", p=128), reads=[w1_d], writes=[w1p])
                P.dma("pool", w3p[:], w3e[:, pi * 256:(pi + 1) * 256].rearrange("(kc p) n -> p kc n", p=128), reads=[w3_d], writes=[w3p])
                for fc in range(2):
                    p1 = p1r.next(); p3 = p3r.next(); s1 = s1r.next()
                    L.proj_feat(p1, hT, N, w1p, fc * 128)
                    L.proj_feat(p3, hT, N, w3p, fc * 128)
                    P.op("act", lambda e, s1=s1, p1=p1, N=N: e.activation(s1[:, 0:N], p1[:, 0:N], AF.Silu), reads=[p1], writes=[s1])
                    P.op("dve", lambda e, s1=s1, p3=p3, N=N, f=pi * 2 + fc: e.tensor_tensor(gT[:, f, 0:N], s1[:, 0:N], p3[:, 0:N], ALU.mult),
                         reads=[s1, p3], writes=[gT])
            for qd in range(4):
                w2q = w2r.next()
                P.dma("pool", w2q[:], w2e[:, qd * 256:(qd + 1) * 256].rearrange("(fc p) n -> p fc n", p=128), reads=[w2_d], writes=[w2q])
                for t in range(nt):
                    po = por.next(); tm = tmr.next()
                    for fc in range(NFC):
                        P.op("pe", lambda e, fc=fc, po=po, t=t, w2q=w2q: e.matmul(po[:, 0:256], gT[:, fc, t * 128:(t + 1) * 128], w2q[:, fc, :],
                                                                                start=(fc == 0), stop=(fc == NFC - 1)),
                             reads=[gT, w2q], writes=[po], inc=(fc == NFC - 1))
                    P.op("dve", lambda e, tm=tm, po=po, qd=qd, j=j: e.tensor_tensor(tm[:, 0:256], po[:, 0:256], g2[j][:, qd * 256:(qd + 1) * 256], ALU.mult),
                         reads=[po, g2[j]], writes=[tm])
                    if moe is None:
                        P.op("dve", lambda e, x=x, t=t, qd=qd, tm=tm: e.tensor_tensor(x[:, t, qd * 256:(qd + 1) * 256], x[:, t, qd * 256:(qd + 1) * 256], tm[:, 0:256], ALU.add),
                             reads=[x, tm], writes=[x])
                    else:
                        P.op("dve", lambda e, x=x, t=t, qd=qd, tm=tm, ex_i=ex_i: e.scalar_tensor_tensor(
                            x[:, t, qd * 256:(qd + 1) * 256], tm[:, 0:256], dg[:, t, ex_i:ex_i + 1], x[:, t, qd * 256:(qd + 1) * 256], ALU.mult, ALU.add),
                            reads=[x, tm, dg], writes=[x])
        xo_ap, xo_t = xo(t0, nt)
        P.dma("sp", xo_ap.rearrange("(t p) d -> p t d", p=128), x[:, 0:nt, :], reads=[x], writes=[xo_t])
    P.phase_end()


STOP1 = None


def emit_l1(L, io):
    nc = L.nc
    P = L.P
    _outer = P.gstack
    P.gstack = ExitStack()
    xall_tile = io["xall_tile"]; xown_blk = io["xown_blk"]; xo_blk = io["xo_blk"]
    csall = io["csall"]; csown = io["csown"]; ident_d = io["ident_d"]; cvec_d = io["cvec_d"]
    sfx = io["sfx"]
    L.modrow_d = io["modrow"]
    gmix_d = L.din("norm_mix_g" + sfx, [D])
    win_d = L.din("o_w_in", [D, 1792])
    qg_d = L.din("c_qnorm_g", [64])
    kg_d = L.din("c_knorm_g", [64])
    lrup_d = L.din("lru_p", [128, 4, 12])
    wa_d = L.din("d_wa", [2, 8, 64, 64])
    wx_d = L.din("d_wx", [2, 8, 64, 64])
    segw_d = L.din("segw", [4])
    KT_d = L.dscratch("KT1_d", [128, NALL], BF)
    VS_d = L.dscratch("VS1_d", [NALL, 128], BF)
    XR_d = L.dscratch("XR_d", [4, 128, NALL], F32)

    gq = P.sb([128, 64], F32, "gq", glob=True); gqs = P.sb([128, 64], F32, "gqs", glob=True)
    gk = P.sb([128, 64], F32, "gk", glob=True); gks = P.sb([128, 64], F32, "gks", glob=True)
    L.load_bc(gq, qg_d.ap, qg_d); L.load_bc(gk, kg_d.ap, kg_d)
    L.swap_gain(gq, gqs); L.swap_gain(gk, gks)
    gmix = P.sb([128, D], F32, "gmix", glob=True)
    L.load_bc(gmix, gmix_d.ap, gmix_d)

    yT = P.sb([128, 8, NOWN], BF, "yT", glob=True)
    gstack = ExitStack()
    gateT = T("gateT", gstack.enter_context(nc.sbuf_tensor("gateT_l1", [128, 4, OWN], BF)).ap())
    qstack = ExitStack()
    QT = T("QT", qstack.enter_context(nc.sbuf_tensor("QT_l1", [128, 2, 4, NOWN], BF)).ap())
    P.op("pool", lambda e: e.memset(QT[64:128, 0, :, :], 0.0), writes=[QT])
    P.op("pool", lambda e: e.memset(QT[0:64, 1, :, :], 0.0), writes=[QT])
    blocks = [(2 + 4 * i, 4) for i in range(4)]

    P.phase_begin()
    A1 = [L.make_A(L.mod_bc(0, 1, "A1"), gmix)]
    sh1 = [L.mod_bc(0, 0, "sh1")]
    Win = P.sb([128, 8, 1024], BF, "Win")
    for g in range(4):
        for n in range(2):
            h = n * 4 + g
            P.dma("pool", Win[:, :, g * 128 + n * 64:g * 128 + (n + 1) * 64],
                  win_d[:, h * 64:(h + 1) * 64].rearrange("(kc p) n -> p kc n", p=128), reads=[win_d], writes=[Win])
    P.dma("pool", Win[:, :, 512:1024], win_d[:, 768:1280].rearrange("(kc p) n -> p kc n", p=128), reads=[win_d], writes=[Win])
    xb = L.ring(1, [128, 4, D], F32, "xb"); csb = L.ring(2, [128, 4, 128], F32, "csb")
    hnr = L.ring(2, [128, D], BF, "hn"); jr = L.ring(2, [128, D], F32, "junk"); ssr = L.ring(2, [128, 2], F32, "ss")
    hTb = L.ring(1, [128, 8, 512], BF, "hTb")
    pTr = L.ring(2, [128, 8, 128], BF, "pT", psum=True)
    pur = L.ring(2, [128, 512], F32, "pu", psum=True)
    CTr = L.ring(2, [128, 64], F32, "CT"); STr = L.ring(2, [128, 64], F32, "ST")
    tmps = [tuple([P.sb([128, 512], F32, "sq"), P.sb([128, 512], F32, "qn"), P.sb([128, 512], F32, "t1"), P.sb([128, 8], F32, "ssq")]) for _ in range(2)]
    qbr = L.ring(2, [128, 512], BF, "qb")
    for bi, (t0, nt) in enumerate(blocks):
        N = nt * 128
        x = xb.next(); cs = csb.next(); hT = hTb.next()
        xb_ap, xb_t = xown_blk(t0, nt)
        P.dma("sp", x[:, 0:nt, :], xb_ap.rearrange("(t p) d -> p t d", p=128), reads=[xb_t], writes=[x])
        P.dma("sp", cs[:, 0:nt, :], csown[t0 * 128:(t0 + nt) * 128, :].rearrange("(t p) d -> p t d", p=128), reads=[csown], writes=[cs])
        for t in range(nt):
            hn = hnr.next(); junk = jr.next(); ss = ssr.next(); pT = pTr.next()
            L.norm_tile(x[:, t, :], x, A1[0], sh1[0], hn, junk, ss)
            L.transpose_cols(hn, 8, pT, lambda hT=hT, t=t: hT[:, :, t * 128:(t + 1) * 128], hT)
        for c in range(4):
            pu = pur.next()
            L.proj_feat(pu, hT, N, Win, 512 + c * 128)
            P.op("act", lambda e, pu=pu, c=c, N=N, t0=t0: e.activation(gateT[:, c, (t0 - 2) * 128:(t0 - 2) * 128 + N], pu[:, 0:N], AF.Gelu_apprx_tanh),
                 reads=[pu], writes=[gateT])
        recs = []
        for t in range(nt):
            tsl = slice(t * 128, (t + 1) * 128)
            gsl = slice((t0 + t) * 128, (t0 + t + 1) * 128)
            P.record_begin()
            pq = pur.next(); CT = CTr.next(); ST = STr.next(); qb = qbr.next(); pT = pTr.next()
            L.proj_tok(pq, hT, tsl, Win, 0, 512)
            P.mark()
            L.rope_tables(T_view(cs, cs[:, t, :]), gq, gqs, CT, ST)
            L.qk_post(pq, 8, CT, ST, qb, tmps[t % 2])
            for c4 in range(4):
                P.op("pe", lambda e, c4=c4, pT=pT, qb=qb: e.transpose(pT[:, c4, :], qb[:, c4 * 128:(c4 + 1) * 128], L.identb[:]),
                     reads=[qb, L.identb], writes=[pT], inc=(c4 == 3))
            P.op("dve", lambda e, pT=pT, gsl=gsl: e.tensor_copy(QT[0:64, 0, :, gsl], pT[0:64, 0:4, :]), reads=[pT], writes=[QT])
            P.op("act", lambda e, pT=pT, gsl=gsl: e.activation(QT[64:128, 1, :, gsl], pT[64:128, 0:4, :], AF.Identity), reads=[pT], writes=[QT])
            recs.append(P.record_end())
        P.replay_skewed(recs)
    P.phase_end()

    P.phase_begin()
    A1 = [L.make_A(L.mod_bc(j, 1, "A1"), gmix) for j in range(2)]
    sh1 = [L.mod_bc(j, 0, "sh1") for j in range(2)]
    Wk = P.sb([128, 8, 768], BF, "Wkvx")
    P.dma("pool", Wk[:, :, 0:256], win_d[:, 512:768].rearrange("(kc p) n -> p kc n", p=128), reads=[win_d], writes=[Wk])
    P.dma("pool", Wk[:, :, 256:768], win_d[:, 1280:1792].rearrange("(kc p) n -> p kc n", p=128), reads=[win_d], writes=[Wk])
    xr = L.ring(3, [128, D], F32, "xa"); csr = L.ring(3, [128, 128], F32, "csa")
    hnr = L.ring(2, [128, D], BF, "hn"); jr = L.ring(2, [128, D], F32, "junk"); ssr = L.ring(2, [128, 2], F32, "ss")
    hTr = L.ring(2, [128, 8, 128], BF, "hT")
    pTr = L.ring(2, [128, 8, 128], BF, "pT", psum=True)
    pkr = L.ring(2, [128, 512], F32, "pk", psum=True); pxr = L.ring(2, [128, 512], F32, "px", psum=True)
    pXr = L.ring(2, [128, 4, 128], F32, "pX", psum=True)
    CTr = L.ring(2, [128, 64], F32, "CT"); STr = L.ring(2, [128, 64], F32, "ST")
    tmps = [tuple([P.sb([128, 512], F32, "sq"), P.sb([128, 512], F32, "qn"), P.sb([128, 512], F32, "t1"), P.sb([128, 8], F32, "ssq")]) for _ in range(2)]
    kbr = L.ring(2, [128, 128], BF, "kb"); vbr = L.ring(2, [128, 128], BF, "vb"); ktr = L.ring(2, [128, 1, 128], BF, "kt")
    xsr = L.ring(2, [128, 512], F32, "xs"); xtr = L.ring(2, [128, 4, 128], F32, "xt")
    def tile_A(t):
        j = 1 if t < 2 else 0
        x = xr.next(); cs = csr.next(); hn = hnr.next(); junk = jr.next(); ss = ssr.next(); hT = hTr.next(); pT = pTr.next()
        xa_ap, xa_t = xall_tile(t)
        P.dma("sp", x[:], xa_ap, reads=[xa_t], writes=[x])
        P.dma("sp", cs[:], csall[t * 128:(t + 1) * 128, :], reads=[csall], writes=[cs])
        L.norm_tile(x[:], x, A1[j], sh1[j], hn, junk, ss)
        L.transpose_cols(hn, 8, pT, lambda hT=hT: hT[:], hT)
        pk = pkr.next(); px = pxr.next()
        L.proj_tok(pk, hT, slice(0, 128), Wk, 0, 256)
        L.proj_tok(px, hT, slice(0, 128), Wk, 256, 512)
        P.mark()
        vb = vbr.next()
        P.op("act", lambda e, vb=vb, pk=pk: e.activation(vb[:], pk[:, 128:256], AF.Identity), reads=[pk], writes=[vb])
        P.dma("pool", VS_d[t * 128:(t + 1) * 128, :], vb[:], reads=[vb], writes=[VS_d])
        CT = CTr.next(); ST = STr.next()
        L.rope_tables(cs, gk, gks, CT, ST)
        kb = kbr.next()
        L.qk_post(pk, 2, CT, ST, kb, tmps[t % 2])
        kt = ktr.next()
        L.transpose_cols(kb, 1, pT, lambda kt=kt: kt[:], kt, eng="dve")
        P.dma("pool", KT_d[:, t * 128:(t + 1) * 128], kt[:, 0, :], reads=[kt], writes=[KT_d])
        xs = xsr.next(); xt = xtr.next(); pX = pXr.next()
        P.op("act", lambda e, xs=xs, px=px: e.activation(xs[:], px[:], AF.Identity), reads=[px], writes=[xs])
        for c in range(4):
            P.op("pe", lambda e, c=c, pX=pX, xs=xs: e.transpose(pX[:, c, :], xs[:, c * 128:(c + 1) * 128], L.ident[:]),
                 reads=[xs, L.ident], writes=[pX], inc=(c == 3))
        P.op("dve", lambda e, xt=xt, pX=pX: e.tensor_copy(xt[:], pX[:]), reads=[pX], writes=[xt])
        P.dma("pool", XR_d[:, :, t * 128:(t + 1) * 128].rearrange("j p t -> p j t"), xt[:], reads=[xt], writes=[XR_d])

    recs = []
    for t in range(NT_ALL):
        P.record_begin(); tile_A(t); recs.append(P.record_end())
    P.replay_skewed(recs)
    P.phase_end()

    P.phase_begin()
    KT = P.sb([128, NALL], BF, "KT")
    for pc in range(6):
        P.dma("sp", KT[:, pc * 1408:(pc + 1) * 1408], KT_d[:, pc * 1408:(pc + 1) * 1408], reads=[KT_d], writes=[KT])
    Vn = [P.sb([128, NT_ALL, 128], BF, "Vn") for _ in range(2)]
    for n in range(2):
        P.op("pool", lambda e, n=n: e.memset(Vn[n][:, :, 64:128], 1.0), writes=[Vn[n]])
        for pc in range(11):
            P.dma("sp", Vn[n][:, pc * 6:(pc + 1) * 6, 0:64],
                  VS_d[pc * 768:(pc + 1) * 768, n * 64:(n + 1) * 64].rearrange("(t p) e -> p t e", p=128), reads=[VS_d], writes=[Vn[n]])
    spr = L.ring(4, [128, 512], F32, "sp", psum=True)
    accr = L.ring(2, [128, 512], F32, "acc", psum=True)
    pXo = L.ring(1, [128, 4, 128], F32, "pXo", psum=True)
    LOOK1 = 3
    pTo = P.ps([128, 4, 128], BF, "pTo")
    ptr_ = L.ring(6, [128, 512], BF, "pts")
    rzr = L.ring(2, [128, 4], F32, "rz")
    osr = L.ring(2, [128, 512], F32, "osb")
    yar = L.ring(2, [128, 512], BF, "yat")
    for qt in range(OWN // 128):
        q0 = LC + qt * 128
        yat = yar.next()
        for n in range(2):
            acc = accr.next()

            def emit_scores1(kt, n=n, q0=q0):
                sp_ = spr.next()
                P.op("pe", lambda e, n=n, sp_=sp_, kt=kt, q0=q0: e.matmul(
                    sp_[:].rearrange("p (g q) -> p g q", g=4), KT[:, kt * 128:(kt + 1) * 128],
                    QT[:, n, :, q0:q0 + 128], start=True, stop=True), reads=[KT, QT], writes=[sp_])
                return sp_

            pend_sc = [emit_scores1(i) for i in range(LOOK1)]
            for kt in range(NT_ALL):
                sp_ = pend_sc.pop(0)
                if kt + LOOK1 < NT_ALL:
                    pend_sc.append(emit_scores1(kt + LOOK1))
                pt = ptr_.next()
                P.op("act", lambda e, pt=pt, sp_=sp_: e.activation(pt[:], sp_[:], AF.Exp, scale=0.125), reads=[sp_], writes=[pt])
                P.op("pe", lambda e, pt=pt, n=n, kt=kt, acc=acc: e.matmul(acc[:], Vn[n][:, kt, :], pt[:], start=(kt == 0), stop=(kt == NT_ALL - 1)),
                     reads=[pt, Vn[n]], writes=[acc], inc=(kt == NT_ALL - 1))
            osb = osr.next(); pX = pXo.next(); rz = rzr.next()
            P.op("act", lambda e, osb=osb, acc=acc: e.activation(osb[:], acc[:], AF.Identity), reads=[acc], writes=[osb])
            for g in range(4):
                P.op("pe", lambda e, g=g, pX=pX, osb=osb: e.transpose(pX[:, g, :], osb[:, g * 128:(g + 1) * 128], L.ident[:]),
                     reads=[osb, L.ident], writes=[pX], inc=(g == 3))
            P.op("dve", lambda e, rz=rz, pX=pX: e.reciprocal(rz[:, 0:4], pX[:, :, 64]), reads=[pX], writes=[rz])
            for g in range(4):
                h = n * 4 + g
                P.op("act", lambda e, g=g, h=h, rz=rz, yat=yat, pX=pX: e.activation(yat[:, h * 64:(h + 1) * 64], pX[:, g, 0:64], AF.Identity, scale=rz[:, g:g + 1]),
                     reads=[pX, rz], writes=[yat])
        for c in range(4):
            P.op("pe", lambda e, c=c, yat=yat: e.transpose(pTo[:, c, :], yat[:, c * 128:(c + 1) * 128], L.identb[:]),
                 reads=[yat, L.identb], writes=[pTo], inc=(c == 3))
        P.op("act", lambda e, q0=q0: e.activation(yT[:, 0:4, q0:q0 + 128], pTo[:], AF.Identity), reads=[pTo], writes=[yT])
    P.phase_end()
    qstack.close()

    P.phase_begin()
    lp = P.sb([128, 4, 12], F32, "lp")
    P.dma("sp", lp[:], lrup_d[:, :, :], reads=[lrup_d], writes=[lp])
    sw = P.sb([128, 4], F32, "segw"); L.load_bc(sw, segw_d.ap, segw_d)
    nsp = P.sb([128, 4, 2], F32, "nsp")
    P.op("act", lambda e: e.activation(nsp[:], lp[:, :, 9:11], AF.Exp, scale=-1.0), reads=[lp], writes=[nsp])
    P.op("act", lambda e: e.activation(nsp[:], nsp[:], AF.Ln, bias=1.0), reads=[nsp], writes=[nsp])
    P.op("dve", lambda e: e.tensor_scalar(nsp[:], nsp[:], -8.0, None, ALU.mult), reads=[nsp], writes=[nsp])
    BD = P.sb([128, 16, 128], BF, "BD")
    P.op("dve", lambda e: e.memset(BD[:], 0.0), writes=[BD])
    for kind, wd in enumerate((wa_d, wx_d)):
        for d in range(2):
            for jj in range(4):
                for hh in range(2):
                    P.dma("pool", BD[hh * 64:(hh + 1) * 64, kind * 8 + d * 4 + jj, hh * 64:(hh + 1) * 64], wd.ap[d, 2 * jj + hh, :, :],
                          reads=[wd], writes=[BD])
    SEGL = 4096
    X = P.sb([128, NALL], F32, "Xr"); XD = P.sb([128, NALL], F32, "XD"); XDB = P.sb([128, NALL], BF, "XDB")
    Ab = T_view(X, X[:, 0:SEGL]); Bb = T_view(X, X[:, SEGL:2 * SEGL]); Hb = P.sb([128, SEGL], F32, "Hb")
    Ib = P.sb([128, SEGL], F32, "Ib")
    acc = P.sb([128, OWN], F32, "hacc")
    st = P.sb([128, 2], F32, "st")
    prr = L.ring(2, [128, 512], F32, "pr", psum=True); pir = L.ring(2, [128, 512], F32, "pi", psum=True)
    for jj in range(4):
        for pc in range(6):
            P.dma("sp", X[:, pc * 1408:(pc + 1) * 1408], XR_d[jj, :, pc * 1408:(pc + 1) * 1408], reads=[XR_d], writes=[X])
        P.op("dve", lambda e, jj=jj: e.tensor_scalar(XD[:], X[:], lp[:, jj, 2:3], lp[:, jj, 4:5], ALU.mult, ALU.add), reads=[X, lp], writes=[XD])
        for (lo, hi) in ((0, LC), (LC, NALL)):
            for tap, off in ((0, -2), (1, -1), (3, 1)):
                a = max(lo, lo - off); b = min(hi, hi - off)
                P.op("dve", lambda e, jj=jj, tap=tap, off=off, a=a, b=b: e.scalar_tensor_tensor(
                    XD[:, a:b], X[:, a + off:b + off], lp[:, jj, tap:tap + 1], XD[:, a:b], ALU.mult, ALU.add), reads=[X, lp, XD], writes=[XD])
        P.op("pool", lambda e: e.tensor_copy(XDB[:], XD[:]), reads=[XD], writes=[XDB])
        P.op("dve", lambda e: e.memset(acc[:], 0.0), writes=[acc])
        for d in range(2):
            segs = [(0, LC), (LC, LC + SEGL), (LC + SEGL, NALL)]
            order = segs if d == 0 else [segs[0], segs[2], segs[1]]
            for si, (lo, hi) in enumerate(order):
                n = hi - lo
                for b0 in range(0, n, 512):
                    bw = min(512, n - b0)
                    pr = prr.next(); pi_ = pir.next()
                    P.op("pe", lambda e, pr=pr, d=d, jj=jj, lo=lo, b0=b0, bw=bw: e.matmul(pr[:, 0:bw], BD[:, d * 4 + jj, :], XDB[:, lo + b0:lo + b0 + bw], start=True, stop=True),
                         reads=[BD, XDB], writes=[pr])
                    P.op("pe", lambda e, pi_=pi_, d=d, jj=jj, lo=lo, b0=b0, bw=bw: e.matmul(pi_[:, 0:bw], BD[:, 8 + d * 4 + jj, :], XDB[:, lo + b0:lo + b0 + bw], start=True, stop=True),
                         reads=[BD, XDB], writes=[pi_])
                    sl = slice(b0, b0 + bw)
                    P.op("act", lambda e, pr=pr, bw=bw, sl=sl, d=d, jj=jj: e.activation(Ab[:, sl], pr[:, 0:bw], AF.Sigmoid, bias=lp[:, jj, 5 + d:6 + d]), reads=[pr, lp], writes=[Ab])
                    P.op("act", lambda e, pi_=pi_, bw=bw, sl=sl, d=d, jj=jj: e.activation(Ib[:, sl], pi_[:, 0:bw], AF.Sigmoid, bias=lp[:, jj, 7 + d:8 + d]), reads=[pi_, lp], writes=[Ib])
                P.op("act", lambda e, n=n, d=d, jj=jj: e.activation(Ab[:, 0:n], Ab[:, 0:n], AF.Exp, scale=nsp[:, jj, d:d + 1]), reads=[Ab, nsp], writes=[Ab])
                P.op("dve", lambda e, n=n: e.tensor_tensor(Hb[:, 0:n], Ab[:, 0:n], Ab[:, 0:n], ALU.mult), reads=[Ab], writes=[Hb])
                P.op("act", lambda e, n=n: e.activation(Hb[:, 0:n], Hb[:, 0:n], AF.Sqrt, bias=1.0, scale=-1.0), reads=[Hb], writes=[Hb])
                P.op("dve", lambda e, n=n: e.tensor_tensor(Hb[:, 0:n], Hb[:, 0:n], Ib[:, 0:n], ALU.mult), reads=[Hb, Ib], writes=[Hb])
                P.op("pool", lambda e, n=n, lo=lo: e.tensor_tensor(Bb[:, 0:n], Hb[:, 0:n], XD[:, lo:lo + n], ALU.mult), reads=[Hb, XD], writes=[Bb])
                init = 0.0 if si == 0 else st[:, d:d + 1]
                if d == 0:
                    P.op("dve", lambda e, n=n, init=init: e.tensor_tensor_scan(Hb[:, 0:n], Ab[:, 0:n], Bb[:, 0:n], init, ALU.mult, ALU.add),
                         reads=[Ab, Bb, st], writes=[Hb])
                    P.op("dve", lambda e, n=n, d=d: e.tensor_copy(st[:, d:d + 1], Hb[:, n - 1:n]), reads=[Hb], writes=[st])
                else:
                    P.op("dve", lambda e, n=n, init=init: e.tensor_tensor_scan(Hb[:, 0:n][:, ::-1], Ab[:, 0:n][:, ::-1], Bb[:, 0:n][:, ::-1], init, ALU.mult, ALU.add),
                         reads=[Ab, Bb, st], writes=[Hb])
                    P.op("dve", lambda e, d=d: e.tensor_copy(st[:, d:d + 1], Hb[:, 0:1]), reads=[Hb], writes=[st])
                if lo >= LC:
                    s0 = (lo - LC) // OWN
                    for k in range(2):
                        P.op("dve", lambda e, k=k, s0=s0: e.scalar_tensor_tensor(acc[:], Hb[:, k * OWN:(k + 1) * OWN], sw[:, s0 + k:s0 + k + 1], acc[:], ALU.mult, ALU.add),
                             reads=[Hb, sw, acc], writes=[acc])
        P.op("dve", lambda e, jj=jj: e.tensor_tensor(yT[:, 4 + jj, LC:LC + OWN], acc[:], gateT[:, jj, :], ALU.mult), reads=[acc, gateT], writes=[yT])
    P.phase_end()
    gstack.close()

    gffn_d = L.din("norm_ffn_g" + sfx, [D])
    wout_d = L.din("w_out" + sfx, [D, D])
    rw_d = L.din("router_w", [D, 8]); rb_d = L.din("router_b", [8])
    w1_d = L.din("moe_w1", [8, D, FF]); w3_d = L.din("moe_w3", [8, D, FF]); w2_d = L.din("moe_w2", [8, FF, D])
    gffn = P.sb([128, D], F32, "gffn", glob=True)
    L.load_bc(gffn, gffn_d.ap, gffn_d)
    ffn_phase(L, xown_blk, xo_blk, blocks, yT, wout_d, gffn, w1_d, w3_d, w2_d, ctx_block=False, moe=(rw_d, rb_d, 8))
    P.barrier()
    P.flush()
    P.gstack.close()
    P.gstack = _outer


def build_fused():
    nc = bass.Bass("TRN2", target_bir_lowering=False)
    L = LK(nc)
    P = L.P
    xall = L.din("xall", [NALL, D])
    xown = L.din("xown", [NOWN, D])
    csall = L.din("csall", [NALL, 128])
    csown = L.din("csown", [NOWN, 128])
    ident_d = L.din("ident", [128, 128])
    cvec_d = L.din("cvec", [128, 16])
    xo = L.dout("xo", [OWN, D])
    xc1_d = L.dscratch("xc1_d", [LC, D])
    x1own_d = L.dscratch("x1own_d", [OWN, D])
    NCH = OWN // 256
    x1g_d = [L.dscratch(f"x1g{k}_d", [4 * 256, D]) for k in range(NCH)]
    L.load_ident(ident_d)
    modrows = []
    for lay in range(2):
        wmod_d = L.din(f"w_mod{lay}", [D, 6 * D]); bmod_d = L.din(f"b_mod{lay}", [6 * D])
        modrows.append(L.setup_common(cvec_d, wmod_d, bmod_d, f"l{lay}"))
    io0 = dict(csall=csall, csown=csown, ident_d=ident_d, cvec_d=cvec_d, sfx="0", modrow=modrows[0],
               xall_tile=lambda t: (xall[t * 128:(t + 1) * 128, :], xall),
               xown_blk=lambda t0, nt: (xown[t0 * 128:(t0 + nt) * 128, :], xown),
               xo_blk=lambda t0, nt: ((xc1_d[0:LC, :], xc1_d) if t0 == 0 else
                                      (x1own_d[(t0 - 2) * 128:(t0 - 2 + nt) * 128, :], x1own_d)))
    emit_l0(L, io0)
    for k in range(NCH):
        csem = nc.alloc_semaphore(f"ccsem{k}")
        waits = P._waits("pool", P._deps([x1own_d], [x1g_d[k]]), False)

        def emit_cc(eng, waits=waits, k=k, csem=csem):
            for (s_, v_) in waits:
                eng.wait_ge(s_, v_)
            eng.collective_compute("AllGather", ALU.bypass, replica_groups=[[0, 1, 2, 3], [4, 5, 6, 7]],
                                   ins=[x1own_d[k * 256:(k + 1) * 256, :].opt()], outs=[x1g_d[k].ap.opt()]).then_inc(csem)

        P.q["pool"].append(emit_cc)
        tok = (csem, 1, "cc")
        x1g_d[k].w = tok; x1g_d[k].r = {}
        x1own_d.r[csem.name] = tok

    def lat_tile(t):
        g = t - 2
        r, i = g // 16, g % 16
        k, h = i // 2, i % 2
        return (x1g_d[k][r * 256 + h * 128:r * 256 + (h + 1) * 128, :], x1g_d[k])

    io1 = dict(csall=csall, csown=csown, ident_d=ident_d, cvec_d=cvec_d, sfx="1", modrow=modrows[1],
               xall_tile=lambda t: ((xc1_d[t * 128:(t + 1) * 128, :], xc1_d) if t < 2 else lat_tile(t)),
               xown_blk=lambda t0, nt: (x1own_d[(t0 - 2) * 128:(t0 - 2 + nt) * 128, :], x1own_d),
               xo_blk=lambda t0, nt: (xo[(t0 - 2) * 128:(t0 - 2 + nt) * 128, :], xo))
    emit_l1(L, io1)
    P.finish([xo])
    return nc, L


_ROPE = None


def _cvec(cb, cctx):
    v = np.stack([cb, cctx], 1).astype(np.float32)
    return np.ascontiguousarray(v.reshape(8, 128, 2).transpose(1, 0, 2).reshape(128, 16))


def _lru_pack(inp):
    cw = np.asarray(inp["d_conv_w"])[0]; cb = np.asarray(inp["d_conv_b"])[0]
    ba = np.asarray(inp["d_ba"])[0]; bx = np.asarray(inp["d_bx"])[0]; lam = np.asarray(inp["d_lambda"])[0]
    cols = [cw[0], cw[1], cw[2], cw[3], cb, ba[0], ba[1], bx[0], bx[1], lam[0], lam[1], np.zeros(512, np.float32)]
    p = np.stack(cols, 1).astype(np.float32)
    return np.ascontiguousarray(p.reshape(4, 128, 12).transpose(1, 0, 2))


_NC = None


def kernel(**inputs):
    global _ROPE, _NC
    if _ROPE is None:
        _ROPE = _rope_table()
    inp = inputs
    nc, L = build_fused()
    x = np.asarray(inp["x"], np.float32); ctx = np.asarray(inp["ctx"], np.float32)
    ident = np.eye(128, dtype=np.float32)
    lru_p = _lru_pack(inp)
    g = lambda k, i=0: np.asarray(inp[k])[i]
    shared = {
        "ident": ident, "csall": _ROPE, "lru_p": lru_p,
        "w_mod0": g("w_mod", 0), "b_mod0": g("b_mod", 0), "norm_mix_g0": g("norm_mix_g", 0), "norm_ffn_g0": g("norm_ffn_g", 0),
        "w_out0": g("w_out", 0), "w_mod1": g("w_mod", 1), "b_mod1": g("b_mod", 1), "norm_mix_g1": g("norm_mix_g", 1),
        "norm_ffn_g1": g("norm_ffn_g", 1), "w_out1": g("w_out", 1),
        "e_w_in": g("e_w_in"), "a_norm_g": g("a_norm_g"), "a_ws": g("a_ws"), "a_bs": g("a_bs"),
        "b_qnorm_g": g("b_qnorm_g"), "b_knorm_g": g("b_knorm_g"), "b_lq1": g("b_lq1"), "b_lk1": g("b_lk1"),
        "b_lq2": g("b_lq2"), "b_lk2": g("b_lk2"), "b_subln_g": g("b_subln_g"),
        "ffn_w1": g("ffn_w1"), "ffn_w3": g("ffn_w3"), "ffn_w2": g("ffn_w2"),
        "o_w_in": g("o_w_in"), "c_qnorm_g": g("c_qnorm_g"), "c_knorm_g": g("c_knorm_g"),
        "d_wa": g("d_wa"), "d_wx": g("d_wx"), "router_w": g("router_w"), "router_b": g("router_b"),
        "moe_w1": g("moe_w1"), "moe_w3": g("moe_w3"), "moe_w2": g("moe_w2"),
    }
    shared = {k: np.ascontiguousarray(v, dtype=np.float32) for k, v in shared.items()}
    maps = []
    for core in range(8):
        b, s = core // 4, core % 4
        xall = np.concatenate([ctx[b], x[b]], 0)
        own = slice(LC + s * OWN, LC + (s + 1) * OWN)
        segw = np.zeros(4, np.float32); segw[s] = 1.0
        m = dict(shared)
        m.update({"xall": xall, "xown": np.concatenate([ctx[b], xall[own]], 0),
                  "csown": np.concatenate([_ROPE[:LC], _ROPE[own]], 0),
                  "cvec": _cvec(np.asarray(inp["c"])[b], np.asarray(inp["c_ctx"])), "segw": segw})
        maps.append({k: np.ascontiguousarray(m[k], dtype=np.float32) for k in L.inputs})
    res = run_bass_kernel_spmd(nc, maps, core_ids=list(range(8)))
    out = np.zeros_like(x)
    for core in range(8):
        b, s = core // 4, core % 4
        out[b, s * OWN:(s + 1) * OWN] = res.results[core]["xo"]
    return out.astype(np.float32)
```
